# Optimizing a Trainium2 kernel written in Bass

```python
import jax, jax.numpy as jnp
from jax import lax
import numpy as np

D_MODEL = 1024
BATCH = 8
SEQ = 2048
DEPTH = 4

GRID_W = 64
N_HEADS = 16
HEAD_DIM = D_MODEL // N_HEADS
NA_ROWS = 8
NA_COLS = 16
NA_QCOLS = NA_COLS
NA_KCOLS = 2 * NA_COLS
GQA_KV_HEADS = 4
GQA_GROUP = N_HEADS // GQA_KV_HEADS
Q_BLOCK = 128
ROPE_THETA = 10000.0
ROPE_PAIRS = HEAD_DIM // 4
N_GROUPS = 4
EXPERTS_PER_GROUP = 8
N_EXPERTS = N_GROUPS * EXPERTS_PER_GROUP
TOP_K = 2
D_EXPERT = D_MODEL // 2
MOE_BLOCK = 128
N_MIXERS = 2
N_NA_LAYERS = (DEPTH + 1) // 2
N_GQA_LAYERS = DEPTH // 2
DEEPNORM_ALPHA = (2 * DEPTH) ** 0.25
DEEPNORM_BETA = (8 * DEPTH) ** -0.25
ADA_INIT = 0.2
LN_EPS = 1e-5
RMS_EPS = 1e-6

kernel_name = "hybrid_natten_gqa_hmoe_deepnorm"


def layer_norm(x, g, b):
    xf = x.astype(jnp.float32)
    mu = xf.mean(-1, keepdims=True)
    var = jnp.square(xf - mu).mean(-1, keepdims=True)
    return ((xf - mu) * lax.rsqrt(var + LN_EPS) * g.astype(jnp.float32) + b.astype(jnp.float32)).astype(x.dtype)


def rms_norm(x, g):
    xf = x.astype(jnp.float32)
    ms = jnp.square(xf).mean(-1, keepdims=True)
    return (xf * lax.rsqrt(ms + RMS_EPS) * g.astype(jnp.float32)).astype(x.dtype)


def neighbourhood_attention(q, k, v, rpb):
    B, S, H, hd = q.shape
    rows = S // GRID_W
    kr = min(NA_ROWS, rows)
    r = jnp.arange(rows, dtype=jnp.int32)
    row_start = jnp.clip(r - kr // 2, 0, rows - kr)
    key_rows = row_start[:, None] + jnp.arange(kr, dtype=jnp.int32)[None, :]
    dr = key_rows - r[:, None] + (NA_ROWS - 1)
    qg = q.reshape(B, rows, GRID_W, H, hd)
    kg = k.reshape(B, rows, GRID_W, H, hd)[:, key_rows]
    vg = v.reshape(B, rows, GRID_W, H, hd)[:, key_rows]
    outs = []
    for q0 in range(0, GRID_W, NA_QCOLS):
        b0 = min(max(q0 - NA_COLS // 2, 0), GRID_W - NA_KCOLS)
        qc = q0 + jnp.arange(NA_QCOLS, dtype=jnp.int32)
        kc = b0 + jnp.arange(NA_KCOLS, dtype=jnp.int32)
        win_start = jnp.clip(qc - NA_COLS // 2, 0, GRID_W - NA_COLS)
        valid = (kc[None, :] >= win_start[:, None]) & (kc[None, :] < win_start[:, None] + NA_COLS)
        dc = jnp.clip(kc[None, :] - qc[:, None] + NA_COLS - 1, 0, 2 * NA_COLS - 2)
        bias = rpb[:, dr[:, None, :, None], dc[None, :, None, :]]
        kb = kg[:, :, :, b0:b0 + NA_KCOLS]
        vb = vg[:, :, :, b0:b0 + NA_KCOLS]
        s = jnp.einsum('brqhd,brikhd->bhrqik', qg[:, :, q0:q0 + NA_QCOLS], kb).astype(jnp.float32)
        s = jnp.where(valid[:, None, :], s + bias.astype(jnp.float32), -jnp.inf)
        p = jax.nn.softmax(s.reshape(B, H, rows, NA_QCOLS, kr * NA_KCOLS), axis=-1)
        p = p.reshape(s.shape).astype(v.dtype)
        outs.append(jnp.einsum('bhrqik,brikhd->brqhd', p, vb))
    return jnp.concatenate(outs, axis=2).reshape(B, S, H, hd)


def neighbourhood_mixer(h, w_qkv, rpb, w_o):
    B, S, D = h.shape
    qkv = (h @ w_qkv).reshape(B, S, 3, N_HEADS, HEAD_DIM)
    q = qkv[:, :, 0] * (HEAD_DIM ** -0.5)
    o = neighbourhood_attention(q, qkv[:, :, 1], qkv[:, :, 2], rpb)
    return o.reshape(B, S, D) @ w_o


def axial_rope_tables(S):
    t = jnp.arange(S, dtype=jnp.int32)
    pos = jnp.stack([t // GRID_W, t % GRID_W], axis=-1).astype(jnp.float32)
    inv_freq = ROPE_THETA ** (-jnp.arange(ROPE_PAIRS, dtype=jnp.float32) / ROPE_PAIRS)
    ang = pos[:, :, None] * inv_freq
    return jnp.cos(ang), jnp.sin(ang)


def apply_axial_rope(x, cos, sin):
    B, S, H, hd = x.shape
    xr = x.astype(jnp.float32).reshape(B, S, H, 2, 2, ROPE_PAIRS)
    x1, x2 = xr[..., 0, :], xr[..., 1, :]
    c, s = cos[None, :, None], sin[None, :, None]
    out = jnp.stack([x1 * c - x2 * s, x2 * c + x1 * s], axis=-2)
    return out.reshape(B, S, H, hd).astype(x.dtype)


def block_gqa_attention(q, k, v):
    B, S, H, hd = q.shape
    nblk = S // Q_BLOCK
    qg = q.reshape(B, nblk, Q_BLOCK, GQA_KV_HEADS, GQA_GROUP, hd).transpose(1, 0, 2, 3, 4, 5)
    scale = HEAD_DIM ** -0.5

    def one_block(qb):
        s = jnp.einsum('bqkgd,bskd->bkgqs', qb, k).astype(jnp.float32) * scale
        p = jax.nn.softmax(s, axis=-1).astype(v.dtype)
        return jnp.einsum('bkgqs,bskd->bqkgd', p, v)

    o = lax.map(one_block, qg)
    return o.transpose(1, 0, 2, 3, 4, 5).reshape(B, S, H * hd)


def gqa_mixer(h, cos, sin, w_qkv, q_gain, k_gain, w_o):
    B, S, D = h.shape
    kvd = GQA_KV_HEADS * HEAD_DIM
    qkv = h @ w_qkv
    q = qkv[..., :D].reshape(B, S, N_HEADS, HEAD_DIM)
    k = qkv[..., D:D + kvd].reshape(B, S, GQA_KV_HEADS, HEAD_DIM)
    v = qkv[..., D + kvd:].reshape(B, S, GQA_KV_HEADS, HEAD_DIM)
    q = apply_axial_rope(rms_norm(q, q_gain), cos, sin)
    k = apply_axial_rope(rms_norm(k, k_gain), cos, sin)
    return block_gqa_attention(q, k, v) @ w_o


def hierarchical_moe(h, w_group, b_group, w_expert, b_expert, w_gate, w_up, w_down):
    B, S, D = h.shape
    N = B * S
    xt = h.reshape(N, D)
    pg = jax.nn.softmax((xt @ w_group).astype(jnp.float32) + b_group.astype(jnp.float32), axis=-1)
    g_prob, g_idx = lax.top_k(pg, 1)
    le = ((xt @ w_expert).astype(jnp.float32) + b_expert.astype(jnp.float32)).reshape(N, N_GROUPS, EXPERTS_PER_GROUP)
    le = le[jnp.arange(N), g_idx[:, 0]]
    e_prob, e_local = lax.top_k(jax.nn.softmax(le, axis=-1), TOP_K)
    e_prob = e_prob / e_prob.sum(-1, keepdims=True)
    gates = g_prob * e_prob
    experts = g_idx * EXPERTS_PER_GROUP + e_local
    A = N * TOP_K
    flat_e = experts.reshape(A).astype(jnp.int32)
    flat_tok = jnp.repeat(jnp.arange(N, dtype=jnp.int32), TOP_K)
    flat_gate = gates.reshape(A)
    order = jnp.argsort(flat_e)
    e_sorted = flat_e[order]
    counts = jnp.bincount(flat_e, length=N_EXPERTS).astype(jnp.int32)
    padded = (counts + MOE_BLOCK - 1) // MOE_BLOCK * MOE_BLOCK
    pad_end = jnp.cumsum(padded)
    pad_start = pad_end - padded
    seg_start = jnp.cumsum(counts) - counts
    dest = pad_start[e_sorted] + (jnp.arange(A, dtype=jnp.int32) - seg_start[e_sorted])
    n_blocks = -(-A // MOE_BLOCK) + N_EXPERTS
    n_rows = n_blocks * MOE_BLOCK
    row_tok = jnp.full((n_rows,), N, jnp.int32).at[dest].set(flat_tok[order])
    row_gate = jnp.zeros((n_rows,), jnp.float32).at[dest].set(flat_gate[order])
    block_expert = jnp.minimum(
        jnp.searchsorted(pad_end, jnp.arange(n_blocks, dtype=jnp.int32) * MOE_BLOCK, side='right'),
        N_EXPERTS - 1)
    x_pad = jnp.concatenate([xt, jnp.zeros((1, D), xt.dtype)], axis=0)
    xs = x_pad[row_tok].reshape(n_blocks, MOE_BLOCK, D)

    def expert_block(args):
        xb, e = args
        return (jax.nn.silu(xb @ w_gate[e]) * (xb @ w_up[e])) @ w_down[e]

    ys = lax.map(expert_block, (xs, block_expert)).reshape(n_rows, D)
    ys = ys * row_gate[:, None].astype(ys.dtype)
    out = jnp.zeros((N + 1, D), ys.dtype).at[row_tok].add(ys)[:N]
    return out.reshape(B, S, D)


def setup_inputs(seed: int = 0) -> dict:
    key = jax.random.key(seed)
    ks = jax.random.split(key, 20)
    f32 = jnp.float32
    D = D_MODEL
    inv = D ** -0.5
    kvd = GQA_KV_HEADS * HEAD_DIM

    def nrm(k, shape, scale):
        return jax.random.normal(k, shape, f32) * scale

    na_cols = jnp.concatenate([jnp.ones((2 * D,), f32), jnp.full((D,), DEEPNORM_BETA, f32)])
    gqa_cols = jnp.concatenate([jnp.ones((D + kvd,), f32), jnp.full((kvd,), DEEPNORM_BETA, f32)])
    return {
        "x": nrm(ks[0], (BATCH, SEQ, D), 1.0),
        "c": nrm(ks[1], (BATCH, D), 1.0),
        "ada_w": nrm(ks[2], (DEPTH, D, 6 * D), ADA_INIT * inv),
        "ada_b": nrm(ks[3], (DEPTH, 6 * D), 0.01),
        "ln_g": 1.0 + nrm(ks[4], (DEPTH, 2, D), 0.01),
        "ln_b": nrm(ks[5], (DEPTH, 2, D), 0.01),
        "na_w_qkv": nrm(ks[6], (N_NA_LAYERS, D, 3 * D), inv) * na_cols,
        "na_rpb": nrm(ks[7], (N_NA_LAYERS, N_HEADS, 2 * NA_ROWS - 1, 2 * NA_COLS - 1), 0.1),
        "na_w_o": nrm(ks[8], (N_NA_LAYERS, D, D), inv * DEEPNORM_BETA),
        "gqa_w_qkv": nrm(ks[9], (N_GQA_LAYERS, D, D + 2 * kvd), inv) * gqa_cols,
        "gqa_q_norm": 1.0 + nrm(ks[10], (N_GQA_LAYERS, HEAD_DIM), 0.01),
        "gqa_k_norm": 1.0 + nrm(ks[11], (N_GQA_LAYERS, HEAD_DIM), 0.01),
        "gqa_w_o": nrm(ks[12], (N_GQA_LAYERS, D, D), inv * DEEPNORM_BETA),
        "moe_w_group": nrm(ks[13], (DEPTH, D, N_GROUPS), inv),
        "moe_b_group": nrm(ks[14], (DEPTH, N_GROUPS), 0.01),
        "moe_w_expert": nrm(ks[15], (DEPTH, D, N_EXPERTS), inv),
        "moe_b_expert": nrm(ks[16], (DEPTH, N_EXPERTS), 0.01),
        "moe_w_gate": nrm(ks[17], (DEPTH, N_EXPERTS, D, D_EXPERT), inv),
        "moe_w_up": nrm(ks[18], (DEPTH, N_EXPERTS, D, D_EXPERT), inv),
        "moe_w_down": nrm(ks[19], (DEPTH, N_EXPERTS, D_EXPERT, D), (D_EXPERT ** -0.5) * DEEPNORM_BETA),
    }


def reference(x, c, ada_w, ada_b, ln_g, ln_b, na_w_qkv, na_rpb, na_w_o, gqa_w_qkv, gqa_q_norm,
              gqa_k_norm, gqa_w_o, moe_w_group, moe_b_group, moe_w_expert, moe_b_expert,
              moe_w_gate, moe_w_up, moe_w_down):
    B, S, D = x.shape
    cos, sin = axial_rope_tables(S)
    c_act = jax.nn.silu(c)
    for i in range(DEPTH):
        mod = c_act @ ada_w[i] + ada_b[i]
        sh1, sc1, g1, sh2, sc2, g2 = jnp.split(mod[:, None, :], 6, axis=-1)
        h = x * (1 + sc1) + sh1
        j = i // N_MIXERS
        if i % N_MIXERS == 0:
            y = neighbourhood_mixer(h, na_w_qkv[j], na_rpb[j], na_w_o[j])
        else:
            y = gqa_mixer(h, cos, sin, gqa_w_qkv[j], gqa_q_norm[j], gqa_k_norm[j], gqa_w_o[j])
        x = layer_norm(DEEPNORM_ALPHA * x + (1 + g1) * y, ln_g[i, 0], ln_b[i, 0])
        h = x * (1 + sc2) + sh2
        y = hierarchical_moe(h, moe_w_group[i], moe_b_group[i], moe_w_expert[i], moe_b_expert[i],
                             moe_w_gate[i], moe_w_up[i], moe_w_down[i])
        x = layer_norm(DEEPNORM_ALPHA * x + (1 + g2) * y, ln_g[i, 1], ln_b[i, 1])
    return x
```

```python
import numpy as np
import concourse.bass as bass
import concourse.mybir as mybir
from concourse.bass_utils import run_bass_kernel_spmd

F32 = mybir.dt.float32
BF16 = mybir.dt.bfloat16
I32 = mybir.dt.int32
AF = mybir.ActivationFunctionType
ALU = mybir.AluOpType
AX = mybir.AxisListType

D = 1024
SEQ = 2048
NT = 16
KC = 8
NH = 16
HD = 64
DEPTH = 4
NE = 32
CAP = 384
NS = CAP // 128
ALPHA = float((2 * DEPTH) ** 0.25)
LN_EPS = 1e-5
RMS_EPS = 1e-6
NEG = -30000.0
BIG = 1.0e4
ARENA_WORDS = 53200


class _Rec:
    def __init__(self):
        self.call = None

    def __getattr__(self, name):
        def f(*a, **k):
            self.call = (name, a, k)
            return self
        return f


def _record(fn):
    r = _Rec()
    fn(r)
    assert r.call is not None
    return r.call


class Sched:
    def __init__(self, nc, n_dma_sems=8, same_engine_sync=True):
        self.nc = nc
        self.prog = {e: [] for e in ("pe", "act", "dve", "pool", "sp")}
        self.cnt = {e: 0 for e in self.prog}
        self.sems = {}
        self.waited = {e: {} for e in self.prog}
        self.last_w = {}
        self.readers = {}
        self.same_engine_sync = same_engine_sync
        self.n_dma_sems = n_dma_sems
        self.dma_ring = {q: {"next": 0, "tot": [0] * n_dma_sems} for q in ("sp", "act", "pool")}
        self._ctx = []

    def open(self):
        nc = self.nc
        for e in self.prog:
            cm = nc.semaphore("s_" + e)
            self.sems["s_" + e] = cm.__enter__()
            self._ctx.append(cm)
        for q in self.dma_ring:
            for i in range(self.n_dma_sems):
                nm = f"d_{q}{i}"
                cm = nc.semaphore(nm)
                self.sems[nm] = cm.__enter__()
                self._ctx.append(cm)

    def close(self):
        for cm in reversed(self._ctx):
            cm.__exit__(None, None, None)

    def _need(self, eng, tok, waits):
        if tok is None:
            return
        sem, val, peng = tok
        if peng == eng and (eng == "pe" or not self.same_engine_sync):
            return
        if self.waited[eng].get(sem, 0) >= val:
            return
        if waits.get(sem, 0) < val:
            waits[sem] = val

    def _collect(self, eng, reads, writes):
        waits = {}
        for k in reads:
            self._need(eng, self.last_w.get(k), waits)
        for k in writes:
            self._need(eng, self.last_w.get(k), waits)
            for t in self.readers.get(k, ()):
                self._need(eng, t, waits)
        for s, v in waits.items():
            self.waited[eng][s] = v
        return list(waits.items())

    def _update(self, tok, reads, writes):
        for k in writes:
            self.last_w[k] = tok
            self.readers[k] = []
        for k in reads:
            if k in writes:
                continue
            lst = self.readers.setdefault(k, [])
            lst.append(tok)
            if len(lst) > 48:
                best = {}
                for t in lst:
                    if t[0] not in best or best[t[0]][1] < t[1]:
                        best[t[0]] = t
                self.readers[k] = list(best.values())

    def op(self, eng, fn, reads=(), writes=()):
        waits = self._collect(eng, reads, writes)
        self.cnt[eng] += 1
        tok = ("s_" + eng, self.cnt[eng], eng)
        self.prog[eng].append((waits, _record(fn), ("s_" + eng, 1)))
        self._update(tok, reads, writes)
        return tok

    def dma(self, q, fn, reads=(), writes=()):
        ring = self.dma_ring[q]
        i = ring["next"]
        ring["next"] = (i + 1) % self.n_dma_sems
        sem = f"d_{q}{i}"
        waits = dict(self._collect(q, reads, writes))
        prev = ring["tot"][i]
        if prev and self.waited[q].get(sem, 0) < prev:
            waits[sem] = prev
            self.waited[q][sem] = prev
        ring["tot"][i] += 16
        tok = (sem, ring["tot"][i], "dma_" + q)
        self.prog[q].append((list(waits.items()), _record(fn), (sem, 16)))
        self._update(tok, reads, writes)
        return tok

    def barrier(self):
        targets = {}
        for e, c in self.cnt.items():
            if c:
                targets["s_" + e] = c
        for q, ring in self.dma_ring.items():
            for i, t in enumerate(ring["tot"]):
                if t:
                    targets[f"d_{q}{i}"] = t
        for eng in self.prog:
            waits = []
            for s, v in targets.items():
                if s == "s_" + eng and eng == "pe":
                    continue
                if self.waited[eng].get(s, 0) < v:
                    waits.append((s, v))
                    self.waited[eng][s] = v
            if waits:
                self.prog[eng].append((waits, None, None))

    def emit(self):
        nc = self.nc
        sems = self.sems
        prog = self.prog

        def run(engh, lst):
            for waits, fn, inc in lst:
                for s, v in waits:
                    engh.wait_ge(sems[s], v)
                if fn is not None:
                    name, a, k = fn
                    ins = getattr(engh, name)(*a, **k)
                    ins.then_inc(sems[inc[0]], inc[1])

        with nc.Block() as block:
            @block.sync
            def _(e):
                run(e, prog["sp"])

            @block.tensor
            def _(e):
                run(e, prog["pe"])

            @block.scalar
            def _(e):
                run(e, prog["act"])

            @block.vector
            def _(e):
                run(e, prog["dve"])

            @block.gpsimd
            def _(e):
                run(e, prog["pool"])


class Arena:
    def __init__(self, t, nwords):
        self.t = t
        self.n = nwords
        self.off = 0

    def alloc(self, shape, dt=F32):
        free = 1
        for s in shape[1:]:
            free *= s
        esz = 4 if dt in (F32, I32) else 2
        words = (free * esz + 3) // 4
        words = (words + 7) // 8 * 8
        assert self.off + words <= self.n, ("arena overflow", self.off, words, self.n)
        v = self.t[:, self.off:self.off + words]
        self.off += words
        if dt != F32:
            v = v.bitcast(dt)
        v = v[:, 0:free]
        if len(shape) == 3:
            v = v.rearrange("p (a b) -> p a b", a=shape[1])
        elif len(shape) == 4:
            v = v.rearrange("p (a b c) -> p a b c", a=shape[1], b=shape[2])
        return v

    def mark(self):
        return self.off

    def reset(self, m):
        self.off = m


def _na_patterns():
    pats = []
    for j in range(NT):
        kt_lo = min(max(j - 2, 0), 11)
        qi = np.arange(128)
        r = 2 * j + qi // 64
        c = qi % 64
        rs = np.clip(r - 4, 0, 24)
        ws = np.clip(c - 8, 0, 48)
        tiles = []
        for i in range(5):
            kt = kt_lo + i
            ki = np.arange(128)
            kr = 2 * kt + ki // 64
            kcol = ki % 64
            vr = (kr[:, None] >= rs[None, :]) & (kr[:, None] < rs[None, :] + 8)
            vc = (kcol[:, None] >= ws[None, :]) & (kcol[:, None] < ws[None, :] + 16)
            dr = np.clip(kr[:, None] - r[None, :] + 7, 0, 14)
            dc = np.clip(kcol[:, None] - c[None, :] + 15, 0, 30)
            valid = vr & vc
            tiles.append((valid, np.where(valid, dr, 0), np.where(valid, dc, 0)))
        pats.append(tiles)
    classes = []
    cls_of_j = []
    for j in range(NT):
        key = b"".join(a.tobytes() for t in pats[j] for a in t)
        found = None
        for ci, (k2, _) in enumerate(classes):
            if k2 == key:
                found = ci
                break
        if found is None:
            classes.append((key, pats[j]))
            found = len(classes) - 1
        cls_of_j.append(found)
    return [c[1] for c in classes], cls_of_j


_NA_CLASSES, _NA_CLS_OF_J = _na_patterns()
NCLS = len(_NA_CLASSES)


def _na_bias_table(rpb):
    L = rpb.shape[0]
    out = np.empty((L, NH, 128, NCLS * 5, 128), np.float32)
    for ci, tiles in enumerate(_NA_CLASSES):
        for i, (valid, dr, dc) in enumerate(tiles):
            g = rpb[:, :, dr, dc]
            out[:, :, :, ci * 5 + i, :] = np.where(valid[None, None], g, np.float32(NEG))
    return out


def _rope_tables():
    t = np.arange(SEQ)
    pos = np.stack([t // 64, t % 64], -1).astype(np.float32)
    inv = (np.float32(10000.0) ** (-np.arange(16, dtype=np.float32) / np.float32(16))).astype(np.float32)
    ang = pos[:, :, None] * inv
    c = np.cos(ang).astype(np.float32)
    s = np.sin(ang).astype(np.float32)
    C64 = np.stack([c, c], 2).reshape(SEQ, 64)
    S64 = np.stack([-s, s], 2).reshape(SEQ, 64)
    C64 = np.ascontiguousarray(C64.reshape(NT, 128, 64).transpose(1, 0, 2))
    S64 = np.ascontiguousarray(S64.reshape(NT, 128, 64).transpose(1, 0, 2))
    return C64, S64


def build_program(n_layers=DEPTH, stop=None, lite=False, decl=None):
    nc = bass.Bass("TRN2", target_bir_lowering=False)
    n_moe = n_layers if stop is None else n_layers - 1
    LD = n_layers if lite else DEPTH
    LNA = max(1, (n_layers + 1) // 2) if lite else 2
    LGQ = max(1, n_layers // 2) if lite else 2
    LMOE = max(1, n_moe) if lite else DEPTH
    EMOE = NE if (n_moe > 0 or not lite) else 1

    def din(name, shape, dt=F32):
        if decl is not None:
            decl[name] = tuple(shape)
        return nc.dram_tensor(name, list(shape), dt, kind="ExternalInput").ap()

    x_d = din("x", [SEQ, D])
    c_d = din("cfm", [128, KC])
    adaw_d = din("ada_w", [LD, D, 6 * D])
    adab_d = din("ada_b", [LD, 6 * D])
    lng_d = din("ln_g", [LD, 2, D])
    lnb_d = din("ln_b", [LD, 2, D])
    nawqkv_d = din("na_w_qkv", [LNA, D, 3 * D])
    nawo_d = din("na_w_o", [LNA, D, D])
    nabias_d = din("na_bias", [LNA, NH, 128, NCLS * 5 * 128])
    gwqkv_d = din("gqa_w_qkv", [LGQ, D, 1536])
    gwo_d = din("gqa_w_o", [LGQ, D, D])
    gqn_d = din("gqa_q_norm", [LGQ, HD])
    gkn_d = din("gqa_k_norm", [LGQ, HD])
    ropec_d = din("rope_c", [128, NT, 64])
    ropes_d = din("rope_s", [128, NT, 64])
    wr_d = din("moe_wr", [LMOE, D, 36])
    br_d = din("moe_br", [LMOE, 36])
    wg_d = din("moe_w_gate", [LMOE, EMOE, D, 512])
    wu_d = din("moe_w_up", [LMOE, EMOE, D, 512])
    wd_d = din("moe_w_down", [LMOE, EMOE, 512, D])
    ident_d = din("ident", [128, 128])
    tri_d = din("tri", [128, 128])
    iotac_d = din("iotac", [128, NE])
    out_d = nc.dram_tensor("out", [SEQ, D], F32, kind="ExternalOutput").ap()
    xs_d = nc.dram_tensor("xs_scr", [NE * CAP + 2 * CAP, D], BF16, kind="Internal").ap()
    ys_d = nc.dram_tensor("ys_scr", [NE * CAP + 2 * CAP, D], BF16, kind="Internal").ap()
    modrow_d = nc.dram_tensor("modrow", [2, D], F32, kind="Internal").ap()

    S = Sched(nc)
    with nc.sbuf_tensor("arena", [128, ARENA_WORDS], F32) as arena_t, \
            nc.psum_tensor("ps", [128, 8, 512], F32) as ps:
        ar = Arena(arena_t, ARENA_WORDS)
        S.open()

        def psb(bank):
            return ps[:, bank, :].bitcast(BF16)

        X = ar.alloc([128, NT, D])
        ident = ar.alloc([128, 128])
        identb = ar.alloc([128, 128], BF16)
        onesb = ar.alloc([128, 128], BF16)
        trib = ar.alloc([128, 128], BF16)
        iotac = ar.alloc([128, NE])
        alphaI = ar.alloc([128, 128])
        cact = ar.alloc([128, KC])
        cA = ar.alloc([128, KC, 128], BF16)
        SC1 = ar.alloc([128, KC]); SH1 = ar.alloc([128, KC])
        SC2 = ar.alloc([128, KC]); SH2 = ar.alloc([128, KC])
        G1B = ar.alloc([128, D]); G2B = ar.alloc([128, D])
        LNG = ar.alloc([128, D]); LNB = ar.alloc([128, D])
        TMPV = [ar.alloc([128, D]) for _ in range(2)]
        st_t = [ar.alloc([128, 12]) for _ in range(4)]
        mv_t = [ar.alloc([128, 2]) for _ in range(4)]
        sd_t = [ar.alloc([128, 2]) for _ in range(4)]
        eps_ln = ar.alloc([128, 1]); eps_rms = ar.alloc([128, 1])
        G1 = ar.alloc([128, NT]); G2 = ar.alloc([128, NT])
        D1i = ar.alloc([128, NT], I32); D2i = ar.alloc([128, NT], I32)
        A0 = ar.mark()
        A = ar.alloc([128, KC, SEQ], BF16)
        B = ar.alloc([128, NT, D], BF16)
        R0 = ar.mark()

        S.dma("sp", lambda e: e.dma_start(out=ident, in_=ident_d[:, :]), writes=["ident"])
        S.dma("pool", lambda e: e.dma_start(out=identb, in_=ident_d[:, :]), writes=["identb"])
        S.dma("pool", lambda e: e.dma_start(out=trib, in_=tri_d[:, :]), writes=["trib"])
        S.dma("sp", lambda e: e.dma_start(out=iotac, in_=iotac_d[:, :]), writes=["iotac"])
        S.dma("sp", lambda e: e.dma_start(out=cact, in_=c_d[:, :]), writes=["cact"])
        S.op("dve", lambda e: e.memset(onesb, 1.0), writes=["onesb"])
        S.op("dve", lambda e: e.tensor_scalar(out=alphaI, in0=ident, scalar1=ALPHA, scalar2=None, op0=ALU.mult),
             reads=["ident"], writes=["alphaI"])
        S.op("dve", lambda e: e.memset(eps_ln, LN_EPS), writes=["eps"])
        S.op("dve", lambda e: e.memset(eps_rms, RMS_EPS), writes=["eps"])
        for g4 in range(4):
            S.dma("sp", lambda e, g4=g4: e.dma_start(
                out=X[:, g4 * 4:(g4 + 1) * 4, :],
                in_=x_d[g4 * 512:(g4 + 1) * 512, :].rearrange("(t p) d -> p t d", p=128)),
                writes=[("X", t) for t in range(g4 * 4, g4 * 4 + 4)])
        S.op("act", lambda e: e.activation(out=cact, in_=cact, func=AF.Silu), reads=["cact"], writes=["cact"])
        S.op("dve", lambda e: e.tensor_copy(out=cA, in_=cact.unsqueeze(2).to_broadcast([128, KC, 128])),
             reads=["cact"], writes=["cA"])

        def mod_phase(i):
            ar.reset(R0)
            AW = [ar.alloc([128, KC, 512], BF16) for _ in range(2)]
            ABb = [ar.alloc([128, 512]) for _ in range(2)]
            T = [ar.alloc([128, 512]) for _ in range(2)]
            aw_v = adaw_d[i].rearrange("(k p) n -> p k n", p=128)
            for cg in range(12):
                b = cg % 2
                S.dma("pool", lambda e, b=b, cg=cg: e.dma_start(out=AW[b], in_=aw_v[:, :, cg * 512:(cg + 1) * 512]),
                      writes=[("AW", b)])
                S.dma("sp", lambda e, b=b, cg=cg: e.dma_start(
                    out=ABb[b], in_=adab_d[i:i + 1, cg * 512:(cg + 1) * 512].partition_broadcast(128)),
                    writes=[("ABb", b)])
                bank = cg % 2
                for kc in range(KC):
                    S.op("pe", lambda e, kc=kc, b=b, bank=bank: e.matmul(
                        ps[:, bank, :], lhsT=cA[:, kc, :], rhs=AW[b][:, kc, :], start=(kc == 0), stop=(kc == KC - 1)),
                        reads=["cA", ("AW", b)], writes=[("ps", bank)])
                kind = cg // 2
                half = cg % 2
                if kind in (2, 5):
                    GB = G1B if kind == 2 else G2B
                    S.op("dve", lambda e, GB=GB, bank=bank, b=b, half=half: e.scalar_tensor_tensor(
                        out=GB[:, half * 512:(half + 1) * 512], in0=ps[:, bank, :], scalar=1.0, in1=ABb[b],
                        op0=ALU.add, op1=ALU.add),
                        reads=[("ps", bank), ("ABb", b)], writes=[("GB", kind)])
                else:
                    add1 = 1.0 if kind in (1, 4) else 0.0
                    S.op("dve", lambda e, bank=bank, b=b, add1=add1: e.scalar_tensor_tensor(
                        out=T[b], in0=ps[:, bank, :], scalar=add1, in1=ABb[b], op0=ALU.add, op1=ALU.add),
                        reads=[("ps", bank), ("ABb", b)], writes=[("T", b)])
                    tb = 2 + b
                    for q in range(4):
                        S.op("pe", lambda e, q=q, b=b, tb=tb: e.transpose(
                            ps[:, tb, q * 128:(q + 1) * 128], T[b][:, q * 128:(q + 1) * 128], ident),
                            reads=[("T", b), "ident"], writes=[("ps", tb)])
                    if kind in (3, 4):
                        S.dma("sp", lambda e, b=b, kind=kind, half=half: e.dma_start(
                            out=modrow_d[kind - 3:kind - 2, half * 512:(half + 1) * 512], in_=T[b][0:1, :]),
                            reads=[("T", b)], writes=[("modrow", kind, half)])
                    dst = {0: SH1, 1: SC1, 3: SH2, 4: SC2}[kind]
                    S.op("dve", lambda e, dst=dst, tb=tb, half=half: e.tensor_copy(
                        out=dst[:, half * 4:(half + 1) * 4],
                        in_=ps[:, tb, :].rearrange("p (q n) -> p q n", q=4)[:, :, 0]),
                        reads=[("ps", tb)], writes=[("modfm", kind)])

        def build_hT(SC, SH, kind_sc, kind_sh):
            for tt in range(NT):
                for half in range(2):
                    bank = (2 * tt + half) % 4
                    for q in range(4):
                        kc = half * 4 + q
                        S.op("pe", lambda e, tt=tt, q=q, kc=kc, bank=bank: e.transpose(
                            ps[:, bank, q * 128:(q + 1) * 128], X[:, tt, kc * 128:(kc + 1) * 128], ident),
                            reads=[("X", tt), "ident"], writes=[("ps", bank)])
                    for q in range(4):
                        kc = half * 4 + q
                        S.op("act", lambda e, tt=tt, q=q, kc=kc, bank=bank: e.activation(
                            out=A[:, kc, tt * 128:(tt + 1) * 128], in_=ps[:, bank, q * 128:(q + 1) * 128],
                            func=AF.Identity, bias=SH[:, kc:kc + 1], scale=SC[:, kc:kc + 1]),
                            reads=[("ps", bank), ("modfm", kind_sc), ("modfm", kind_sh)], writes=[("A", tt)])

        def ln_stats(tt, v_ps, vkeys, vbufs):
            nb_ = len(vbufs)
            tb = tt % nb_
            v = vbufs[tb]
            sb_ = tt % 4
            for half in range(2):
                S.op("dve", lambda e, half=half: e.bn_stats(
                    out=st_t[sb_][:, half * 6:(half + 1) * 6], in_=v_ps[:, half * 512:(half + 1) * 512]),
                    reads=[vkeys[half]], writes=[("st", sb_)])
            S.op("dve", lambda e: e.bn_aggr(out=mv_t[sb_], in_=st_t[sb_]), reads=[("st", sb_)], writes=[("mv", sb_)])
            S.op("act", lambda e: e.activation(out=sd_t[sb_][:, 0:1], in_=mv_t[sb_][:, 1:2], func=AF.Sqrt, bias=eps_ln, scale=1.0),
                 reads=[("mv", sb_), "eps"], writes=[("sd", sb_)])
            S.op("dve", lambda e: e.reciprocal(out=sd_t[sb_][:, 0:1], in_=sd_t[sb_][:, 0:1]), reads=[("sd", sb_)], writes=[("sd", sb_)])
            S.op("dve", lambda e: e.scalar_tensor_tensor(
                out=sd_t[sb_][:, 1:2], in0=mv_t[sb_][:, 0:1], scalar=-1.0, in1=sd_t[sb_][:, 0:1], op0=ALU.mult, op1=ALU.mult),
                reads=[("mv", sb_), ("sd", sb_)], writes=[("sd2", sb_)])
            S.op("act", lambda e: e.activation(out=v, in_=v_ps, func=AF.Identity, bias=sd_t[sb_][:, 1:2], scale=sd_t[sb_][:, 0:1]),
                 reads=list(vkeys) + [("sd", sb_), ("sd2", sb_)], writes=[("v", tb)])

        def ln_finish(tt, store_out, vbufs):
            tb = tt % len(vbufs)
            v = vbufs[tb]
            S.op("dve", lambda e: e.tensor_tensor(out=v, in0=v, in1=LNG, op=ALU.mult),
                 reads=[("v", tb), "LNG"], writes=[("v", tb)])
            S.op("dve", lambda e: e.tensor_tensor(out=X[:, tt, :], in0=v, in1=LNB, op=ALU.add),
                 reads=[("v", tb), "LNB"], writes=[("X", tt)])
            if store_out:
                S.dma("sp", lambda e: e.dma_start(out=out_d[tt * 128:(tt + 1) * 128, :], in_=X[:, tt, :]),
                      reads=[("X", tt)], writes=[("out", tt)])

        def load_ln_params(i, which):
            S.dma("sp", lambda e: e.dma_start(out=LNG, in_=lng_d[i, which:which + 1, :].partition_broadcast(128)),
                  writes=["LNG"])
            S.dma("sp", lambda e: e.dma_start(out=LNB, in_=lnb_d[i, which:which + 1, :].partition_broadcast(128)),
                  writes=["LNB"])

        def na_phase(l):
            ar.reset(R0)
            Wb = [[ar.alloc([128, KC, 128], BF16) for _ in range(3)] for _ in range(2)]
            BI = [ar.alloc([128, NCLS * 5, 128], BF16) for _ in range(3)]
            QT = ar.alloc([128, SEQ], BF16)
            KT = ar.alloc([128, SEQ], BF16)
            V = ar.alloc([128, NT, 2, 66], BF16)
            PT = [ar.alloc([128, 640], BF16) for _ in range(3)]
            rc = [ar.alloc([128, 1]) for _ in range(2)]
            w_v = nawqkv_d[l].rearrange("(k p) n -> p k n", p=128)
            S.op("dve", lambda e: e.memset(V[:, :, :, 64:66], 1.0), writes=["V"])

            def load_w(p):
                wb = p % 2
                for m in range(3):
                    c0 = m * D + p * 128
                    S.dma("pool", lambda e, wb=wb, m=m, c0=c0: e.dma_start(out=Wb[wb][m], in_=w_v[:, :, c0:c0 + 128]),
                          writes=[("W", wb, m)])

            def load_bias(h):
                nchunk = NCLS * 5 // 5
                S.dma("pool", lambda e, h=h: e.dma_start(
                    out=BI[h % 3].rearrange("p (a b) n -> p a (b n)", a=nchunk),
                    in_=nabias_d[l, h].rearrange("p (a m) -> p a m", a=nchunk)), writes=[("BI", h % 3)])

            load_w(0)
            load_bias(0)
            load_bias(1)

            def exp_bias(h):
                S.op("act", lambda e: e.activation(
                    out=BI[h % 3].rearrange("p a n -> p (a n)"), in_=BI[h % 3].rearrange("p a n -> p (a n)"), func=AF.Exp),
                    reads=[("BI", h % 3)], writes=[("BI", h % 3)])

            def emit_ST(n, h, j):
                hh = h % 2
                r0 = hh * 64
                kt_lo = min(max(j - 2, 0), 11)
                sb = (n % 3) * 2
                for i in range(5):
                    bank = sb + i // 4
                    col = (i % 4) * 128
                    S.op("pe", lambda e, bank=bank, col=col, i=i: e.matmul(
                        ps[:, bank, col:col + 128], lhsT=KT[r0:r0 + 64, (kt_lo + i) * 128:(kt_lo + i + 1) * 128],
                        rhs=QT[r0:r0 + 64, j * 128:(j + 1) * 128], start=True, stop=True),
                        reads=["KT", "QT"], writes=[("ps", bank)])

            def emit_B(n, h, j):
                c = _NA_CLS_OF_J[j]
                sb = (n % 3) * 2
                pb = n % 3
                S.op("act", lambda e: e.activation(out=PT[pb][:, 0:512], in_=ps[:, sb, :], func=AF.Exp),
                     reads=[("ps", sb)], writes=[("PT", pb)])
                S.op("act", lambda e: e.activation(out=PT[pb][:, 512:640], in_=ps[:, sb + 1, 0:128], func=AF.Exp),
                     reads=[("ps", sb + 1)], writes=[("PT", pb)])
                S.op("dve", lambda e: e.tensor_tensor(
                    out=PT[pb], in0=PT[pb], in1=BI[h % 3][:, c * 5:(c + 1) * 5, :].rearrange("p a n -> p (a n)"),
                    op=ALU.mult), reads=[("PT", pb), ("BI", h % 3)], writes=[("PT", pb)])

            def emit_C(n, h, j):
                hh = h % 2
                kt_lo = min(max(j - 2, 0), 11)
                pb = n % 3
                ob = 6 + (n % 2)
                for i in range(5):
                    S.op("pe", lambda e, i=i: e.matmul(
                        ps[:, ob, 0:65], lhsT=PT[pb][:, i * 128:(i + 1) * 128], rhs=V[:, kt_lo + i, hh, 0:65],
                        start=(i == 0), stop=(i == 4)),
                        reads=[("PT", pb), "V"], writes=[("ps", ob)])
                rb_ = n % 2
                S.op("dve", lambda e: e.reciprocal(out=rc[rb_], in_=ps[:, ob, 64:65]),
                     reads=[("ps", ob)], writes=[("rc", rb_)])
                S.op("dve", lambda e: e.tensor_scalar(
                    out=B[:, j, h * 64:(h + 1) * 64], in0=ps[:, ob, 0:64], scalar1=rc[rb_], scalar2=None, op0=ALU.mult),
                    reads=[("ps", ob), ("rc", rb_)], writes=[("B", j)])

            exp_bias(0)
            exp_bias(1)
            for p in range(8):
                wb = p % 2
                if p + 1 < 8:
                    load_w(p + 1)
                for m in range(2):
                    for tg in range(4):
                        bank = 4 + (tg % 2)
                        for kc in range(KC):
                            S.op("pe", lambda e, m=m, tg=tg, kc=kc, bank=bank: e.matmul(
                                ps[:, bank, :], lhsT=Wb[wb][m][:, kc, :], rhs=A[:, kc, tg * 512:(tg + 1) * 512],
                                start=(kc == 0), stop=(kc == KC - 1)),
                                reads=[("W", wb, m)] + [("A", t) for t in range(tg * 4, tg * 4 + 4)],
                                writes=[("ps", bank)])
                        if m == 0:
                            S.op("act", lambda e, tg=tg, bank=bank: e.activation(
                                out=QT[:, tg * 512:(tg + 1) * 512], in_=ps[:, bank, :], func=AF.Copy, scale=0.125),
                                reads=[("ps", bank)], writes=["QT"])
                        else:
                            S.op("dve", lambda e, tg=tg, bank=bank: e.tensor_copy(
                                out=KT[:, tg * 512:(tg + 1) * 512], in_=ps[:, bank, :]),
                                reads=[("ps", bank)], writes=["KT"])
                for tq in range(4):
                    bank = 6 + (tq % 2)
                    for t4 in range(4):
                        tt = tq * 4 + t4
                        for kc in range(KC):
                            S.op("pe", lambda e, tt=tt, t4=t4, kc=kc, bank=bank: e.matmul(
                                ps[:, bank, t4 * 128:(t4 + 1) * 128], lhsT=A[:, kc, tt * 128:(tt + 1) * 128],
                                rhs=Wb[wb][2][:, kc, :], start=(kc == 0), stop=(kc == KC - 1)),
                                reads=[("W", wb, 2), ("A", tt)], writes=[("ps", bank)])
                    S.op("dve", lambda e, tq=tq, bank=bank: e.tensor_copy(
                        out=V[:, tq * 4:(tq + 1) * 4, :, 0:64],
                        in_=ps[:, bank, :].rearrange("p (t h d) -> p t h d", t=4, h=2)),
                        reads=[("ps", bank)], writes=["V"])
                steps = [(2 * p + hh, j) for hh in range(2) for j in range(NT)]
                emit_ST(0, *steps[0])
                emit_ST(1, *steps[1])
                emit_B(0, *steps[0])
                for n, (h, j) in enumerate(steps):
                    if j == 0 and h + 2 < NH:
                        load_bias(h + 2)
                    if j == 8 and h + 2 < NH:
                        exp_bias(h + 2)
                    if n + 2 < len(steps):
                        emit_ST(n + 2, *steps[n + 2])
                    if n + 1 < len(steps):
                        emit_B(n + 1, *steps[n + 1])
                    emit_C(n, h, j)

        def gqa_phase(l):
            ar.reset(R0)
            KTd = ar.alloc([128, 4, SEQ], BF16)
            V = ar.alloc([128, NT, 4, 66], BF16)
            QN = ar.alloc([128, HD]); KN = ar.alloc([128, HD])
            ssq = ar.alloc([128, 20])
            m1 = ar.mark()
            ar_b = Arena(arena_t, ARENA_WORDS)
            ar_b.off = A0 + KC * SEQ // 2
            Wg = [ar_b.alloc([128, KC, 512], BF16) for _ in range(3)]
            RC = ar_b.alloc([128, NT, 64]); RS = ar_b.alloc([128, NT, 64])
            assert ar_b.off <= R0
            SQ = ar.alloc([128, 1280]); QF = ar.alloc([128, 1280]); T1 = ar.alloc([128, 1280])
            QB = ar.alloc([128, 1024], BF16); KD = ar.alloc([128, 4, 2, 64], BF16)
            w_v = gwqkv_d[l].rearrange("(k p) n -> p k n", p=128)
            for cg in range(3):
                S.dma("pool", lambda e, cg=cg: e.dma_start(out=Wg[cg], in_=w_v[:, :, cg * 512:(cg + 1) * 512]),
                      writes=[("Wg", cg)])
            S.dma("sp", lambda e: e.dma_start(out=RC, in_=ropec_d[:, :, :]), writes=["RC"])
            S.dma("sp", lambda e: e.dma_start(out=RS, in_=ropes_d[:, :, :]), writes=["RS"])
            S.dma("sp", lambda e: e.dma_start(out=QN, in_=gqn_d[l:l + 1, :].partition_broadcast(128)), writes=["QN"])
            S.dma("sp", lambda e: e.dma_start(out=KN, in_=gkn_d[l:l + 1, :].partition_broadcast(128)), writes=["KN"])
            S.op("dve", lambda e: e.memset(V[:, :, :, 64:66], 1.0), writes=["V"])

            def hd(ap, nh):
                return ap.rearrange("p (h d) -> p h d", h=nh)

            for tt in range(NT):
                b0 = (tt % 2) * 3
                for cg in range(3):
                    bank = b0 + cg
                    for kc in range(KC):
                        S.op("pe", lambda e, cg=cg, kc=kc, bank=bank, tt=tt: e.matmul(
                            ps[:, bank, :], lhsT=A[:, kc, tt * 128:(tt + 1) * 128], rhs=Wg[cg][:, kc, :],
                            start=(kc == 0), stop=(kc == KC - 1)),
                            reads=[("A", tt), ("Wg", cg)], writes=[("ps", bank)])
                parts = [(ps[:, b0, :], 0, 8, ("ps", b0)), (ps[:, b0 + 1, :], 512, 8, ("ps", b0 + 1)),
                         (ps[:, b0 + 2, 0:256], 1024, 4, ("ps", b0 + 2))]
                for (pap, co, nh, pk) in parts:
                    S.op("act", lambda e, pap=pap, co=co, nh=nh: e.activation(
                        out=SQ[:, co:co + nh * 64], in_=pap, func=AF.Square),
                        reads=[pk], writes=["SQ"])
                S.op("act", lambda e, tt=tt, b0=b0: e.activation(
                    out=V[:, tt, :, 0:64], in_=hd(ps[:, b0 + 2, 256:512], 4), func=AF.Copy),
                    reads=[("ps", b0 + 2)], writes=["V"])
                S.op("dve", lambda e: e.tensor_reduce(out=ssq, in_=hd(SQ, 20), axis=AX.X, op=ALU.add),
                     reads=["SQ"], writes=["ssq"])
                S.op("act", lambda e: e.activation(out=ssq, in_=ssq, func=AF.Sqrt, bias=eps_rms, scale=1.0 / 64),
                     reads=["ssq", "eps"], writes=["ssq"])
                S.op("dve", lambda e: e.reciprocal(out=ssq, in_=ssq), reads=["ssq"], writes=["ssq"])
                h0 = 0
                for (pap, co, nh, pk) in parts:
                    S.op("dve", lambda e, pap=pap, co=co, nh=nh, h0=h0: e.tensor_tensor(
                        out=hd(QF[:, co:co + nh * 64], nh), in0=hd(pap, nh),
                        in1=ssq[:, h0:h0 + nh].unsqueeze(2).to_broadcast([128, nh, 64]), op=ALU.mult),
                        reads=[pk, "ssq"], writes=["QF"])
                    h0 += nh
                S.op("dve", lambda e: e.tensor_tensor(
                    out=hd(QF[:, 0:1024], 16), in0=hd(QF[:, 0:1024], 16),
                    in1=QN.unsqueeze(1).to_broadcast([128, 16, 64]), op=ALU.mult),
                    reads=["QF", "QN"], writes=["QF"])
                S.op("dve", lambda e: e.tensor_tensor(
                    out=hd(QF[:, 1024:1280], 4), in0=hd(QF[:, 1024:1280], 4),
                    in1=KN.unsqueeze(1).to_broadcast([128, 4, 64]), op=ALU.mult),
                    reads=["QF", "KN"], writes=["QF"])
                S.op("dve", lambda e, tt=tt: e.tensor_tensor(
                    out=hd(T1, 20), in0=hd(QF, 20), in1=RC[:, tt, :].unsqueeze(1).to_broadcast([128, 20, 64]),
                    op=ALU.mult), reads=["QF", "RC"], writes=["T1"])

                def v5(ap):
                    return ap.rearrange("p (h a f q) -> p h a f q", h=20, a=2, f=2)

                for f in range(2):
                    S.op("dve", lambda e, tt=tt, f=f: e.tensor_tensor(
                        out=v5(SQ)[:, :, :, f, :], in0=v5(QF)[:, :, :, 1 - f, :],
                        in1=RS[:, tt, :].rearrange("p (a f q) -> p a f q", a=2, f=2)[:, :, f, :]
                        .unsqueeze(1).to_broadcast([128, 20, 2, 16]), op=ALU.mult),
                        reads=["QF", "RS", "ssq"], writes=["SQ"])
                S.op("dve", lambda e: e.tensor_tensor(out=QB, in0=T1[:, 0:1024], in1=SQ[:, 0:1024], op=ALU.add),
                     reads=["T1", "SQ"], writes=["QB"])
                for dup in range(2):
                    S.op("dve", lambda e, dup=dup: e.tensor_tensor(
                        out=KD[:, :, dup, :], in0=hd(T1[:, 1024:1280], 4), in1=hd(SQ[:, 1024:1280], 4), op=ALU.add),
                        reads=["T1", "SQ"], writes=["KD"])
                for pr in range(8):
                    S.op("pe", lambda e, pr=pr: e.transpose(
                        psb(6)[:, pr * 128:(pr + 1) * 128], QB[:, pr * 128:(pr + 1) * 128], identb),
                        reads=["QB", "identb"], writes=[("ps", 6)])
                for g in range(4):
                    S.op("pe", lambda e, g=g: e.transpose(
                        psb(7)[:, g * 128:(g + 1) * 128], KD[:, g, :, :].rearrange("p a d -> p (a d)"), identb),
                        reads=["KD", "identb"], writes=[("ps", 7)])
                S.op("act", lambda e, tt=tt: e.activation(
                    out=A[:, :, tt * 128:(tt + 1) * 128], in_=psb(6).rearrange("p (k n) -> p k n", k=8), func=AF.Copy),
                    reads=[("ps", 6)], writes=[("A", tt)])
                S.op("dve", lambda e, tt=tt: e.tensor_copy(
                    out=KTd[:, :, tt * 128:(tt + 1) * 128], in_=psb(7)[:, 0:512].rearrange("p (g n) -> p g n", g=4)),
                    reads=[("ps", 7)], writes=["KTd"])

            S.barrier()
            ar.reset(m1)
            PT = [ar.alloc([128, 512], BF16) for _ in range(4)]
            rc = [ar.alloc([128, 4]) for _ in range(2)]
            KT2 = ar.alloc([128, 4, SEQ], BF16)
            S.op("act", lambda e: e.activation(out=KT2[64:128, 0:2, :], in_=KTd[64:128, 0:2, :], func=AF.Copy),
                 reads=["KTd"], writes=["KT2"])
            S.op("pool", lambda e: e.tensor_copy(out=KT2[64:128, 2:4, :], in_=KTd[64:128, 2:4, :]),
                 reads=["KTd"], writes=["KT2b"])
            S.op("dve", lambda e: e.memset(KT2[0:64, :, :], 0.0), writes=["KT2c"])
            S.op("dve", lambda e: e.memset(KTd[64:128, :, :], 0.0), reads=["KT2", "KT2b"], writes=["KTd"])
            steps = [(h, qg, kt) for h in range(NH) for qg in range(4) for kt in range(NT)]

            def emit_ST(n):
                h, qg, kt = steps[n]
                g = h // 4
                r0 = (h % 2) * 64
                bank = n % 4
                KK = KTd if h % 2 == 0 else KT2
                S.op("pe", lambda e: e.matmul(
                    ps[:, bank, :], lhsT=KK[:, g, kt * 128:(kt + 1) * 128],
                    rhs=A[:, h // 2, qg * 512:(qg + 1) * 512], start=True, stop=True),
                    reads=["KTd", "KT2", "KT2b", "KT2c"] + [("A", t) for t in range(qg * 4, qg * 4 + 4)], writes=[("ps", bank)])

            def emit_rest(n):
                h, qg, kt = steps[n]
                g = h // 4
                bank = n % 4
                pt = PT[n % 4]
                grp = n // NT
                ob = 4 + (grp % 2)
                S.op("act", lambda e: e.activation(out=pt, in_=ps[:, bank, :], func=AF.Exp, scale=0.125),
                     reads=[("ps", bank)], writes=[("PT", n % 4)])
                for qt in range(4):
                    S.op("pe", lambda e, qt=qt: e.matmul(
                        ps[:, ob, qt * 128:qt * 128 + 65], lhsT=pt[:, qt * 128:(qt + 1) * 128],
                        rhs=V[:, kt, g, 0:65], start=(kt == 0), stop=(kt == NT - 1)),
                        reads=[("PT", n % 4), "V"], writes=[("ps", ob)])
                if kt == NT - 1:
                    rb = grp % 2
                    S.op("dve", lambda e: e.reciprocal(
                        out=rc[rb], in_=ps[:, ob, :].rearrange("p (q n) -> p q n", q=4)[:, :, 64]),
                        reads=[("ps", ob)], writes=[("rc", rb)])
                    for qt in range(4):
                        tt = qg * 4 + qt
                        S.op("dve", lambda e, qt=qt, tt=tt: e.tensor_scalar(
                            out=B[:, tt, h * 64:(h + 1) * 64], in0=ps[:, ob, qt * 128:qt * 128 + 64],
                            scalar1=rc[rb][:, qt:qt + 1], scalar2=None, op0=ALU.mult),
                            reads=[("ps", ob), ("rc", rb)], writes=[("B", tt)])

            emit_ST(0)
            emit_ST(1)
            for n in range(len(steps)):
                if n + 2 < len(steps):
                    emit_ST(n + 2)
                emit_rest(n)

        def wo_ln_phase(i, wo_ap):
            ar.reset(R0)
            WO = ar.alloc([128, KC, D], BF16)
            VB = TMPV + [ar.alloc([128, D]) for _ in range(2)]
            wo_v = wo_ap.rearrange("(k p) n -> p k n", p=128)
            for half in range(2):
                S.dma("pool", lambda e, half=half: e.dma_start(
                    out=WO[:, :, half * 512:(half + 1) * 512], in_=wo_v[:, :, half * 512:(half + 1) * 512]),
                    writes=[("WO", half)])
            load_ln_params(i, 0)
            for half in range(2):
                S.op("dve", lambda e, half=half: e.tensor_tensor(
                    out=WO[:, :, half * 512:(half + 1) * 512], in0=WO[:, :, half * 512:(half + 1) * 512],
                    in1=G1B[:, half * 512:(half + 1) * 512].unsqueeze(1).to_broadcast([128, KC, 512]), op=ALU.mult),
                    reads=[("WO", half), ("GB", 2)], writes=[("WO", half)])
            for tt in range(NT):
                bank = tt % 2
                for kc in range(KC):
                    S.op("pe", lambda e, tt=tt, kc=kc, bank=bank: e.transpose(
                        psb(bank)[:, kc * 128:(kc + 1) * 128], B[:, tt, kc * 128:(kc + 1) * 128], identb),
                        reads=[("B", tt), "identb"], writes=[("ps", bank)])
                eng = "act" if tt % 2 == 0 else "dve"
                if eng == "act":
                    S.op("act", lambda e, tt=tt, bank=bank: e.activation(
                        out=A[:, :, tt * 128:(tt + 1) * 128], in_=psb(bank).rearrange("p (k n) -> p k n", k=8),
                        func=AF.Copy), reads=[("ps", bank)], writes=[("A", tt)])
                else:
                    S.op("dve", lambda e, tt=tt, bank=bank: e.tensor_copy(
                        out=A[:, :, tt * 128:(tt + 1) * 128], in_=psb(bank).rearrange("p (k n) -> p k n", k=8)),
                        reads=[("ps", bank)], writes=[("A", tt)])
            def wo_mm(tt):
                banks = [2 + 2 * (tt % 3), 3 + 2 * (tt % 3)]
                for half in range(2):
                    S.op("pe", lambda e, half=half, bank=banks[half]: e.matmul(
                        ps[:, bank, :], lhsT=alphaI, rhs=X[:, tt, half * 512:(half + 1) * 512], start=True, stop=False),
                        reads=["alphaI", ("X", tt)], writes=[("ps", banks[half])])
                    for kc in range(KC):
                        S.op("pe", lambda e, kc=kc, half=half, bank=banks[half]: e.matmul(
                            ps[:, bank, :], lhsT=A[:, kc, tt * 128:(tt + 1) * 128],
                            rhs=WO[:, kc, half * 512:(half + 1) * 512], start=False, stop=(kc == KC - 1)),
                            reads=[("A", tt), ("WO", half)], writes=[("ps", banks[half])])

            def wo_stats(tt):
                banks = [2 + 2 * (tt % 3), 3 + 2 * (tt % 3)]
                ln_stats(tt, ps[:, banks[0]:banks[0] + 2, :].rearrange("p a n -> p (a n)"),
                         [("ps", banks[0]), ("ps", banks[1])], vbufs=VB)

            wo_mm(0)
            wo_mm(1)
            wo_stats(0)
            for tt in range(NT):
                if tt + 2 < NT:
                    wo_mm(tt + 2)
                if tt + 1 < NT:
                    wo_stats(tt + 1)
                ln_finish(tt, store_out=False, vbufs=VB)

        def moe_phase(i, store_out):
            ar.reset(A0)
            NWB = 3
            WE = [[ar.alloc([128, KC, 512], BF16), ar.alloc([128, KC, 512], BF16), ar.alloc([128, 4, D], BF16)]
                  for _ in range(NWB)]
            m0 = ar.mark()

            def load_expert(e_):
                wb = e_ % NWB
                S.dma("pool", lambda e: e.dma_start(out=WE[wb][0], in_=wg_d[i, e_].rearrange("(k p) n -> p k n", p=128)),
                      writes=[("WE", wb, 0)])
                S.dma("pool", lambda e: e.dma_start(out=WE[wb][1], in_=wu_d[i, e_].rearrange("(k p) n -> p k n", p=128)),
                      writes=[("WE", wb, 1)])
                S.dma("pool", lambda e: e.dma_start(out=WE[wb][2], in_=wd_d[i, e_].rearrange("(k p) n -> p k n", p=128)),
                      writes=[("WE", wb, 2)])

            load_ln_params(i, 1)
            WR = ar.alloc([128, KC, 36]); RB = ar.alloc([128, 36])
            HT32 = [ar.alloc([128, KC, 128]) for _ in range(2)]
            L = ar.alloc([128, NT, 36])
            gmax = ar.alloc([128, NT]); gsum = ar.alloc([128, NT]); m1_ = ar.alloc([128, NT]); m2_ = ar.alloc([128, NT])
            dd = ar.alloc([128, NT]); e21 = ar.alloc([128, NT])
            gsel = ar.alloc([128, NT, 4]); gex = ar.alloc([128, NT, 4])
            lem = ar.alloc([128, NT, NE]); lem2 = ar.alloc([128, NT, NE])
            OH1 = ar.alloc([128, NT, NE]); OH2 = ar.alloc([128, NT, NE])
            Mall = ar.alloc([128, NT, NE], BF16)
            PEf = ar.alloc([128, NT, NE]); PR = ar.alloc([128, NT, NE])
            D1f = ar.alloc([128, NT]); D2f = ar.alloc([128, NT])
            HB = [ar.alloc([128, D], BF16) for _ in range(2)]
            HF = [ar.alloc([128, D]) for _ in range(2)]
            SHB, SCB = TMPV[0], TMPV[1]
            S.dma("sp", lambda e: e.dma_start(out=SHB, in_=modrow_d[0:1, :].partition_broadcast(128)),
                  reads=[("modrow", 3, 0), ("modrow", 3, 1)], writes=["SHB"])
            S.dma("sp", lambda e: e.dma_start(out=SCB, in_=modrow_d[1:2, :].partition_broadcast(128)),
                  reads=[("modrow", 4, 0), ("modrow", 4, 1)], writes=["SCB"])
            S.dma("sp", lambda e: e.dma_start(out=WR, in_=wr_d[i].rearrange("(k p) n -> p k n", p=128)), writes=["WR"])
            S.dma("sp", lambda e: e.dma_start(out=RB, in_=br_d[i:i + 1, :].partition_broadcast(128)), writes=["RB"])
            load_expert(0)
            load_expert(1)
            load_expert(2)
            for tt in range(NT):
                hb = tt % 2
                for half in range(2):
                    bank = (2 * tt + half) % 4
                    for q in range(4):
                        kc = half * 4 + q
                        S.op("pe", lambda e, tt=tt, q=q, kc=kc, bank=bank: e.transpose(
                            ps[:, bank, q * 128:(q + 1) * 128], X[:, tt, kc * 128:(kc + 1) * 128], ident),
                            reads=[("X", tt), "ident"], writes=[("ps", bank)])
                    for q in range(4):
                        kc = half * 4 + q
                        S.op("act", lambda e, q=q, kc=kc, bank=bank, hb=hb: e.activation(
                            out=HT32[hb][:, kc, :], in_=ps[:, bank, q * 128:(q + 1) * 128],
                            func=AF.Identity, bias=SH2[:, kc:kc + 1], scale=SC2[:, kc:kc + 1]),
                            reads=[("ps", bank), ("modfm", 3), ("modfm", 4)], writes=[("HT32", hb)])
                rbank = 4 + tt // 8
                c0 = (tt % 8) * 36
                for kc in range(KC):
                    S.op("pe", lambda e, kc=kc, hb=hb, rbank=rbank, c0=c0: e.matmul(
                        ps[:, rbank, c0:c0 + 36], lhsT=HT32[hb][:, kc, :], rhs=WR[:, kc, :],
                        start=(kc == 0), stop=(kc == KC - 1)),
                        reads=[("HT32", hb), "WR"], writes=[("ps", rbank)])
            k_ = "rt"
            for hf in range(2):
                S.op("dve", lambda e, hf=hf: e.tensor_tensor(
                    out=L[:, hf * 8:(hf + 1) * 8, :], in0=ps[:, 4 + hf, 0:288].rearrange("p (t x) -> p t x", t=8),
                    in1=RB.unsqueeze(1).to_broadcast([128, 8, 36]), op=ALU.add),
                    reads=[("ps", 4 + hf), "RB"], writes=[k_])
            LG = L[:, :, 0:4]
            LE = L[:, :, 4:36]
            S.op("dve", lambda e: e.tensor_reduce(out=gmax, in_=LG, axis=AX.X, op=ALU.max), reads=[k_], writes=[k_])
            S.op("dve", lambda e: e.tensor_tensor(out=gsel, in0=LG, in1=gmax.unsqueeze(2).to_broadcast([128, NT, 4]),
                                                  op=ALU.is_equal), reads=[k_], writes=[k_])
            S.op("dve", lambda e: e.tensor_tensor(out=gex, in0=LG, in1=gmax.unsqueeze(2).to_broadcast([128, NT, 4]),
                                                  op=ALU.subtract), reads=[k_], writes=[k_])
            S.op("act", lambda e: e.activation(out=gex, in_=gex, func=AF.Exp), reads=[k_], writes=[k_])
            S.op("dve", lambda e: e.tensor_reduce(out=gsum, in_=gex, axis=AX.X, op=ALU.add), reads=[k_], writes=[k_])
            S.op("dve", lambda e: e.reciprocal(out=gsum, in_=gsum), reads=[k_], writes=[k_])
            S.op("dve", lambda e: e.tensor_scalar(out=gsel, in0=gsel, scalar1=BIG, scalar2=-BIG, op0=ALU.mult, op1=ALU.add),
                 reads=[k_], writes=[k_])
            for hf in range(2):
                S.op("dve", lambda e, hf=hf: e.tensor_tensor(
                    out=lem[:, hf * 8:(hf + 1) * 8, :].rearrange("p t (g x) -> p (t g) x", g=4),
                    in0=LE[:, hf * 8:(hf + 1) * 8, :].rearrange("p t (g x) -> p t g x", g=4),
                    in1=gsel[:, hf * 8:(hf + 1) * 8, :].unsqueeze(3).to_broadcast([128, 8, 4, 8]), op=ALU.add),
                    reads=[k_], writes=[k_])
            S.op("dve", lambda e: e.tensor_reduce(out=m1_, in_=lem, axis=AX.X, op=ALU.max), reads=[k_], writes=[k_])
            S.op("dve", lambda e: e.tensor_tensor(out=OH1, in0=lem, in1=m1_.unsqueeze(2).to_broadcast([128, NT, NE]),
                                                  op=ALU.is_equal), reads=[k_], writes=["OH"])
            S.op("dve", lambda e: e.scalar_tensor_tensor(
                out=lem2.rearrange("p t x -> p (t x)"), in0=OH1.rearrange("p t x -> p (t x)"), scalar=-BIG,
                in1=lem.rearrange("p t x -> p (t x)"), op0=ALU.mult, op1=ALU.add), reads=[k_, "OH"], writes=[k_])
            S.op("dve", lambda e: e.tensor_reduce(out=m2_, in_=lem2, axis=AX.X, op=ALU.max), reads=[k_], writes=[k_])
            S.op("dve", lambda e: e.tensor_tensor(out=OH2, in0=lem2, in1=m2_.unsqueeze(2).to_broadcast([128, NT, NE]),
                                                  op=ALU.is_equal), reads=[k_], writes=["OH"])
            S.op("dve", lambda e: e.tensor_tensor(out=Mall, in0=OH1, in1=OH2, op=ALU.add), reads=["OH"], writes=["Mall"])
            S.op("dve", lambda e: e.tensor_tensor(out=dd, in0=m2_, in1=m1_, op=ALU.subtract), reads=[k_], writes=[k_])
            S.op("act", lambda e: e.activation(out=e21, in_=dd, func=AF.Exp), reads=[k_], writes=[k_])
            S.op("dve", lambda e: e.tensor_scalar(out=e21, in0=e21, scalar1=1.0, scalar2=None, op0=ALU.add), reads=[k_], writes=[k_])
            S.op("dve", lambda e: e.reciprocal(out=e21, in_=e21), reads=[k_], writes=[k_])
            S.op("dve", lambda e: e.tensor_tensor(out=G1, in0=e21, in1=gsum, op=ALU.mult), reads=[k_], writes=["G"])
            S.op("dve", lambda e: e.tensor_tensor(out=G2, in0=gsum, in1=G1, op=ALU.subtract), reads=[k_, "G"], writes=["G"])
            for tt in range(NT):
                S.op("pe", lambda e, tt=tt: e.matmul(
                    ps[:, 6, tt * NE:(tt + 1) * NE], lhsT=trib, rhs=Mall[:, tt, :], start=True, stop=(tt == 0)),
                    reads=["trib", "Mall"], writes=[("ps", 6)])
                for t2 in range(tt):
                    S.op("pe", lambda e, tt=tt, t2=t2: e.matmul(
                        ps[:, 6, tt * NE:(tt + 1) * NE], lhsT=onesb, rhs=Mall[:, t2, :], start=False, stop=(t2 == tt - 1)),
                        reads=["onesb", "Mall"], writes=[("ps", 6)])
            S.op("dve", lambda e: e.tensor_tensor(
                out=PEf, in0=ps[:, 6, :].rearrange("p (t x) -> p t x", t=NT),
                in1=iotac.unsqueeze(1).to_broadcast([128, NT, NE]), op=ALU.add),
                reads=[("ps", 6), "iotac"], writes=["PEf"])
            for (OH, Df, Di) in ((OH1, D1f, D1i), (OH2, D2f, D2i)):
                S.op("dve", lambda e, OH=OH: e.tensor_tensor(out=PR, in0=OH, in1=PEf, op=ALU.mult),
                     reads=["OH", "PEf"], writes=["PR"])
                S.op("dve", lambda e, Df=Df: e.tensor_reduce(out=Df, in_=PR, axis=AX.X, op=ALU.add),
                     reads=["PR"], writes=["Df"])
                S.op("dve", lambda e, Df=Df, Di=Di: e.tensor_copy(out=Di, in_=Df), reads=["Df"], writes=["Di"])
            for tt in range(NT):
                hb = tt % 2
                S.op("dve", lambda e, tt=tt, hb=hb: e.tensor_tensor(out=HF[hb], in0=X[:, tt, :], in1=SCB, op=ALU.mult),
                     reads=[("X", tt), "SCB"], writes=[("HF", hb)])
                S.op("dve", lambda e, hb=hb: e.tensor_tensor(out=HB[hb], in0=HF[hb], in1=SHB, op=ALU.add),
                     reads=[("HF", hb), "SHB"], writes=[("HB", hb)])
                for Di in (D1i, D2i):
                    S.dma("pool", lambda e, tt=tt, Di=Di, hb=hb: e.indirect_dma_start(
                        out=xs_d[:, :], out_offset=bass.IndirectOffsetOnAxis(ap=Di[:, tt:tt + 1], axis=0),
                        in_=HB[hb], in_offset=None),
                        reads=[("HB", hb), "Di"], writes=["XS"])
            S.barrier()
            ar.reset(m0)
            NXG = 6
            NYO = 4
            XG = [ar.alloc([128, D], BF16) for _ in range(NXG)]
            XeT = [ar.alloc([128, KC, CAP], BF16) for _ in range(2)]
            HTb = [ar.alloc([128, 4, CAP], BF16) for _ in range(2)]
            SG = [ar.alloc([128, CAP]) for _ in range(2)]
            YO = [ar.alloc([128, D], BF16) for _ in range(NYO)]

            def prep_load(e_):
                for s_ in range(NS):
                    xb = (e_ * NS + s_) % NXG
                    r0 = e_ * CAP + s_ * 128
                    S.dma("sp", lambda e, xb=xb, r0=r0: e.dma_start(out=XG[xb], in_=xs_d[r0:r0 + 128, :]),
                          reads=["XS"], writes=[("XG", xb)])

            def prep(e_):
                wb = e_ % 2
                for s_ in range(NS):
                    xb = (e_ * NS + s_) % NXG
                    bank = (e_ * NS + s_) % 2
                    for kc in range(KC):
                        S.op("pe", lambda e, xb=xb, kc=kc, bank=bank: e.transpose(
                            psb(bank)[:, kc * 128:(kc + 1) * 128], XG[xb][:, kc * 128:(kc + 1) * 128], identb),
                            reads=[("XG", xb), "identb"], writes=[("ps", bank)])
                    S.op("act", lambda e, s_=s_, bank=bank, wb=wb: e.activation(
                        out=XeT[wb][:, :, s_ * 128:(s_ + 1) * 128], in_=psb(bank).rearrange("p (k n) -> p k n", k=KC),
                        func=AF.Copy), reads=[("ps", bank)], writes=[("XeT", wb)])

            def compute(e_):
                wb = e_ % 2
                ww = e_ % NWB
                Wg_, Wu_, Wd_ = WE[ww]
                for fc in range(4):
                    gbank = 2 + 2 * (fc % 2)
                    ubank = gbank + 1
                    for (wi, W_, bank) in ((0, Wg_, gbank), (1, Wu_, ubank)):
                        for kc in range(KC):
                            S.op("pe", lambda e, fc=fc, kc=kc, W_=W_, bank=bank: e.matmul(
                                ps[:, bank, 0:CAP], lhsT=W_[:, kc, fc * 128:(fc + 1) * 128], rhs=XeT[wb][:, kc, :],
                                start=(kc == 0), stop=(kc == KC - 1)),
                                reads=[("WE", ww, wi), ("XeT", wb)], writes=[("ps", bank)])
                    S.op("act", lambda e, fc=fc, gbank=gbank: e.activation(out=SG[fc % 2], in_=ps[:, gbank, 0:CAP], func=AF.Silu),
                         reads=[("ps", gbank)], writes=[("SG", fc % 2)])
                    S.op("dve", lambda e, fc=fc, ubank=ubank: e.tensor_tensor(
                        out=HTb[wb][:, fc, :], in0=SG[fc % 2], in1=ps[:, ubank, 0:CAP], op=ALU.mult),
                        reads=[("SG", fc % 2), ("ps", ubank)], writes=[("HTb", wb)])
                for s_ in range(NS):
                    yb_ = (e_ * NS + s_) % NYO
                    for half in range(2):
                        bank = 6 + half
                        for fc in range(4):
                            S.op("pe", lambda e, s_=s_, half=half, fc=fc, bank=bank: e.matmul(
                                ps[:, bank, :], lhsT=HTb[wb][:, fc, s_ * 128:(s_ + 1) * 128],
                                rhs=Wd_[:, fc, half * 512:(half + 1) * 512], start=(fc == 0), stop=(fc == 3)),
                                reads=[("HTb", wb), ("WE", ww, 2)], writes=[("ps", bank)])
                        S.op("dve", lambda e, yb_=yb_, bank=bank, half=half: e.tensor_tensor(
                            out=YO[yb_][:, half * 512:(half + 1) * 512], in0=ps[:, bank, :],
                            in1=G2B[:, half * 512:(half + 1) * 512], op=ALU.mult),
                            reads=[("ps", bank), ("GB", 5)], writes=[("YO", yb_)])
                    r0 = e_ * CAP + s_ * 128
                    S.dma("pool", lambda e, yb_=yb_, r0=r0: e.dma_start(out=ys_d[r0:r0 + 128, :], in_=YO[yb_]),
                          reads=[("YO", yb_)], writes=["YS"])
                if e_ + NWB < NE:
                    load_expert(e_ + NWB)

            prep_load(0)
            prep_load(1)
            prep(0)
            for e_ in range(NE):
                if e_ + 2 < NE:
                    prep_load(e_ + 2)
                if e_ + 1 < NE:
                    prep(e_ + 1)
                compute(e_)
            S.barrier()
            ar.reset(A0)
            NYB = 4
            Y1 = [ar.alloc([128, D], BF16) for _ in range(NYB)]
            Y2 = [ar.alloc([128, D], BF16) for _ in range(NYB)]
            VB = TMPV + [ar.alloc([128, D]) for _ in range(2)]
            def gather(tt):
                yb = tt % NYB
                S.dma("pool", lambda e: e.indirect_dma_start(
                    out=Y1[yb], out_offset=None, in_=ys_d[:, :],
                    in_offset=bass.IndirectOffsetOnAxis(ap=D1i[:, tt:tt + 1], axis=0)),
                    reads=["YS", "Di"], writes=[("Y1", yb)])
                S.dma("pool", lambda e: e.indirect_dma_start(
                    out=Y2[yb], out_offset=None, in_=ys_d[:, :],
                    in_offset=bass.IndirectOffsetOnAxis(ap=D2i[:, tt:tt + 1], axis=0)),
                    reads=["YS", "Di"], writes=[("Y2", yb)])

            DG = [[ar.alloc([128, 128], BF16) for _ in range(2)] for _ in range(2)]
            for tt in range(NYB):
                gather(tt)

            def build(tt):
                yb = tt % NYB
                db = tt % 2
                for k_, Gk in enumerate((G1, G2)):
                    S.op("dve", lambda e, k_=k_, Gk=Gk: e.tensor_scalar(
                        out=DG[db][k_], in0=ident, scalar1=Gk[:, tt:tt + 1], scalar2=None, op0=ALU.mult),
                        reads=["ident", "G"], writes=[("DG", db, k_)])
                banks = [2 * (tt % 4), 2 * (tt % 4) + 1]
                for half in range(2):
                    hs = slice(half * 512, (half + 1) * 512)
                    S.op("pe", lambda e, half=half, hs=hs: e.matmul(
                        ps[:, banks[half], :], lhsT=alphaI, rhs=X[:, tt, hs], start=True, stop=False),
                        reads=["alphaI", ("X", tt)], writes=[("ps", banks[half])])
                    S.op("pe", lambda e, half=half, hs=hs: e.matmul(
                        ps[:, banks[half], :], lhsT=DG[db][0], rhs=Y1[yb][:, hs], start=False, stop=False),
                        reads=[("DG", db, 0), ("Y1", yb)], writes=[("ps", banks[half])])
                    S.op("pe", lambda e, half=half, hs=hs: e.matmul(
                        ps[:, banks[half], :], lhsT=DG[db][1], rhs=Y2[yb][:, hs], start=False, stop=True),
                        reads=[("DG", db, 1), ("Y2", yb)], writes=[("ps", banks[half])])

            def cstats(tt):
                banks = [2 * (tt % 4), 2 * (tt % 4) + 1]
                ln_stats(tt, ps[:, banks[0]:banks[0] + 2, :].rearrange("p a n -> p (a n)"),
                         [("ps", banks[0]), ("ps", banks[1])], vbufs=VB)

            build(0)
            build(1)
            cstats(0)
            for tt in range(NT):
                if tt + 2 < NT:
                    build(tt + 2)
                if tt + 1 < NT:
                    cstats(tt + 1)
                ln_finish(tt, store_out=store_out, vbufs=VB)
                if tt + NYB < NT:
                    gather(tt + NYB)

        def dump_dbg(kind):
            if kind == "mod":
                S.dma("sp", lambda e: e.dma_start(out=out_d[0:128, :], in_=G1B), reads=[("GB", 2)], writes=["o0"])
                S.dma("sp", lambda e: e.dma_start(out=out_d[128:256, :], in_=G2B), reads=[("GB", 5)], writes=["o1"])
                for n_, t_ in enumerate((SC1, SH1, SC2, SH2)):
                    S.dma("sp", lambda e, n_=n_, t_=t_: e.dma_start(out=out_d[256:384, n_ * 8:(n_ + 1) * 8], in_=t_),
                          reads=[("modfm", k) for k in (0, 1, 3, 4)], writes=[("o2", n_)])
            elif kind == "hT":
                for kc in range(KC):
                    S.dma("pool", lambda e, kc=kc: e.dma_start(out=out_d[kc * 128:(kc + 1) * 128, :], in_=A[:, kc, 0:1024]),
                          reads=[("A", t) for t in range(NT)], writes=[("o", kc)])
            elif kind == "attn":
                for tt in range(NT):
                    S.dma("pool", lambda e, tt=tt: e.dma_start(out=out_d[tt * 128:(tt + 1) * 128, :], in_=B[:, tt, :]),
                          reads=[("B", tt)], writes=[("o", tt)])

        def dump_x():
            for tt in range(NT):
                S.dma("sp", lambda e, tt=tt: e.dma_start(out=out_d[tt * 128:(tt + 1) * 128, :], in_=X[:, tt, :]),
                      reads=[("X", tt)], writes=[("out", tt)])

        done = False
        for i in range(n_layers):
            S.barrier()
            mod_phase(i)
            S.barrier()
            if stop == ("mod", i):
                dump_dbg("mod")
                done = True
                break
            build_hT(SC1, SH1, 1, 0)
            if stop == ("hT", i):
                S.barrier()
                dump_dbg("hT")
                done = True
                break
            l = i // 2
            if i % 2 == 0:
                na_phase(l)
                wo_ap = nawo_d[l]
            else:
                gqa_phase(l)
                wo_ap = gwo_d[l]
            S.barrier()
            if stop == ("attn", i):
                dump_dbg("attn")
                done = True
                break
            wo_ln_phase(i, wo_ap)
            S.barrier()
            if stop == ("mix", i):
                dump_x()
                done = True
                break
            last = (i == n_layers - 1)
            moe_phase(i, store_out=last)
            if last:
                done = True
        assert done
        S.barrier()
        S.emit()
        S.close()
    return nc


_CACHE = {}


def _prep_inputs(x, c, ada_w, ada_b, ln_g, ln_b, na_w_qkv, na_rpb, na_w_o, gqa_w_qkv, gqa_q_norm,
                 gqa_k_norm, gqa_w_o, moe_w_group, moe_b_group, moe_w_expert, moe_b_expert,
                 moe_w_gate, moe_w_up, moe_w_down):
    f = lambda a: np.ascontiguousarray(np.asarray(a), dtype=np.float32)
    C64, S64 = _rope_tables()
    shared = {
        "ada_w": f(ada_w), "ada_b": f(ada_b), "ln_g": f(ln_g), "ln_b": f(ln_b),
        "na_w_qkv": f(na_w_qkv), "na_w_o": f(na_w_o), "na_bias": _na_bias_table(f(na_rpb)).reshape(-1, NH, 128, NCLS * 5 * 128),
        "gqa_w_qkv": f(gqa_w_qkv), "gqa_w_o": f(gqa_w_o), "gqa_q_norm": f(gqa_q_norm), "gqa_k_norm": f(gqa_k_norm),
        "rope_c": C64, "rope_s": S64,
        "moe_wr": np.ascontiguousarray(np.concatenate([f(moe_w_group), f(moe_w_expert)], axis=-1)),
        "moe_br": np.ascontiguousarray(np.concatenate([f(moe_b_group), f(moe_b_expert)], axis=-1)),
        "moe_w_gate": f(moe_w_gate), "moe_w_up": f(moe_w_up), "moe_w_down": f(moe_w_down),
        "ident": np.eye(128, dtype=np.float32),
        "tri": np.triu(np.ones((128, 128), np.float32), 1),
        "iotac": np.ascontiguousarray(np.broadcast_to((np.arange(NE) * CAP).astype(np.float32), (128, NE))),
    }
    x = f(x)
    c = f(c)
    in_maps = []
    for b in range(8):
        m = dict(shared)
        m["x"] = x[b]
        m["cfm"] = np.ascontiguousarray(c[b].reshape(KC, 128).T)
        in_maps.append(m)
    return in_maps


def kernel(**inputs):
    in_maps = _prep_inputs(**inputs)
    key = "full"
    if key not in _CACHE:
        _CACHE[key] = build_program()
    nc = _CACHE[key]
    res = run_bass_kernel_spmd(nc, in_maps, core_ids=list(range(8)))
    return np.stack([np.asarray(r["out"], dtype=np.float32) for r in res.results], axis=0)
```

```python
import numpy as np
import concourse.bass as bass
import concourse.mybir as mybir
from concourse.bass_utils import run_bass_kernel_spmd

F32 = mybir.dt.float32
BF16 = mybir.dt.bfloat16
I32 = mybir.dt.int32
AF = mybir.ActivationFunctionType
ALU = mybir.AluOpType
AX = mybir.AxisListType

D = 1024
SEQ = 2048
NT = 16
KC = 8
NH = 16
HD = 64
DEPTH = 4
NE = 32
CAP = 384
NS = CAP // 128
ALPHA = float((2 * DEPTH) ** 0.25)
LN_EPS = 1e-5
RMS_EPS = 1e-6
NEG = -30000.0
BIG = 1.0e4
ARENA_WORDS = 53200


class _Rec:
    def __init__(self):
        self.call = None

    def __getattr__(self, name):
        def f(*a, **k):
            self.call = (name, a, k)
            return self
        return f


def _record(fn):
    r = _Rec()
    fn(r)
    assert r.call is not None
    return r.call


class Sched:
    def __init__(self, nc, n_dma_sems=8, same_engine_sync=True):
        self.nc = nc
        self.prog = {e: [] for e in ("pe", "act", "dve", "pool", "sp")}
        self.cnt = {e: 0 for e in self.prog}
        self.sems = {}
        self.waited = {e: {} for e in self.prog}
        self.last_w = {}
        self.readers = {}
        self.same_engine_sync = same_engine_sync
        self.n_dma_sems = n_dma_sems
        self.dma_ring = {q: {"next": 0, "tot": [0] * n_dma_sems} for q in ("sp", "act", "pool")}
        self._ctx = []

    def open(self):
        nc = self.nc
        for e in self.prog:
            cm = nc.semaphore("s_" + e)
            self.sems["s_" + e] = cm.__enter__()
            self._ctx.append(cm)
        for q in self.dma_ring:
            for i in range(self.n_dma_sems):
                nm = f"d_{q}{i}"
                cm = nc.semaphore(nm)
                self.sems[nm] = cm.__enter__()
                self._ctx.append(cm)

    def close(self):
        for cm in reversed(self._ctx):
            cm.__exit__(None, None, None)

    def _need(self, eng, tok, waits):
        if tok is None:
            return
        sem, val, peng = tok
        if peng == eng and (eng == "pe" or not self.same_engine_sync):
            return
        if self.waited[eng].get(sem, 0) >= val:
            return
        if waits.get(sem, 0) < val:
            waits[sem] = val

    def _collect(self, eng, reads, writes):
        waits = {}
        for k in reads:
            self._need(eng, self.last_w.get(k), waits)
        for k in writes:
            self._need(eng, self.last_w.get(k), waits)
            for t in self.readers.get(k, ()):
                self._need(eng, t, waits)
        for s, v in waits.items():
            self.waited[eng][s] = v
        return list(waits.items())

    def _update(self, tok, reads, writes):
        for k in writes:
            self.last_w[k] = tok
            self.readers[k] = []
        for k in reads:
            if k in writes:
                continue
            lst = self.readers.setdefault(k, [])
            lst.append(tok)
            if len(lst) > 48:
                best = {}
                for t in lst:
                    if t[0] not in best or best[t[0]][1] < t[1]:
                        best[t[0]] = t
                self.readers[k] = list(best.values())

    def op(self, eng, fn, reads=(), writes=()):
        waits = self._collect(eng, reads, writes)
        self.cnt[eng] += 1
        tok = ("s_" + eng, self.cnt[eng], eng)
        self.prog[eng].append((waits, _record(fn), ("s_" + eng, 1)))
        self._update(tok, reads, writes)
        return tok

    def dma(self, q, fn, reads=(), writes=()):
        ring = self.dma_ring[q]
        i = ring["next"]
        ring["next"] = (i + 1) % self.n_dma_sems
        sem = f"d_{q}{i}"
        waits = dict(self._collect(q, reads, writes))
        prev = ring["tot"][i]
        if prev and self.waited[q].get(sem, 0) < prev:
            waits[sem] = prev
            self.waited[q][sem] = prev
        ring["tot"][i] += 16
        tok = (sem, ring["tot"][i], "dma_" + q)
        self.prog[q].append((list(waits.items()), _record(fn), (sem, 16)))
        self._update(tok, reads, writes)
        return tok

    def barrier(self):
        targets = {}
        for e, c in self.cnt.items():
            if c:
                targets["s_" + e] = c
        for q, ring in self.dma_ring.items():
            for i, t in enumerate(ring["tot"]):
                if t:
                    targets[f"d_{q}{i}"] = t
        for eng in self.prog:
            waits = []
            for s, v in targets.items():
                if s == "s_" + eng and eng == "pe":
                    continue
                if self.waited[eng].get(s, 0) < v:
                    waits.append((s, v))
                    self.waited[eng][s] = v
            if waits:
                self.prog[eng].append((waits, None, None))

    def emit(self):
        nc = self.nc
        sems = self.sems
        prog = self.prog

        def run(engh, lst):
            for waits, fn, inc in lst:
                for s, v in waits:
                    engh.wait_ge(sems[s], v)
                if fn is not None:
                    name, a, k = fn
                    ins = getattr(engh, name)(*a, **k)
                    ins.then_inc(sems[inc[0]], inc[1])

        with nc.Block() as block:
            @block.sync
            def _(e):
                run(e, prog["sp"])

            @block.tensor
            def _(e):
                run(e, prog["pe"])

            @block.scalar
            def _(e):
                run(e, prog["act"])

            @block.vector
            def _(e):
                run(e, prog["dve"])

            @block.gpsimd
            def _(e):
                run(e, prog["pool"])


class Arena:
    def __init__(self, t, nwords):
        self.t = t
        self.n = nwords
        self.off = 0

    def alloc(self, shape, dt=F32):
        free = 1
        for s in shape[1:]:
            free *= s
        esz = 4 if dt in (F32, I32) else 2
        words = (free * esz + 3) // 4
        words = (words + 7) // 8 * 8
        assert self.off + words <= self.n, ("arena overflow", self.off, words, self.n)
        v = self.t[:, self.off:self.off + words]
        self.off += words
        if dt != F32:
            v = v.bitcast(dt)
        v = v[:, 0:free]
        if len(shape) == 3:
            v = v.rearrange("p (a b) -> p a b", a=shape[1])
        elif len(shape) == 4:
            v = v.rearrange("p (a b c) -> p a b c", a=shape[1], b=shape[2])
        return v

    def mark(self):
        return self.off

    def reset(self, m):
        self.off = m


def _na_patterns():
    pats = []
    for j in range(NT):
        kt_lo = min(max(j - 2, 0), 11)
        qi = np.arange(128)
        r = 2 * j + qi // 64
        c = qi % 64
        rs = np.clip(r - 4, 0, 24)
        ws = np.clip(c - 8, 0, 48)
        tiles = []
        for i in range(5):
            kt = kt_lo + i
            ki = np.arange(128)
            kr = 2 * kt + ki // 64
            kcol = ki % 64
            vr = (kr[:, None] >= rs[None, :]) & (kr[:, None] < rs[None, :] + 8)
            vc = (kcol[:, None] >= ws[None, :]) & (kcol[:, None] < ws[None, :] + 16)
            dr = np.clip(kr[:, None] - r[None, :] + 7, 0, 14)
            dc = np.clip(kcol[:, None] - c[None, :] + 15, 0, 30)
            valid = vr & vc
            tiles.append((valid, np.where(valid, dr, 0), np.where(valid, dc, 0)))
        pats.append(tiles)
    classes = []
    cls_of_j = []
    for j in range(NT):
        key = b"".join(a.tobytes() for t in pats[j] for a in t)
        found = None
        for ci, (k2, _) in enumerate(classes):
            if k2 == key:
                found = ci
                break
        if found is None:
            classes.append((key, pats[j]))
            found = len(classes) - 1
        cls_of_j.append(found)
    return [c[1] for c in classes], cls_of_j


_NA_CLASSES, _NA_CLS_OF_J = _na_patterns()
NCLS = len(_NA_CLASSES)


def _na_bias_table(rpb):
    L = rpb.shape[0]
    out = np.empty((L, NH, 128, NCLS * 5, 128), np.float32)
    for ci, tiles in enumerate(_NA_CLASSES):
        for i, (valid, dr, dc) in enumerate(tiles):
            g = rpb[:, :, dr, dc]
            out[:, :, :, ci * 5 + i, :] = np.where(valid[None, None], g, np.float32(NEG))
    return out


def _rope_tables():
    t = np.arange(SEQ)
    pos = np.stack([t // 64, t % 64], -1).astype(np.float32)
    inv = (np.float32(10000.0) ** (-np.arange(16, dtype=np.float32) / np.float32(16))).astype(np.float32)
    ang = pos[:, :, None] * inv
    c = np.cos(ang).astype(np.float32)
    s = np.sin(ang).astype(np.float32)
    C64 = np.stack([c, c], 2).reshape(SEQ, 64)
    S64 = np.stack([-s, s], 2).reshape(SEQ, 64)
    C64 = np.ascontiguousarray(C64.reshape(NT, 128, 64).transpose(1, 0, 2))
    S64 = np.ascontiguousarray(S64.reshape(NT, 128, 64).transpose(1, 0, 2))
    return C64, S64


def build_program(n_layers=DEPTH, stop=None, lite=False, decl=None):
    nc = bass.Bass("TRN2", target_bir_lowering=False)
    n_moe = n_layers if stop is None else n_layers - 1
    LD = n_layers if lite else DEPTH
    LNA = max(1, (n_layers + 1) // 2) if lite else 2
    LGQ = max(1, n_layers // 2) if lite else 2
    LMOE = max(1, n_moe) if lite else DEPTH
    EMOE = NE if (n_moe > 0 or not lite) else 1

    def din(name, shape, dt=F32):
        if decl is not None:
            decl[name] = tuple(shape)
        return nc.dram_tensor(name, list(shape), dt, kind="ExternalInput").ap()

    x_d = din("x", [SEQ, D])
    c_d = din("cfm", [128, KC])
    adaw_d = din("ada_w", [LD, D, 6 * D])
    adab_d = din("ada_b", [LD, 6 * D])
    lng_d = din("ln_g", [LD, 2, D])
    lnb_d = din("ln_b", [LD, 2, D])
    nawqkv_d = din("na_w_qkv", [LNA, D, 3 * D])
    nawo_d = din("na_w_o", [LNA, D, D])
    nabias_d = din("na_bias", [LNA, NH, 128, NCLS * 5 * 128])
    gwqkv_d = din("gqa_w_qkv", [LGQ, D, 1536])
    gwo_d = din("gqa_w_o", [LGQ, D, D])
    gqn_d = din("gqa_q_norm", [LGQ, HD])
    gkn_d = din("gqa_k_norm", [LGQ, HD])
    ropec_d = din("rope_c", [128, NT, 64])
    ropes_d = din("rope_s", [128, NT, 64])
    wr_d = din("moe_wr", [LMOE, D, 36])
    br_d = din("moe_br", [LMOE, 36])
    wg_d = din("moe_w_gate", [LMOE, EMOE, D, 512])
    wu_d = din("moe_w_up", [LMOE, EMOE, D, 512])
    wd_d = din("moe_w_down", [LMOE, EMOE, 512, D])
    ident_d = din("ident", [128, 128])
    tri_d = din("tri", [128, 128])
    iotac_d = din("iotac", [128, NE])
    out_d = nc.dram_tensor("out", [SEQ, D], F32, kind="ExternalOutput").ap()
    xs_d = nc.dram_tensor("xs_scr", [NE * CAP + 2 * CAP, D], BF16, kind="Internal").ap()
    ys_d = nc.dram_tensor("ys_scr", [NE * CAP + 2 * CAP, D], BF16, kind="Internal").ap()
    modrow_d = nc.dram_tensor("modrow", [2, D], F32, kind="Internal").ap()

    S = Sched(nc)
    with nc.sbuf_tensor("arena", [128, ARENA_WORDS], F32) as arena_t, \
            nc.psum_tensor("ps", [128, 8, 512], F32) as ps:
        ar = Arena(arena_t, ARENA_WORDS)
        S.open()

        def psb(bank):
            return ps[:, bank, :].bitcast(BF16)

        X = ar.alloc([128, NT, D])
        ident = ar.alloc([128, 128])
        identb = ar.alloc([128, 128], BF16)
        onesb = ar.alloc([128, 128], BF16)
        trib = ar.alloc([128, 128], BF16)
        iotac = ar.alloc([128, NE])
        alphaI = ar.alloc([128, 128])
        cact = ar.alloc([128, KC])
        cA = ar.alloc([128, KC, 128], BF16)
        SC1 = ar.alloc([128, KC]); SH1 = ar.alloc([128, KC])
        SC2 = ar.alloc([128, KC]); SH2 = ar.alloc([128, KC])
        G1B = ar.alloc([128, D]); G2B = ar.alloc([128, D])
        LNG = ar.alloc([128, D]); LNB = ar.alloc([128, D])
        TMPV = [ar.alloc([128, D]) for _ in range(2)]
        st_t = [ar.alloc([128, 12]) for _ in range(4)]
        mv_t = [ar.alloc([128, 2]) for _ in range(4)]
        sd_t = [ar.alloc([128, 2]) for _ in range(4)]
        eps_ln = ar.alloc([128, 1]); eps_rms = ar.alloc([128, 1])
        G1 = ar.alloc([128, NT]); G2 = ar.alloc([128, NT])
        D1i = ar.alloc([128, NT], I32); D2i = ar.alloc([128, NT], I32)
        A0 = ar.mark()
        A = ar.alloc([128, KC, SEQ], BF16)
        B = ar.alloc([128, NT, D], BF16)
        R0 = ar.mark()

        S.dma("sp", lambda e: e.dma_start(out=ident, in_=ident_d[:, :]), writes=["ident"])
        S.dma("pool", lambda e: e.dma_start(out=identb, in_=ident_d[:, :]), writes=["identb"])
        S.dma("pool", lambda e: e.dma_start(out=trib, in_=tri_d[:, :]), writes=["trib"])
        S.dma("sp", lambda e: e.dma_start(out=iotac, in_=iotac_d[:, :]), writes=["iotac"])
        S.dma("sp", lambda e: e.dma_start(out=cact, in_=c_d[:, :]), writes=["cact"])
        S.op("dve", lambda e: e.memset(onesb, 1.0), writes=["onesb"])
        S.op("dve", lambda e: e.tensor_scalar(out=alphaI, in0=ident, scalar1=ALPHA, scalar2=None, op0=ALU.mult),
             reads=["ident"], writes=["alphaI"])
        S.op("dve", lambda e: e.memset(eps_ln, LN_EPS), writes=["eps"])
        S.op("dve", lambda e: e.memset(eps_rms, RMS_EPS), writes=["eps"])
        for g4 in range(4):
            S.dma("sp", lambda e, g4=g4: e.dma_start(
                out=X[:, g4 * 4:(g4 + 1) * 4, :],
                in_=x_d[g4 * 512:(g4 + 1) * 512, :].rearrange("(t p) d -> p t d", p=128)),
                writes=[("X", t) for t in range(g4 * 4, g4 * 4 + 4)])
        S.op("act", lambda e: e.activation(out=cact, in_=cact, func=AF.Silu), reads=["cact"], writes=["cact"])
        S.op("dve", lambda e: e.tensor_copy(out=cA, in_=cact.unsqueeze(2).to_broadcast([128, KC, 128])),
             reads=["cact"], writes=["cA"])

        def mod_phase(i):
            ar.reset(R0)
            AW = [ar.alloc([128, KC, 512], BF16) for _ in range(2)]
            ABb = [ar.alloc([128, 512]) for _ in range(2)]
            T = [ar.alloc([128, 512]) for _ in range(2)]
            aw_v = adaw_d[i].rearrange("(k p) n -> p k n", p=128)
            for cg in range(12):
                b = cg % 2
                S.dma("pool", lambda e, b=b, cg=cg: e.dma_start(out=AW[b], in_=aw_v[:, :, cg * 512:(cg + 1) * 512]),
                      writes=[("AW", b)])
                S.dma("sp", lambda e, b=b, cg=cg: e.dma_start(
                    out=ABb[b], in_=adab_d[i:i + 1, cg * 512:(cg + 1) * 512].partition_broadcast(128)),
                    writes=[("ABb", b)])
                bank = cg % 2
                for kc in range(KC):
                    S.op("pe", lambda e, kc=kc, b=b, bank=bank: e.matmul(
                        ps[:, bank, :], lhsT=cA[:, kc, :], rhs=AW[b][:, kc, :], start=(kc == 0), stop=(kc == KC - 1)),
                        reads=["cA", ("AW", b)], writes=[("ps", bank)])
                kind = cg // 2
                half = cg % 2
                if kind in (2, 5):
                    GB = G1B if kind == 2 else G2B
                    S.op("dve", lambda e, GB=GB, bank=bank, b=b, half=half: e.scalar_tensor_tensor(
                        out=GB[:, half * 512:(half + 1) * 512], in0=ps[:, bank, :], scalar=1.0, in1=ABb[b],
                        op0=ALU.add, op1=ALU.add),
                        reads=[("ps", bank), ("ABb", b)], writes=[("GB", kind)])
                else:
                    add1 = 1.0 if kind in (1, 4) else 0.0
                    S.op("dve", lambda e, bank=bank, b=b, add1=add1: e.scalar_tensor_tensor(
                        out=T[b], in0=ps[:, bank, :], scalar=add1, in1=ABb[b], op0=ALU.add, op1=ALU.add),
                        reads=[("ps", bank), ("ABb", b)], writes=[("T", b)])
                    tb = 2 + b
                    for q in range(4):
                        S.op("pe", lambda e, q=q, b=b, tb=tb: e.transpose(
                            ps[:, tb, q * 128:(q + 1) * 128], T[b][:, q * 128:(q + 1) * 128], ident),
                            reads=[("T", b), "ident"], writes=[("ps", tb)])
                    if kind in (3, 4):
                        S.dma("sp", lambda e, b=b, kind=kind, half=half: e.dma_start(
                            out=modrow_d[kind - 3:kind - 2, half * 512:(half + 1) * 512], in_=T[b][0:1, :]),
                            reads=[("T", b)], writes=[("modrow", kind, half)])
                    dst = {0: SH1, 1: SC1, 3: SH2, 4: SC2}[kind]
                    S.op("dve", lambda e, dst=dst, tb=tb, half=half: e.tensor_copy(
                        out=dst[:, half * 4:(half + 1) * 4],
                        in_=ps[:, tb, :].rearrange("p (q n) -> p q n", q=4)[:, :, 0]),
                        reads=[("ps", tb)], writes=[("modfm", kind)])

        def build_hT(SC, SH, kind_sc, kind_sh):
            for tt in range(NT):
                for half in range(2):
                    bank = (2 * tt + half) % 4
                    for q in range(4):
                        kc = half * 4 + q
                        S.op("pe", lambda e, tt=tt, q=q, kc=kc, bank=bank: e.transpose(
                            ps[:, bank, q * 128:(q + 1) * 128], X[:, tt, kc * 128:(kc + 1) * 128], ident),
                            reads=[("X", tt), "ident"], writes=[("ps", bank)])
                    for q in range(4):
                        kc = half * 4 + q
                        S.op("act", lambda e, tt=tt, q=q, kc=kc, bank=bank: e.activation(
                            out=A[:, kc, tt * 128:(tt + 1) * 128], in_=ps[:, bank, q * 128:(q + 1) * 128],
                            func=AF.Identity, bias=SH[:, kc:kc + 1], scale=SC[:, kc:kc + 1]),
                            reads=[("ps", bank), ("modfm", kind_sc), ("modfm", kind_sh)], writes=[("A", tt)])

        def ln_stats(tt, v_ps, vkeys, vbufs):
            nb_ = len(vbufs)
            tb = tt % nb_
            v = vbufs[tb]
            sb_ = tt % 4
            for half in range(2):
                S.op("dve", lambda e, half=half: e.bn_stats(
                    out=st_t[sb_][:, half * 6:(half + 1) * 6], in_=v_ps[:, half * 512:(half + 1) * 512]),
                    reads=[vkeys[half]], writes=[("st", sb_)])
            S.op("dve", lambda e: e.bn_aggr(out=mv_t[sb_], in_=st_t[sb_]), reads=[("st", sb_)], writes=[("mv", sb_)])
            S.op("act", lambda e: e.activation(out=sd_t[sb_][:, 0:1], in_=mv_t[sb_][:, 1:2], func=AF.Sqrt, bias=eps_ln, scale=1.0),
                 reads=[("mv", sb_), "eps"], writes=[("sd", sb_)])
            S.op("dve", lambda e: e.reciprocal(out=sd_t[sb_][:, 0:1], in_=sd_t[sb_][:, 0:1]), reads=[("sd", sb_)], writes=[("sd", sb_)])
            S.op("dve", lambda e: e.scalar_tensor_tensor(
                out=sd_t[sb_][:, 1:2], in0=mv_t[sb_][:, 0:1], scalar=-1.0, in1=sd_t[sb_][:, 0:1], op0=ALU.mult, op1=ALU.mult),
                reads=[("mv", sb_), ("sd", sb_)], writes=[("sd2", sb_)])
            S.op("act", lambda e: e.activation(out=v, in_=v_ps, func=AF.Identity, bias=sd_t[sb_][:, 1:2], scale=sd_t[sb_][:, 0:1]),
                 reads=list(vkeys) + [("sd", sb_), ("sd2", sb_)], writes=[("v", tb)])

        def ln_finish(tt, store_out, vbufs):
            tb = tt % len(vbufs)
            v = vbufs[tb]
            S.op("dve", lambda e: e.tensor_tensor(out=v, in0=v, in1=LNG, op=ALU.mult),
                 reads=[("v", tb), "LNG"], writes=[("v", tb)])
            S.op("dve", lambda e: e.tensor_tensor(out=X[:, tt, :], in0=v, in1=LNB, op=ALU.add),
                 reads=[("v", tb), "LNB"], writes=[("X", tt)])
            if store_out:
                S.dma("sp", lambda e: e.dma_start(out=out_d[tt * 128:(tt + 1) * 128, :], in_=X[:, tt, :]),
                      reads=[("X", tt)], writes=[("out", tt)])

        def load_ln_params(i, which):
            S.dma("sp", lambda e: e.dma_start(out=LNG, in_=lng_d[i, which:which + 1, :].partition_broadcast(128)),
                  writes=["LNG"])
            S.dma("sp", lambda e: e.dma_start(out=LNB, in_=lnb_d[i, which:which + 1, :].partition_broadcast(128)),
                  writes=["LNB"])

        def na_phase(l):
            ar.reset(R0)
            Wb = [[ar.alloc([128, KC, 128], BF16) for _ in range(3)] for _ in range(2)]
            BI = [ar.alloc([128, NCLS * 5, 128], BF16) for _ in range(3)]
            QT = ar.alloc([128, SEQ], BF16)
            KT = ar.alloc([128, SEQ], BF16)
            V = ar.alloc([128, NT, 2, 66], BF16)
            PT = [ar.alloc([128, 640], BF16) for _ in range(3)]
            rc = [ar.alloc([128, 1]) for _ in range(2)]
            w_v = nawqkv_d[l].rearrange("(k p) n -> p k n", p=128)
            S.op("dve", lambda e: e.memset(V[:, :, :, 64:66], 1.0), writes=["V"])

            def load_w(p):
                wb = p % 2
                for m in range(3):
                    c0 = m * D + p * 128
                    S.dma("pool", lambda e, wb=wb, m=m, c0=c0: e.dma_start(out=Wb[wb][m], in_=w_v[:, :, c0:c0 + 128]),
                          writes=[("W", wb, m)])

            def load_bias(h):
                nchunk = NCLS * 5 // 5
                S.dma("pool", lambda e, h=h: e.dma_start(
                    out=BI[h % 3].rearrange("p (a b) n -> p a (b n)", a=nchunk),
                    in_=nabias_d[l, h].rearrange("p (a m) -> p a m", a=nchunk)), writes=[("BI", h % 3)])

            load_w(0)
            load_bias(0)
            load_bias(1)

            def exp_bias(h):
                S.op("act", lambda e: e.activation(
                    out=BI[h % 3].rearrange("p a n -> p (a n)"), in_=BI[h % 3].rearrange("p a n -> p (a n)"), func=AF.Exp),
                    reads=[("BI", h % 3)], writes=[("BI", h % 3)])

            def emit_ST(n, h, j):
                hh = h % 2
                r0 = hh * 64
                kt_lo = min(max(j - 2, 0), 11)
                sb = (n % 3) * 2
                for i in range(5):
                    bank = sb + i // 4
                    col = (i % 4) * 128
                    S.op("pe", lambda e, bank=bank, col=col, i=i: e.matmul(
                        ps[:, bank, col:col + 128], lhsT=KT[r0:r0 + 64, (kt_lo + i) * 128:(kt_lo + i + 1) * 128],
                        rhs=QT[r0:r0 + 64, j * 128:(j + 1) * 128], start=True, stop=True),
                        reads=["KT", "QT"], writes=[("ps", bank)])

            def emit_B(n, h, j):
                c = _NA_CLS_OF_J[j]
                sb = (n % 3) * 2
                pb = n % 3
                S.op("act", lambda e: e.activation(out=PT[pb][:, 0:512], in_=ps[:, sb, :], func=AF.Exp),
                     reads=[("ps", sb)], writes=[("PT", pb)])
                S.op("act", lambda e: e.activation(out=PT[pb][:, 512:640], in_=ps[:, sb + 1, 0:128], func=AF.Exp),
                     reads=[("ps", sb + 1)], writes=[("PT", pb)])
                S.op("dve", lambda e: e.tensor_tensor(
                    out=PT[pb], in0=PT[pb], in1=BI[h % 3][:, c * 5:(c + 1) * 5, :].rearrange("p a n -> p (a n)"),
                    op=ALU.mult), reads=[("PT", pb), ("BI", h % 3)], writes=[("PT", pb)])

            def emit_C(n, h, j):
                hh = h % 2
                kt_lo = min(max(j - 2, 0), 11)
                pb = n % 3
                ob = 6 + (n % 2)
                for i in range(5):
                    S.op("pe", lambda e, i=i: e.matmul(
                        ps[:, ob, 0:65], lhsT=PT[pb][:, i * 128:(i + 1) * 128], rhs=V[:, kt_lo + i, hh, 0:65],
                        start=(i == 0), stop=(i == 4)),
                        reads=[("PT", pb), "V"], writes=[("ps", ob)])
                rb_ = n % 2
                S.op("dve", lambda e: e.reciprocal(out=rc[rb_], in_=ps[:, ob, 64:65]),
                     reads=[("ps", ob)], writes=[("rc", rb_)])
                S.op("dve", lambda e: e.tensor_scalar(
                    out=B[:, j, h * 64:(h + 1) * 64], in0=ps[:, ob, 0:64], scalar1=rc[rb_], scalar2=None, op0=ALU.mult),
                    reads=[("ps", ob), ("rc", rb_)], writes=[("B", j)])

            exp_bias(0)
            exp_bias(1)
            for p in range(8):
                wb = p % 2
                if p + 1 < 8:
                    load_w(p + 1)
                for m in range(2):
                    for tg in range(4):
                        bank = 4 + (tg % 2)
                        for kc in range(KC):
                            S.op("pe", lambda e, m=m, tg=tg, kc=kc, bank=bank: e.matmul(
                                ps[:, bank, :], lhsT=Wb[wb][m][:, kc, :], rhs=A[:, kc, tg * 512:(tg + 1) * 512],
                                start=(kc == 0), stop=(kc == KC - 1)),
                                reads=[("W", wb, m)] + [("A", t) for t in range(tg * 4, tg * 4 + 4)],
                                writes=[("ps", bank)])
                        if m == 0:
                            S.op("act", lambda e, tg=tg, bank=bank: e.activation(
                                out=QT[:, tg * 512:(tg + 1) * 512], in_=ps[:, bank, :], func=AF.Copy, scale=0.125),
                                reads=[("ps", bank)], writes=["QT"])
                        else:
                            S.op("dve", lambda e, tg=tg, bank=bank: e.tensor_copy(
                                out=KT[:, tg * 512:(tg + 1) * 512], in_=ps[:, bank, :]),
                                reads=[("ps", bank)], writes=["KT"])
                for tq in range(4):
                    bank = 6 + (tq % 2)
                    for t4 in range(4):
                        tt = tq * 4 + t4
                        for kc in range(KC):
                            S.op("pe", lambda e, tt=tt, t4=t4, kc=kc, bank=bank: e.matmul(
                                ps[:, bank, t4 * 128:(t4 + 1) * 128], lhsT=A[:, kc, tt * 128:(tt + 1) * 128],
                                rhs=Wb[wb][2][:, kc, :], start=(kc == 0), stop=(kc == KC - 1)),
                                reads=[("W", wb, 2), ("A", tt)], writes=[("ps", bank)])
                    S.op("dve", lambda e, tq=tq, bank=bank: e.tensor_copy(
                        out=V[:, tq * 4:(tq + 1) * 4, :, 0:64],
                        in_=ps[:, bank, :].rearrange("p (t h d) -> p t h d", t=4, h=2)),
                        reads=[("ps", bank)], writes=["V"])
                steps = [(2 * p + hh, j) for hh in range(2) for j in range(NT)]
                emit_ST(0, *steps[0])
                emit_ST(1, *steps[1])
                emit_B(0, *steps[0])
                for n, (h, j) in enumerate(steps):
                    if j == 0 and h + 2 < NH:
                        load_bias(h + 2)
                    if j == 8 and h + 2 < NH:
                        exp_bias(h + 2)
                    if n + 2 < len(steps):
                        emit_ST(n + 2, *steps[n + 2])
                    if n + 1 < len(steps):
                        emit_B(n + 1, *steps[n + 1])
                    emit_C(n, h, j)

        def gqa_phase(l):
            ar.reset(R0)
            KTd = ar.alloc([128, 4, SEQ], BF16)
            V = ar.alloc([128, NT, 4, 66], BF16)
            QN = ar.alloc([128, HD]); KN = ar.alloc([128, HD])
            ssq = ar.alloc([128, 20])
            m1 = ar.mark()
            ar_b = Arena(arena_t, ARENA_WORDS)
            ar_b.off = A0 + KC * SEQ // 2
            Wg = [ar_b.alloc([128, KC, 512], BF16) for _ in range(3)]
            RC = ar_b.alloc([128, NT, 64]); RS = ar_b.alloc([128, NT, 64])
            assert ar_b.off <= R0
            SQ = ar.alloc([128, 1280]); QF = ar.alloc([128, 1280]); T1 = ar.alloc([128, 1280])
            QB = ar.alloc([128, 1024], BF16); KD = ar.alloc([128, 4, 2, 64], BF16)
            w_v = gwqkv_d[l].rearrange("(k p) n -> p k n", p=128)
            for cg in range(3):
                S.dma("pool", lambda e, cg=cg: e.dma_start(out=Wg[cg], in_=w_v[:, :, cg * 512:(cg + 1) * 512]),
                      writes=[("Wg", cg)])
            S.dma("sp", lambda e: e.dma_start(out=RC, in_=ropec_d[:, :, :]), writes=["RC"])
            S.dma("sp", lambda e: e.dma_start(out=RS, in_=ropes_d[:, :, :]), writes=["RS"])
            S.dma("sp", lambda e: e.dma_start(out=QN, in_=gqn_d[l:l + 1, :].partition_broadcast(128)), writes=["QN"])
            S.dma("sp", lambda e: e.dma_start(out=KN, in_=gkn_d[l:l + 1, :].partition_broadcast(128)), writes=["KN"])
            S.op("dve", lambda e: e.memset(V[:, :, :, 64:66], 1.0), writes=["V"])

            def hd(ap, nh):
                return ap.rearrange("p (h d) -> p h d", h=nh)

            for tt in range(NT):
                b0 = (tt % 2) * 3
                for cg in range(3):
                    bank = b0 + cg
                    for kc in range(KC):
                        S.op("pe", lambda e, cg=cg, kc=kc, bank=bank, tt=tt: e.matmul(
                            ps[:, bank, :], lhsT=A[:, kc, tt * 128:(tt + 1) * 128], rhs=Wg[cg][:, kc, :],
                            start=(kc == 0), stop=(kc == KC - 1)),
                            reads=[("A", tt), ("Wg", cg)], writes=[("ps", bank)])
                parts = [(ps[:, b0, :], 0, 8, ("ps", b0)), (ps[:, b0 + 1, :], 512, 8, ("ps", b0 + 1)),
                         (ps[:, b0 + 2, 0:256], 1024, 4, ("ps", b0 + 2))]
                for (pap, co, nh, pk) in parts:
                    S.op("act", lambda e, pap=pap, co=co, nh=nh: e.activation(
                        out=SQ[:, co:co + nh * 64], in_=pap, func=AF.Square),
                        reads=[pk], writes=["SQ"])
                S.op("act", lambda e, tt=tt, b0=b0: e.activation(
                    out=V[:, tt, :, 0:64], in_=hd(ps[:, b0 + 2, 256:512], 4), func=AF.Copy),
                    reads=[("ps", b0 + 2)], writes=["V"])
                S.op("dve", lambda e: e.tensor_reduce(out=ssq, in_=hd(SQ, 20), axis=AX.X, op=ALU.add),
                     reads=["SQ"], writes=["ssq"])
                S.op("act", lambda e: e.activation(out=ssq, in_=ssq, func=AF.Sqrt, bias=eps_rms, scale=1.0 / 64),
                     reads=["ssq", "eps"], writes=["ssq"])
                S.op("dve", lambda e: e.reciprocal(out=ssq, in_=ssq), reads=["ssq"], writes=["ssq"])
                h0 = 0
                for (pap, co, nh, pk) in parts:
                    S.op("dve", lambda e, pap=pap, co=co, nh=nh, h0=h0: e.tensor_tensor(
                        out=hd(QF[:, co:co + nh * 64], nh), in0=hd(pap, nh),
                        in1=ssq[:, h0:h0 + nh].unsqueeze(2).to_broadcast([128, nh, 64]), op=ALU.mult),
                        reads=[pk, "ssq"], writes=["QF"])
                    h0 += nh
                S.op("dve", lambda e: e.tensor_tensor(
                    out=hd(QF[:, 0:1024], 16), in0=hd(QF[:, 0:1024], 16),
                    in1=QN.unsqueeze(1).to_broadcast([128, 16, 64]), op=ALU.mult),
                    reads=["QF", "QN"], writes=["QF"])
                S.op("dve", lambda e: e.tensor_tensor(
                    out=hd(QF[:, 1024:1280], 4), in0=hd(QF[:, 1024:1280], 4),
                    in1=KN.unsqueeze(1).to_broadcast([128, 4, 64]), op=ALU.mult),
                    reads=["QF", "KN"], writes=["QF"])
                S.op("dve", lambda e, tt=tt: e.tensor_tensor(
                    out=hd(T1, 20), in0=hd(QF, 20), in1=RC[:, tt, :].unsqueeze(1).to_broadcast([128, 20, 64]),
                    op=ALU.mult), reads=["QF", "RC"], writes=["T1"])

                def v5(ap):
                    return ap.rearrange("p (h a f q) -> p h a f q", h=20, a=2, f=2)

                for f in range(2):
                    S.op("dve", lambda e, tt=tt, f=f: e.tensor_tensor(
                        out=v5(SQ)[:, :, :, f, :], in0=v5(QF)[:, :, :, 1 - f, :],
                        in1=RS[:, tt, :].rearrange("p (a f q) -> p a f q", a=2, f=2)[:, :, f, :]
                        .unsqueeze(1).to_broadcast([128, 20, 2, 16]), op=ALU.mult),
                        reads=["QF", "RS", "ssq"], writes=["SQ"])
                S.op("dve", lambda e: e.tensor_tensor(out=QB, in0=T1[:, 0:1024], in1=SQ[:, 0:1024], op=ALU.add),
                     reads=["T1", "SQ"], writes=["QB"])
                for dup in range(2):
                    S.op("dve", lambda e, dup=dup: e.tensor_tensor(
                        out=KD[:, :, dup, :], in0=hd(T1[:, 1024:1280], 4), in1=hd(SQ[:, 1024:1280], 4), op=ALU.add),
                        reads=["T1", "SQ"], writes=["KD"])
                for pr in range(8):
                    S.op("pe", lambda e, pr=pr: e.transpose(
                        psb(6)[:, pr * 128:(pr + 1) * 128], QB[:, pr * 128:(pr + 1) * 128], identb),
                        reads=["QB", "identb"], writes=[("ps", 6)])
                for g in range(4):
                    S.op("pe", lambda e, g=g: e.transpose(
                        psb(7)[:, g * 128:(g + 1) * 128], KD[:, g, :, :].rearrange("p a d -> p (a d)"), identb),
                        reads=["KD", "identb"], writes=[("ps", 7)])
                S.op("act", lambda e, tt=tt: e.activation(
                    out=A[:, :, tt * 128:(tt + 1) * 128], in_=psb(6).rearrange("p (k n) -> p k n", k=8), func=AF.Copy),
                    reads=[("ps", 6)], writes=[("A", tt)])
                S.op("dve", lambda e, tt=tt: e.tensor_copy(
                    out=KTd[:, :, tt * 128:(tt + 1) * 128], in_=psb(7)[:, 0:512].rearrange("p (g n) -> p g n", g=4)),
                    reads=[("ps", 7)], writes=["KTd"])

            S.barrier()
            ar.reset(m1)
            PT = [ar.alloc([128, 512], BF16) for _ in range(4)]
            rc = [ar.alloc([128, 4]) for _ in range(2)]
            KT2 = ar.alloc([128, 4, SEQ], BF16)
            S.op("act", lambda e: e.activation(out=KT2[64:128, 0:2, :], in_=KTd[64:128, 0:2, :], func=AF.Copy),
                 reads=["KTd"], writes=["KT2"])
            S.op("pool", lambda e: e.tensor_copy(out=KT2[64:128, 2:4, :], in_=KTd[64:128, 2:4, :]),
                 reads=["KTd"], writes=["KT2b"])
            S.op("dve", lambda e: e.memset(KT2[0:64, :, :], 0.0), writes=["KT2c"])
            S.op("dve", lambda e: e.memset(KTd[64:128, :, :], 0.0), reads=["KT2", "KT2b"], writes=["KTd"])
            steps = [(h, qg, kt) for h in range(NH) for qg in range(4) for kt in range(NT)]

            def emit_ST(n):
                h, qg, kt = steps[n]
                g = h // 4
                r0 = (h % 2) * 64
                bank = n % 4
                KK = KTd if h % 2 == 0 else KT2
                S.op("pe", lambda e: e.matmul(
                    ps[:, bank, :], lhsT=KK[:, g, kt * 128:(kt + 1) * 128],
                    rhs=A[:, h // 2, qg * 512:(qg + 1) * 512], start=True, stop=True),
                    reads=["KTd", "KT2", "KT2b", "KT2c"] + [("A", t) for t in range(qg * 4, qg * 4 + 4)], writes=[("ps", bank)])

            def emit_rest(n):
                h, qg, kt = steps[n]
                g = h // 4
                bank = n % 4
                pt = PT[n % 4]
                grp = n // NT
                ob = 4 + (grp % 2)
                S.op("act", lambda e: e.activation(out=pt, in_=ps[:, bank, :], func=AF.Exp, scale=0.125),
                     reads=[("ps", bank)], writes=[("PT", n % 4)])
                for qt in range(4):
                    S.op("pe", lambda e, qt=qt: e.matmul(
                        ps[:, ob, qt * 128:qt * 128 + 65], lhsT=pt[:, qt * 128:(qt + 1) * 128],
                        rhs=V[:, kt, g, 0:65], start=(kt == 0), stop=(kt == NT - 1)),
                        reads=[("PT", n % 4), "V"], writes=[("ps", ob)])
                if kt == NT - 1:
                    rb = grp % 2
                    S.op("dve", lambda e: e.reciprocal(
                        out=rc[rb], in_=ps[:, ob, :].rearrange("p (q n) -> p q n", q=4)[:, :, 64]),
                        reads=[("ps", ob)], writes=[("rc", rb)])
                    for qt in range(4):
                        tt = qg * 4 + qt
                        S.op("dve", lambda e, qt=qt, tt=tt: e.tensor_scalar(
                            out=B[:, tt, h * 64:(h + 1) * 64], in0=ps[:, ob, qt * 128:qt * 128 + 64],
                            scalar1=rc[rb][:, qt:qt + 1], scalar2=None, op0=ALU.mult),
                            reads=[("ps", ob), ("rc", rb)], writes=[("B", tt)])

            emit_ST(0)
            emit_ST(1)
            for n in range(len(steps)):
                if n + 2 < len(steps):
                    emit_ST(n + 2)
                emit_rest(n)

        def wo_ln_phase(i, wo_ap):
            ar.reset(R0)
            WO = ar.alloc([128, KC, D], BF16)
            VB = TMPV + [ar.alloc([128, D]) for _ in range(2)]
            wo_v = wo_ap.rearrange("(k p) n -> p k n", p=128)
            for half in range(2):
                S.dma("pool", lambda e, half=half: e.dma_start(
                    out=WO[:, :, half * 512:(half + 1) * 512], in_=wo_v[:, :, half * 512:(half + 1) * 512]),
                    writes=[("WO", half)])
            load_ln_params(i, 0)
            for half in range(2):
                S.op("dve", lambda e, half=half: e.tensor_tensor(
                    out=WO[:, :, half * 512:(half + 1) * 512], in0=WO[:, :, half * 512:(half + 1) * 512],
                    in1=G1B[:, half * 512:(half + 1) * 512].unsqueeze(1).to_broadcast([128, KC, 512]), op=ALU.mult),
                    reads=[("WO", half), ("GB", 2)], writes=[("WO", half)])
            for tt in range(NT):
                bank = tt % 2
                for kc in range(KC):
                    S.op("pe", lambda e, tt=tt, kc=kc, bank=bank: e.transpose(
                        psb(bank)[:, kc * 128:(kc + 1) * 128], B[:, tt, kc * 128:(kc + 1) * 128], identb),
                        reads=[("B", tt), "identb"], writes=[("ps", bank)])
                eng = "act" if tt % 2 == 0 else "dve"
                if eng == "act":
                    S.op("act", lambda e, tt=tt, bank=bank: e.activation(
                        out=A[:, :, tt * 128:(tt + 1) * 128], in_=psb(bank).rearrange("p (k n) -> p k n", k=8),
                        func=AF.Copy), reads=[("ps", bank)], writes=[("A", tt)])
                else:
                    S.op("dve", lambda e, tt=tt, bank=bank: e.tensor_copy(
                        out=A[:, :, tt * 128:(tt + 1) * 128], in_=psb(bank).rearrange("p (k n) -> p k n", k=8)),
                        reads=[("ps", bank)], writes=[("A", tt)])
            def wo_mm(tt):
                banks = [2 + 2 * (tt % 3), 3 + 2 * (tt % 3)]
                for half in range(2):
                    S.op("pe", lambda e, half=half, bank=banks[half]: e.matmul(
                        ps[:, bank, :], lhsT=alphaI, rhs=X[:, tt, half * 512:(half + 1) * 512], start=True, stop=False),
                        reads=["alphaI", ("X", tt)], writes=[("ps", banks[half])])
                    for kc in range(KC):
                        S.op("pe", lambda e, kc=kc, half=half, bank=banks[half]: e.matmul(
                            ps[:, bank, :], lhsT=A[:, kc, tt * 128:(tt + 1) * 128],
                            rhs=WO[:, kc, half * 512:(half + 1) * 512], start=False, stop=(kc == KC - 1)),
                            reads=[("A", tt), ("WO", half)], writes=[("ps", banks[half])])

            def wo_stats(tt):
                banks = [2 + 2 * (tt % 3), 3 + 2 * (tt % 3)]
                ln_stats(tt, ps[:, banks[0]:banks[0] + 2, :].rearrange("p a n -> p (a n)"),
                         [("ps", banks[0]), ("ps", banks[1])], vbufs=VB)

            wo_mm(0)
            wo_mm(1)
            wo_stats(0)
            for tt in range(NT):
                if tt + 2 < NT:
                    wo_mm(tt + 2)
                if tt + 1 < NT:
                    wo_stats(tt + 1)
                ln_finish(tt, store_out=False, vbufs=VB)

        def moe_phase(i, store_out):
            ar.reset(A0)
            NWB = 3
            WE = [[ar.alloc([128, KC, 512], BF16), ar.alloc([128, KC, 512], BF16), ar.alloc([128, 4, D], BF16)]
                  for _ in range(NWB)]
            m0 = ar.mark()

            def load_expert(e_, which=(0, 1, 2)):
                wb = e_ % NWB
                srcs = (wg_d, wu_d, wd_d)
                for wi in which:
                    S.dma("pool", lambda e, wi=wi: e.dma_start(
                        out=WE[wb][wi], in_=srcs[wi][i, e_].rearrange("(k p) n -> p k n", p=128)),
                        writes=[("WE", wb, wi)])

            load_ln_params(i, 1)
            WR = ar.alloc([128, KC, 36]); RB = ar.alloc([128, 36])
            HT32 = [ar.alloc([128, KC, 128]) for _ in range(2)]
            L = ar.alloc([128, NT, 36])
            gmax = ar.alloc([128, NT]); gsum = ar.alloc([128, NT]); m1_ = ar.alloc([128, NT]); m2_ = ar.alloc([128, NT])
            dd = ar.alloc([128, NT]); e21 = ar.alloc([128, NT])
            gsel = ar.alloc([128, NT, 4]); gex = ar.alloc([128, NT, 4])
            lem = ar.alloc([128, NT, NE]); lem2 = ar.alloc([128, NT, NE])
            OH1 = ar.alloc([128, NT, NE]); OH2 = ar.alloc([128, NT, NE])
            Mall = ar.alloc([128, NT, NE], BF16)
            PEf = ar.alloc([128, NT, NE]); PR = ar.alloc([128, NT, NE])
            D1f = ar.alloc([128, NT]); D2f = ar.alloc([128, NT])
            HB = [ar.alloc([128, D], BF16) for _ in range(2)]
            HF = [ar.alloc([128, D]) for _ in range(2)]
            SHB, SCB = TMPV[0], TMPV[1]
            S.dma("sp", lambda e: e.dma_start(out=SHB, in_=modrow_d[0:1, :].partition_broadcast(128)),
                  reads=[("modrow", 3, 0), ("modrow", 3, 1)], writes=["SHB"])
            S.dma("sp", lambda e: e.dma_start(out=SCB, in_=modrow_d[1:2, :].partition_broadcast(128)),
                  reads=[("modrow", 4, 0), ("modrow", 4, 1)], writes=["SCB"])
            S.dma("sp", lambda e: e.dma_start(out=WR, in_=wr_d[i].rearrange("(k p) n -> p k n", p=128)), writes=["WR"])
            S.dma("sp", lambda e: e.dma_start(out=RB, in_=br_d[i:i + 1, :].partition_broadcast(128)), writes=["RB"])
            load_expert(0)
            load_expert(1)
            load_expert(2)
            for tt in range(NT):
                hb = tt % 2
                for half in range(2):
                    bank = (2 * tt + half) % 4
                    for q in range(4):
                        kc = half * 4 + q
                        S.op("pe", lambda e, tt=tt, q=q, kc=kc, bank=bank: e.transpose(
                            ps[:, bank, q * 128:(q + 1) * 128], X[:, tt, kc * 128:(kc + 1) * 128], ident),
                            reads=[("X", tt), "ident"], writes=[("ps", bank)])
                    for q in range(4):
                        kc = half * 4 + q
                        S.op("act", lambda e, q=q, kc=kc, bank=bank, hb=hb: e.activation(
                            out=HT32[hb][:, kc, :], in_=ps[:, bank, q * 128:(q + 1) * 128],
                            func=AF.Identity, bias=SH2[:, kc:kc + 1], scale=SC2[:, kc:kc + 1]),
                            reads=[("ps", bank), ("modfm", 3), ("modfm", 4)], writes=[("HT32", hb)])
                rbank = 4 + tt // 8
                c0 = (tt % 8) * 36
                for kc in range(KC):
                    S.op("pe", lambda e, kc=kc, hb=hb, rbank=rbank, c0=c0: e.matmul(
                        ps[:, rbank, c0:c0 + 36], lhsT=HT32[hb][:, kc, :], rhs=WR[:, kc, :],
                        start=(kc == 0), stop=(kc == KC - 1)),
                        reads=[("HT32", hb), "WR"], writes=[("ps", rbank)])
            k_ = "rt"
            for hf in range(2):
                S.op("dve", lambda e, hf=hf: e.tensor_tensor(
                    out=L[:, hf * 8:(hf + 1) * 8, :], in0=ps[:, 4 + hf, 0:288].rearrange("p (t x) -> p t x", t=8),
                    in1=RB.unsqueeze(1).to_broadcast([128, 8, 36]), op=ALU.add),
                    reads=[("ps", 4 + hf), "RB"], writes=[k_])
            LG = L[:, :, 0:4]
            LE = L[:, :, 4:36]
            S.op("dve", lambda e: e.tensor_reduce(out=gmax, in_=LG, axis=AX.X, op=ALU.max), reads=[k_], writes=[k_])
            S.op("dve", lambda e: e.tensor_tensor(out=gsel, in0=LG, in1=gmax.unsqueeze(2).to_broadcast([128, NT, 4]),
                                                  op=ALU.is_equal), reads=[k_], writes=[k_])
            S.op("dve", lambda e: e.tensor_tensor(out=gex, in0=LG, in1=gmax.unsqueeze(2).to_broadcast([128, NT, 4]),
                                                  op=ALU.subtract), reads=[k_], writes=[k_])
            S.op("act", lambda e: e.activation(out=gex, in_=gex, func=AF.Exp), reads=[k_], writes=[k_])
            S.op("dve", lambda e: e.tensor_reduce(out=gsum, in_=gex, axis=AX.X, op=ALU.add), reads=[k_], writes=[k_])
            S.op("dve", lambda e: e.reciprocal(out=gsum, in_=gsum), reads=[k_], writes=[k_])
            S.op("dve", lambda e: e.tensor_scalar(out=gsel, in0=gsel, scalar1=BIG, scalar2=-BIG, op0=ALU.mult, op1=ALU.add),
                 reads=[k_], writes=[k_])
            for hf in range(2):
                S.op("dve", lambda e, hf=hf: e.tensor_tensor(
                    out=lem[:, hf * 8:(hf + 1) * 8, :].rearrange("p t (g x) -> p (t g) x", g=4),
                    in0=LE[:, hf * 8:(hf + 1) * 8, :].rearrange("p t (g x) -> p t g x", g=4),
                    in1=gsel[:, hf * 8:(hf + 1) * 8, :].unsqueeze(3).to_broadcast([128, 8, 4, 8]), op=ALU.add),
                    reads=[k_], writes=[k_])
            S.op("dve", lambda e: e.tensor_reduce(out=m1_, in_=lem, axis=AX.X, op=ALU.max), reads=[k_], writes=[k_])
            S.op("dve", lambda e: e.tensor_tensor(out=OH1, in0=lem, in1=m1_.unsqueeze(2).to_broadcast([128, NT, NE]),
                                                  op=ALU.is_equal), reads=[k_], writes=["OH"])
            S.op("dve", lambda e: e.scalar_tensor_tensor(
                out=lem2.rearrange("p t x -> p (t x)"), in0=OH1.rearrange("p t x -> p (t x)"), scalar=-BIG,
                in1=lem.rearrange("p t x -> p (t x)"), op0=ALU.mult, op1=ALU.add), reads=[k_, "OH"], writes=[k_])
            S.op("dve", lambda e: e.tensor_reduce(out=m2_, in_=lem2, axis=AX.X, op=ALU.max), reads=[k_], writes=[k_])
            S.op("dve", lambda e: e.tensor_tensor(out=OH2, in0=lem2, in1=m2_.unsqueeze(2).to_broadcast([128, NT, NE]),
                                                  op=ALU.is_equal), reads=[k_], writes=["OH"])
            S.op("dve", lambda e: e.tensor_tensor(out=Mall, in0=OH1, in1=OH2, op=ALU.add), reads=["OH"], writes=["Mall"])
            S.op("dve", lambda e: e.tensor_tensor(out=dd, in0=m2_, in1=m1_, op=ALU.subtract), reads=[k_], writes=[k_])
            S.op("act", lambda e: e.activation(out=e21, in_=dd, func=AF.Exp), reads=[k_], writes=[k_])
            S.op("dve", lambda e: e.tensor_scalar(out=e21, in0=e21, scalar1=1.0, scalar2=None, op0=ALU.add), reads=[k_], writes=[k_])
            S.op("dve", lambda e: e.reciprocal(out=e21, in_=e21), reads=[k_], writes=[k_])
            S.op("dve", lambda e: e.tensor_tensor(out=G1, in0=e21, in1=gsum, op=ALU.mult), reads=[k_], writes=["G"])
            S.op("dve", lambda e: e.tensor_tensor(out=G2, in0=gsum, in1=G1, op=ALU.subtract), reads=[k_, "G"], writes=["G"])
            for tt in range(NT):
                S.op("pe", lambda e, tt=tt: e.matmul(
                    ps[:, 6, tt * NE:(tt + 1) * NE], lhsT=trib, rhs=Mall[:, tt, :], start=True, stop=(tt == 0)),
                    reads=["trib", "Mall"], writes=[("ps", 6)])
                for t2 in range(tt):
                    S.op("pe", lambda e, tt=tt, t2=t2: e.matmul(
                        ps[:, 6, tt * NE:(tt + 1) * NE], lhsT=onesb, rhs=Mall[:, t2, :], start=False, stop=(t2 == tt - 1)),
                        reads=["onesb", "Mall"], writes=[("ps", 6)])
            S.op("dve", lambda e: e.tensor_tensor(
                out=PEf, in0=ps[:, 6, :].rearrange("p (t x) -> p t x", t=NT),
                in1=iotac.unsqueeze(1).to_broadcast([128, NT, NE]), op=ALU.add),
                reads=[("ps", 6), "iotac"], writes=["PEf"])
            for (OH, Df, Di) in ((OH1, D1f, D1i), (OH2, D2f, D2i)):
                S.op("dve", lambda e, OH=OH: e.tensor_tensor(out=PR, in0=OH, in1=PEf, op=ALU.mult),
                     reads=["OH", "PEf"], writes=["PR"])
                S.op("dve", lambda e, Df=Df: e.tensor_reduce(out=Df, in_=PR, axis=AX.X, op=ALU.add),
                     reads=["PR"], writes=["Df"])
                S.op("dve", lambda e, Df=Df, Di=Di: e.tensor_copy(out=Di, in_=Df), reads=["Df"], writes=["Di"])
            for tt in range(NT):
                hb = tt % 2
                S.op("dve", lambda e, tt=tt, hb=hb: e.tensor_tensor(out=HF[hb], in0=X[:, tt, :], in1=SCB, op=ALU.mult),
                     reads=[("X", tt), "SCB"], writes=[("HF", hb)])
                S.op("dve", lambda e, hb=hb: e.tensor_tensor(out=HB[hb], in0=HF[hb], in1=SHB, op=ALU.add),
                     reads=[("HF", hb), "SHB"], writes=[("HB", hb)])
                for Di in (D1i, D2i):
                    S.dma("pool", lambda e, tt=tt, Di=Di, hb=hb: e.indirect_dma_start(
                        out=xs_d[:, :], out_offset=bass.IndirectOffsetOnAxis(ap=Di[:, tt:tt + 1], axis=0),
                        in_=HB[hb], in_offset=None),
                        reads=[("HB", hb), "Di"], writes=["XS"])
            S.barrier()
            ar.reset(m0)
            NXG = 6
            NYO = 4
            XG = [ar.alloc([128, D], BF16) for _ in range(NXG)]
            XeT = [ar.alloc([128, KC, CAP], BF16) for _ in range(2)]
            HTb = [ar.alloc([128, 4, CAP], BF16) for _ in range(2)]
            SG = [ar.alloc([128, CAP]) for _ in range(2)]
            YO = [ar.alloc([128, D], BF16) for _ in range(NYO)]

            def prep_load(e_):
                for s_ in range(NS):
                    xb = (e_ * NS + s_) % NXG
                    r0 = e_ * CAP + s_ * 128
                    S.dma("sp", lambda e, xb=xb, r0=r0: e.dma_start(out=XG[xb], in_=xs_d[r0:r0 + 128, :]),
                          reads=["XS"], writes=[("XG", xb)])

            def prep(e_):
                wb = e_ % 2
                for s_ in range(NS):
                    xb = (e_ * NS + s_) % NXG
                    bank = (e_ * NS + s_) % 2
                    for kc in range(KC):
                        S.op("pe", lambda e, xb=xb, kc=kc, bank=bank: e.transpose(
                            psb(bank)[:, kc * 128:(kc + 1) * 128], XG[xb][:, kc * 128:(kc + 1) * 128], identb),
                            reads=[("XG", xb), "identb"], writes=[("ps", bank)])
                    S.op("act", lambda e, s_=s_, bank=bank, wb=wb: e.activation(
                        out=XeT[wb][:, :, s_ * 128:(s_ + 1) * 128], in_=psb(bank).rearrange("p (k n) -> p k n", k=KC),
                        func=AF.Copy), reads=[("ps", bank)], writes=[("XeT", wb)])

            def compute(e_):
                wb = e_ % 2
                ww = e_ % NWB
                Wg_, Wu_, Wd_ = WE[ww]
                for fc in range(4):
                    gbank = 2 + 2 * (fc % 2)
                    ubank = gbank + 1
                    for (wi, W_, bank) in ((0, Wg_, gbank), (1, Wu_, ubank)):
                        for kc in range(KC):
                            S.op("pe", lambda e, fc=fc, kc=kc, W_=W_, bank=bank: e.matmul(
                                ps[:, bank, 0:CAP], lhsT=W_[:, kc, fc * 128:(fc + 1) * 128], rhs=XeT[wb][:, kc, :],
                                start=(kc == 0), stop=(kc == KC - 1)),
                                reads=[("WE", ww, wi), ("XeT", wb)], writes=[("ps", bank)])
                    S.op("act", lambda e, fc=fc, gbank=gbank: e.activation(out=SG[fc % 2], in_=ps[:, gbank, 0:CAP], func=AF.Silu),
                         reads=[("ps", gbank)], writes=[("SG", fc % 2)])
                    S.op("dve", lambda e, fc=fc, ubank=ubank: e.tensor_tensor(
                        out=HTb[wb][:, fc, :], in0=SG[fc % 2], in1=ps[:, ubank, 0:CAP], op=ALU.mult),
                        reads=[("SG", fc % 2), ("ps", ubank)], writes=[("HTb", wb)])
                if e_ + NWB < NE:
                    load_expert(e_ + NWB, which=(0, 1))
                for s_ in range(NS):
                    yb_ = (e_ * NS + s_) % NYO
                    for half in range(2):
                        bank = 6 + half
                        for fc in range(4):
                            S.op("pe", lambda e, s_=s_, half=half, fc=fc, bank=bank: e.matmul(
                                ps[:, bank, :], lhsT=HTb[wb][:, fc, s_ * 128:(s_ + 1) * 128],
                                rhs=Wd_[:, fc, half * 512:(half + 1) * 512], start=(fc == 0), stop=(fc == 3)),
                                reads=[("HTb", wb), ("WE", ww, 2)], writes=[("ps", bank)])
                        S.op("dve", lambda e, yb_=yb_, bank=bank, half=half: e.tensor_tensor(
                            out=YO[yb_][:, half * 512:(half + 1) * 512], in0=ps[:, bank, :],
                            in1=G2B[:, half * 512:(half + 1) * 512], op=ALU.mult),
                            reads=[("ps", bank), ("GB", 5)], writes=[("YO", yb_)])
                    r0 = e_ * CAP + s_ * 128
                    S.dma("sp", lambda e, yb_=yb_, r0=r0: e.dma_start(out=ys_d[r0:r0 + 128, :], in_=YO[yb_]),
                          reads=[("YO", yb_)], writes=["YS"])
                if e_ + NWB < NE:
                    load_expert(e_ + NWB, which=(2,))

            prep_load(0)
            prep_load(1)
            prep(0)
            for e_ in range(NE):
                if e_ + 2 < NE:
                    prep_load(e_ + 2)
                if e_ + 1 < NE:
                    prep(e_ + 1)
                compute(e_)
            S.barrier()
            ar.reset(A0)
            NYB = 4
            Y1 = [ar.alloc([128, D], BF16) for _ in range(NYB)]
            Y2 = [ar.alloc([128, D], BF16) for _ in range(NYB)]
            VB = TMPV + [ar.alloc([128, D]) for _ in range(2)]
            def gather(tt):
                yb = tt % NYB
                S.dma("pool", lambda e: e.indirect_dma_start(
                    out=Y1[yb], out_offset=None, in_=ys_d[:, :],
                    in_offset=bass.IndirectOffsetOnAxis(ap=D1i[:, tt:tt + 1], axis=0)),
                    reads=["YS", "Di"], writes=[("Y1", yb)])
                S.dma("pool", lambda e: e.indirect_dma_start(
                    out=Y2[yb], out_offset=None, in_=ys_d[:, :],
                    in_offset=bass.IndirectOffsetOnAxis(ap=D2i[:, tt:tt + 1], axis=0)),
                    reads=["YS", "Di"], writes=[("Y2", yb)])

            DG = [[ar.alloc([128, 128], BF16) for _ in range(2)] for _ in range(2)]
            for tt in range(NYB):
                gather(tt)

            def build(tt):
                yb = tt % NYB
                db = tt % 2
                for k_, Gk in enumerate((G1, G2)):
                    S.op("dve", lambda e, k_=k_, Gk=Gk: e.tensor_scalar(
                        out=DG[db][k_], in0=ident, scalar1=Gk[:, tt:tt + 1], scalar2=None, op0=ALU.mult),
                        reads=["ident", "G"], writes=[("DG", db, k_)])
                banks = [2 * (tt % 4), 2 * (tt % 4) + 1]
                for half in range(2):
                    hs = slice(half * 512, (half + 1) * 512)
                    S.op("pe", lambda e, half=half, hs=hs: e.matmul(
                        ps[:, banks[half], :], lhsT=alphaI, rhs=X[:, tt, hs], start=True, stop=False),
                        reads=["alphaI", ("X", tt)], writes=[("ps", banks[half])])
                    S.op("pe", lambda e, half=half, hs=hs: e.matmul(
                        ps[:, banks[half], :], lhsT=DG[db][0], rhs=Y1[yb][:, hs], start=False, stop=False),
                        reads=[("DG", db, 0), ("Y1", yb)], writes=[("ps", banks[half])])
                    S.op("pe", lambda e, half=half, hs=hs: e.matmul(
                        ps[:, banks[half], :], lhsT=DG[db][1], rhs=Y2[yb][:, hs], start=False, stop=True),
                        reads=[("DG", db, 1), ("Y2", yb)], writes=[("ps", banks[half])])

            def cstats(tt):
                banks = [2 * (tt % 4), 2 * (tt % 4) + 1]
                ln_stats(tt, ps[:, banks[0]:banks[0] + 2, :].rearrange("p a n -> p (a n)"),
                         [("ps", banks[0]), ("ps", banks[1])], vbufs=VB)

            build(0)
            build(1)
            cstats(0)
            for tt in range(NT):
                if tt + 2 < NT:
                    build(tt + 2)
                if tt + 1 < NT:
                    cstats(tt + 1)
                ln_finish(tt, store_out=store_out, vbufs=VB)
                if tt + NYB < NT:
                    gather(tt + NYB)

        def dump_dbg(kind):
            if kind == "mod":
                S.dma("sp", lambda e: e.dma_start(out=out_d[0:128, :], in_=G1B), reads=[("GB", 2)], writes=["o0"])
                S.dma("sp", lambda e: e.dma_start(out=out_d[128:256, :], in_=G2B), reads=[("GB", 5)], writes=["o1"])
                for n_, t_ in enumerate((SC1, SH1, SC2, SH2)):
                    S.dma("sp", lambda e, n_=n_, t_=t_: e.dma_start(out=out_d[256:384, n_ * 8:(n_ + 1) * 8], in_=t_),
                          reads=[("modfm", k) for k in (0, 1, 3, 4)], writes=[("o2", n_)])
            elif kind == "hT":
                for kc in range(KC):
                    S.dma("pool", lambda e, kc=kc: e.dma_start(out=out_d[kc * 128:(kc + 1) * 128, :], in_=A[:, kc, 0:1024]),
                          reads=[("A", t) for t in range(NT)], writes=[("o", kc)])
            elif kind == "attn":
                for tt in range(NT):
                    S.dma("pool", lambda e, tt=tt: e.dma_start(out=out_d[tt * 128:(tt + 1) * 128, :], in_=B[:, tt, :]),
                          reads=[("B", tt)], writes=[("o", tt)])

        def dump_x():
            for tt in range(NT):
                S.dma("sp", lambda e, tt=tt: e.dma_start(out=out_d[tt * 128:(tt + 1) * 128, :], in_=X[:, tt, :]),
                      reads=[("X", tt)], writes=[("out", tt)])

        done = False
        for i in range(n_layers):
            S.barrier()
            mod_phase(i)
            S.barrier()
            if stop == ("mod", i):
                dump_dbg("mod")
                done = True
                break
            build_hT(SC1, SH1, 1, 0)
            if stop == ("hT", i):
                S.barrier()
                dump_dbg("hT")
                done = True
                break
            l = i // 2
            if i % 2 == 0:
                na_phase(l)
                wo_ap = nawo_d[l]
            else:
                gqa_phase(l)
                wo_ap = gwo_d[l]
            S.barrier()
            if stop == ("attn", i):
                dump_dbg("attn")
                done = True
                break
            wo_ln_phase(i, wo_ap)
            S.barrier()
            if stop == ("mix", i):
                dump_x()
                done = True
                break
            last = (i == n_layers - 1)
            moe_phase(i, store_out=last)
            if last:
                done = True
        assert done
        S.barrier()
        S.emit()
        S.close()
    return nc


_CACHE = {}


def _prep_inputs(x, c, ada_w, ada_b, ln_g, ln_b, na_w_qkv, na_rpb, na_w_o, gqa_w_qkv, gqa_q_norm,
                 gqa_k_norm, gqa_w_o, moe_w_group, moe_b_group, moe_w_expert, moe_b_expert,
                 moe_w_gate, moe_w_up, moe_w_down):
    f = lambda a: np.ascontiguousarray(np.asarray(a), dtype=np.float32)
    C64, S64 = _rope_tables()
    shared = {
        "ada_w": f(ada_w), "ada_b": f(ada_b), "ln_g": f(ln_g), "ln_b": f(ln_b),
        "na_w_qkv": f(na_w_qkv), "na_w_o": f(na_w_o), "na_bias": _na_bias_table(f(na_rpb)).reshape(-1, NH, 128, NCLS * 5 * 128),
        "gqa_w_qkv": f(gqa_w_qkv), "gqa_w_o": f(gqa_w_o), "gqa_q_norm": f(gqa_q_norm), "gqa_k_norm": f(gqa_k_norm),
        "rope_c": C64, "rope_s": S64,
        "moe_wr": np.ascontiguousarray(np.concatenate([f(moe_w_group), f(moe_w_expert)], axis=-1)),
        "moe_br": np.ascontiguousarray(np.concatenate([f(moe_b_group), f(moe_b_expert)], axis=-1)),
        "moe_w_gate": f(moe_w_gate), "moe_w_up": f(moe_w_up), "moe_w_down": f(moe_w_down),
        "ident": np.eye(128, dtype=np.float32),
        "tri": np.triu(np.ones((128, 128), np.float32), 1),
        "iotac": np.ascontiguousarray(np.broadcast_to((np.arange(NE) * CAP).astype(np.float32), (128, NE))),
    }
    x = f(x)
    c = f(c)
    in_maps = []
    for b in range(8):
        m = dict(shared)
        m["x"] = x[b]
        m["cfm"] = np.ascontiguousarray(c[b].reshape(KC, 128).T)
        in_maps.append(m)
    return in_maps


def kernel(**inputs):
    in_maps = _prep_inputs(**inputs)
    key = "full"
    if key not in _CACHE:
        _CACHE[key] = build_program()
    nc = _CACHE[key]
    res = run_bass_kernel_spmd(nc, in_maps, core_ids=list(range(8)))
    return np.stack([np.asarray(r["out"], dtype=np.float32) for r in res.results], axis=0)
```

```python
import numpy as np
import concourse.bass as bass
import concourse.mybir as mybir
from concourse.bass_utils import run_bass_kernel_spmd

F32 = mybir.dt.float32
BF16 = mybir.dt.bfloat16
I32 = mybir.dt.int32
AF = mybir.ActivationFunctionType
ALU = mybir.AluOpType
AX = mybir.AxisListType

D = 1024
SEQ = 2048
NT = 16
KC = 8
NH = 16
HD = 64
DEPTH = 4
NE = 32
CAP = 384
NS = CAP // 128
ALPHA = float((2 * DEPTH) ** 0.25)
LN_EPS = 1e-5
RMS_EPS = 1e-6
NEG = -30000.0
BIG = 1.0e4
ARENA_WORDS = 53200


class _Rec:
    def __init__(self):
        self.call = None

    def __getattr__(self, name):
        def f(*a, **k):
            self.call = (name, a, k)
            return self
        return f


def _record(fn):
    r = _Rec()
    fn(r)
    assert r.call is not None
    return r.call


class Sched:
    def __init__(self, nc, n_dma_sems=8, same_engine_sync=True):
        self.nc = nc
        self.prog = {e: [] for e in ("pe", "act", "dve", "pool", "sp")}
        self.cnt = {e: 0 for e in self.prog}
        self.sems = {}
        self.waited = {e: {} for e in self.prog}
        self.last_w = {}
        self.readers = {}
        self.same_engine_sync = same_engine_sync
        self.n_dma_sems = n_dma_sems
        self.dma_ring = {q: {"next": 0, "tot": [0] * n_dma_sems} for q in ("sp", "act", "pool")}
        self._ctx = []

    def open(self):
        nc = self.nc
        for e in self.prog:
            cm = nc.semaphore("s_" + e)
            self.sems["s_" + e] = cm.__enter__()
            self._ctx.append(cm)
        for q in self.dma_ring:
            for i in range(self.n_dma_sems):
                nm = f"d_{q}{i}"
                cm = nc.semaphore(nm)
                self.sems[nm] = cm.__enter__()
                self._ctx.append(cm)

    def close(self):
        for cm in reversed(self._ctx):
            cm.__exit__(None, None, None)

    def _need(self, eng, tok, waits):
        if tok is None:
            return
        sem, val, peng = tok
        if peng == eng and (eng == "pe" or not self.same_engine_sync):
            return
        if self.waited[eng].get(sem, 0) >= val:
            return
        if waits.get(sem, 0) < val:
            waits[sem] = val

    def _collect(self, eng, reads, writes):
        waits = {}
        for k in reads:
            self._need(eng, self.last_w.get(k), waits)
        for k in writes:
            self._need(eng, self.last_w.get(k), waits)
            for t in self.readers.get(k, ()):
                self._need(eng, t, waits)
        for s, v in waits.items():
            self.waited[eng][s] = v
        return list(waits.items())

    def _update(self, tok, reads, writes):
        for k in writes:
            self.last_w[k] = tok
            self.readers[k] = []
        for k in reads:
            if k in writes:
                continue
            lst = self.readers.setdefault(k, [])
            lst.append(tok)
            if len(lst) > 48:
                best = {}
                for t in lst:
                    if t[0] not in best or best[t[0]][1] < t[1]:
                        best[t[0]] = t
                self.readers[k] = list(best.values())

    def op(self, eng, fn, reads=(), writes=()):
        waits = self._collect(eng, reads, writes)
        self.cnt[eng] += 1
        tok = ("s_" + eng, self.cnt[eng], eng)
        self.prog[eng].append((waits, _record(fn), ("s_" + eng, 1)))
        self._update(tok, reads, writes)
        return tok

    def dma(self, q, fn, reads=(), writes=()):
        ring = self.dma_ring[q]
        i = ring["next"]
        ring["next"] = (i + 1) % self.n_dma_sems
        sem = f"d_{q}{i}"
        waits = dict(self._collect(q, reads, writes))
        prev = ring["tot"][i]
        if prev and self.waited[q].get(sem, 0) < prev:
            waits[sem] = prev
            self.waited[q][sem] = prev
        ring["tot"][i] += 16
        tok = (sem, ring["tot"][i], "dma_" + q)
        self.prog[q].append((list(waits.items()), _record(fn), (sem, 16)))
        self._update(tok, reads, writes)
        return tok

    def barrier(self):
        targets = {}
        for e, c in self.cnt.items():
            if c:
                targets["s_" + e] = c
        for q, ring in self.dma_ring.items():
            for i, t in enumerate(ring["tot"]):
                if t:
                    targets[f"d_{q}{i}"] = t
        for eng in self.prog:
            waits = []
            for s, v in targets.items():
                if s == "s_" + eng and eng == "pe":
                    continue
                if self.waited[eng].get(s, 0) < v:
                    waits.append((s, v))
                    self.waited[eng][s] = v
            if waits:
                self.prog[eng].append((waits, None, None))

    def emit(self):
        nc = self.nc
        sems = self.sems
        prog = self.prog

        def run(engh, lst):
            for waits, fn, inc in lst:
                for s, v in waits:
                    engh.wait_ge(sems[s], v)
                if fn is not None:
                    name, a, k = fn
                    ins = getattr(engh, name)(*a, **k)
                    ins.then_inc(sems[inc[0]], inc[1])

        with nc.Block() as block:
            @block.sync
            def _(e):
                run(e, prog["sp"])

            @block.tensor
            def _(e):
                run(e, prog["pe"])

            @block.scalar
            def _(e):
                run(e, prog["act"])

            @block.vector
            def _(e):
                run(e, prog["dve"])

            @block.gpsimd
            def _(e):
                run(e, prog["pool"])


class Arena:
    def __init__(self, t, nwords):
        self.t = t
        self.n = nwords
        self.off = 0

    def alloc(self, shape, dt=F32):
        free = 1
        for s in shape[1:]:
            free *= s
        esz = 4 if dt in (F32, I32) else 2
        words = (free * esz + 3) // 4
        words = (words + 7) // 8 * 8
        assert self.off + words <= self.n, ("arena overflow", self.off, words, self.n)
        v = self.t[:, self.off:self.off + words]
        self.off += words
        if dt != F32:
            v = v.bitcast(dt)
        v = v[:, 0:free]
        if len(shape) == 3:
            v = v.rearrange("p (a b) -> p a b", a=shape[1])
        elif len(shape) == 4:
            v = v.rearrange("p (a b c) -> p a b c", a=shape[1], b=shape[2])
        return v

    def mark(self):
        return self.off

    def reset(self, m):
        self.off = m


def _na_patterns():
    pats = []
    for j in range(NT):
        kt_lo = min(max(j - 2, 0), 11)
        qi = np.arange(128)
        r = 2 * j + qi // 64
        c = qi % 64
        rs = np.clip(r - 4, 0, 24)
        ws = np.clip(c - 8, 0, 48)
        tiles = []
        for i in range(5):
            kt = kt_lo + i
            ki = np.arange(128)
            kr = 2 * kt + ki // 64
            kcol = ki % 64
            vr = (kr[:, None] >= rs[None, :]) & (kr[:, None] < rs[None, :] + 8)
            vc = (kcol[:, None] >= ws[None, :]) & (kcol[:, None] < ws[None, :] + 16)
            dr = np.clip(kr[:, None] - r[None, :] + 7, 0, 14)
            dc = np.clip(kcol[:, None] - c[None, :] + 15, 0, 30)
            valid = vr & vc
            tiles.append((valid, np.where(valid, dr, 0), np.where(valid, dc, 0)))
        pats.append(tiles)
    classes = []
    cls_of_j = []
    for j in range(NT):
        key = b"".join(a.tobytes() for t in pats[j] for a in t)
        found = None
        for ci, (k2, _) in enumerate(classes):
            if k2 == key:
                found = ci
                break
        if found is None:
            classes.append((key, pats[j]))
            found = len(classes) - 1
        cls_of_j.append(found)
    return [c[1] for c in classes], cls_of_j


_NA_CLASSES, _NA_CLS_OF_J = _na_patterns()
NCLS = len(_NA_CLASSES)


def _na_bias_table(rpb):
    L = rpb.shape[0]
    out = np.empty((L, NH, 128, NCLS * 5, 128), np.float32)
    for ci, tiles in enumerate(_NA_CLASSES):
        for i, (valid, dr, dc) in enumerate(tiles):
            g = rpb[:, :, dr, dc]
            out[:, :, :, ci * 5 + i, :] = np.where(valid[None, None], g, np.float32(NEG))
    return out


def _rope_tables():
    t = np.arange(SEQ)
    pos = np.stack([t // 64, t % 64], -1).astype(np.float32)
    inv = (np.float32(10000.0) ** (-np.arange(16, dtype=np.float32) / np.float32(16))).astype(np.float32)
    ang = pos[:, :, None] * inv
    c = np.cos(ang).astype(np.float32)
    s = np.sin(ang).astype(np.float32)
    C64 = np.stack([c, c], 2).reshape(SEQ, 64)
    S64 = np.stack([-s, s], 2).reshape(SEQ, 64)
    C64 = np.ascontiguousarray(C64.reshape(NT, 128, 64).transpose(1, 0, 2))
    S64 = np.ascontiguousarray(S64.reshape(NT, 128, 64).transpose(1, 0, 2))
    return C64, S64


def build_program(n_layers=DEPTH, stop=None, lite=False, decl=None):
    nc = bass.Bass("TRN2", target_bir_lowering=False)
    n_moe = n_layers if stop is None else n_layers - 1
    LD = n_layers if lite else DEPTH
    LNA = max(1, (n_layers + 1) // 2) if lite else 2
    LGQ = max(1, n_layers // 2) if lite else 2
    LMOE = max(1, n_moe) if lite else DEPTH
    EMOE = NE if (n_moe > 0 or not lite) else 1

    def din(name, shape, dt=F32):
        if decl is not None:
            decl[name] = tuple(shape)
        return nc.dram_tensor(name, list(shape), dt, kind="ExternalInput").ap()

    x_d = din("x", [SEQ, D])
    c_d = din("cfm", [128, KC])
    adaw_d = din("ada_w", [LD, D, 6 * D])
    adab_d = din("ada_b", [LD, 6 * D])
    lng_d = din("ln_g", [LD, 2, D])
    lnb_d = din("ln_b", [LD, 2, D])
    nawqkv_d = din("na_w_qkv", [LNA, D, 3 * D])
    nawo_d = din("na_w_o", [LNA, D, D])
    nabias_d = din("na_bias", [LNA, NH, 128, NCLS * 5 * 128])
    gwqkv_d = din("gqa_w_qkv", [LGQ, D, 1536])
    gwo_d = din("gqa_w_o", [LGQ, D, D])
    gqn_d = din("gqa_q_norm", [LGQ, HD])
    gkn_d = din("gqa_k_norm", [LGQ, HD])
    ropec_d = din("rope_c", [128, NT, 64])
    ropes_d = din("rope_s", [128, NT, 64])
    wr_d = din("moe_wr", [LMOE, D, 36])
    br_d = din("moe_br", [LMOE, 36])
    wg_d = din("moe_w_gate", [LMOE, EMOE, D, 512])
    wu_d = din("moe_w_up", [LMOE, EMOE, D, 512])
    wd_d = din("moe_w_down", [LMOE, EMOE, 512, D])
    ident_d = din("ident", [128, 128])
    tri_d = din("tri", [128, 128])
    iotac_d = din("iotac", [128, NE])
    out_d = nc.dram_tensor("out", [SEQ, D], F32, kind="ExternalOutput").ap()
    xs_d = nc.dram_tensor("xs_scr", [NE * CAP + 2 * CAP, D], BF16, kind="Internal").ap()
    ys_d = nc.dram_tensor("ys_scr", [NE * CAP + 2 * CAP, D], BF16, kind="Internal").ap()
    modrow_d = nc.dram_tensor("modrow", [2, D], F32, kind="Internal").ap()

    S = Sched(nc)
    with nc.sbuf_tensor("arena", [128, ARENA_WORDS], F32) as arena_t, \
            nc.psum_tensor("ps", [128, 8, 512], F32) as ps:
        ar = Arena(arena_t, ARENA_WORDS)
        S.open()

        def psb(bank):
            return ps[:, bank, :].bitcast(BF16)

        X = ar.alloc([128, NT, D])
        ident = ar.alloc([128, 128])
        identb = ar.alloc([128, 128], BF16)
        onesb = ar.alloc([128, 128], BF16)
        trib = ar.alloc([128, 128], BF16)
        iotac = ar.alloc([128, NE])
        alphaI = ar.alloc([128, 128])
        cact = ar.alloc([128, KC])
        cA = ar.alloc([128, KC, 128], BF16)
        SC1 = ar.alloc([128, KC]); SH1 = ar.alloc([128, KC])
        SC2 = ar.alloc([128, KC]); SH2 = ar.alloc([128, KC])
        G1B = ar.alloc([128, D]); G2B = ar.alloc([128, D])
        LNG = ar.alloc([128, D]); LNB = ar.alloc([128, D])
        TMPV = [ar.alloc([128, D]) for _ in range(2)]
        st_t = [ar.alloc([128, 12]) for _ in range(4)]
        mv_t = [ar.alloc([128, 2]) for _ in range(4)]
        sd_t = [ar.alloc([128, 2]) for _ in range(4)]
        eps_ln = ar.alloc([128, 1]); eps_rms = ar.alloc([128, 1])
        G1 = ar.alloc([128, NT]); G2 = ar.alloc([128, NT])
        D1i = ar.alloc([128, NT], I32); D2i = ar.alloc([128, NT], I32)
        A0 = ar.mark()
        A = ar.alloc([128, KC, SEQ], BF16)
        B = ar.alloc([128, NT, D], BF16)
        R0 = ar.mark()

        S.dma("sp", lambda e: e.dma_start(out=ident, in_=ident_d[:, :]), writes=["ident"])
        S.dma("pool", lambda e: e.dma_start(out=identb, in_=ident_d[:, :]), writes=["identb"])
        S.dma("pool", lambda e: e.dma_start(out=trib, in_=tri_d[:, :]), writes=["trib"])
        S.dma("sp", lambda e: e.dma_start(out=iotac, in_=iotac_d[:, :]), writes=["iotac"])
        S.dma("sp", lambda e: e.dma_start(out=cact, in_=c_d[:, :]), writes=["cact"])
        S.op("dve", lambda e: e.memset(onesb, 1.0), writes=["onesb"])
        S.op("dve", lambda e: e.tensor_scalar(out=alphaI, in0=ident, scalar1=ALPHA, scalar2=None, op0=ALU.mult),
             reads=["ident"], writes=["alphaI"])
        S.op("dve", lambda e: e.memset(eps_ln, LN_EPS), writes=["eps"])
        S.op("dve", lambda e: e.memset(eps_rms, RMS_EPS), writes=["eps"])
        for g4 in range(4):
            S.dma("sp", lambda e, g4=g4: e.dma_start(
                out=X[:, g4 * 4:(g4 + 1) * 4, :],
                in_=x_d[g4 * 512:(g4 + 1) * 512, :].rearrange("(t p) d -> p t d", p=128)),
                writes=[("X", t) for t in range(g4 * 4, g4 * 4 + 4)])
        S.op("act", lambda e: e.activation(out=cact, in_=cact, func=AF.Silu), reads=["cact"], writes=["cact"])
        S.op("dve", lambda e: e.tensor_copy(out=cA, in_=cact.unsqueeze(2).to_broadcast([128, KC, 128])),
             reads=["cact"], writes=["cA"])

        def mod_groups(i, base, mm_banks, tr_banks):
            arm = Arena(arena_t, ARENA_WORDS)
            arm.off = base
            AW = [arm.alloc([128, KC, 512], BF16) for _ in range(2)]
            ABb = [arm.alloc([128, 512]) for _ in range(2)]
            T = [arm.alloc([128, 512]) for _ in range(2)]
            aw_v = adaw_d[i].rearrange("(k p) n -> p k n", p=128)

            def group(cg):
                b = cg % 2
                S.dma("pool", lambda e: e.dma_start(out=AW[b], in_=aw_v[:, :, cg * 512:(cg + 1) * 512]),
                      writes=[("AW", b)])
                S.dma("sp", lambda e: e.dma_start(
                    out=ABb[b], in_=adab_d[i:i + 1, cg * 512:(cg + 1) * 512].partition_broadcast(128)),
                    writes=[("ABb", b)])
                bank = mm_banks[cg % len(mm_banks)]
                for kc in range(KC):
                    S.op("pe", lambda e, kc=kc: e.matmul(
                        ps[:, bank, :], lhsT=cA[:, kc, :], rhs=AW[b][:, kc, :], start=(kc == 0), stop=(kc == KC - 1)),
                        reads=["cA", ("AW", b)], writes=[("ps", bank)])
                kind = cg // 2
                half = cg % 2
                if kind in (2, 5):
                    GB = G1B if kind == 2 else G2B
                    S.op("dve", lambda e: e.scalar_tensor_tensor(
                        out=GB[:, half * 512:(half + 1) * 512], in0=ps[:, bank, :], scalar=1.0, in1=ABb[b],
                        op0=ALU.add, op1=ALU.add),
                        reads=[("ps", bank), ("ABb", b)], writes=[("GB", kind)])
                else:
                    add1 = 1.0 if kind in (1, 4) else 0.0
                    S.op("dve", lambda e: e.scalar_tensor_tensor(
                        out=T[b], in0=ps[:, bank, :], scalar=add1, in1=ABb[b], op0=ALU.add, op1=ALU.add),
                        reads=[("ps", bank), ("ABb", b)], writes=[("T", b)])
                    tb = tr_banks[b % len(tr_banks)]
                    for q in range(4):
                        S.op("pe", lambda e, q=q: e.transpose(
                            ps[:, tb, q * 128:(q + 1) * 128], T[b][:, q * 128:(q + 1) * 128], ident),
                            reads=[("T", b), "ident"], writes=[("ps", tb)])
                    if kind in (3, 4):
                        S.dma("sp", lambda e: e.dma_start(
                            out=modrow_d[kind - 3:kind - 2, half * 512:(half + 1) * 512], in_=T[b][0:1, :]),
                            reads=[("T", b)], writes=[("modrow", kind, half)])
                    dst = {0: SH1, 1: SC1, 3: SH2, 4: SC2}[kind]
                    S.op("dve", lambda e: e.tensor_copy(
                        out=dst[:, half * 4:(half + 1) * 4],
                        in_=ps[:, tb, :].rearrange("p (q n) -> p q n", q=4)[:, :, 0]),
                        reads=[("ps", tb)], writes=[("modfm", kind)])
            return group

        def mod_phase(i):
            g = mod_groups(i, R0, (0, 1), (2, 3))
            for cg in range(12):
                g(cg)

        def hT_tile(tt, banks, SC, SH, kind_sc, kind_sh):
            for half in range(2):
                bank = banks[half % len(banks)]
                for q in range(4):
                    kc = half * 4 + q
                    S.op("pe", lambda e, q=q, kc=kc: e.transpose(
                        ps[:, bank, q * 128:(q + 1) * 128], X[:, tt, kc * 128:(kc + 1) * 128], ident),
                        reads=[("X", tt), "ident"], writes=[("ps", bank)])
                for q in range(4):
                    kc = half * 4 + q
                    S.op("act", lambda e, q=q, kc=kc: e.activation(
                        out=A[:, kc, tt * 128:(tt + 1) * 128], in_=ps[:, bank, q * 128:(q + 1) * 128],
                        func=AF.Identity, bias=SH[:, kc:kc + 1], scale=SC[:, kc:kc + 1]),
                        reads=[("ps", bank), ("modfm", kind_sc), ("modfm", kind_sh)], writes=[("A", tt)])

        def build_hT(SC, SH, kind_sc, kind_sh):
            for tt in range(NT):
                hT_tile(tt, [(2 * tt) % 4, (2 * tt + 1) % 4], SC, SH, kind_sc, kind_sh)

        def ln_stats(tt, v_ps, vkeys, vbufs):
            nb_ = len(vbufs)
            tb = tt % nb_
            v = vbufs[tb]
            sb_ = tt % 4
            for half in range(2):
                S.op("dve", lambda e, half=half: e.bn_stats(
                    out=st_t[sb_][:, half * 6:(half + 1) * 6], in_=v_ps[:, half * 512:(half + 1) * 512]),
                    reads=[vkeys[half]], writes=[("st", sb_)])
            S.op("dve", lambda e: e.bn_aggr(out=mv_t[sb_], in_=st_t[sb_]), reads=[("st", sb_)], writes=[("mv", sb_)])
            S.op("act", lambda e: e.activation(out=sd_t[sb_][:, 0:1], in_=mv_t[sb_][:, 1:2], func=AF.Sqrt, bias=eps_ln, scale=1.0),
                 reads=[("mv", sb_), "eps"], writes=[("sd", sb_)])
            S.op("dve", lambda e: e.reciprocal(out=sd_t[sb_][:, 0:1], in_=sd_t[sb_][:, 0:1]), reads=[("sd", sb_)], writes=[("sd", sb_)])
            S.op("dve", lambda e: e.scalar_tensor_tensor(
                out=sd_t[sb_][:, 1:2], in0=mv_t[sb_][:, 0:1], scalar=-1.0, in1=sd_t[sb_][:, 0:1], op0=ALU.mult, op1=ALU.mult),
                reads=[("mv", sb_), ("sd", sb_)], writes=[("sd2", sb_)])
            S.op("act", lambda e: e.activation(out=v, in_=v_ps, func=AF.Identity, bias=sd_t[sb_][:, 1:2], scale=sd_t[sb_][:, 0:1]),
                 reads=list(vkeys) + [("sd", sb_), ("sd2", sb_)], writes=[("v", tb)])

        def ln_finish(tt, store_out, vbufs):
            tb = tt % len(vbufs)
            v = vbufs[tb]
            S.op("dve", lambda e: e.tensor_tensor(out=v, in0=v, in1=LNG, op=ALU.mult),
                 reads=[("v", tb), "LNG"], writes=[("v", tb)])
            S.op("dve", lambda e: e.tensor_tensor(out=X[:, tt, :], in0=v, in1=LNB, op=ALU.add),
                 reads=[("v", tb), "LNB"], writes=[("X", tt)])
            if store_out:
                S.dma("sp", lambda e: e.dma_start(out=out_d[tt * 128:(tt + 1) * 128, :], in_=X[:, tt, :]),
                      reads=[("X", tt)], writes=[("out", tt)])

        def load_ln_params(i, which):
            S.dma("sp", lambda e: e.dma_start(out=LNG, in_=lng_d[i, which:which + 1, :].partition_broadcast(128)),
                  writes=["LNG"])
            S.dma("sp", lambda e: e.dma_start(out=LNB, in_=lnb_d[i, which:which + 1, :].partition_broadcast(128)),
                  writes=["LNB"])

        def na_phase(l):
            ar.reset(R0)
            Wb = [[ar.alloc([128, KC, 128], BF16) for _ in range(3)] for _ in range(2)]
            BI = [ar.alloc([128, NCLS * 5, 128], BF16) for _ in range(3)]
            QT = ar.alloc([128, SEQ], BF16)
            KT = ar.alloc([128, SEQ], BF16)
            V = ar.alloc([128, NT, 2, 66], BF16)
            PT = [ar.alloc([128, 640], BF16) for _ in range(3)]
            rc = [ar.alloc([128, 1]) for _ in range(2)]
            w_v = nawqkv_d[l].rearrange("(k p) n -> p k n", p=128)
            S.op("dve", lambda e: e.memset(V[:, :, :, 64:66], 1.0), writes=["V"])

            def load_w(p):
                wb = p % 2
                for m in range(3):
                    c0 = m * D + p * 128
                    S.dma("pool", lambda e, wb=wb, m=m, c0=c0: e.dma_start(out=Wb[wb][m], in_=w_v[:, :, c0:c0 + 128]),
                          writes=[("W", wb, m)])

            def load_bias(h):
                nchunk = NCLS * 5 // 5
                S.dma("pool", lambda e, h=h: e.dma_start(
                    out=BI[h % 3].rearrange("p (a b) n -> p a (b n)", a=nchunk),
                    in_=nabias_d[l, h].rearrange("p (a m) -> p a m", a=nchunk)), writes=[("BI", h % 3)])

            load_w(0)
            load_bias(0)
            load_bias(1)

            def exp_bias(h):
                S.op("act", lambda e: e.activation(
                    out=BI[h % 3].rearrange("p a n -> p (a n)"), in_=BI[h % 3].rearrange("p a n -> p (a n)"), func=AF.Exp),
                    reads=[("BI", h % 3)], writes=[("BI", h % 3)])

            def emit_ST(n, h, j):
                hh = h % 2
                r0 = hh * 64
                kt_lo = min(max(j - 2, 0), 11)
                sb = (n % 3) * 2
                for i in range(5):
                    bank = sb + i // 4
                    col = (i % 4) * 128
                    S.op("pe", lambda e, bank=bank, col=col, i=i: e.matmul(
                        ps[:, bank, col:col + 128], lhsT=KT[r0:r0 + 64, (kt_lo + i) * 128:(kt_lo + i + 1) * 128],
                        rhs=QT[r0:r0 + 64, j * 128:(j + 1) * 128], start=True, stop=True),
                        reads=["KT", "QT"], writes=[("ps", bank)])

            def emit_B(n, h, j):
                c = _NA_CLS_OF_J[j]
                sb = (n % 3) * 2
                pb = n % 3
                S.op("act", lambda e: e.activation(out=PT[pb][:, 0:512], in_=ps[:, sb, :], func=AF.Exp),
                     reads=[("ps", sb)], writes=[("PT", pb)])
                S.op("act", lambda e: e.activation(out=PT[pb][:, 512:640], in_=ps[:, sb + 1, 0:128], func=AF.Exp),
                     reads=[("ps", sb + 1)], writes=[("PT", pb)])
                S.op("dve", lambda e: e.tensor_tensor(
                    out=PT[pb], in0=PT[pb], in1=BI[h % 3][:, c * 5:(c + 1) * 5, :].rearrange("p a n -> p (a n)"),
                    op=ALU.mult), reads=[("PT", pb), ("BI", h % 3)], writes=[("PT", pb)])

            def emit_C(n, h, j):
                hh = h % 2
                kt_lo = min(max(j - 2, 0), 11)
                pb = n % 3
                ob = 6 + (n % 2)
                for i in range(5):
                    S.op("pe", lambda e, i=i: e.matmul(
                        ps[:, ob, 0:65], lhsT=PT[pb][:, i * 128:(i + 1) * 128], rhs=V[:, kt_lo + i, hh, 0:65],
                        start=(i == 0), stop=(i == 4)),
                        reads=[("PT", pb), "V"], writes=[("ps", ob)])
                rb_ = n % 2
                S.op("dve", lambda e: e.reciprocal(out=rc[rb_], in_=ps[:, ob, 64:65]),
                     reads=[("ps", ob)], writes=[("rc", rb_)])
                S.op("dve", lambda e: e.tensor_scalar(
                    out=B[:, j, h * 64:(h + 1) * 64], in0=ps[:, ob, 0:64], scalar1=rc[rb_], scalar2=None, op0=ALU.mult),
                    reads=[("ps", ob), ("rc", rb_)], writes=[("B", j)])

            exp_bias(0)
            exp_bias(1)
            for p in range(8):
                wb = p % 2
                if p + 1 < 8:
                    load_w(p + 1)
                for m in range(2):
                    for tg in range(4):
                        bank = 4 + (tg % 2)
                        for kc in range(KC):
                            S.op("pe", lambda e, m=m, tg=tg, kc=kc, bank=bank: e.matmul(
                                ps[:, bank, :], lhsT=Wb[wb][m][:, kc, :], rhs=A[:, kc, tg * 512:(tg + 1) * 512],
                                start=(kc == 0), stop=(kc == KC - 1)),
                                reads=[("W", wb, m)] + [("A", t) for t in range(tg * 4, tg * 4 + 4)],
                                writes=[("ps", bank)])
                        if m == 0:
                            S.op("act", lambda e, tg=tg, bank=bank: e.activation(
                                out=QT[:, tg * 512:(tg + 1) * 512], in_=ps[:, bank, :], func=AF.Copy, scale=0.125),
                                reads=[("ps", bank)], writes=["QT"])
                        else:
                            S.op("dve", lambda e, tg=tg, bank=bank: e.tensor_copy(
                                out=KT[:, tg * 512:(tg + 1) * 512], in_=ps[:, bank, :]),
                                reads=[("ps", bank)], writes=["KT"])
                for tq in range(4):
                    bank = 6 + (tq % 2)
                    for t4 in range(4):
                        tt = tq * 4 + t4
                        for kc in range(KC):
                            S.op("pe", lambda e, tt=tt, t4=t4, kc=kc, bank=bank: e.matmul(
                                ps[:, bank, t4 * 128:(t4 + 1) * 128], lhsT=A[:, kc, tt * 128:(tt + 1) * 128],
                                rhs=Wb[wb][2][:, kc, :], start=(kc == 0), stop=(kc == KC - 1)),
                                reads=[("W", wb, 2), ("A", tt)], writes=[("ps", bank)])
                    S.op("dve", lambda e, tq=tq, bank=bank: e.tensor_copy(
                        out=V[:, tq * 4:(tq + 1) * 4, :, 0:64],
                        in_=ps[:, bank, :].rearrange("p (t h d) -> p t h d", t=4, h=2)),
                        reads=[("ps", bank)], writes=["V"])
                steps = [(2 * p + hh, j) for hh in range(2) for j in range(NT)]
                emit_ST(0, *steps[0])
                emit_ST(1, *steps[1])
                emit_B(0, *steps[0])
                for n, (h, j) in enumerate(steps):
                    if j == 0 and h + 2 < NH:
                        load_bias(h + 2)
                    if j == 8 and h + 2 < NH:
                        exp_bias(h + 2)
                    if n + 2 < len(steps):
                        emit_ST(n + 2, *steps[n + 2])
                    if n + 1 < len(steps):
                        emit_B(n + 1, *steps[n + 1])
                    emit_C(n, h, j)

        def gqa_phase(l):
            ar.reset(R0)
            KTd = ar.alloc([128, 4, SEQ], BF16)
            V = ar.alloc([128, NT, 4, 66], BF16)
            QN = ar.alloc([128, HD]); KN = ar.alloc([128, HD])
            ssq = ar.alloc([128, 20])
            m1 = ar.mark()
            ar_b = Arena(arena_t, ARENA_WORDS)
            ar_b.off = A0 + KC * SEQ // 2
            Wg = [ar_b.alloc([128, KC, 512], BF16) for _ in range(3)]
            RC = ar_b.alloc([128, NT, 64]); RS = ar_b.alloc([128, NT, 64])
            assert ar_b.off <= R0
            SQ = ar.alloc([128, 1280]); QF = ar.alloc([128, 1280]); T1 = ar.alloc([128, 1280])
            QB = ar.alloc([128, 1024], BF16); KD = ar.alloc([128, 4, 2, 64], BF16)
            w_v = gwqkv_d[l].rearrange("(k p) n -> p k n", p=128)
            for cg in range(3):
                S.dma("pool", lambda e, cg=cg: e.dma_start(out=Wg[cg], in_=w_v[:, :, cg * 512:(cg + 1) * 512]),
                      writes=[("Wg", cg)])
            S.dma("sp", lambda e: e.dma_start(out=RC, in_=ropec_d[:, :, :]), writes=["RC"])
            S.dma("sp", lambda e: e.dma_start(out=RS, in_=ropes_d[:, :, :]), writes=["RS"])
            S.dma("sp", lambda e: e.dma_start(out=QN, in_=gqn_d[l:l + 1, :].partition_broadcast(128)), writes=["QN"])
            S.dma("sp", lambda e: e.dma_start(out=KN, in_=gkn_d[l:l + 1, :].partition_broadcast(128)), writes=["KN"])
            S.op("dve", lambda e: e.memset(V[:, :, :, 64:66], 1.0), writes=["V"])

            def hd(ap, nh):
                return ap.rearrange("p (h d) -> p h d", h=nh)

            for tt in range(NT):
                b0 = (tt % 2) * 3
                for cg in range(3):
                    bank = b0 + cg
                    for kc in range(KC):
                        S.op("pe", lambda e, cg=cg, kc=kc, bank=bank, tt=tt: e.matmul(
                            ps[:, bank, :], lhsT=A[:, kc, tt * 128:(tt + 1) * 128], rhs=Wg[cg][:, kc, :],
                            start=(kc == 0), stop=(kc == KC - 1)),
                            reads=[("A", tt), ("Wg", cg)], writes=[("ps", bank)])
                parts = [(ps[:, b0, :], 0, 8, ("ps", b0)), (ps[:, b0 + 1, :], 512, 8, ("ps", b0 + 1)),
                         (ps[:, b0 + 2, 0:256], 1024, 4, ("ps", b0 + 2))]
                for (pap, co, nh, pk) in parts:
                    S.op("act", lambda e, pap=pap, co=co, nh=nh: e.activation(
                        out=SQ[:, co:co + nh * 64], in_=pap, func=AF.Square),
                        reads=[pk], writes=["SQ"])
                S.op("act", lambda e, tt=tt, b0=b0: e.activation(
                    out=V[:, tt, :, 0:64], in_=hd(ps[:, b0 + 2, 256:512], 4), func=AF.Copy),
                    reads=[("ps", b0 + 2)], writes=["V"])
                S.op("dve", lambda e: e.tensor_reduce(out=ssq, in_=hd(SQ, 20), axis=AX.X, op=ALU.add),
                     reads=["SQ"], writes=["ssq"])
                S.op("act", lambda e: e.activation(out=ssq, in_=ssq, func=AF.Sqrt, bias=eps_rms, scale=1.0 / 64),
                     reads=["ssq", "eps"], writes=["ssq"])
                S.op("dve", lambda e: e.reciprocal(out=ssq, in_=ssq), reads=["ssq"], writes=["ssq"])
                h0 = 0
                for (pap, co, nh, pk) in parts:
                    S.op("dve", lambda e, pap=pap, co=co, nh=nh, h0=h0: e.tensor_tensor(
                        out=hd(QF[:, co:co + nh * 64], nh), in0=hd(pap, nh),
                        in1=ssq[:, h0:h0 + nh].unsqueeze(2).to_broadcast([128, nh, 64]), op=ALU.mult),
                        reads=[pk, "ssq"], writes=["QF"])
                    h0 += nh
                S.op("dve", lambda e: e.tensor_tensor(
                    out=hd(QF[:, 0:1024], 16), in0=hd(QF[:, 0:1024], 16),
                    in1=QN.unsqueeze(1).to_broadcast([128, 16, 64]), op=ALU.mult),
                    reads=["QF", "QN"], writes=["QF"])
                S.op("dve", lambda e: e.tensor_tensor(
                    out=hd(QF[:, 1024:1280], 4), in0=hd(QF[:, 1024:1280], 4),
                    in1=KN.unsqueeze(1).to_broadcast([128, 4, 64]), op=ALU.mult),
                    reads=["QF", "KN"], writes=["QF"])
                S.op("dve", lambda e, tt=tt: e.tensor_tensor(
                    out=hd(T1, 20), in0=hd(QF, 20), in1=RC[:, tt, :].unsqueeze(1).to_broadcast([128, 20, 64]),
                    op=ALU.mult), reads=["QF", "RC"], writes=["T1"])

                def v5(ap):
                    return ap.rearrange("p (h a f q) -> p h a f q", h=20, a=2, f=2)

                for f in range(2):
                    S.op("dve", lambda e, tt=tt, f=f: e.tensor_tensor(
                        out=v5(SQ)[:, :, :, f, :], in0=v5(QF)[:, :, :, 1 - f, :],
                        in1=RS[:, tt, :].rearrange("p (a f q) -> p a f q", a=2, f=2)[:, :, f, :]
                        .unsqueeze(1).to_broadcast([128, 20, 2, 16]), op=ALU.mult),
                        reads=["QF", "RS", "ssq"], writes=["SQ"])
                S.op("dve", lambda e: e.tensor_tensor(out=QB, in0=T1[:, 0:1024], in1=SQ[:, 0:1024], op=ALU.add),
                     reads=["T1", "SQ"], writes=["QB"])
                for dup in range(2):
                    S.op("dve", lambda e, dup=dup: e.tensor_tensor(
                        out=KD[:, :, dup, :], in0=hd(T1[:, 1024:1280], 4), in1=hd(SQ[:, 1024:1280], 4), op=ALU.add),
                        reads=["T1", "SQ"], writes=["KD"])
                for pr in range(8):
                    S.op("pe", lambda e, pr=pr: e.transpose(
                        psb(6)[:, pr * 128:(pr + 1) * 128], QB[:, pr * 128:(pr + 1) * 128], identb),
                        reads=["QB", "identb"], writes=[("ps", 6)])
                for g in range(4):
                    S.op("pe", lambda e, g=g: e.transpose(
                        psb(7)[:, g * 128:(g + 1) * 128], KD[:, g, :, :].rearrange("p a d -> p (a d)"), identb),
                        reads=["KD", "identb"], writes=[("ps", 7)])
                S.op("act", lambda e, tt=tt: e.activation(
                    out=A[:, :, tt * 128:(tt + 1) * 128], in_=psb(6).rearrange("p (k n) -> p k n", k=8), func=AF.Copy),
                    reads=[("ps", 6)], writes=[("A", tt)])
                S.op("dve", lambda e, tt=tt: e.tensor_copy(
                    out=KTd[:, :, tt * 128:(tt + 1) * 128], in_=psb(7)[:, 0:512].rearrange("p (g n) -> p g n", g=4)),
                    reads=[("ps", 7)], writes=["KTd"])

            S.barrier()
            ar.reset(m1)
            PT = [ar.alloc([128, 512], BF16) for _ in range(4)]
            rc = [ar.alloc([128, 4]) for _ in range(2)]
            KT2 = ar.alloc([128, 4, SEQ], BF16)
            S.op("act", lambda e: e.activation(out=KT2[64:128, 0:2, :], in_=KTd[64:128, 0:2, :], func=AF.Copy),
                 reads=["KTd"], writes=["KT2"])
            S.op("pool", lambda e: e.tensor_copy(out=KT2[64:128, 2:4, :], in_=KTd[64:128, 2:4, :]),
                 reads=["KTd"], writes=["KT2b"])
            S.op("dve", lambda e: e.memset(KT2[0:64, :, :], 0.0), writes=["KT2c"])
            S.op("dve", lambda e: e.memset(KTd[64:128, :, :], 0.0), reads=["KT2", "KT2b"], writes=["KTd"])
            steps = [(h, qg, kt) for h in range(NH) for qg in range(4) for kt in range(NT)]

            def emit_ST(n):
                h, qg, kt = steps[n]
                g = h // 4
                r0 = (h % 2) * 64
                bank = n % 4
                KK = KTd if h % 2 == 0 else KT2
                S.op("pe", lambda e: e.matmul(
                    ps[:, bank, :], lhsT=KK[:, g, kt * 128:(kt + 1) * 128],
                    rhs=A[:, h // 2, qg * 512:(qg + 1) * 512], start=True, stop=True),
                    reads=["KTd", "KT2", "KT2b", "KT2c"] + [("A", t) for t in range(qg * 4, qg * 4 + 4)], writes=[("ps", bank)])

            def emit_rest(n):
                h, qg, kt = steps[n]
                g = h // 4
                bank = n % 4
                pt = PT[n % 4]
                grp = n // NT
                ob = 4 + (grp % 2)
                S.op("act", lambda e: e.activation(out=pt, in_=ps[:, bank, :], func=AF.Exp, scale=0.125),
                     reads=[("ps", bank)], writes=[("PT", n % 4)])
                for qt in range(4):
                    S.op("pe", lambda e, qt=qt: e.matmul(
                        ps[:, ob, qt * 128:qt * 128 + 65], lhsT=pt[:, qt * 128:(qt + 1) * 128],
                        rhs=V[:, kt, g, 0:65], start=(kt == 0), stop=(kt == NT - 1)),
                        reads=[("PT", n % 4), "V"], writes=[("ps", ob)])
                if kt == NT - 1:
                    rb = grp % 2
                    S.op("dve", lambda e: e.reciprocal(
                        out=rc[rb], in_=ps[:, ob, :].rearrange("p (q n) -> p q n", q=4)[:, :, 64]),
                        reads=[("ps", ob)], writes=[("rc", rb)])
                    for qt in range(4):
                        tt = qg * 4 + qt
                        S.op("dve", lambda e, qt=qt, tt=tt: e.tensor_scalar(
                            out=B[:, tt, h * 64:(h + 1) * 64], in0=ps[:, ob, qt * 128:qt * 128 + 64],
                            scalar1=rc[rb][:, qt:qt + 1], scalar2=None, op0=ALU.mult),
                            reads=[("ps", ob), ("rc", rb)], writes=[("B", tt)])

            emit_ST(0)
            emit_ST(1)
            for n in range(len(steps)):
                if n + 2 < len(steps):
                    emit_ST(n + 2)
                emit_rest(n)

        def wo_ln_phase(i, wo_ap):
            ar.reset(R0)
            WO = ar.alloc([128, KC, D], BF16)
            VB = TMPV + [ar.alloc([128, D]) for _ in range(2)]
            wo_v = wo_ap.rearrange("(k p) n -> p k n", p=128)
            for half in range(2):
                S.dma("pool", lambda e, half=half: e.dma_start(
                    out=WO[:, :, half * 512:(half + 1) * 512], in_=wo_v[:, :, half * 512:(half + 1) * 512]),
                    writes=[("WO", half)])
            load_ln_params(i, 0)
            for half in range(2):
                S.op("dve", lambda e, half=half: e.tensor_tensor(
                    out=WO[:, :, half * 512:(half + 1) * 512], in0=WO[:, :, half * 512:(half + 1) * 512],
                    in1=G1B[:, half * 512:(half + 1) * 512].unsqueeze(1).to_broadcast([128, KC, 512]), op=ALU.mult),
                    reads=[("WO", half), ("GB", 2)], writes=[("WO", half)])
            for tt in range(NT):
                bank = tt % 2
                for kc in range(KC):
                    S.op("pe", lambda e, tt=tt, kc=kc, bank=bank: e.transpose(
                        psb(bank)[:, kc * 128:(kc + 1) * 128], B[:, tt, kc * 128:(kc + 1) * 128], identb),
                        reads=[("B", tt), "identb"], writes=[("ps", bank)])
                eng = "act" if tt % 2 == 0 else "dve"
                if eng == "act":
                    S.op("act", lambda e, tt=tt, bank=bank: e.activation(
                        out=A[:, :, tt * 128:(tt + 1) * 128], in_=psb(bank).rearrange("p (k n) -> p k n", k=8),
                        func=AF.Copy), reads=[("ps", bank)], writes=[("A", tt)])
                else:
                    S.op("dve", lambda e, tt=tt, bank=bank: e.tensor_copy(
                        out=A[:, :, tt * 128:(tt + 1) * 128], in_=psb(bank).rearrange("p (k n) -> p k n", k=8)),
                        reads=[("ps", bank)], writes=[("A", tt)])
            def wo_mm(tt):
                banks = [2 + 2 * (tt % 3), 3 + 2 * (tt % 3)]
                for half in range(2):
                    S.op("pe", lambda e, half=half, bank=banks[half]: e.matmul(
                        ps[:, bank, :], lhsT=alphaI, rhs=X[:, tt, half * 512:(half + 1) * 512], start=True, stop=False),
                        reads=["alphaI", ("X", tt)], writes=[("ps", banks[half])])
                    for kc in range(KC):
                        S.op("pe", lambda e, kc=kc, half=half, bank=banks[half]: e.matmul(
                            ps[:, bank, :], lhsT=A[:, kc, tt * 128:(tt + 1) * 128],
                            rhs=WO[:, kc, half * 512:(half + 1) * 512], start=False, stop=(kc == KC - 1)),
                            reads=[("A", tt), ("WO", half)], writes=[("ps", banks[half])])

            def wo_stats(tt):
                banks = [2 + 2 * (tt % 3), 3 + 2 * (tt % 3)]
                ln_stats(tt, ps[:, banks[0]:banks[0] + 2, :].rearrange("p a n -> p (a n)"),
                         [("ps", banks[0]), ("ps", banks[1])], vbufs=VB)

            wo_mm(0)
            wo_mm(1)
            wo_stats(0)
            for tt in range(NT):
                if tt + 2 < NT:
                    wo_mm(tt + 2)
                if tt + 1 < NT:
                    wo_stats(tt + 1)
                ln_finish(tt, store_out=False, vbufs=VB)

        def moe_phase(i, store_out, next_layer=None):
            ar.reset(A0)
            NWB = 3
            WE = [[ar.alloc([128, KC, 512], BF16), ar.alloc([128, KC, 512], BF16), ar.alloc([128, 4, D], BF16)]
                  for _ in range(NWB)]
            m0 = ar.mark()

            def load_expert(e_, which=(0, 1, 2)):
                wb = e_ % NWB
                srcs = (wg_d, wu_d, wd_d)
                for wi in which:
                    S.dma("pool", lambda e, wi=wi: e.dma_start(
                        out=WE[wb][wi], in_=srcs[wi][i, e_].rearrange("(k p) n -> p k n", p=128)),
                        writes=[("WE", wb, wi)])

            load_ln_params(i, 1)
            WR = ar.alloc([128, KC, 36]); RB = ar.alloc([128, 36])
            HT32 = [ar.alloc([128, KC, 128]) for _ in range(2)]
            L = ar.alloc([128, NT, 36])
            gmax = ar.alloc([128, NT]); gsum = ar.alloc([128, NT]); m1_ = ar.alloc([128, NT]); m2_ = ar.alloc([128, NT])
            dd = ar.alloc([128, NT]); e21 = ar.alloc([128, NT])
            gsel = ar.alloc([128, NT, 4]); gex = ar.alloc([128, NT, 4])
            lem = ar.alloc([128, NT, NE]); lem2 = ar.alloc([128, NT, NE])
            OH1 = ar.alloc([128, NT, NE]); OH2 = ar.alloc([128, NT, NE])
            Mall = ar.alloc([128, NT, NE], BF16)
            PEf = ar.alloc([128, NT, NE]); PR = ar.alloc([128, NT, NE])
            D1f = ar.alloc([128, NT]); D2f = ar.alloc([128, NT])
            HB = [ar.alloc([128, D], BF16) for _ in range(2)]
            HF = [ar.alloc([128, D]) for _ in range(2)]
            SHB, SCB = TMPV[0], TMPV[1]
            S.dma("sp", lambda e: e.dma_start(out=SHB, in_=modrow_d[0:1, :].partition_broadcast(128)),
                  reads=[("modrow", 3, 0), ("modrow", 3, 1)], writes=["SHB"])
            S.dma("sp", lambda e: e.dma_start(out=SCB, in_=modrow_d[1:2, :].partition_broadcast(128)),
                  reads=[("modrow", 4, 0), ("modrow", 4, 1)], writes=["SCB"])
            S.dma("sp", lambda e: e.dma_start(out=WR, in_=wr_d[i].rearrange("(k p) n -> p k n", p=128)), writes=["WR"])
            S.dma("sp", lambda e: e.dma_start(out=RB, in_=br_d[i:i + 1, :].partition_broadcast(128)), writes=["RB"])
            load_expert(0)
            load_expert(1)
            load_expert(2)
            for tt in range(NT):
                hb = tt % 2
                for half in range(2):
                    bank = (2 * tt + half) % 4
                    for q in range(4):
                        kc = half * 4 + q
                        S.op("pe", lambda e, tt=tt, q=q, kc=kc, bank=bank: e.transpose(
                            ps[:, bank, q * 128:(q + 1) * 128], X[:, tt, kc * 128:(kc + 1) * 128], ident),
                            reads=[("X", tt), "ident"], writes=[("ps", bank)])
                    for q in range(4):
                        kc = half * 4 + q
                        S.op("act", lambda e, q=q, kc=kc, bank=bank, hb=hb: e.activation(
                            out=HT32[hb][:, kc, :], in_=ps[:, bank, q * 128:(q + 1) * 128],
                            func=AF.Identity, bias=SH2[:, kc:kc + 1], scale=SC2[:, kc:kc + 1]),
                            reads=[("ps", bank), ("modfm", 3), ("modfm", 4)], writes=[("HT32", hb)])
                rbank = 4 + tt // 8
                c0 = (tt % 8) * 36
                for kc in range(KC):
                    S.op("pe", lambda e, kc=kc, hb=hb, rbank=rbank, c0=c0: e.matmul(
                        ps[:, rbank, c0:c0 + 36], lhsT=HT32[hb][:, kc, :], rhs=WR[:, kc, :],
                        start=(kc == 0), stop=(kc == KC - 1)),
                        reads=[("HT32", hb), "WR"], writes=[("ps", rbank)])
            k_ = "rt"
            for hf in range(2):
                S.op("dve", lambda e, hf=hf: e.tensor_tensor(
                    out=L[:, hf * 8:(hf + 1) * 8, :], in0=ps[:, 4 + hf, 0:288].rearrange("p (t x) -> p t x", t=8),
                    in1=RB.unsqueeze(1).to_broadcast([128, 8, 36]), op=ALU.add),
                    reads=[("ps", 4 + hf), "RB"], writes=[k_])
            LG = L[:, :, 0:4]
            LE = L[:, :, 4:36]
            S.op("dve", lambda e: e.tensor_reduce(out=gmax, in_=LG, axis=AX.X, op=ALU.max), reads=[k_], writes=[k_])
            S.op("dve", lambda e: e.tensor_tensor(out=gsel, in0=LG, in1=gmax.unsqueeze(2).to_broadcast([128, NT, 4]),
                                                  op=ALU.is_equal), reads=[k_], writes=[k_])
            S.op("dve", lambda e: e.tensor_tensor(out=gex, in0=LG, in1=gmax.unsqueeze(2).to_broadcast([128, NT, 4]),
                                                  op=ALU.subtract), reads=[k_], writes=[k_])
            S.op("act", lambda e: e.activation(out=gex, in_=gex, func=AF.Exp), reads=[k_], writes=[k_])
            S.op("dve", lambda e: e.tensor_reduce(out=gsum, in_=gex, axis=AX.X, op=ALU.add), reads=[k_], writes=[k_])
            S.op("dve", lambda e: e.reciprocal(out=gsum, in_=gsum), reads=[k_], writes=[k_])
            S.op("dve", lambda e: e.tensor_scalar(out=gsel, in0=gsel, scalar1=BIG, scalar2=-BIG, op0=ALU.mult, op1=ALU.add),
                 reads=[k_], writes=[k_])
            for hf in range(2):
                S.op("dve", lambda e, hf=hf: e.tensor_tensor(
                    out=lem[:, hf * 8:(hf + 1) * 8, :].rearrange("p t (g x) -> p (t g) x", g=4),
                    in0=LE[:, hf * 8:(hf + 1) * 8, :].rearrange("p t (g x) -> p t g x", g=4),
                    in1=gsel[:, hf * 8:(hf + 1) * 8, :].unsqueeze(3).to_broadcast([128, 8, 4, 8]), op=ALU.add),
                    reads=[k_], writes=[k_])
            S.op("dve", lambda e: e.tensor_reduce(out=m1_, in_=lem, axis=AX.X, op=ALU.max), reads=[k_], writes=[k_])
            S.op("dve", lambda e: e.tensor_tensor(out=OH1, in0=lem, in1=m1_.unsqueeze(2).to_broadcast([128, NT, NE]),
                                                  op=ALU.is_equal), reads=[k_], writes=["OH"])
            S.op("dve", lambda e: e.scalar_tensor_tensor(
                out=lem2.rearrange("p t x -> p (t x)"), in0=OH1.rearrange("p t x -> p (t x)"), scalar=-BIG,
                in1=lem.rearrange("p t x -> p (t x)"), op0=ALU.mult, op1=ALU.add), reads=[k_, "OH"], writes=[k_])
            S.op("dve", lambda e: e.tensor_reduce(out=m2_, in_=lem2, axis=AX.X, op=ALU.max), reads=[k_], writes=[k_])
            S.op("dve", lambda e: e.tensor_tensor(out=OH2, in0=lem2, in1=m2_.unsqueeze(2).to_broadcast([128, NT, NE]),
                                                  op=ALU.is_equal), reads=[k_], writes=["OH"])
            S.op("dve", lambda e: e.tensor_tensor(out=Mall, in0=OH1, in1=OH2, op=ALU.add), reads=["OH"], writes=["Mall"])
            S.op("dve", lambda e: e.tensor_tensor(out=dd, in0=m2_, in1=m1_, op=ALU.subtract), reads=[k_], writes=[k_])
            S.op("act", lambda e: e.activation(out=e21, in_=dd, func=AF.Exp), reads=[k_], writes=[k_])
            S.op("dve", lambda e: e.tensor_scalar(out=e21, in0=e21, scalar1=1.0, scalar2=None, op0=ALU.add), reads=[k_], writes=[k_])
            S.op("dve", lambda e: e.reciprocal(out=e21, in_=e21), reads=[k_], writes=[k_])
            S.op("dve", lambda e: e.tensor_tensor(out=G1, in0=e21, in1=gsum, op=ALU.mult), reads=[k_], writes=["G"])
            S.op("dve", lambda e: e.tensor_tensor(out=G2, in0=gsum, in1=G1, op=ALU.subtract), reads=[k_, "G"], writes=["G"])
            for tt in range(NT):
                S.op("pe", lambda e, tt=tt: e.matmul(
                    ps[:, 6, tt * NE:(tt + 1) * NE], lhsT=trib, rhs=Mall[:, tt, :], start=True, stop=(tt == 0)),
                    reads=["trib", "Mall"], writes=[("ps", 6)])
                for t2 in range(tt):
                    S.op("pe", lambda e, tt=tt, t2=t2: e.matmul(
                        ps[:, 6, tt * NE:(tt + 1) * NE], lhsT=onesb, rhs=Mall[:, t2, :], start=False, stop=(t2 == tt - 1)),
                        reads=["onesb", "Mall"], writes=[("ps", 6)])
            S.op("dve", lambda e: e.tensor_tensor(
                out=PEf, in0=ps[:, 6, :].rearrange("p (t x) -> p t x", t=NT),
                in1=iotac.unsqueeze(1).to_broadcast([128, NT, NE]), op=ALU.add),
                reads=[("ps", 6), "iotac"], writes=["PEf"])
            for (OH, Df, Di) in ((OH1, D1f, D1i), (OH2, D2f, D2i)):
                S.op("dve", lambda e, OH=OH: e.tensor_tensor(out=PR, in0=OH, in1=PEf, op=ALU.mult),
                     reads=["OH", "PEf"], writes=["PR"])
                S.op("dve", lambda e, Df=Df: e.tensor_reduce(out=Df, in_=PR, axis=AX.X, op=ALU.add),
                     reads=["PR"], writes=["Df"])
                S.op("dve", lambda e, Df=Df, Di=Di: e.tensor_copy(out=Di, in_=Df), reads=["Df"], writes=["Di"])
            for tt in range(NT):
                hb = tt % 2
                S.op("dve", lambda e, tt=tt, hb=hb: e.tensor_tensor(out=HF[hb], in0=X[:, tt, :], in1=SCB, op=ALU.mult),
                     reads=[("X", tt), "SCB"], writes=[("HF", hb)])
                S.op("dve", lambda e, hb=hb: e.tensor_tensor(out=HB[hb], in0=HF[hb], in1=SHB, op=ALU.add),
                     reads=[("HF", hb), "SHB"], writes=[("HB", hb)])
                for Di in (D1i, D2i):
                    S.dma("pool", lambda e, tt=tt, Di=Di, hb=hb: e.indirect_dma_start(
                        out=xs_d[:, :], out_offset=bass.IndirectOffsetOnAxis(ap=Di[:, tt:tt + 1], axis=0),
                        in_=HB[hb], in_offset=None),
                        reads=[("HB", hb), "Di"], writes=["XS"])
            S.barrier()
            ar.reset(m0)
            NXG = 6
            NYO = 4
            XG = [ar.alloc([128, D], BF16) for _ in range(NXG)]
            XeT = [ar.alloc([128, KC, CAP], BF16) for _ in range(2)]
            HTb = [ar.alloc([128, 4, CAP], BF16) for _ in range(2)]
            SG = [ar.alloc([128, CAP]) for _ in range(2)]
            YO = [ar.alloc([128, D], BF16) for _ in range(NYO)]

            def prep_load(e_):
                for s_ in range(NS):
                    xb = (e_ * NS + s_) % NXG
                    r0 = e_ * CAP + s_ * 128
                    S.dma("sp", lambda e, xb=xb, r0=r0: e.dma_start(out=XG[xb], in_=xs_d[r0:r0 + 128, :]),
                          reads=["XS"], writes=[("XG", xb)])

            def prep(e_):
                wb = e_ % 2
                for s_ in range(NS):
                    xb = (e_ * NS + s_) % NXG
                    bank = (e_ * NS + s_) % 2
                    for kc in range(KC):
                        S.op("pe", lambda e, xb=xb, kc=kc, bank=bank: e.transpose(
                            psb(bank)[:, kc * 128:(kc + 1) * 128], XG[xb][:, kc * 128:(kc + 1) * 128], identb),
                            reads=[("XG", xb), "identb"], writes=[("ps", bank)])
                    S.op("act", lambda e, s_=s_, bank=bank, wb=wb: e.activation(
                        out=XeT[wb][:, :, s_ * 128:(s_ + 1) * 128], in_=psb(bank).rearrange("p (k n) -> p k n", k=KC),
                        func=AF.Copy), reads=[("ps", bank)], writes=[("XeT", wb)])

            def compute(e_):
                wb = e_ % 2
                ww = e_ % NWB
                Wg_, Wu_, Wd_ = WE[ww]
                for fc in range(4):
                    gbank = 2 + 2 * (fc % 2)
                    ubank = gbank + 1
                    for (wi, W_, bank) in ((0, Wg_, gbank), (1, Wu_, ubank)):
                        for kc in range(KC):
                            S.op("pe", lambda e, fc=fc, kc=kc, W_=W_, bank=bank: e.matmul(
                                ps[:, bank, 0:CAP], lhsT=W_[:, kc, fc * 128:(fc + 1) * 128], rhs=XeT[wb][:, kc, :],
                                start=(kc == 0), stop=(kc == KC - 1)),
                                reads=[("WE", ww, wi), ("XeT", wb)], writes=[("ps", bank)])
                    S.op("act", lambda e, fc=fc, gbank=gbank: e.activation(out=SG[fc % 2], in_=ps[:, gbank, 0:CAP], func=AF.Silu),
                         reads=[("ps", gbank)], writes=[("SG", fc % 2)])
                    S.op("dve", lambda e, fc=fc, ubank=ubank: e.tensor_tensor(
                        out=HTb[wb][:, fc, :], in0=SG[fc % 2], in1=ps[:, ubank, 0:CAP], op=ALU.mult),
                        reads=[("SG", fc % 2), ("ps", ubank)], writes=[("HTb", wb)])
                if e_ + NWB < NE:
                    load_expert(e_ + NWB, which=(0, 1))
                for s_ in range(NS):
                    yb_ = (e_ * NS + s_) % NYO
                    for half in range(2):
                        bank = 6 + half
                        for fc in range(4):
                            S.op("pe", lambda e, s_=s_, half=half, fc=fc, bank=bank: e.matmul(
                                ps[:, bank, :], lhsT=HTb[wb][:, fc, s_ * 128:(s_ + 1) * 128],
                                rhs=Wd_[:, fc, half * 512:(half + 1) * 512], start=(fc == 0), stop=(fc == 3)),
                                reads=[("HTb", wb), ("WE", ww, 2)], writes=[("ps", bank)])
                        S.op("dve", lambda e, yb_=yb_, bank=bank, half=half: e.tensor_tensor(
                            out=YO[yb_][:, half * 512:(half + 1) * 512], in0=ps[:, bank, :],
                            in1=G2B[:, half * 512:(half + 1) * 512], op=ALU.mult),
                            reads=[("ps", bank), ("GB", 5)], writes=[("YO", yb_)])
                    r0 = e_ * CAP + s_ * 128
                    S.dma("sp", lambda e, yb_=yb_, r0=r0: e.dma_start(out=ys_d[r0:r0 + 128, :], in_=YO[yb_]),
                          reads=[("YO", yb_)], writes=["YS"])
                if e_ + NWB < NE:
                    load_expert(e_ + NWB, which=(2,))

            prep_load(0)
            prep_load(1)
            prep(0)
            for e_ in range(NE):
                if e_ + 2 < NE:
                    prep_load(e_ + 2)
                if e_ + 1 < NE:
                    prep(e_ + 1)
                compute(e_)
            S.barrier()
            ar.reset(A0 + KC * SEQ // 2)
            modg = mod_groups(next_layer, R0, (0,), (1,)) if next_layer is not None else None
            NYB = 4
            Y1 = [ar.alloc([128, D], BF16) for _ in range(NYB)]
            Y2 = [ar.alloc([128, D], BF16) for _ in range(NYB)]
            VB = TMPV + [ar.alloc([128, D]) for _ in range(2)]
            def gather(tt):
                yb = tt % NYB
                S.dma("pool", lambda e: e.indirect_dma_start(
                    out=Y1[yb], out_offset=None, in_=ys_d[:, :],
                    in_offset=bass.IndirectOffsetOnAxis(ap=D1i[:, tt:tt + 1], axis=0)),
                    reads=["YS", "Di"], writes=[("Y1", yb)])
                S.dma("pool", lambda e: e.indirect_dma_start(
                    out=Y2[yb], out_offset=None, in_=ys_d[:, :],
                    in_offset=bass.IndirectOffsetOnAxis(ap=D2i[:, tt:tt + 1], axis=0)),
                    reads=["YS", "Di"], writes=[("Y2", yb)])

            DG = [[ar.alloc([128, 128], BF16) for _ in range(2)] for _ in range(2)]
            for tt in range(NYB):
                gather(tt)

            def build(tt):
                yb = tt % NYB
                db = tt % 2
                for k_, Gk in enumerate((G1, G2)):
                    S.op("dve", lambda e, k_=k_, Gk=Gk: e.tensor_scalar(
                        out=DG[db][k_], in0=ident, scalar1=Gk[:, tt:tt + 1], scalar2=None, op0=ALU.mult),
                        reads=["ident", "G"], writes=[("DG", db, k_)])
                banks = [2 + 2 * (tt % 3), 3 + 2 * (tt % 3)]
                for half in range(2):
                    hs = slice(half * 512, (half + 1) * 512)
                    S.op("pe", lambda e, half=half, hs=hs: e.matmul(
                        ps[:, banks[half], :], lhsT=alphaI, rhs=X[:, tt, hs], start=True, stop=False),
                        reads=["alphaI", ("X", tt)], writes=[("ps", banks[half])])
                    S.op("pe", lambda e, half=half, hs=hs: e.matmul(
                        ps[:, banks[half], :], lhsT=DG[db][0], rhs=Y1[yb][:, hs], start=False, stop=False),
                        reads=[("DG", db, 0), ("Y1", yb)], writes=[("ps", banks[half])])
                    S.op("pe", lambda e, half=half, hs=hs: e.matmul(
                        ps[:, banks[half], :], lhsT=DG[db][1], rhs=Y2[yb][:, hs], start=False, stop=True),
                        reads=[("DG", db, 1), ("Y2", yb)], writes=[("ps", banks[half])])

            def cstats(tt):
                banks = [2 + 2 * (tt % 3), 3 + 2 * (tt % 3)]
                ln_stats(tt, ps[:, banks[0]:banks[0] + 2, :].rearrange("p a n -> p (a n)"),
                         [("ps", banks[0]), ("ps", banks[1])], vbufs=VB)

            build(0)
            build(1)
            cstats(0)
            for tt in range(NT):
                if tt + 2 < NT:
                    build(tt + 2)
                if tt + 1 < NT:
                    cstats(tt + 1)
                ln_finish(tt, store_out=store_out, vbufs=VB)
                if tt + NYB < NT:
                    gather(tt + NYB)
                if modg is not None:
                    if tt < 12:
                        modg(tt)
                    if tt >= 4:
                        hT_tile(tt - 4, (1,), SC1, SH1, 1, 0)
            if modg is not None:
                for k in range(NT - 4, NT):
                    hT_tile(k, (1,), SC1, SH1, 1, 0)

        def dump_dbg(kind):
            if kind == "mod":
                S.dma("sp", lambda e: e.dma_start(out=out_d[0:128, :], in_=G1B), reads=[("GB", 2)], writes=["o0"])
                S.dma("sp", lambda e: e.dma_start(out=out_d[128:256, :], in_=G2B), reads=[("GB", 5)], writes=["o1"])
                for n_, t_ in enumerate((SC1, SH1, SC2, SH2)):
                    S.dma("sp", lambda e, n_=n_, t_=t_: e.dma_start(out=out_d[256:384, n_ * 8:(n_ + 1) * 8], in_=t_),
                          reads=[("modfm", k) for k in (0, 1, 3, 4)], writes=[("o2", n_)])
            elif kind == "hT":
                for kc in range(KC):
                    S.dma("pool", lambda e, kc=kc: e.dma_start(out=out_d[kc * 128:(kc + 1) * 128, :], in_=A[:, kc, 0:1024]),
                          reads=[("A", t) for t in range(NT)], writes=[("o", kc)])
            elif kind == "attn":
                for tt in range(NT):
                    S.dma("pool", lambda e, tt=tt: e.dma_start(out=out_d[tt * 128:(tt + 1) * 128, :], in_=B[:, tt, :]),
                          reads=[("B", tt)], writes=[("o", tt)])

        def dump_x():
            for tt in range(NT):
                S.dma("sp", lambda e, tt=tt: e.dma_start(out=out_d[tt * 128:(tt + 1) * 128, :], in_=X[:, tt, :]),
                      reads=[("X", tt)], writes=[("out", tt)])

        done = False
        premod = False
        for i in range(n_layers):
            S.barrier()
            if not premod:
                mod_phase(i)
                S.barrier()
                if stop == ("mod", i):
                    dump_dbg("mod")
                    done = True
                    break
                build_hT(SC1, SH1, 1, 0)
                if stop == ("hT", i):
                    S.barrier()
                    dump_dbg("hT")
                    done = True
                    break
            l = i // 2
            if i % 2 == 0:
                na_phase(l)
                wo_ap = nawo_d[l]
            else:
                gqa_phase(l)
                wo_ap = gwo_d[l]
            S.barrier()
            if stop == ("attn", i):
                dump_dbg("attn")
                done = True
                break
            wo_ln_phase(i, wo_ap)
            S.barrier()
            if stop == ("mix", i):
                dump_x()
                done = True
                break
            last = (i == n_layers - 1)
            moe_phase(i, store_out=last, next_layer=(None if last else i + 1))
            premod = not last
            if last:
                done = True
        assert done
        S.barrier()
        S.emit()
        S.close()
    return nc


_CACHE = {}


def _prep_inputs(x, c, ada_w, ada_b, ln_g, ln_b, na_w_qkv, na_rpb, na_w_o, gqa_w_qkv, gqa_q_norm,
                 gqa_k_norm, gqa_w_o, moe_w_group, moe_b_group, moe_w_expert, moe_b_expert,
                 moe_w_gate, moe_w_up, moe_w_down):
    f = lambda a: np.ascontiguousarray(np.asarray(a), dtype=np.float32)
    C64, S64 = _rope_tables()
    shared = {
        "ada_w": f(ada_w), "ada_b": f(ada_b), "ln_g": f(ln_g), "ln_b": f(ln_b),
        "na_w_qkv": f(na_w_qkv), "na_w_o": f(na_w_o), "na_bias": _na_bias_table(f(na_rpb)).reshape(-1, NH, 128, NCLS * 5 * 128),
        "gqa_w_qkv": f(gqa_w_qkv), "gqa_w_o": f(gqa_w_o), "gqa_q_norm": f(gqa_q_norm), "gqa_k_norm": f(gqa_k_norm),
        "rope_c": C64, "rope_s": S64,
        "moe_wr": np.ascontiguousarray(np.concatenate([f(moe_w_group), f(moe_w_expert)], axis=-1)),
        "moe_br": np.ascontiguousarray(np.concatenate([f(moe_b_group), f(moe_b_expert)], axis=-1)),
        "moe_w_gate": f(moe_w_gate), "moe_w_up": f(moe_w_up), "moe_w_down": f(moe_w_down),
        "ident": np.eye(128, dtype=np.float32),
        "tri": np.triu(np.ones((128, 128), np.float32), 1),
        "iotac": np.ascontiguousarray(np.broadcast_to((np.arange(NE) * CAP).astype(np.float32), (128, NE))),
    }
    x = f(x)
    c = f(c)
    in_maps = []
    for b in range(8):
        m = dict(shared)
        m["x"] = x[b]
        m["cfm"] = np.ascontiguousarray(c[b].reshape(KC, 128).T)
        in_maps.append(m)
    return in_maps


def kernel(**inputs):
    in_maps = _prep_inputs(**inputs)
    key = "full"
    if key not in _CACHE:
        _CACHE[key] = build_program()
    nc = _CACHE[key]
    res = run_bass_kernel_spmd(nc, in_maps, core_ids=list(range(8)))
    return np.stack([np.asarray(r["out"], dtype=np.float32) for r in res.results], axis=0)
```

```python
import numpy as np
import concourse.bass as bass
import concourse.mybir as mybir
from concourse.bass_utils import run_bass_kernel_spmd

F32 = mybir.dt.float32
BF16 = mybir.dt.bfloat16
I32 = mybir.dt.int32
AF = mybir.ActivationFunctionType
ALU = mybir.AluOpType
AX = mybir.AxisListType

D = 1024
SEQ = 2048
NT = 16
KC = 8
NH = 16
HD = 64
DEPTH = 4
NE = 32
CAP = 384
NS = CAP // 128
ALPHA = float((2 * DEPTH) ** 0.25)
LN_EPS = 1e-5
RMS_EPS = 1e-6
NEG = -30000.0
BIG = 1.0e4
ARENA_WORDS = 53200


class _Rec:
    def __init__(self):
        self.call = None

    def __getattr__(self, name):
        def f(*a, **k):
            self.call = (name, a, k)
            return self
        return f


def _record(fn):
    r = _Rec()
    fn(r)
    assert r.call is not None
    return r.call


class Sched:
    def __init__(self, nc, n_dma_sems=8, same_engine_sync=True):
        self.nc = nc
        self.prog = {e: [] for e in ("pe", "act", "dve", "pool", "sp")}
        self.cnt = {e: 0 for e in self.prog}
        self.sems = {}
        self.waited = {e: {} for e in self.prog}
        self.last_w = {}
        self.readers = {}
        self.same_engine_sync = same_engine_sync
        self.n_dma_sems = n_dma_sems
        self.dma_ring = {q: {"next": 0, "tot": [0] * n_dma_sems} for q in ("sp", "act", "pool")}
        self._ctx = []

    def open(self):
        nc = self.nc
        for e in self.prog:
            cm = nc.semaphore("s_" + e)
            self.sems["s_" + e] = cm.__enter__()
            self._ctx.append(cm)
        for q in self.dma_ring:
            for i in range(self.n_dma_sems):
                nm = f"d_{q}{i}"
                cm = nc.semaphore(nm)
                self.sems[nm] = cm.__enter__()
                self._ctx.append(cm)

    def close(self):
        for cm in reversed(self._ctx):
            cm.__exit__(None, None, None)

    def _need(self, eng, tok, waits):
        if tok is None:
            return
        sem, val, peng = tok
        if peng == eng and (eng == "pe" or not self.same_engine_sync):
            return
        if self.waited[eng].get(sem, 0) >= val:
            return
        if waits.get(sem, 0) < val:
            waits[sem] = val

    def _collect(self, eng, reads, writes):
        waits = {}
        for k in reads:
            self._need(eng, self.last_w.get(k), waits)
        for k in writes:
            self._need(eng, self.last_w.get(k), waits)
            for t in self.readers.get(k, ()):
                self._need(eng, t, waits)
        for s, v in waits.items():
            self.waited[eng][s] = v
        return list(waits.items())

    def _update(self, tok, reads, writes):
        for k in writes:
            self.last_w[k] = tok
            self.readers[k] = []
        for k in reads:
            if k in writes:
                continue
            lst = self.readers.setdefault(k, [])
            lst.append(tok)
            if len(lst) > 48:
                best = {}
                for t in lst:
                    if t[0] not in best or best[t[0]][1] < t[1]:
                        best[t[0]] = t
                self.readers[k] = list(best.values())

    def op(self, eng, fn, reads=(), writes=()):
        waits = self._collect(eng, reads, writes)
        self.cnt[eng] += 1
        tok = ("s_" + eng, self.cnt[eng], eng)
        self.prog[eng].append((waits, _record(fn), ("s_" + eng, 1)))
        self._update(tok, reads, writes)
        return tok

    def dma(self, q, fn, reads=(), writes=()):
        ring = self.dma_ring[q]
        i = ring["next"]
        ring["next"] = (i + 1) % self.n_dma_sems
        sem = f"d_{q}{i}"
        waits = dict(self._collect(q, reads, writes))
        prev = ring["tot"][i]
        if prev and self.waited[q].get(sem, 0) < prev:
            waits[sem] = prev
            self.waited[q][sem] = prev
        ring["tot"][i] += 16
        tok = (sem, ring["tot"][i], "dma_" + q)
        self.prog[q].append((list(waits.items()), _record(fn), (sem, 16)))
        self._update(tok, reads, writes)
        return tok

    def barrier(self):
        targets = {}
        for e, c in self.cnt.items():
            if c:
                targets["s_" + e] = c
        for q, ring in self.dma_ring.items():
            for i, t in enumerate(ring["tot"]):
                if t:
                    targets[f"d_{q}{i}"] = t
        for eng in self.prog:
            waits = []
            for s, v in targets.items():
                if s == "s_" + eng and eng == "pe":
                    continue
                if self.waited[eng].get(s, 0) < v:
                    waits.append((s, v))
                    self.waited[eng][s] = v
            if waits:
                self.prog[eng].append((waits, None, None))

    def emit(self):
        nc = self.nc
        sems = self.sems
        prog = self.prog

        def run(engh, lst):
            for waits, fn, inc in lst:
                for s, v in waits:
                    engh.wait_ge(sems[s], v)
                if fn is not None:
                    name, a, k = fn
                    ins = getattr(engh, name)(*a, **k)
                    ins.then_inc(sems[inc[0]], inc[1])

        with nc.Block() as block:
            @block.sync
            def _(e):
                run(e, prog["sp"])

            @block.tensor
            def _(e):
                run(e, prog["pe"])

            @block.scalar
            def _(e):
                run(e, prog["act"])

            @block.vector
            def _(e):
                run(e, prog["dve"])

            @block.gpsimd
            def _(e):
                run(e, prog["pool"])


class Arena:
    def __init__(self, t, nwords):
        self.t = t
        self.n = nwords
        self.off = 0

    def alloc(self, shape, dt=F32):
        free = 1
        for s in shape[1:]:
            free *= s
        esz = 4 if dt in (F32, I32) else 2
        words = (free * esz + 3) // 4
        words = (words + 7) // 8 * 8
        assert self.off + words <= self.n, ("arena overflow", self.off, words, self.n)
        v = self.t[:, self.off:self.off + words]
        self.off += words
        if dt != F32:
            v = v.bitcast(dt)
        v = v[:, 0:free]
        if len(shape) == 3:
            v = v.rearrange("p (a b) -> p a b", a=shape[1])
        elif len(shape) == 4:
            v = v.rearrange("p (a b c) -> p a b c", a=shape[1], b=shape[2])
        return v

    def mark(self):
        return self.off

    def reset(self, m):
        self.off = m


def _na_patterns():
    pats = []
    for j in range(NT):
        kt_lo = min(max(j - 2, 0), 11)
        qi = np.arange(128)
        r = 2 * j + qi // 64
        c = qi % 64
        rs = np.clip(r - 4, 0, 24)
        ws = np.clip(c - 8, 0, 48)
        tiles = []
        for i in range(5):
            kt = kt_lo + i
            ki = np.arange(128)
            kr = 2 * kt + ki // 64
            kcol = ki % 64
            vr = (kr[:, None] >= rs[None, :]) & (kr[:, None] < rs[None, :] + 8)
            vc = (kcol[:, None] >= ws[None, :]) & (kcol[:, None] < ws[None, :] + 16)
            dr = np.clip(kr[:, None] - r[None, :] + 7, 0, 14)
            dc = np.clip(kcol[:, None] - c[None, :] + 15, 0, 30)
            valid = vr & vc
            tiles.append((valid, np.where(valid, dr, 0), np.where(valid, dc, 0)))
        pats.append(tiles)
    classes = []
    cls_of_j = []
    for j in range(NT):
        key = b"".join(a.tobytes() for t in pats[j] for a in t)
        found = None
        for ci, (k2, _) in enumerate(classes):
            if k2 == key:
                found = ci
                break
        if found is None:
            classes.append((key, pats[j]))
            found = len(classes) - 1
        cls_of_j.append(found)
    return [c[1] for c in classes], cls_of_j


_NA_CLASSES, _NA_CLS_OF_J = _na_patterns()
NCLS = len(_NA_CLASSES)


def _na_bias_table(rpb):
    L = rpb.shape[0]
    out = np.empty((L, NH, 128, NCLS * 5, 128), np.float32)
    for ci, tiles in enumerate(_NA_CLASSES):
        for i, (valid, dr, dc) in enumerate(tiles):
            g = rpb[:, :, dr, dc]
            out[:, :, :, ci * 5 + i, :] = np.where(valid[None, None], g, np.float32(NEG))
    return out


def _rope_tables():
    t = np.arange(SEQ)
    pos = np.stack([t // 64, t % 64], -1).astype(np.float32)
    inv = (np.float32(10000.0) ** (-np.arange(16, dtype=np.float32) / np.float32(16))).astype(np.float32)
    ang = pos[:, :, None] * inv
    c = np.cos(ang).astype(np.float32)
    s = np.sin(ang).astype(np.float32)
    C64 = np.stack([c, c], 2).reshape(SEQ, 64)
    S64 = np.stack([-s, s], 2).reshape(SEQ, 64)
    C64 = np.ascontiguousarray(C64.reshape(NT, 128, 64).transpose(1, 0, 2))
    S64 = np.ascontiguousarray(S64.reshape(NT, 128, 64).transpose(1, 0, 2))
    return C64, S64


def build_program(n_layers=DEPTH, stop=None, lite=False, decl=None):
    nc = bass.Bass("TRN2", target_bir_lowering=False)
    n_moe = n_layers if stop is None else n_layers - 1
    LD = n_layers if lite else DEPTH
    LNA = max(1, (n_layers + 1) // 2) if lite else 2
    LGQ = max(1, n_layers // 2) if lite else 2
    LMOE = max(1, n_moe) if lite else DEPTH
    EMOE = NE if (n_moe > 0 or not lite) else 1

    def din(name, shape, dt=F32):
        if decl is not None:
            decl[name] = tuple(shape)
        return nc.dram_tensor(name, list(shape), dt, kind="ExternalInput").ap()

    x_d = din("x", [SEQ, D])
    c_d = din("cfm", [128, KC])
    adaw_d = din("ada_w", [LD, D, 6 * D])
    adab_d = din("ada_b", [LD, 6 * D])
    lng_d = din("ln_g", [LD, 2, D])
    lnb_d = din("ln_b", [LD, 2, D])
    nawqkv_d = din("na_w_qkv", [LNA, D, 3 * D])
    nawo_d = din("na_w_o", [LNA, D, D])
    nabias_d = din("na_bias", [LNA, NH, 128, NCLS * 5 * 128])
    gwqkv_d = din("gqa_w_qkv", [LGQ, D, 1536])
    gwo_d = din("gqa_w_o", [LGQ, D, D])
    gqn_d = din("gqa_q_norm", [LGQ, HD])
    gkn_d = din("gqa_k_norm", [LGQ, HD])
    ropec_d = din("rope_c", [128, NT, 64])
    ropes_d = din("rope_s", [128, NT, 64])
    wr_d = din("moe_wr", [LMOE, D, 36])
    br_d = din("moe_br", [LMOE, 36])
    wg_d = din("moe_w_gate", [LMOE, EMOE, D, 512])
    wu_d = din("moe_w_up", [LMOE, EMOE, D, 512])
    wd_d = din("moe_w_down", [LMOE, EMOE, 512, D])
    ident_d = din("ident", [128, 128])
    tri_d = din("tri", [128, 128])
    iotac_d = din("iotac", [128, NE])
    out_d = nc.dram_tensor("out", [SEQ, D], F32, kind="ExternalOutput").ap()
    xs_d = nc.dram_tensor("xs_scr", [NE * CAP + 2 * CAP, D], BF16, kind="Internal").ap()
    ys_d = nc.dram_tensor("ys_scr", [NE * CAP + 2 * CAP, D], BF16, kind="Internal").ap()
    modrow_d = nc.dram_tensor("modrow", [2, D], F32, kind="Internal").ap()

    S = Sched(nc)
    with nc.sbuf_tensor("arena", [128, ARENA_WORDS], F32) as arena_t, \
            nc.psum_tensor("ps", [128, 8, 512], F32) as ps:
        ar = Arena(arena_t, ARENA_WORDS)
        S.open()

        def psb(bank):
            return ps[:, bank, :].bitcast(BF16)

        X = ar.alloc([128, NT, D])
        ident = ar.alloc([128, 128])
        identb = ar.alloc([128, 128], BF16)
        onesb = ar.alloc([128, 128], BF16)
        trib = ar.alloc([128, 128], BF16)
        iotac = ar.alloc([128, NE])
        alphaI = ar.alloc([128, 128])
        cact = ar.alloc([128, KC])
        cA = ar.alloc([128, KC, 128], BF16)
        SC1 = ar.alloc([128, KC]); SH1 = ar.alloc([128, KC])
        SC2 = ar.alloc([128, KC]); SH2 = ar.alloc([128, KC])
        G1B = ar.alloc([128, D]); G2B = ar.alloc([128, D])
        LNG = ar.alloc([128, D]); LNB = ar.alloc([128, D])
        TMPV = [ar.alloc([128, D]) for _ in range(2)]
        st_t = [ar.alloc([128, 12]) for _ in range(4)]
        mv_t = [ar.alloc([128, 2]) for _ in range(4)]
        sd_t = [ar.alloc([128, 2]) for _ in range(4)]
        eps_ln = ar.alloc([128, 1]); eps_rms = ar.alloc([128, 1])
        G1 = ar.alloc([128, NT]); G2 = ar.alloc([128, NT])
        D1i = ar.alloc([128, NT], I32); D2i = ar.alloc([128, NT], I32)
        A0 = ar.mark()
        A = ar.alloc([128, KC, SEQ], BF16)
        B = ar.alloc([128, NT, D], BF16)
        R0 = ar.mark()

        S.dma("sp", lambda e: e.dma_start(out=ident, in_=ident_d[:, :]), writes=["ident"])
        S.dma("pool", lambda e: e.dma_start(out=identb, in_=ident_d[:, :]), writes=["identb"])
        S.dma("pool", lambda e: e.dma_start(out=trib, in_=tri_d[:, :]), writes=["trib"])
        S.dma("sp", lambda e: e.dma_start(out=iotac, in_=iotac_d[:, :]), writes=["iotac"])
        S.dma("sp", lambda e: e.dma_start(out=cact, in_=c_d[:, :]), writes=["cact"])
        S.op("dve", lambda e: e.memset(onesb, 1.0), writes=["onesb"])
        S.op("dve", lambda e: e.tensor_scalar(out=alphaI, in0=ident, scalar1=ALPHA, scalar2=None, op0=ALU.mult),
             reads=["ident"], writes=["alphaI"])
        S.op("dve", lambda e: e.memset(eps_ln, LN_EPS), writes=["eps"])
        S.op("dve", lambda e: e.memset(eps_rms, RMS_EPS), writes=["eps"])
        for g4 in range(4):
            S.dma("sp", lambda e, g4=g4: e.dma_start(
                out=X[:, g4 * 4:(g4 + 1) * 4, :],
                in_=x_d[g4 * 512:(g4 + 1) * 512, :].rearrange("(t p) d -> p t d", p=128)),
                writes=[("X", t) for t in range(g4 * 4, g4 * 4 + 4)])
        S.op("act", lambda e: e.activation(out=cact, in_=cact, func=AF.Silu), reads=["cact"], writes=["cact"])
        S.op("dve", lambda e: e.tensor_copy(out=cA, in_=cact.unsqueeze(2).to_broadcast([128, KC, 128])),
             reads=["cact"], writes=["cA"])

        def mod_groups(i, base, mm_banks, tr_banks):
            arm = Arena(arena_t, ARENA_WORDS)
            arm.off = base
            AW = [arm.alloc([128, KC, 512], BF16) for _ in range(2)]
            ABb = [arm.alloc([128, 512]) for _ in range(2)]
            T = [arm.alloc([128, 512]) for _ in range(2)]
            aw_v = adaw_d[i].rearrange("(k p) n -> p k n", p=128)

            def group(cg):
                b = cg % 2
                S.dma("pool", lambda e: e.dma_start(out=AW[b], in_=aw_v[:, :, cg * 512:(cg + 1) * 512]),
                      writes=[("AW", b)])
                S.dma("sp", lambda e: e.dma_start(
                    out=ABb[b], in_=adab_d[i:i + 1, cg * 512:(cg + 1) * 512].partition_broadcast(128)),
                    writes=[("ABb", b)])
                bank = mm_banks[cg % len(mm_banks)]
                for kc in range(KC):
                    S.op("pe", lambda e, kc=kc: e.matmul(
                        ps[:, bank, :], lhsT=cA[:, kc, :], rhs=AW[b][:, kc, :], start=(kc == 0), stop=(kc == KC - 1)),
                        reads=["cA", ("AW", b)], writes=[("ps", bank)])
                kind = cg // 2
                half = cg % 2
                if kind in (2, 5):
                    GB = G1B if kind == 2 else G2B
                    S.op("dve", lambda e: e.scalar_tensor_tensor(
                        out=GB[:, half * 512:(half + 1) * 512], in0=ps[:, bank, :], scalar=1.0, in1=ABb[b],
                        op0=ALU.add, op1=ALU.add),
                        reads=[("ps", bank), ("ABb", b)], writes=[("GB", kind)])
                else:
                    add1 = 1.0 if kind in (1, 4) else 0.0
                    S.op("dve", lambda e: e.scalar_tensor_tensor(
                        out=T[b], in0=ps[:, bank, :], scalar=add1, in1=ABb[b], op0=ALU.add, op1=ALU.add),
                        reads=[("ps", bank), ("ABb", b)], writes=[("T", b)])
                    tb = tr_banks[b % len(tr_banks)]
                    for q in range(4):
                        S.op("pe", lambda e, q=q: e.transpose(
                            ps[:, tb, q * 128:(q + 1) * 128], T[b][:, q * 128:(q + 1) * 128], ident),
                            reads=[("T", b), "ident"], writes=[("ps", tb)])
                    if kind in (3, 4):
                        S.dma("sp", lambda e: e.dma_start(
                            out=modrow_d[kind - 3:kind - 2, half * 512:(half + 1) * 512], in_=T[b][0:1, :]),
                            reads=[("T", b)], writes=[("modrow", kind, half)])
                    dst = {0: SH1, 1: SC1, 3: SH2, 4: SC2}[kind]
                    S.op("dve", lambda e: e.tensor_copy(
                        out=dst[:, half * 4:(half + 1) * 4],
                        in_=ps[:, tb, :].rearrange("p (q n) -> p q n", q=4)[:, :, 0]),
                        reads=[("ps", tb)], writes=[("modfm", kind)])
            return group

        def mod_phase(i):
            g = mod_groups(i, R0, (0, 1), (2, 3))
            for cg in range(12):
                g(cg)

        def hT_tile(tt, banks, SC, SH, kind_sc, kind_sh):
            for half in range(2):
                bank = banks[half % len(banks)]
                for q in range(4):
                    kc = half * 4 + q
                    S.op("pe", lambda e, q=q, kc=kc: e.transpose(
                        ps[:, bank, q * 128:(q + 1) * 128], X[:, tt, kc * 128:(kc + 1) * 128], ident),
                        reads=[("X", tt), "ident"], writes=[("ps", bank)])
                for q in range(4):
                    kc = half * 4 + q
                    S.op("act", lambda e, q=q, kc=kc: e.activation(
                        out=A[:, kc, tt * 128:(tt + 1) * 128], in_=ps[:, bank, q * 128:(q + 1) * 128],
                        func=AF.Identity, bias=SH[:, kc:kc + 1], scale=SC[:, kc:kc + 1]),
                        reads=[("ps", bank), ("modfm", kind_sc), ("modfm", kind_sh)], writes=[("A", tt)])

        def build_hT(SC, SH, kind_sc, kind_sh):
            for tt in range(NT):
                hT_tile(tt, [(2 * tt) % 4, (2 * tt + 1) % 4], SC, SH, kind_sc, kind_sh)

        def ln_stats(tt, v_ps, vkeys, vbufs):
            nb_ = len(vbufs)
            tb = tt % nb_
            v = vbufs[tb]
            sb_ = tt % 4
            for half in range(2):
                S.op("dve", lambda e, half=half: e.bn_stats(
                    out=st_t[sb_][:, half * 6:(half + 1) * 6], in_=v_ps[:, half * 512:(half + 1) * 512]),
                    reads=[vkeys[half]], writes=[("st", sb_)])
            S.op("dve", lambda e: e.bn_aggr(out=mv_t[sb_], in_=st_t[sb_]), reads=[("st", sb_)], writes=[("mv", sb_)])
            S.op("act", lambda e: e.activation(out=sd_t[sb_][:, 0:1], in_=mv_t[sb_][:, 1:2], func=AF.Sqrt, bias=eps_ln, scale=1.0),
                 reads=[("mv", sb_), "eps"], writes=[("sd", sb_)])
            S.op("dve", lambda e: e.reciprocal(out=sd_t[sb_][:, 0:1], in_=sd_t[sb_][:, 0:1]), reads=[("sd", sb_)], writes=[("sd", sb_)])
            S.op("dve", lambda e: e.scalar_tensor_tensor(
                out=sd_t[sb_][:, 1:2], in0=mv_t[sb_][:, 0:1], scalar=-1.0, in1=sd_t[sb_][:, 0:1], op0=ALU.mult, op1=ALU.mult),
                reads=[("mv", sb_), ("sd", sb_)], writes=[("sd2", sb_)])
            S.op("act", lambda e: e.activation(out=v, in_=v_ps, func=AF.Identity, bias=sd_t[sb_][:, 1:2], scale=sd_t[sb_][:, 0:1]),
                 reads=list(vkeys) + [("sd", sb_), ("sd2", sb_)], writes=[("v", tb)])

        def ln_finish(tt, store_out, vbufs):
            tb = tt % len(vbufs)
            v = vbufs[tb]
            S.op("dve", lambda e: e.tensor_tensor(out=v, in0=v, in1=LNG, op=ALU.mult),
                 reads=[("v", tb), "LNG"], writes=[("v", tb)])
            S.op("dve", lambda e: e.tensor_tensor(out=X[:, tt, :], in0=v, in1=LNB, op=ALU.add),
                 reads=[("v", tb), "LNB"], writes=[("X", tt)])
            if store_out:
                S.dma("sp", lambda e: e.dma_start(out=out_d[tt * 128:(tt + 1) * 128, :], in_=X[:, tt, :]),
                      reads=[("X", tt)], writes=[("out", tt)])

        def load_ln_params(i, which):
            S.dma("sp", lambda e: e.dma_start(out=LNG, in_=lng_d[i, which:which + 1, :].partition_broadcast(128)),
                  writes=["LNG"])
            S.dma("sp", lambda e: e.dma_start(out=LNB, in_=lnb_d[i, which:which + 1, :].partition_broadcast(128)),
                  writes=["LNB"])

        def na_phase(l):
            ar.reset(R0)
            Wb = [[ar.alloc([128, KC, 128], BF16) for _ in range(3)] for _ in range(2)]
            BI = [ar.alloc([128, NCLS * 5, 128], BF16) for _ in range(3)]
            QT = ar.alloc([128, SEQ], BF16)
            KT = ar.alloc([128, SEQ], BF16)
            V = ar.alloc([128, NT, 2, 66], BF16)
            PT = [ar.alloc([128, 640], BF16) for _ in range(3)]
            rc = [ar.alloc([128, 1]) for _ in range(2)]
            w_v = nawqkv_d[l].rearrange("(k p) n -> p k n", p=128)
            S.op("dve", lambda e: e.memset(V[:, :, :, 64:66], 1.0), writes=["V"])

            def load_w(p):
                wb = p % 2
                for m in range(3):
                    c0 = m * D + p * 128
                    S.dma("pool", lambda e, wb=wb, m=m, c0=c0: e.dma_start(out=Wb[wb][m], in_=w_v[:, :, c0:c0 + 128]),
                          writes=[("W", wb, m)])

            def load_bias(h):
                nchunk = NCLS * 5 // 5
                S.dma("pool", lambda e, h=h: e.dma_start(
                    out=BI[h % 3].rearrange("p (a b) n -> p a (b n)", a=nchunk),
                    in_=nabias_d[l, h].rearrange("p (a m) -> p a m", a=nchunk)), writes=[("BI", h % 3)])

            load_w(0)
            load_bias(0)
            load_bias(1)

            def exp_bias(h):
                S.op("act", lambda e: e.activation(
                    out=BI[h % 3].rearrange("p a n -> p (a n)"), in_=BI[h % 3].rearrange("p a n -> p (a n)"), func=AF.Exp),
                    reads=[("BI", h % 3)], writes=[("BI", h % 3)])

            def emit_ST(n, h, j):
                hh = h % 2
                r0 = hh * 64
                kt_lo = min(max(j - 2, 0), 11)
                sb = (n % 3) * 2
                for i in range(5):
                    bank = sb + i // 4
                    col = (i % 4) * 128
                    S.op("pe", lambda e, bank=bank, col=col, i=i: e.matmul(
                        ps[:, bank, col:col + 128], lhsT=KT[r0:r0 + 64, (kt_lo + i) * 128:(kt_lo + i + 1) * 128],
                        rhs=QT[r0:r0 + 64, j * 128:(j + 1) * 128], start=True, stop=True),
                        reads=["KT", "QT"], writes=[("ps", bank)])

            def emit_B(n, h, j):
                c = _NA_CLS_OF_J[j]
                sb = (n % 3) * 2
                pb = n % 3
                S.op("act", lambda e: e.activation(out=PT[pb][:, 0:512], in_=ps[:, sb, :], func=AF.Exp),
                     reads=[("ps", sb)], writes=[("PT", pb)])
                S.op("act", lambda e: e.activation(out=PT[pb][:, 512:640], in_=ps[:, sb + 1, 0:128], func=AF.Exp),
                     reads=[("ps", sb + 1)], writes=[("PT", pb)])
                S.op("dve", lambda e: e.tensor_tensor(
                    out=PT[pb], in0=PT[pb], in1=BI[h % 3][:, c * 5:(c + 1) * 5, :].rearrange("p a n -> p (a n)"),
                    op=ALU.mult), reads=[("PT", pb), ("BI", h % 3)], writes=[("PT", pb)])

            def emit_C(n, h, j):
                hh = h % 2
                kt_lo = min(max(j - 2, 0), 11)
                pb = n % 3
                ob = 6 + (n % 2)
                for i in range(5):
                    S.op("pe", lambda e, i=i: e.matmul(
                        ps[:, ob, 0:65], lhsT=PT[pb][:, i * 128:(i + 1) * 128], rhs=V[:, kt_lo + i, hh, 0:65],
                        start=(i == 0), stop=(i == 4)),
                        reads=[("PT", pb), "V"], writes=[("ps", ob)])
                rb_ = n % 2
                S.op("dve", lambda e: e.reciprocal(out=rc[rb_], in_=ps[:, ob, 64:65]),
                     reads=[("ps", ob)], writes=[("rc", rb_)])
                S.op("dve", lambda e: e.tensor_scalar(
                    out=B[:, j, h * 64:(h + 1) * 64], in0=ps[:, ob, 0:64], scalar1=rc[rb_], scalar2=None, op0=ALU.mult),
                    reads=[("ps", ob), ("rc", rb_)], writes=[("B", j)])

            exp_bias(0)
            exp_bias(1)
            for p in range(8):
                wb = p % 2
                if p + 1 < 8:
                    load_w(p + 1)
                for m in range(2):
                    for tg in range(4):
                        bank = 4 + (tg % 2)
                        for kc in range(KC):
                            S.op("pe", lambda e, m=m, tg=tg, kc=kc, bank=bank: e.matmul(
                                ps[:, bank, :], lhsT=Wb[wb][m][:, kc, :], rhs=A[:, kc, tg * 512:(tg + 1) * 512],
                                start=(kc == 0), stop=(kc == KC - 1)),
                                reads=[("W", wb, m)] + [("A", t) for t in range(tg * 4, tg * 4 + 4)],
                                writes=[("ps", bank)])
                        if m == 0:
                            S.op("act", lambda e, tg=tg, bank=bank: e.activation(
                                out=QT[:, tg * 512:(tg + 1) * 512], in_=ps[:, bank, :], func=AF.Copy, scale=0.125),
                                reads=[("ps", bank)], writes=["QT"])
                        else:
                            S.op("dve", lambda e, tg=tg, bank=bank: e.tensor_copy(
                                out=KT[:, tg * 512:(tg + 1) * 512], in_=ps[:, bank, :]),
                                reads=[("ps", bank)], writes=["KT"])
                for tq in range(4):
                    bank = 6 + (tq % 2)
                    for t4 in range(4):
                        tt = tq * 4 + t4
                        for kc in range(KC):
                            S.op("pe", lambda e, tt=tt, t4=t4, kc=kc, bank=bank: e.matmul(
                                ps[:, bank, t4 * 128:(t4 + 1) * 128], lhsT=A[:, kc, tt * 128:(tt + 1) * 128],
                                rhs=Wb[wb][2][:, kc, :], start=(kc == 0), stop=(kc == KC - 1)),
                                reads=[("W", wb, 2), ("A", tt)], writes=[("ps", bank)])
                    S.op("dve", lambda e, tq=tq, bank=bank: e.tensor_copy(
                        out=V[:, tq * 4:(tq + 1) * 4, :, 0:64],
                        in_=ps[:, bank, :].rearrange("p (t h d) -> p t h d", t=4, h=2)),
                        reads=[("ps", bank)], writes=["V"])
                steps = [(2 * p + hh, j) for hh in range(2) for j in range(NT)]
                emit_ST(0, *steps[0])
                emit_ST(1, *steps[1])
                emit_B(0, *steps[0])
                for n, (h, j) in enumerate(steps):
                    if j == 0 and h + 2 < NH:
                        load_bias(h + 2)
                    if j == 8 and h + 2 < NH:
                        exp_bias(h + 2)
                    if n + 2 < len(steps):
                        emit_ST(n + 2, *steps[n + 2])
                    if n + 1 < len(steps):
                        emit_B(n + 1, *steps[n + 1])
                    emit_C(n, h, j)

        def gqa_phase(l):
            ar.reset(R0)
            KTd = ar.alloc([128, 4, SEQ], BF16)
            V = ar.alloc([128, NT, 4, 66], BF16)
            QN = ar.alloc([128, HD]); KN = ar.alloc([128, HD])
            ssq = ar.alloc([128, 20])
            m1 = ar.mark()
            ar_b = Arena(arena_t, ARENA_WORDS)
            ar_b.off = A0 + KC * SEQ // 2
            Wg = [ar_b.alloc([128, KC, 512], BF16) for _ in range(3)]
            RC = ar_b.alloc([128, NT, 64]); RS = ar_b.alloc([128, NT, 64])
            assert ar_b.off <= R0
            SQ = ar.alloc([128, 1280]); QF = ar.alloc([128, 1280]); T1 = ar.alloc([128, 1280])
            QB = ar.alloc([128, 1024], BF16); KD = ar.alloc([128, 4, 2, 64], BF16)
            w_v = gwqkv_d[l].rearrange("(k p) n -> p k n", p=128)
            for cg in range(3):
                S.dma("pool", lambda e, cg=cg: e.dma_start(out=Wg[cg], in_=w_v[:, :, cg * 512:(cg + 1) * 512]),
                      writes=[("Wg", cg)])
            S.dma("sp", lambda e: e.dma_start(out=RC, in_=ropec_d[:, :, :]), writes=["RC"])
            S.dma("sp", lambda e: e.dma_start(out=RS, in_=ropes_d[:, :, :]), writes=["RS"])
            S.dma("sp", lambda e: e.dma_start(out=QN, in_=gqn_d[l:l + 1, :].partition_broadcast(128)), writes=["QN"])
            S.dma("sp", lambda e: e.dma_start(out=KN, in_=gkn_d[l:l + 1, :].partition_broadcast(128)), writes=["KN"])
            S.op("dve", lambda e: e.memset(V[:, :, :, 64:66], 1.0), writes=["V"])

            def hd(ap, nh):
                return ap.rearrange("p (h d) -> p h d", h=nh)

            for tt in range(NT):
                b0 = (tt % 2) * 3
                for cg in range(3):
                    bank = b0 + cg
                    for kc in range(KC):
                        S.op("pe", lambda e, cg=cg, kc=kc, bank=bank, tt=tt: e.matmul(
                            ps[:, bank, :], lhsT=A[:, kc, tt * 128:(tt + 1) * 128], rhs=Wg[cg][:, kc, :],
                            start=(kc == 0), stop=(kc == KC - 1)),
                            reads=[("A", tt), ("Wg", cg)], writes=[("ps", bank)])
                parts = [(ps[:, b0, :], 0, 8, ("ps", b0)), (ps[:, b0 + 1, :], 512, 8, ("ps", b0 + 1)),
                         (ps[:, b0 + 2, 0:256], 1024, 4, ("ps", b0 + 2))]
                for (pap, co, nh, pk) in parts:
                    S.op("act", lambda e, pap=pap, co=co, nh=nh: e.activation(
                        out=SQ[:, co:co + nh * 64], in_=pap, func=AF.Square),
                        reads=[pk], writes=["SQ"])
                S.op("act", lambda e, tt=tt, b0=b0: e.activation(
                    out=V[:, tt, :, 0:64], in_=hd(ps[:, b0 + 2, 256:512], 4), func=AF.Copy),
                    reads=[("ps", b0 + 2)], writes=["V"])
                S.op("dve", lambda e: e.tensor_reduce(out=ssq, in_=hd(SQ, 20), axis=AX.X, op=ALU.add),
                     reads=["SQ"], writes=["ssq"])
                S.op("act", lambda e: e.activation(out=ssq, in_=ssq, func=AF.Sqrt, bias=eps_rms, scale=1.0 / 64),
                     reads=["ssq", "eps"], writes=["ssq"])
                S.op("dve", lambda e: e.reciprocal(out=ssq, in_=ssq), reads=["ssq"], writes=["ssq"])
                h0 = 0
                for (pap, co, nh, pk) in parts:
                    S.op("dve", lambda e, pap=pap, co=co, nh=nh, h0=h0: e.tensor_tensor(
                        out=hd(QF[:, co:co + nh * 64], nh), in0=hd(pap, nh),
                        in1=ssq[:, h0:h0 + nh].unsqueeze(2).to_broadcast([128, nh, 64]), op=ALU.mult),
                        reads=[pk, "ssq"], writes=["QF"])
                    h0 += nh
                S.op("dve", lambda e: e.tensor_tensor(
                    out=hd(QF[:, 0:1024], 16), in0=hd(QF[:, 0:1024], 16),
                    in1=QN.unsqueeze(1).to_broadcast([128, 16, 64]), op=ALU.mult),
                    reads=["QF", "QN"], writes=["QF"])
                S.op("dve", lambda e: e.tensor_tensor(
                    out=hd(QF[:, 1024:1280], 4), in0=hd(QF[:, 1024:1280], 4),
                    in1=KN.unsqueeze(1).to_broadcast([128, 4, 64]), op=ALU.mult),
                    reads=["QF", "KN"], writes=["QF"])
                S.op("dve", lambda e, tt=tt: e.tensor_tensor(
                    out=hd(T1, 20), in0=hd(QF, 20), in1=RC[:, tt, :].unsqueeze(1).to_broadcast([128, 20, 64]),
                    op=ALU.mult), reads=["QF", "RC"], writes=["T1"])

                def v5(ap):
                    return ap.rearrange("p (h a f q) -> p h a f q", h=20, a=2, f=2)

                for f in range(2):
                    S.op("dve", lambda e, tt=tt, f=f: e.tensor_tensor(
                        out=v5(SQ)[:, :, :, f, :], in0=v5(QF)[:, :, :, 1 - f, :],
                        in1=RS[:, tt, :].rearrange("p (a f q) -> p a f q", a=2, f=2)[:, :, f, :]
                        .unsqueeze(1).to_broadcast([128, 20, 2, 16]), op=ALU.mult),
                        reads=["QF", "RS", "ssq"], writes=["SQ"])
                S.op("dve", lambda e: e.tensor_tensor(out=QB, in0=T1[:, 0:1024], in1=SQ[:, 0:1024], op=ALU.add),
                     reads=["T1", "SQ"], writes=["QB"])
                for dup in range(2):
                    S.op("dve", lambda e, dup=dup: e.tensor_tensor(
                        out=KD[:, :, dup, :], in0=hd(T1[:, 1024:1280], 4), in1=hd(SQ[:, 1024:1280], 4), op=ALU.add),
                        reads=["T1", "SQ"], writes=["KD"])
                for pr in range(8):
                    S.op("pe", lambda e, pr=pr: e.transpose(
                        psb(6)[:, pr * 128:(pr + 1) * 128], QB[:, pr * 128:(pr + 1) * 128], identb),
                        reads=["QB", "identb"], writes=[("ps", 6)])
                for g in range(4):
                    S.op("pe", lambda e, g=g: e.transpose(
                        psb(7)[:, g * 128:(g + 1) * 128], KD[:, g, :, :].rearrange("p a d -> p (a d)"), identb),
                        reads=["KD", "identb"], writes=[("ps", 7)])
                S.op("act", lambda e, tt=tt: e.activation(
                    out=A[:, :, tt * 128:(tt + 1) * 128], in_=psb(6).rearrange("p (k n) -> p k n", k=8), func=AF.Copy),
                    reads=[("ps", 6)], writes=[("A", tt)])
                S.op("dve", lambda e, tt=tt: e.tensor_copy(
                    out=KTd[:, :, tt * 128:(tt + 1) * 128], in_=psb(7)[:, 0:512].rearrange("p (g n) -> p g n", g=4)),
                    reads=[("ps", 7)], writes=["KTd"])

            S.barrier()
            ar.reset(m1)
            PT = [ar.alloc([128, 512], BF16) for _ in range(4)]
            rc = [ar.alloc([128, 4]) for _ in range(2)]
            KT2 = ar.alloc([128, 4, SEQ], BF16)
            S.op("act", lambda e: e.activation(out=KT2[64:128, 0:2, :], in_=KTd[64:128, 0:2, :], func=AF.Copy),
                 reads=["KTd"], writes=["KT2"])
            S.op("pool", lambda e: e.tensor_copy(out=KT2[64:128, 2:4, :], in_=KTd[64:128, 2:4, :]),
                 reads=["KTd"], writes=["KT2b"])
            S.op("dve", lambda e: e.memset(KT2[0:64, :, :], 0.0), writes=["KT2c"])
            S.op("dve", lambda e: e.memset(KTd[64:128, :, :], 0.0), reads=["KT2", "KT2b"], writes=["KTd"])
            steps = [(h, qg, kt) for h in range(NH) for qg in range(4) for kt in range(NT)]

            def emit_ST(n):
                h, qg, kt = steps[n]
                g = h // 4
                r0 = (h % 2) * 64
                bank = n % 4
                KK = KTd if h % 2 == 0 else KT2
                S.op("pe", lambda e: e.matmul(
                    ps[:, bank, :], lhsT=KK[:, g, kt * 128:(kt + 1) * 128],
                    rhs=A[:, h // 2, qg * 512:(qg + 1) * 512], start=True, stop=True),
                    reads=["KTd", "KT2", "KT2b", "KT2c"] + [("A", t) for t in range(qg * 4, qg * 4 + 4)], writes=[("ps", bank)])

            def emit_rest(n):
                h, qg, kt = steps[n]
                g = h // 4
                bank = n % 4
                pt = PT[n % 4]
                grp = n // NT
                ob = 4 + (grp % 2)
                S.op("act", lambda e: e.activation(out=pt, in_=ps[:, bank, :], func=AF.Exp, scale=0.125),
                     reads=[("ps", bank)], writes=[("PT", n % 4)])
                for qt in range(4):
                    S.op("pe", lambda e, qt=qt: e.matmul(
                        ps[:, ob, qt * 128:qt * 128 + 65], lhsT=pt[:, qt * 128:(qt + 1) * 128],
                        rhs=V[:, kt, g, 0:65], start=(kt == 0), stop=(kt == NT - 1)),
                        reads=[("PT", n % 4), "V"], writes=[("ps", ob)])
                if kt == NT - 1:
                    rb = grp % 2
                    S.op("dve", lambda e: e.reciprocal(
                        out=rc[rb], in_=ps[:, ob, :].rearrange("p (q n) -> p q n", q=4)[:, :, 64]),
                        reads=[("ps", ob)], writes=[("rc", rb)])
                    for qt in range(4):
                        tt = qg * 4 + qt
                        S.op("dve", lambda e, qt=qt, tt=tt: e.tensor_scalar(
                            out=B[:, tt, h * 64:(h + 1) * 64], in0=ps[:, ob, qt * 128:qt * 128 + 64],
                            scalar1=rc[rb][:, qt:qt + 1], scalar2=None, op0=ALU.mult),
                            reads=[("ps", ob), ("rc", rb)], writes=[("B", tt)])

            emit_ST(0)
            emit_ST(1)
            for n in range(len(steps)):
                if n + 2 < len(steps):
                    emit_ST(n + 2)
                emit_rest(n)

        def wo_ln_phase(i, wo_ap):
            ar.reset(R0)
            WO = ar.alloc([128, KC, D], BF16)
            VB = TMPV + [ar.alloc([128, D]) for _ in range(2)]
            wo_v = wo_ap.rearrange("(k p) n -> p k n", p=128)
            for half in range(2):
                S.dma("pool", lambda e, half=half: e.dma_start(
                    out=WO[:, :, half * 512:(half + 1) * 512], in_=wo_v[:, :, half * 512:(half + 1) * 512]),
                    writes=[("WO", half)])
            load_ln_params(i, 0)
            for half in range(2):
                S.op("dve", lambda e, half=half: e.tensor_tensor(
                    out=WO[:, :, half * 512:(half + 1) * 512], in0=WO[:, :, half * 512:(half + 1) * 512],
                    in1=G1B[:, half * 512:(half + 1) * 512].unsqueeze(1).to_broadcast([128, KC, 512]), op=ALU.mult),
                    reads=[("WO", half), ("GB", 2)], writes=[("WO", half)])
            for tt in range(NT):
                bank = tt % 2
                for kc in range(KC):
                    S.op("pe", lambda e, tt=tt, kc=kc, bank=bank: e.transpose(
                        psb(bank)[:, kc * 128:(kc + 1) * 128], B[:, tt, kc * 128:(kc + 1) * 128], identb),
                        reads=[("B", tt), "identb"], writes=[("ps", bank)])
                eng = "act" if tt % 2 == 0 else "dve"
                if eng == "act":
                    S.op("act", lambda e, tt=tt, bank=bank: e.activation(
                        out=A[:, :, tt * 128:(tt + 1) * 128], in_=psb(bank).rearrange("p (k n) -> p k n", k=8),
                        func=AF.Copy), reads=[("ps", bank)], writes=[("A", tt)])
                else:
                    S.op("dve", lambda e, tt=tt, bank=bank: e.tensor_copy(
                        out=A[:, :, tt * 128:(tt + 1) * 128], in_=psb(bank).rearrange("p (k n) -> p k n", k=8)),
                        reads=[("ps", bank)], writes=[("A", tt)])
            def wo_mm(tt):
                banks = [2 + 2 * (tt % 3), 3 + 2 * (tt % 3)]
                for half in range(2):
                    S.op("pe", lambda e, half=half, bank=banks[half]: e.matmul(
                        ps[:, bank, :], lhsT=alphaI, rhs=X[:, tt, half * 512:(half + 1) * 512], start=True, stop=False),
                        reads=["alphaI", ("X", tt)], writes=[("ps", banks[half])])
                    for kc in range(KC):
                        S.op("pe", lambda e, kc=kc, half=half, bank=banks[half]: e.matmul(
                            ps[:, bank, :], lhsT=A[:, kc, tt * 128:(tt + 1) * 128],
                            rhs=WO[:, kc, half * 512:(half + 1) * 512], start=False, stop=(kc == KC - 1)),
                            reads=[("A", tt), ("WO", half)], writes=[("ps", banks[half])])

            def wo_stats(tt):
                banks = [2 + 2 * (tt % 3), 3 + 2 * (tt % 3)]
                ln_stats(tt, ps[:, banks[0]:banks[0] + 2, :].rearrange("p a n -> p (a n)"),
                         [("ps", banks[0]), ("ps", banks[1])], vbufs=VB)

            wo_mm(0)
            wo_mm(1)
            wo_stats(0)
            for tt in range(NT):
                if tt + 2 < NT:
                    wo_mm(tt + 2)
                if tt + 1 < NT:
                    wo_stats(tt + 1)
                ln_finish(tt, store_out=False, vbufs=VB)

        def moe_phase(i, store_out, next_layer=None):
            ar.reset(A0)
            NWB = 3
            WE = [[ar.alloc([128, KC, 512], BF16), ar.alloc([128, KC, 512], BF16), ar.alloc([128, 4, D], BF16)]
                  for _ in range(NWB)]
            m0 = ar.mark()

            def load_expert(e_, which=(0, 1, 2)):
                wb = e_ % NWB
                srcs = (wg_d, wu_d, wd_d)
                for wi in which:
                    S.dma("pool", lambda e, wi=wi: e.dma_start(
                        out=WE[wb][wi], in_=srcs[wi][i, e_].rearrange("(k p) n -> p k n", p=128)),
                        writes=[("WE", wb, wi)])

            load_ln_params(i, 1)
            WR = ar.alloc([128, KC, 36]); RB = ar.alloc([128, 36])
            HT32 = [ar.alloc([128, KC, 128]) for _ in range(2)]
            L = ar.alloc([128, NT, 36])
            gmax = ar.alloc([128, NT]); gsum = ar.alloc([128, NT]); m1_ = ar.alloc([128, NT]); m2_ = ar.alloc([128, NT])
            dd = ar.alloc([128, NT]); e21 = ar.alloc([128, NT])
            gsel = ar.alloc([128, NT, 4]); gex = ar.alloc([128, NT, 4])
            lem = ar.alloc([128, NT, NE]); lem2 = ar.alloc([128, NT, NE])
            OH1 = ar.alloc([128, NT, NE]); OH2 = ar.alloc([128, NT, NE])
            Mall = ar.alloc([128, NT, NE], BF16)
            PEf = ar.alloc([128, NT, NE]); PR = ar.alloc([128, NT, NE])
            D1f = ar.alloc([128, NT]); D2f = ar.alloc([128, NT])
            HB = [ar.alloc([128, D], BF16) for _ in range(4)]
            HF = [ar.alloc([128, D]) for _ in range(2)]
            SHB, SCB = TMPV[0], TMPV[1]
            S.dma("sp", lambda e: e.dma_start(out=SHB, in_=modrow_d[0:1, :].partition_broadcast(128)),
                  reads=[("modrow", 3, 0), ("modrow", 3, 1)], writes=["SHB"])
            S.dma("sp", lambda e: e.dma_start(out=SCB, in_=modrow_d[1:2, :].partition_broadcast(128)),
                  reads=[("modrow", 4, 0), ("modrow", 4, 1)], writes=["SCB"])
            S.dma("sp", lambda e: e.dma_start(out=WR, in_=wr_d[i].rearrange("(k p) n -> p k n", p=128)), writes=["WR"])
            S.dma("sp", lambda e: e.dma_start(out=RB, in_=br_d[i:i + 1, :].partition_broadcast(128)), writes=["RB"])
            load_expert(0)
            load_expert(1)
            load_expert(2)
            def r_T(tt):
                hb = tt % 2
                for half in range(2):
                    bank = (2 * tt + half) % 4
                    for q in range(4):
                        kc = half * 4 + q
                        S.op("pe", lambda e, q=q, kc=kc, bank=bank: e.transpose(
                            ps[:, bank, q * 128:(q + 1) * 128], X[:, tt, kc * 128:(kc + 1) * 128], ident),
                            reads=[("X", tt), "ident"], writes=[("ps", bank)])
                    for q in range(4):
                        kc = half * 4 + q
                        S.op("act", lambda e, q=q, kc=kc, bank=bank: e.activation(
                            out=HT32[hb][:, kc, :], in_=ps[:, bank, q * 128:(q + 1) * 128],
                            func=AF.Identity, bias=SH2[:, kc:kc + 1], scale=SC2[:, kc:kc + 1]),
                            reads=[("ps", bank), ("modfm", 3), ("modfm", 4)], writes=[("HT32", hb)])

            def r_M(tt):
                hb = tt % 2
                rbank = 4 + tt // 8
                c0 = (tt % 8) * 36
                for kc in range(KC):
                    S.op("pe", lambda e, kc=kc: e.matmul(
                        ps[:, rbank, c0:c0 + 36], lhsT=HT32[hb][:, kc, :], rhs=WR[:, kc, :],
                        start=(kc == 0), stop=(kc == KC - 1)),
                        reads=[("HT32", hb), "WR"], writes=[("ps", rbank)])

            r_T(0)
            for tt in range(NT):
                if tt + 1 < NT:
                    r_T(tt + 1)
                r_M(tt)
            k_ = "rt"
            for hf in range(2):
                S.op("dve", lambda e, hf=hf: e.tensor_tensor(
                    out=L[:, hf * 8:(hf + 1) * 8, :], in0=ps[:, 4 + hf, 0:288].rearrange("p (t x) -> p t x", t=8),
                    in1=RB.unsqueeze(1).to_broadcast([128, 8, 36]), op=ALU.add),
                    reads=[("ps", 4 + hf), "RB"], writes=[k_])
            LG = L[:, :, 0:4]
            LE = L[:, :, 4:36]
            S.op("dve", lambda e: e.tensor_reduce(out=gmax, in_=LG, axis=AX.X, op=ALU.max), reads=[k_], writes=[k_])
            S.op("dve", lambda e: e.tensor_tensor(out=gsel, in0=LG, in1=gmax.unsqueeze(2).to_broadcast([128, NT, 4]),
                                                  op=ALU.is_equal), reads=[k_], writes=[k_])
            S.op("dve", lambda e: e.tensor_tensor(out=gex, in0=LG, in1=gmax.unsqueeze(2).to_broadcast([128, NT, 4]),
                                                  op=ALU.subtract), reads=[k_], writes=[k_])
            S.op("act", lambda e: e.activation(out=gex, in_=gex, func=AF.Exp), reads=[k_], writes=[k_])
            S.op("dve", lambda e: e.tensor_reduce(out=gsum, in_=gex, axis=AX.X, op=ALU.add), reads=[k_], writes=[k_])
            S.op("dve", lambda e: e.reciprocal(out=gsum, in_=gsum), reads=[k_], writes=[k_])
            S.op("dve", lambda e: e.tensor_scalar(out=gsel, in0=gsel, scalar1=BIG, scalar2=-BIG, op0=ALU.mult, op1=ALU.add),
                 reads=[k_], writes=[k_])
            for hf in range(2):
                S.op("dve", lambda e, hf=hf: e.tensor_tensor(
                    out=lem[:, hf * 8:(hf + 1) * 8, :].rearrange("p t (g x) -> p (t g) x", g=4),
                    in0=LE[:, hf * 8:(hf + 1) * 8, :].rearrange("p t (g x) -> p t g x", g=4),
                    in1=gsel[:, hf * 8:(hf + 1) * 8, :].unsqueeze(3).to_broadcast([128, 8, 4, 8]), op=ALU.add),
                    reads=[k_], writes=[k_])
            S.op("dve", lambda e: e.tensor_reduce(out=m1_, in_=lem, axis=AX.X, op=ALU.max), reads=[k_], writes=[k_])
            S.op("dve", lambda e: e.tensor_tensor(out=OH1, in0=lem, in1=m1_.unsqueeze(2).to_broadcast([128, NT, NE]),
                                                  op=ALU.is_equal), reads=[k_], writes=["OH"])
            S.op("dve", lambda e: e.scalar_tensor_tensor(
                out=lem2.rearrange("p t x -> p (t x)"), in0=OH1.rearrange("p t x -> p (t x)"), scalar=-BIG,
                in1=lem.rearrange("p t x -> p (t x)"), op0=ALU.mult, op1=ALU.add), reads=[k_, "OH"], writes=[k_])
            S.op("dve", lambda e: e.tensor_reduce(out=m2_, in_=lem2, axis=AX.X, op=ALU.max), reads=[k_], writes=[k_])
            S.op("dve", lambda e: e.tensor_tensor(out=OH2, in0=lem2, in1=m2_.unsqueeze(2).to_broadcast([128, NT, NE]),
                                                  op=ALU.is_equal), reads=[k_], writes=["OH"])
            S.op("dve", lambda e: e.tensor_tensor(out=Mall, in0=OH1, in1=OH2, op=ALU.add), reads=["OH"], writes=["Mall"])
            S.op("dve", lambda e: e.tensor_tensor(out=dd, in0=m2_, in1=m1_, op=ALU.subtract), reads=[k_], writes=[k_])
            S.op("act", lambda e: e.activation(out=e21, in_=dd, func=AF.Exp), reads=[k_], writes=[k_])
            S.op("dve", lambda e: e.tensor_scalar(out=e21, in0=e21, scalar1=1.0, scalar2=None, op0=ALU.add), reads=[k_], writes=[k_])
            S.op("dve", lambda e: e.reciprocal(out=e21, in_=e21), reads=[k_], writes=[k_])
            S.op("dve", lambda e: e.tensor_tensor(out=G1, in0=e21, in1=gsum, op=ALU.mult), reads=[k_], writes=["G"])
            S.op("dve", lambda e: e.tensor_tensor(out=G2, in0=gsum, in1=G1, op=ALU.subtract), reads=[k_, "G"], writes=["G"])
            for tt in range(NT):
                S.op("pe", lambda e, tt=tt: e.matmul(
                    ps[:, 6, tt * NE:(tt + 1) * NE], lhsT=trib, rhs=Mall[:, tt, :], start=True, stop=(tt == 0)),
                    reads=["trib", "Mall"], writes=[("ps", 6)])
                for t2 in range(tt):
                    S.op("pe", lambda e, tt=tt, t2=t2: e.matmul(
                        ps[:, 6, tt * NE:(tt + 1) * NE], lhsT=onesb, rhs=Mall[:, t2, :], start=False, stop=(t2 == tt - 1)),
                        reads=["onesb", "Mall"], writes=[("ps", 6)])
            S.op("dve", lambda e: e.tensor_tensor(
                out=PEf, in0=ps[:, 6, :].rearrange("p (t x) -> p t x", t=NT),
                in1=iotac.unsqueeze(1).to_broadcast([128, NT, NE]), op=ALU.add),
                reads=[("ps", 6), "iotac"], writes=["PEf"])
            for (OH, Df, Di) in ((OH1, D1f, D1i), (OH2, D2f, D2i)):
                S.op("dve", lambda e, OH=OH: e.tensor_tensor(out=PR, in0=OH, in1=PEf, op=ALU.mult),
                     reads=["OH", "PEf"], writes=["PR"])
                S.op("dve", lambda e, Df=Df: e.tensor_reduce(out=Df, in_=PR, axis=AX.X, op=ALU.add),
                     reads=["PR"], writes=["Df"])
                S.op("dve", lambda e, Df=Df, Di=Di: e.tensor_copy(out=Di, in_=Df), reads=["Df"], writes=["Di"])
            for tt in range(NT):
                hb = tt % 4
                hf = tt % 2
                S.op("dve", lambda e, tt=tt, hf=hf: e.tensor_tensor(out=HF[hf], in0=X[:, tt, :], in1=SCB, op=ALU.mult),
                     reads=[("X", tt), "SCB"], writes=[("HF", hf)])
                S.op("dve", lambda e, hb=hb, hf=hf: e.tensor_tensor(out=HB[hb], in0=HF[hf], in1=SHB, op=ALU.add),
                     reads=[("HF", hf), "SHB"], writes=[("HB", hb)])
                for Di in (D1i, D2i):
                    S.dma("pool", lambda e, tt=tt, Di=Di, hb=hb: e.indirect_dma_start(
                        out=xs_d[:, :], out_offset=bass.IndirectOffsetOnAxis(ap=Di[:, tt:tt + 1], axis=0),
                        in_=HB[hb], in_offset=None),
                        reads=[("HB", hb), "Di"], writes=[("XSw", tt, id(Di))])
            S.barrier()
            ar.reset(m0)
            NXG = 6
            NYO = 4
            XG = [ar.alloc([128, D], BF16) for _ in range(NXG)]
            XeT = [ar.alloc([128, KC, CAP], BF16) for _ in range(2)]
            HTb = [ar.alloc([128, 4, CAP], BF16) for _ in range(2)]
            SG = [ar.alloc([128, CAP]) for _ in range(2)]
            YO = [ar.alloc([128, D], BF16) for _ in range(NYO)]

            def prep_load(e_):
                for s_ in range(NS):
                    xb = (e_ * NS + s_) % NXG
                    r0 = e_ * CAP + s_ * 128
                    S.dma("sp", lambda e, xb=xb, r0=r0: e.dma_start(out=XG[xb], in_=xs_d[r0:r0 + 128, :]),
                          writes=[("XG", xb)])

            def prep(e_):
                wb = e_ % 2
                for s_ in range(NS):
                    xb = (e_ * NS + s_) % NXG
                    bank = (e_ * NS + s_) % 2
                    for kc in range(KC):
                        S.op("pe", lambda e, xb=xb, kc=kc, bank=bank: e.transpose(
                            psb(bank)[:, kc * 128:(kc + 1) * 128], XG[xb][:, kc * 128:(kc + 1) * 128], identb),
                            reads=[("XG", xb), "identb"], writes=[("ps", bank)])
                    S.op("act", lambda e, s_=s_, bank=bank, wb=wb: e.activation(
                        out=XeT[wb][:, :, s_ * 128:(s_ + 1) * 128], in_=psb(bank).rearrange("p (k n) -> p k n", k=KC),
                        func=AF.Copy), reads=[("ps", bank)], writes=[("XeT", wb)])

            def compute(e_):
                wb = e_ % 2
                ww = e_ % NWB
                Wg_, Wu_, Wd_ = WE[ww]
                for fc in range(4):
                    gbank = 2 + 2 * (fc % 2)
                    ubank = gbank + 1
                    for (wi, W_, bank) in ((0, Wg_, gbank), (1, Wu_, ubank)):
                        for kc in range(KC):
                            S.op("pe", lambda e, fc=fc, kc=kc, W_=W_, bank=bank: e.matmul(
                                ps[:, bank, 0:CAP], lhsT=W_[:, kc, fc * 128:(fc + 1) * 128], rhs=XeT[wb][:, kc, :],
                                start=(kc == 0), stop=(kc == KC - 1)),
                                reads=[("WE", ww, wi), ("XeT", wb)], writes=[("ps", bank)])
                    S.op("act", lambda e, fc=fc, gbank=gbank: e.activation(out=SG[fc % 2], in_=ps[:, gbank, 0:CAP], func=AF.Silu),
                         reads=[("ps", gbank)], writes=[("SG", fc % 2)])
                    S.op("dve", lambda e, fc=fc, ubank=ubank: e.tensor_tensor(
                        out=HTb[wb][:, fc, :], in0=SG[fc % 2], in1=ps[:, ubank, 0:CAP], op=ALU.mult),
                        reads=[("SG", fc % 2), ("ps", ubank)], writes=[("HTb", wb)])
                if e_ + NWB < NE:
                    load_expert(e_ + NWB, which=(0, 1))
                for s_ in range(NS):
                    yb_ = (e_ * NS + s_) % NYO
                    for half in range(2):
                        bank = 6 + half
                        for fc in range(4):
                            S.op("pe", lambda e, s_=s_, half=half, fc=fc, bank=bank: e.matmul(
                                ps[:, bank, :], lhsT=HTb[wb][:, fc, s_ * 128:(s_ + 1) * 128],
                                rhs=Wd_[:, fc, half * 512:(half + 1) * 512], start=(fc == 0), stop=(fc == 3)),
                                reads=[("HTb", wb), ("WE", ww, 2)], writes=[("ps", bank)])
                        S.op("dve", lambda e, yb_=yb_, bank=bank, half=half: e.tensor_tensor(
                            out=YO[yb_][:, half * 512:(half + 1) * 512], in0=ps[:, bank, :],
                            in1=G2B[:, half * 512:(half + 1) * 512], op=ALU.mult),
                            reads=[("ps", bank), ("GB", 5)], writes=[("YO", yb_)])
                    r0 = e_ * CAP + s_ * 128
                    S.dma("sp", lambda e, yb_=yb_, r0=r0: e.dma_start(out=ys_d[r0:r0 + 128, :], in_=YO[yb_]),
                          reads=[("YO", yb_)], writes=[("YSw", e_, s_)])
                if e_ + NWB < NE:
                    load_expert(e_ + NWB, which=(2,))

            prep_load(0)
            prep_load(1)
            prep(0)
            for e_ in range(NE):
                if e_ + 2 < NE:
                    prep_load(e_ + 2)
                if e_ + 1 < NE:
                    prep(e_ + 1)
                compute(e_)
            S.barrier()
            ar.reset(A0 + KC * SEQ // 2)
            modg = mod_groups(next_layer, R0, (0,), (1,)) if next_layer is not None else None
            NYB = 4
            Y1 = [ar.alloc([128, D], BF16) for _ in range(NYB)]
            Y2 = [ar.alloc([128, D], BF16) for _ in range(NYB)]
            VB = TMPV + [ar.alloc([128, D]) for _ in range(2)]
            def gather(tt):
                yb = tt % NYB
                S.dma("pool", lambda e: e.indirect_dma_start(
                    out=Y1[yb], out_offset=None, in_=ys_d[:, :],
                    in_offset=bass.IndirectOffsetOnAxis(ap=D1i[:, tt:tt + 1], axis=0)),
                    reads=["Di"], writes=[("Y1", yb)])
                S.dma("pool", lambda e: e.indirect_dma_start(
                    out=Y2[yb], out_offset=None, in_=ys_d[:, :],
                    in_offset=bass.IndirectOffsetOnAxis(ap=D2i[:, tt:tt + 1], axis=0)),
                    reads=["Di"], writes=[("Y2", yb)])

            DG = [[ar.alloc([128, 128], BF16) for _ in range(2)] for _ in range(2)]
            for tt in range(NYB):
                gather(tt)

            def build(tt):
                yb = tt % NYB
                db = tt % 2
                for k_, Gk in enumerate((G1, G2)):
                    S.op("dve", lambda e, k_=k_, Gk=Gk: e.tensor_scalar(
                        out=DG[db][k_], in0=ident, scalar1=Gk[:, tt:tt + 1], scalar2=None, op0=ALU.mult),
                        reads=["ident", "G"], writes=[("DG", db, k_)])
                banks = [2 + 2 * (tt % 3), 3 + 2 * (tt % 3)]
                for half in range(2):
                    hs = slice(half * 512, (half + 1) * 512)
                    S.op("pe", lambda e, half=half, hs=hs: e.matmul(
                        ps[:, banks[half], :], lhsT=alphaI, rhs=X[:, tt, hs], start=True, stop=False),
                        reads=["alphaI", ("X", tt)], writes=[("ps", banks[half])])
                    S.op("pe", lambda e, half=half, hs=hs: e.matmul(
                        ps[:, banks[half], :], lhsT=DG[db][0], rhs=Y1[yb][:, hs], start=False, stop=False),
                        reads=[("DG", db, 0), ("Y1", yb)], writes=[("ps", banks[half])])
                    S.op("pe", lambda e, half=half, hs=hs: e.matmul(
                        ps[:, banks[half], :], lhsT=DG[db][1], rhs=Y2[yb][:, hs], start=False, stop=True),
                        reads=[("DG", db, 1), ("Y2", yb)], writes=[("ps", banks[half])])

            def cstats(tt):
                banks = [2 + 2 * (tt % 3), 3 + 2 * (tt % 3)]
                ln_stats(tt, ps[:, banks[0]:banks[0] + 2, :].rearrange("p a n -> p (a n)"),
                         [("ps", banks[0]), ("ps", banks[1])], vbufs=VB)

            build(0)
            build(1)
            cstats(0)
            for tt in range(NT):
                if tt + 2 < NT:
                    build(tt + 2)
                if tt + 1 < NT:
                    cstats(tt + 1)
                ln_finish(tt, store_out=store_out, vbufs=VB)
                if tt + NYB < NT:
                    gather(tt + NYB)
                if modg is not None:
                    if tt < 12:
                        modg(tt)
                    if tt >= 4:
                        hT_tile(tt - 4, (1,), SC1, SH1, 1, 0)
            if modg is not None:
                for k in range(NT - 4, NT):
                    hT_tile(k, (1,), SC1, SH1, 1, 0)

        def dump_dbg(kind):
            if kind == "mod":
                S.dma("sp", lambda e: e.dma_start(out=out_d[0:128, :], in_=G1B), reads=[("GB", 2)], writes=["o0"])
                S.dma("sp", lambda e: e.dma_start(out=out_d[128:256, :], in_=G2B), reads=[("GB", 5)], writes=["o1"])
                for n_, t_ in enumerate((SC1, SH1, SC2, SH2)):
                    S.dma("sp", lambda e, n_=n_, t_=t_: e.dma_start(out=out_d[256:384, n_ * 8:(n_ + 1) * 8], in_=t_),
                          reads=[("modfm", k) for k in (0, 1, 3, 4)], writes=[("o2", n_)])
            elif kind == "hT":
                for kc in range(KC):
                    S.dma("pool", lambda e, kc=kc: e.dma_start(out=out_d[kc * 128:(kc + 1) * 128, :], in_=A[:, kc, 0:1024]),
                          reads=[("A", t) for t in range(NT)], writes=[("o", kc)])
            elif kind == "attn":
                for tt in range(NT):
                    S.dma("pool", lambda e, tt=tt: e.dma_start(out=out_d[tt * 128:(tt + 1) * 128, :], in_=B[:, tt, :]),
                          reads=[("B", tt)], writes=[("o", tt)])

        def dump_x():
            for tt in range(NT):
                S.dma("sp", lambda e, tt=tt: e.dma_start(out=out_d[tt * 128:(tt + 1) * 128, :], in_=X[:, tt, :]),
                      reads=[("X", tt)], writes=[("out", tt)])

        done = False
        premod = False
        for i in range(n_layers):
            S.barrier()
            if not premod:
                mod_phase(i)
                S.barrier()
                if stop == ("mod", i):
                    dump_dbg("mod")
                    done = True
                    break
                build_hT(SC1, SH1, 1, 0)
                if stop == ("hT", i):
                    S.barrier()
                    dump_dbg("hT")
                    done = True
                    break
            l = i // 2
            if i % 2 == 0:
                na_phase(l)
                wo_ap = nawo_d[l]
            else:
                gqa_phase(l)
                wo_ap = gwo_d[l]
            S.barrier()
            if stop == ("attn", i):
                dump_dbg("attn")
                done = True
                break
            wo_ln_phase(i, wo_ap)
            S.barrier()
            if stop == ("mix", i):
                dump_x()
                done = True
                break
            last = (i == n_layers - 1)
            moe_phase(i, store_out=last, next_layer=(None if last else i + 1))
            premod = not last
            if last:
                done = True
        assert done
        S.barrier()
        S.emit()
        S.close()
    return nc


_CACHE = {}


def _prep_inputs(x, c, ada_w, ada_b, ln_g, ln_b, na_w_qkv, na_rpb, na_w_o, gqa_w_qkv, gqa_q_norm,
                 gqa_k_norm, gqa_w_o, moe_w_group, moe_b_group, moe_w_expert, moe_b_expert,
                 moe_w_gate, moe_w_up, moe_w_down):
    f = lambda a: np.ascontiguousarray(np.asarray(a), dtype=np.float32)
    C64, S64 = _rope_tables()
    shared = {
        "ada_w": f(ada_w), "ada_b": f(ada_b), "ln_g": f(ln_g), "ln_b": f(ln_b),
        "na_w_qkv": f(na_w_qkv), "na_w_o": f(na_w_o), "na_bias": _na_bias_table(f(na_rpb)).reshape(-1, NH, 128, NCLS * 5 * 128),
        "gqa_w_qkv": f(gqa_w_qkv), "gqa_w_o": f(gqa_w_o), "gqa_q_norm": f(gqa_q_norm), "gqa_k_norm": f(gqa_k_norm),
        "rope_c": C64, "rope_s": S64,
        "moe_wr": np.ascontiguousarray(np.concatenate([f(moe_w_group), f(moe_w_expert)], axis=-1)),
        "moe_br": np.ascontiguousarray(np.concatenate([f(moe_b_group), f(moe_b_expert)], axis=-1)),
        "moe_w_gate": f(moe_w_gate), "moe_w_up": f(moe_w_up), "moe_w_down": f(moe_w_down),
        "ident": np.eye(128, dtype=np.float32),
        "tri": np.triu(np.ones((128, 128), np.float32), 1),
        "iotac": np.ascontiguousarray(np.broadcast_to((np.arange(NE) * CAP).astype(np.float32), (128, NE))),
    }
    x = f(x)
    c = f(c)
    in_maps = []
    for b in range(8):
        m = dict(shared)
        m["x"] = x[b]
        m["cfm"] = np.ascontiguousarray(c[b].reshape(KC, 128).T)
        in_maps.append(m)
    return in_maps


def kernel(**inputs):
    in_maps = _prep_inputs(**inputs)
    key = "full"
    if key not in _CACHE:
        _CACHE[key] = build_program()
    nc = _CACHE[key]
    res = run_bass_kernel_spmd(nc, in_maps, core_ids=list(range(8)))
    return np.stack([np.asarray(r["out"], dtype=np.float32) for r in res.results], axis=0)
```

```python
import numpy as np
import concourse.bass as bass
import concourse.mybir as mybir
from concourse.bass_utils import run_bass_kernel_spmd

F32 = mybir.dt.float32
BF16 = mybir.dt.bfloat16
I32 = mybir.dt.int32
AF = mybir.ActivationFunctionType
ALU = mybir.AluOpType
AX = mybir.AxisListType

D = 1024
SEQ = 2048
NT = 16
KC = 8
NH = 16
HD = 64
DEPTH = 4
NE = 32
CAP = 384
NS = CAP // 128
ALPHA = float((2 * DEPTH) ** 0.25)
LN_EPS = 1e-5
RMS_EPS = 1e-6
NEG = -30000.0
BIG = 1.0e4
ARENA_WORDS = 53200


class _Rec:
    def __init__(self):
        self.call = None

    def __getattr__(self, name):
        def f(*a, **k):
            self.call = (name, a, k)
            return self
        return f


def _record(fn):
    r = _Rec()
    fn(r)
    assert r.call is not None
    return r.call


class Sched:
    def __init__(self, nc, n_dma_sems=8, same_engine_sync=True):
        self.nc = nc
        self.prog = {e: [] for e in ("pe", "act", "dve", "pool", "sp")}
        self.cnt = {e: 0 for e in self.prog}
        self.sems = {}
        self.waited = {e: {} for e in self.prog}
        self.last_w = {}
        self.readers = {}
        self.same_engine_sync = same_engine_sync
        self.n_dma_sems = n_dma_sems
        self.dma_ring = {q: {"next": 0, "tot": [0] * n_dma_sems} for q in ("sp", "act", "pool")}
        self._ctx = []

    def open(self):
        nc = self.nc
        for e in self.prog:
            cm = nc.semaphore("s_" + e)
            self.sems["s_" + e] = cm.__enter__()
            self._ctx.append(cm)
        for q in self.dma_ring:
            for i in range(self.n_dma_sems):
                nm = f"d_{q}{i}"
                cm = nc.semaphore(nm)
                self.sems[nm] = cm.__enter__()
                self._ctx.append(cm)

    def close(self):
        for cm in reversed(self._ctx):
            cm.__exit__(None, None, None)

    def _need(self, eng, tok, waits):
        if tok is None:
            return
        sem, val, peng = tok
        if peng == eng and (eng == "pe" or not self.same_engine_sync):
            return
        if self.waited[eng].get(sem, 0) >= val:
            return
        if waits.get(sem, 0) < val:
            waits[sem] = val

    def _collect(self, eng, reads, writes):
        waits = {}
        for k in reads:
            self._need(eng, self.last_w.get(k), waits)
        for k in writes:
            self._need(eng, self.last_w.get(k), waits)
            for t in self.readers.get(k, ()):
                self._need(eng, t, waits)
        for s, v in waits.items():
            self.waited[eng][s] = v
        return list(waits.items())

    def _update(self, tok, reads, writes):
        for k in writes:
            self.last_w[k] = tok
            self.readers[k] = []
        for k in reads:
            if k in writes:
                continue
            lst = self.readers.setdefault(k, [])
            lst.append(tok)
            if len(lst) > 48:
                best = {}
                for t in lst:
                    if t[0] not in best or best[t[0]][1] < t[1]:
                        best[t[0]] = t
                self.readers[k] = list(best.values())

    def op(self, eng, fn, reads=(), writes=()):
        waits = self._collect(eng, reads, writes)
        self.cnt[eng] += 1
        tok = ("s_" + eng, self.cnt[eng], eng)
        self.prog[eng].append((waits, _record(fn), ("s_" + eng, 1)))
        self._update(tok, reads, writes)
        return tok

    def dma(self, q, fn, reads=(), writes=()):
        ring = self.dma_ring[q]
        i = ring["next"]
        ring["next"] = (i + 1) % self.n_dma_sems
        sem = f"d_{q}{i}"
        waits = dict(self._collect(q, reads, writes))
        prev = ring["tot"][i]
        if prev and self.waited[q].get(sem, 0) < prev:
            waits[sem] = prev
            self.waited[q][sem] = prev
        ring["tot"][i] += 16
        tok = (sem, ring["tot"][i], "dma_" + q)
        self.prog[q].append((list(waits.items()), _record(fn), (sem, 16)))
        self._update(tok, reads, writes)
        return tok

    def barrier(self):
        targets = {}
        for e, c in self.cnt.items():
            if c:
                targets["s_" + e] = c
        for q, ring in self.dma_ring.items():
            for i, t in enumerate(ring["tot"]):
                if t:
                    targets[f"d_{q}{i}"] = t
        for eng in self.prog:
            waits = []
            for s, v in targets.items():
                if s == "s_" + eng and eng == "pe":
                    continue
                if self.waited[eng].get(s, 0) < v:
                    waits.append((s, v))
                    self.waited[eng][s] = v
            if waits:
                self.prog[eng].append((waits, None, None))

    def emit(self):
        nc = self.nc
        sems = self.sems
        prog = self.prog

        def run(engh, lst):
            for waits, fn, inc in lst:
                for s, v in waits:
                    engh.wait_ge(sems[s], v)
                if fn is not None:
                    name, a, k = fn
                    ins = getattr(engh, name)(*a, **k)
                    ins.then_inc(sems[inc[0]], inc[1])

        with nc.Block() as block:
            @block.sync
            def _(e):
                run(e, prog["sp"])

            @block.tensor
            def _(e):
                run(e, prog["pe"])

            @block.scalar
            def _(e):
                run(e, prog["act"])

            @block.vector
            def _(e):
                run(e, prog["dve"])

            @block.gpsimd
            def _(e):
                run(e, prog["pool"])


class Arena:
    def __init__(self, t, nwords):
        self.t = t
        self.n = nwords
        self.off = 0

    def alloc(self, shape, dt=F32):
        free = 1
        for s in shape[1:]:
            free *= s
        esz = 4 if dt in (F32, I32) else 2
        words = (free * esz + 3) // 4
        words = (words + 7) // 8 * 8
        assert self.off + words <= self.n, ("arena overflow", self.off, words, self.n)
        v = self.t[:, self.off:self.off + words]
        self.off += words
        if dt != F32:
            v = v.bitcast(dt)
        v = v[:, 0:free]
        if len(shape) == 3:
            v = v.rearrange("p (a b) -> p a b", a=shape[1])
        elif len(shape) == 4:
            v = v.rearrange("p (a b c) -> p a b c", a=shape[1], b=shape[2])
        return v

    def mark(self):
        return self.off

    def reset(self, m):
        self.off = m


def _na_patterns():
    pats = []
    for j in range(NT):
        kt_lo = min(max(j - 2, 0), 11)
        qi = np.arange(128)
        r = 2 * j + qi // 64
        c = qi % 64
        rs = np.clip(r - 4, 0, 24)
        ws = np.clip(c - 8, 0, 48)
        tiles = []
        for i in range(5):
            kt = kt_lo + i
            ki = np.arange(128)
            kr = 2 * kt + ki // 64
            kcol = ki % 64
            vr = (kr[:, None] >= rs[None, :]) & (kr[:, None] < rs[None, :] + 8)
            vc = (kcol[:, None] >= ws[None, :]) & (kcol[:, None] < ws[None, :] + 16)
            dr = np.clip(kr[:, None] - r[None, :] + 7, 0, 14)
            dc = np.clip(kcol[:, None] - c[None, :] + 15, 0, 30)
            valid = vr & vc
            tiles.append((valid, np.where(valid, dr, 0), np.where(valid, dc, 0)))
        pats.append(tiles)
    classes = []
    cls_of_j = []
    for j in range(NT):
        key = b"".join(a.tobytes() for t in pats[j] for a in t)
        found = None
        for ci, (k2, _) in enumerate(classes):
            if k2 == key:
                found = ci
                break
        if found is None:
            classes.append((key, pats[j]))
            found = len(classes) - 1
        cls_of_j.append(found)
    return [c[1] for c in classes], cls_of_j


_NA_CLASSES, _NA_CLS_OF_J = _na_patterns()
NCLS = len(_NA_CLASSES)


def _na_bias_table(rpb):
    L = rpb.shape[0]
    out = np.empty((L, NH, 128, NCLS * 5, 128), np.float32)
    for ci, tiles in enumerate(_NA_CLASSES):
        for i, (valid, dr, dc) in enumerate(tiles):
            g = rpb[:, :, dr, dc]
            out[:, :, :, ci * 5 + i, :] = np.where(valid[None, None], g, np.float32(NEG))
    return out


def _rope_tables():
    t = np.arange(SEQ)
    pos = np.stack([t // 64, t % 64], -1).astype(np.float32)
    inv = (np.float32(10000.0) ** (-np.arange(16, dtype=np.float32) / np.float32(16))).astype(np.float32)
    ang = pos[:, :, None] * inv
    c = np.cos(ang).astype(np.float32)
    s = np.sin(ang).astype(np.float32)
    C64 = np.stack([c, c], 2).reshape(SEQ, 64)
    S64 = np.stack([-s, s], 2).reshape(SEQ, 64)
    C64 = np.ascontiguousarray(C64.reshape(NT, 128, 64).transpose(1, 0, 2))
    S64 = np.ascontiguousarray(S64.reshape(NT, 128, 64).transpose(1, 0, 2))
    return C64, S64


def build_program(n_layers=DEPTH, stop=None, lite=False, decl=None):
    nc = bass.Bass("TRN2", target_bir_lowering=False)
    n_moe = n_layers if stop is None else n_layers - 1
    LD = n_layers if lite else DEPTH
    LNA = max(1, (n_layers + 1) // 2) if lite else 2
    LGQ = max(1, n_layers // 2) if lite else 2
    LMOE = max(1, n_moe) if lite else DEPTH
    EMOE = NE if (n_moe > 0 or not lite) else 1

    def din(name, shape, dt=F32):
        if decl is not None:
            decl[name] = tuple(shape)
        return nc.dram_tensor(name, list(shape), dt, kind="ExternalInput").ap()

    x_d = din("x", [SEQ, D])
    c_d = din("cfm", [128, KC])
    adaw_d = din("ada_w", [LD, D, 6 * D])
    adab_d = din("ada_b", [LD, 6 * D])
    lng_d = din("ln_g", [LD, 2, D])
    lnb_d = din("ln_b", [LD, 2, D])
    nawqkv_d = din("na_w_qkv", [LNA, D, 3 * D])
    nawo_d = din("na_w_o", [LNA, D, D])
    nabias_d = din("na_bias", [LNA, NH, 128, NCLS * 5 * 128])
    gwqkv_d = din("gqa_w_qkv", [LGQ, D, 1536])
    gwo_d = din("gqa_w_o", [LGQ, D, D])
    gqn_d = din("gqa_q_norm", [LGQ, HD])
    gkn_d = din("gqa_k_norm", [LGQ, HD])
    ropec_d = din("rope_c", [128, NT, 64])
    ropes_d = din("rope_s", [128, NT, 64])
    wr_d = din("moe_wr", [LMOE, D, 36])
    br_d = din("moe_br", [LMOE, 36])
    wg_d = din("moe_w_gate", [LMOE, EMOE, D, 512])
    wu_d = din("moe_w_up", [LMOE, EMOE, D, 512])
    wd_d = din("moe_w_down", [LMOE, EMOE, 512, D])
    ident_d = din("ident", [128, 128])
    tri_d = din("tri", [128, 128])
    iotac_d = din("iotac", [128, NE])
    out_d = nc.dram_tensor("out", [SEQ, D], F32, kind="ExternalOutput").ap()
    xs_d = nc.dram_tensor("xs_scr", [NE * CAP + 2 * CAP, D], BF16, kind="Internal").ap()
    ys_d = nc.dram_tensor("ys_scr", [NE * CAP + 2 * CAP, D], BF16, kind="Internal").ap()
    modrow_d = nc.dram_tensor("modrow", [2, D], F32, kind="Internal").ap()

    S = Sched(nc)
    with nc.sbuf_tensor("arena", [128, ARENA_WORDS], F32) as arena_t, \
            nc.psum_tensor("ps", [128, 8, 512], F32) as ps:
        ar = Arena(arena_t, ARENA_WORDS)
        S.open()

        def psb(bank):
            return ps[:, bank, :].bitcast(BF16)

        X = ar.alloc([128, NT, D])
        ident = ar.alloc([128, 128])
        identb = ar.alloc([128, 128], BF16)
        onesb = ar.alloc([128, 128], BF16)
        trib = ar.alloc([128, 128], BF16)
        iotac = ar.alloc([128, NE])
        alphaI = ar.alloc([128, 128])
        cact = ar.alloc([128, KC])
        cA = ar.alloc([128, KC, 128], BF16)
        SC1 = ar.alloc([128, KC]); SH1 = ar.alloc([128, KC])
        SC2 = ar.alloc([128, KC]); SH2 = ar.alloc([128, KC])
        G1B = ar.alloc([128, D]); G2B = ar.alloc([128, D])
        LNG = ar.alloc([128, D]); LNB = ar.alloc([128, D])
        TMPV = [ar.alloc([128, D]) for _ in range(2)]
        st_t = [ar.alloc([128, 12]) for _ in range(4)]
        mv_t = [ar.alloc([128, 2]) for _ in range(4)]
        sd_t = [ar.alloc([128, 2]) for _ in range(4)]
        eps_ln = ar.alloc([128, 1]); eps_rms = ar.alloc([128, 1])
        G1 = ar.alloc([128, NT]); G2 = ar.alloc([128, NT])
        D1i = ar.alloc([128, NT], I32); D2i = ar.alloc([128, NT], I32)
        A0 = ar.mark()
        A = ar.alloc([128, KC, SEQ], BF16)
        B = ar.alloc([128, NT, D], BF16)
        R0 = ar.mark()

        S.dma("sp", lambda e: e.dma_start(out=ident, in_=ident_d[:, :]), writes=["ident"])
        S.dma("pool", lambda e: e.dma_start(out=identb, in_=ident_d[:, :]), writes=["identb"])
        S.dma("pool", lambda e: e.dma_start(out=trib, in_=tri_d[:, :]), writes=["trib"])
        S.dma("sp", lambda e: e.dma_start(out=iotac, in_=iotac_d[:, :]), writes=["iotac"])
        S.dma("sp", lambda e: e.dma_start(out=cact, in_=c_d[:, :]), writes=["cact"])
        S.op("dve", lambda e: e.memset(onesb, 1.0), writes=["onesb"])
        S.op("dve", lambda e: e.tensor_scalar(out=alphaI, in0=ident, scalar1=ALPHA, scalar2=None, op0=ALU.mult),
             reads=["ident"], writes=["alphaI"])
        S.op("dve", lambda e: e.memset(eps_ln, LN_EPS), writes=["eps"])
        S.op("dve", lambda e: e.memset(eps_rms, RMS_EPS), writes=["eps"])
        for g4 in range(4):
            S.dma("sp", lambda e, g4=g4: e.dma_start(
                out=X[:, g4 * 4:(g4 + 1) * 4, :],
                in_=x_d[g4 * 512:(g4 + 1) * 512, :].rearrange("(t p) d -> p t d", p=128)),
                writes=[("X", t) for t in range(g4 * 4, g4 * 4 + 4)])
        S.op("act", lambda e: e.activation(out=cact, in_=cact, func=AF.Silu), reads=["cact"], writes=["cact"])
        S.op("dve", lambda e: e.tensor_copy(out=cA, in_=cact.unsqueeze(2).to_broadcast([128, KC, 128])),
             reads=["cact"], writes=["cA"])

        def mod_groups(i, base, mm_banks, tr_banks):
            arm = Arena(arena_t, ARENA_WORDS)
            arm.off = base
            AW = [arm.alloc([128, KC, 512], BF16) for _ in range(2)]
            ABb = [arm.alloc([128, 512]) for _ in range(2)]
            T = [arm.alloc([128, 512]) for _ in range(2)]
            aw_v = adaw_d[i].rearrange("(k p) n -> p k n", p=128)

            def group(cg):
                b = cg % 2
                S.dma("pool", lambda e: e.dma_start(out=AW[b], in_=aw_v[:, :, cg * 512:(cg + 1) * 512]),
                      writes=[("AW", b)])
                S.dma("sp", lambda e: e.dma_start(
                    out=ABb[b], in_=adab_d[i:i + 1, cg * 512:(cg + 1) * 512].partition_broadcast(128)),
                    writes=[("ABb", b)])
                bank = mm_banks[cg % len(mm_banks)]
                for kc in range(KC):
                    S.op("pe", lambda e, kc=kc: e.matmul(
                        ps[:, bank, :], lhsT=cA[:, kc, :], rhs=AW[b][:, kc, :], start=(kc == 0), stop=(kc == KC - 1)),
                        reads=["cA", ("AW", b)], writes=[("ps", bank)])
                kind = cg // 2
                half = cg % 2
                if kind in (2, 5):
                    GB = G1B if kind == 2 else G2B
                    S.op("dve", lambda e: e.scalar_tensor_tensor(
                        out=GB[:, half * 512:(half + 1) * 512], in0=ps[:, bank, :], scalar=1.0, in1=ABb[b],
                        op0=ALU.add, op1=ALU.add),
                        reads=[("ps", bank), ("ABb", b)], writes=[("GB", kind)])
                else:
                    add1 = 1.0 if kind in (1, 4) else 0.0
                    S.op("dve", lambda e: e.scalar_tensor_tensor(
                        out=T[b], in0=ps[:, bank, :], scalar=add1, in1=ABb[b], op0=ALU.add, op1=ALU.add),
                        reads=[("ps", bank), ("ABb", b)], writes=[("T", b)])
                    tb = tr_banks[b % len(tr_banks)]
                    for q in range(4):
                        S.op("pe", lambda e, q=q: e.transpose(
                            ps[:, tb, q * 128:(q + 1) * 128], T[b][:, q * 128:(q + 1) * 128], ident),
                            reads=[("T", b), "ident"], writes=[("ps", tb)])
                    if kind in (3, 4):
                        S.dma("sp", lambda e: e.dma_start(
                            out=modrow_d[kind - 3:kind - 2, half * 512:(half + 1) * 512], in_=T[b][0:1, :]),
                            reads=[("T", b)], writes=[("modrow", kind, half)])
                    dst = {0: SH1, 1: SC1, 3: SH2, 4: SC2}[kind]
                    S.op("dve", lambda e: e.tensor_copy(
                        out=dst[:, half * 4:(half + 1) * 4],
                        in_=ps[:, tb, :].rearrange("p (q n) -> p q n", q=4)[:, :, 0]),
                        reads=[("ps", tb)], writes=[("modfm", kind)])
            return group

        def mod_phase(i):
            g = mod_groups(i, R0, (0, 1), (2, 3))
            for cg in range(12):
                g(cg)

        def hT_tile(tt, banks, SC, SH, kind_sc, kind_sh):
            for half in range(2):
                bank = banks[half % len(banks)]
                for q in range(4):
                    kc = half * 4 + q
                    S.op("pe", lambda e, q=q, kc=kc: e.transpose(
                        ps[:, bank, q * 128:(q + 1) * 128], X[:, tt, kc * 128:(kc + 1) * 128], ident),
                        reads=[("X", tt), "ident"], writes=[("ps", bank)])
                for q in range(4):
                    kc = half * 4 + q
                    S.op("act", lambda e, q=q, kc=kc: e.activation(
                        out=A[:, kc, tt * 128:(tt + 1) * 128], in_=ps[:, bank, q * 128:(q + 1) * 128],
                        func=AF.Identity, bias=SH[:, kc:kc + 1], scale=SC[:, kc:kc + 1]),
                        reads=[("ps", bank), ("modfm", kind_sc), ("modfm", kind_sh)], writes=[("A", tt)])

        def build_hT(SC, SH, kind_sc, kind_sh):
            for tt in range(NT):
                hT_tile(tt, [(2 * tt) % 4, (2 * tt + 1) % 4], SC, SH, kind_sc, kind_sh)

        def ln_stats(tt, v_ps, vkeys, vbufs):
            nb_ = len(vbufs)
            tb = tt % nb_
            v = vbufs[tb]
            sb_ = tt % 4
            for half in range(2):
                S.op("dve", lambda e, half=half: e.bn_stats(
                    out=st_t[sb_][:, half * 6:(half + 1) * 6], in_=v_ps[:, half * 512:(half + 1) * 512]),
                    reads=[vkeys[half]], writes=[("st", sb_)])
            S.op("dve", lambda e: e.bn_aggr(out=mv_t[sb_], in_=st_t[sb_]), reads=[("st", sb_)], writes=[("mv", sb_)])
            S.op("act", lambda e: e.activation(out=sd_t[sb_][:, 0:1], in_=mv_t[sb_][:, 1:2], func=AF.Sqrt, bias=eps_ln, scale=1.0),
                 reads=[("mv", sb_), "eps"], writes=[("sd", sb_)])
            S.op("dve", lambda e: e.reciprocal(out=sd_t[sb_][:, 0:1], in_=sd_t[sb_][:, 0:1]), reads=[("sd", sb_)], writes=[("sd", sb_)])
            S.op("dve", lambda e: e.scalar_tensor_tensor(
                out=sd_t[sb_][:, 1:2], in0=mv_t[sb_][:, 0:1], scalar=-1.0, in1=sd_t[sb_][:, 0:1], op0=ALU.mult, op1=ALU.mult),
                reads=[("mv", sb_), ("sd", sb_)], writes=[("sd2", sb_)])
            S.op("act", lambda e: e.activation(out=v, in_=v_ps, func=AF.Identity, bias=sd_t[sb_][:, 1:2], scale=sd_t[sb_][:, 0:1]),
                 reads=list(vkeys) + [("sd", sb_), ("sd2", sb_)], writes=[("v", tb)])

        def ln_finish(tt, store_out, vbufs):
            tb = tt % len(vbufs)
            v = vbufs[tb]
            S.op("dve", lambda e: e.tensor_tensor(out=v, in0=v, in1=LNG, op=ALU.mult),
                 reads=[("v", tb), "LNG"], writes=[("v", tb)])
            S.op("dve", lambda e: e.tensor_tensor(out=X[:, tt, :], in0=v, in1=LNB, op=ALU.add),
                 reads=[("v", tb), "LNB"], writes=[("X", tt)])
            if store_out:
                S.dma("sp", lambda e: e.dma_start(out=out_d[tt * 128:(tt + 1) * 128, :], in_=X[:, tt, :]),
                      reads=[("X", tt)], writes=[("out", tt)])

        def load_ln_params(i, which):
            S.dma("sp", lambda e: e.dma_start(out=LNG, in_=lng_d[i, which:which + 1, :].partition_broadcast(128)),
                  writes=["LNG"])
            S.dma("sp", lambda e: e.dma_start(out=LNB, in_=lnb_d[i, which:which + 1, :].partition_broadcast(128)),
                  writes=["LNB"])

        def na_phase(l):
            ar.reset(R0)
            Wb = [[ar.alloc([128, KC, 128], BF16) for _ in range(3)] for _ in range(2)]
            BI = [ar.alloc([128, NCLS * 5, 128], BF16) for _ in range(3)]
            QT = ar.alloc([128, SEQ], BF16)
            KT = ar.alloc([128, SEQ], BF16)
            V = ar.alloc([128, NT, 2, 66], BF16)
            PT = [ar.alloc([128, 640], BF16) for _ in range(3)]
            rc = [ar.alloc([128, 1]) for _ in range(2)]
            w_v = nawqkv_d[l].rearrange("(k p) n -> p k n", p=128)
            S.op("dve", lambda e: e.memset(V[:, :, :, 64:66], 1.0), writes=["V"])

            def load_w(p):
                wb = p % 2
                for m in range(3):
                    c0 = m * D + p * 128
                    S.dma("pool", lambda e, wb=wb, m=m, c0=c0: e.dma_start(out=Wb[wb][m], in_=w_v[:, :, c0:c0 + 128]),
                          writes=[("W", wb, m)])

            def load_bias(h):
                nchunk = NCLS * 5 // 5
                S.dma("pool", lambda e, h=h: e.dma_start(
                    out=BI[h % 3].rearrange("p (a b) n -> p a (b n)", a=nchunk),
                    in_=nabias_d[l, h].rearrange("p (a m) -> p a m", a=nchunk)), writes=[("BI", h % 3)])

            load_w(0)
            load_bias(0)
            load_bias(1)

            def exp_bias(h):
                S.op("act", lambda e: e.activation(
                    out=BI[h % 3].rearrange("p a n -> p (a n)"), in_=BI[h % 3].rearrange("p a n -> p (a n)"), func=AF.Exp),
                    reads=[("BI", h % 3)], writes=[("BI", h % 3)])

            def emit_ST(n, h, j):
                hh = h % 2
                r0 = hh * 64
                kt_lo = min(max(j - 2, 0), 11)
                sb = (n % 3) * 2
                for i in range(5):
                    bank = sb + i // 4
                    col = (i % 4) * 128
                    S.op("pe", lambda e, bank=bank, col=col, i=i: e.matmul(
                        ps[:, bank, col:col + 128], lhsT=KT[r0:r0 + 64, (kt_lo + i) * 128:(kt_lo + i + 1) * 128],
                        rhs=QT[r0:r0 + 64, j * 128:(j + 1) * 128], start=True, stop=True),
                        reads=["KT", "QT"], writes=[("ps", bank)])

            def emit_B(n, h, j):
                c = _NA_CLS_OF_J[j]
                sb = (n % 3) * 2
                pb = n % 3
                S.op("act", lambda e: e.activation(out=PT[pb][:, 0:512], in_=ps[:, sb, :], func=AF.Exp),
                     reads=[("ps", sb)], writes=[("PT", pb)])
                S.op("act", lambda e: e.activation(out=PT[pb][:, 512:640], in_=ps[:, sb + 1, 0:128], func=AF.Exp),
                     reads=[("ps", sb + 1)], writes=[("PT", pb)])
                S.op("dve", lambda e: e.tensor_tensor(
                    out=PT[pb], in0=PT[pb], in1=BI[h % 3][:, c * 5:(c + 1) * 5, :].rearrange("p a n -> p (a n)"),
                    op=ALU.mult), reads=[("PT", pb), ("BI", h % 3)], writes=[("PT", pb)])

            def emit_C(n, h, j):
                hh = h % 2
                kt_lo = min(max(j - 2, 0), 11)
                pb = n % 3
                ob = 6 + (n % 2)
                for i in range(5):
                    S.op("pe", lambda e, i=i: e.matmul(
                        ps[:, ob, 0:65], lhsT=PT[pb][:, i * 128:(i + 1) * 128], rhs=V[:, kt_lo + i, hh, 0:65],
                        start=(i == 0), stop=(i == 4)),
                        reads=[("PT", pb), "V"], writes=[("ps", ob)])
                rb_ = n % 2
                S.op("dve", lambda e: e.reciprocal(out=rc[rb_], in_=ps[:, ob, 64:65]),
                     reads=[("ps", ob)], writes=[("rc", rb_)])
                S.op("dve", lambda e: e.tensor_scalar(
                    out=B[:, j, h * 64:(h + 1) * 64], in0=ps[:, ob, 0:64], scalar1=rc[rb_], scalar2=None, op0=ALU.mult),
                    reads=[("ps", ob), ("rc", rb_)], writes=[("B", j)])

            exp_bias(0)
            exp_bias(1)
            for p in range(8):
                wb = p % 2
                if p + 1 < 8:
                    load_w(p + 1)
                for m in range(2):
                    for tg in range(4):
                        bank = 4 + (tg % 2)
                        for kc in range(KC):
                            S.op("pe", lambda e, m=m, tg=tg, kc=kc, bank=bank: e.matmul(
                                ps[:, bank, :], lhsT=Wb[wb][m][:, kc, :], rhs=A[:, kc, tg * 512:(tg + 1) * 512],
                                start=(kc == 0), stop=(kc == KC - 1)),
                                reads=[("W", wb, m)] + [("A", t) for t in range(tg * 4, tg * 4 + 4)],
                                writes=[("ps", bank)])
                        if m == 0:
                            S.op("act", lambda e, tg=tg, bank=bank: e.activation(
                                out=QT[:, tg * 512:(tg + 1) * 512], in_=ps[:, bank, :], func=AF.Copy, scale=0.125),
                                reads=[("ps", bank)], writes=["QT"])
                        else:
                            S.op("dve", lambda e, tg=tg, bank=bank: e.tensor_copy(
                                out=KT[:, tg * 512:(tg + 1) * 512], in_=ps[:, bank, :]),
                                reads=[("ps", bank)], writes=["KT"])
                for tq in range(4):
                    bank = 6 + (tq % 2)
                    for t4 in range(4):
                        tt = tq * 4 + t4
                        for kc in range(KC):
                            S.op("pe", lambda e, tt=tt, t4=t4, kc=kc, bank=bank: e.matmul(
                                ps[:, bank, t4 * 128:(t4 + 1) * 128], lhsT=A[:, kc, tt * 128:(tt + 1) * 128],
                                rhs=Wb[wb][2][:, kc, :], start=(kc == 0), stop=(kc == KC - 1)),
                                reads=[("W", wb, 2), ("A", tt)], writes=[("ps", bank)])
                    S.op("dve", lambda e, tq=tq, bank=bank: e.tensor_copy(
                        out=V[:, tq * 4:(tq + 1) * 4, :, 0:64],
                        in_=ps[:, bank, :].rearrange("p (t h d) -> p t h d", t=4, h=2)),
                        reads=[("ps", bank)], writes=["V"])
                steps = [(2 * p + hh, j) for hh in range(2) for j in range(NT)]
                emit_ST(0, *steps[0])
                emit_ST(1, *steps[1])
                emit_B(0, *steps[0])
                for n, (h, j) in enumerate(steps):
                    if j == 0 and h + 2 < NH:
                        load_bias(h + 2)
                    if j == 8 and h + 2 < NH:
                        exp_bias(h + 2)
                    if n + 2 < len(steps):
                        emit_ST(n + 2, *steps[n + 2])
                    if n + 1 < len(steps):
                        emit_B(n + 1, *steps[n + 1])
                    emit_C(n, h, j)

        def gqa_phase(l):
            ar.reset(R0)
            KTd = ar.alloc([128, 4, SEQ], BF16)
            V = ar.alloc([128, NT, 4, 66], BF16)
            QN = ar.alloc([128, HD]); KN = ar.alloc([128, HD])
            ssq = ar.alloc([128, 20])
            m1 = ar.mark()
            ar_b = Arena(arena_t, ARENA_WORDS)
            ar_b.off = A0 + KC * SEQ // 2
            Wg = [ar_b.alloc([128, KC, 512], BF16) for _ in range(3)]
            RC = ar_b.alloc([128, NT, 64]); RS = ar_b.alloc([128, NT, 64])
            assert ar_b.off <= R0
            SQ = ar.alloc([128, 1280]); QF = ar.alloc([128, 1280]); T1 = ar.alloc([128, 1280]); T2 = ar.alloc([128, 1280])
            QB = ar.alloc([128, 1024], BF16); KD = ar.alloc([128, 4, 2, 64], BF16)
            w_v = gwqkv_d[l].rearrange("(k p) n -> p k n", p=128)
            for cg in range(3):
                S.dma("pool", lambda e, cg=cg: e.dma_start(out=Wg[cg], in_=w_v[:, :, cg * 512:(cg + 1) * 512]),
                      writes=[("Wg", cg)])
            S.dma("sp", lambda e: e.dma_start(out=RC, in_=ropec_d[:, :, :]), writes=["RC"])
            S.dma("sp", lambda e: e.dma_start(out=RS, in_=ropes_d[:, :, :]), writes=["RS"])
            S.dma("sp", lambda e: e.dma_start(out=QN, in_=gqn_d[l:l + 1, :].partition_broadcast(128)), writes=["QN"])
            S.dma("sp", lambda e: e.dma_start(out=KN, in_=gkn_d[l:l + 1, :].partition_broadcast(128)), writes=["KN"])
            S.op("dve", lambda e: e.memset(V[:, :, :, 64:66], 1.0), writes=["V"])

            def hd(ap, nh):
                return ap.rearrange("p (h d) -> p h d", h=nh)

            def g_mm(tt):
                b0 = (tt % 2) * 3
                for cg in range(3):
                    bank = b0 + cg
                    for kc in range(KC):
                        S.op("pe", lambda e, cg=cg, kc=kc, bank=bank, tt=tt: e.matmul(
                            ps[:, bank, :], lhsT=A[:, kc, tt * 128:(tt + 1) * 128], rhs=Wg[cg][:, kc, :],
                            start=(kc == 0), stop=(kc == KC - 1)),
                            reads=[("A", tt), ("Wg", cg)], writes=[("ps", bank)])

            def g_chain(tt):
                b0 = (tt % 2) * 3
                parts = [(ps[:, b0, :], 0, 8, ("ps", b0)), (ps[:, b0 + 1, :], 512, 8, ("ps", b0 + 1)),
                         (ps[:, b0 + 2, 0:256], 1024, 4, ("ps", b0 + 2))]
                for (pap, co, nh, pk) in parts:
                    S.op("act", lambda e, pap=pap, co=co, nh=nh: e.activation(
                        out=SQ[:, co:co + nh * 64], in_=pap, func=AF.Square),
                        reads=[pk], writes=["SQ"])
                S.op("act", lambda e, tt=tt, b0=b0: e.activation(
                    out=V[:, tt, :, 0:64], in_=hd(ps[:, b0 + 2, 256:512], 4), func=AF.Copy),
                    reads=[("ps", b0 + 2)], writes=["V"])
                S.op("dve", lambda e: e.tensor_reduce(out=ssq, in_=hd(SQ, 20), axis=AX.X, op=ALU.add),
                     reads=["SQ"], writes=["ssq"])
                S.op("act", lambda e: e.activation(out=ssq, in_=ssq, func=AF.Sqrt, bias=eps_rms, scale=1.0 / 64),
                     reads=["ssq", "eps"], writes=["ssq"])
                S.op("dve", lambda e: e.reciprocal(out=ssq, in_=ssq), reads=["ssq"], writes=["ssq"])
                h0 = 0
                for (pap, co, nh, pk) in parts:
                    S.op("dve", lambda e, pap=pap, co=co, nh=nh, h0=h0: e.tensor_tensor(
                        out=hd(QF[:, co:co + nh * 64], nh), in0=hd(pap, nh),
                        in1=ssq[:, h0:h0 + nh].unsqueeze(2).to_broadcast([128, nh, 64]), op=ALU.mult),
                        reads=[pk, "ssq"], writes=["QF"])
                    h0 += nh
                S.op("dve", lambda e: e.tensor_tensor(
                    out=hd(QF[:, 0:1024], 16), in0=hd(QF[:, 0:1024], 16),
                    in1=QN.unsqueeze(1).to_broadcast([128, 16, 64]), op=ALU.mult),
                    reads=["QF", "QN"], writes=["QF"])
                S.op("dve", lambda e: e.tensor_tensor(
                    out=hd(QF[:, 1024:1280], 4), in0=hd(QF[:, 1024:1280], 4),
                    in1=KN.unsqueeze(1).to_broadcast([128, 4, 64]), op=ALU.mult),
                    reads=["QF", "KN"], writes=["QF"])
                S.op("dve", lambda e, tt=tt: e.tensor_tensor(
                    out=hd(T1, 20), in0=hd(QF, 20), in1=RC[:, tt, :].unsqueeze(1).to_broadcast([128, 20, 64]),
                    op=ALU.mult), reads=["QF", "RC"], writes=["T1"])

                def v5(ap):
                    return ap.rearrange("p (h a f q) -> p h a f q", h=20, a=2, f=2)

                for f in range(2):
                    S.op("dve", lambda e, tt=tt, f=f: e.tensor_tensor(
                        out=v5(T2)[:, :, :, f, :], in0=v5(QF)[:, :, :, 1 - f, :],
                        in1=RS[:, tt, :].rearrange("p (a f q) -> p a f q", a=2, f=2)[:, :, f, :]
                        .unsqueeze(1).to_broadcast([128, 20, 2, 16]), op=ALU.mult),
                        reads=["QF", "RS"], writes=["T2"])
                S.op("dve", lambda e: e.tensor_tensor(out=QB, in0=T1[:, 0:1024], in1=T2[:, 0:1024], op=ALU.add),
                     reads=["T1", "T2"], writes=["QB"])
                for dup in range(2):
                    S.op("dve", lambda e, dup=dup: e.tensor_tensor(
                        out=KD[:, :, dup, :], in0=hd(T1[:, 1024:1280], 4), in1=hd(T2[:, 1024:1280], 4), op=ALU.add),
                        reads=["T1", "T2"], writes=["KD"])

            def g_tr(tt):
                for pr in range(8):
                    S.op("pe", lambda e, pr=pr: e.transpose(
                        psb(6)[:, pr * 128:(pr + 1) * 128], QB[:, pr * 128:(pr + 1) * 128], identb),
                        reads=["QB", "identb"], writes=[("ps", 6)])
                for g in range(4):
                    S.op("pe", lambda e, g=g: e.transpose(
                        psb(7)[:, g * 128:(g + 1) * 128], KD[:, g, :, :].rearrange("p a d -> p (a d)"), identb),
                        reads=["KD", "identb"], writes=[("ps", 7)])
                S.op("act", lambda e, tt=tt: e.activation(
                    out=A[:, :, tt * 128:(tt + 1) * 128], in_=psb(6).rearrange("p (k n) -> p k n", k=8), func=AF.Copy),
                    reads=[("ps", 6)], writes=[("A", tt)])
                S.op("dve", lambda e, tt=tt: e.tensor_copy(
                    out=KTd[:, :, tt * 128:(tt + 1) * 128], in_=psb(7)[:, 0:512].rearrange("p (g n) -> p g n", g=4)),
                    reads=[("ps", 7)], writes=["KTd"])

            g_mm(0)
            for tt in range(NT):
                if tt + 1 < NT:
                    g_mm(tt + 1)
                g_chain(tt)
                g_tr(tt)

            S.barrier()
            ar.reset(m1)
            PT = [ar.alloc([128, 512], BF16) for _ in range(4)]
            rc = [ar.alloc([128, 4]) for _ in range(2)]
            KT2 = ar.alloc([128, 4, SEQ], BF16)
            S.op("act", lambda e: e.activation(out=KT2[64:128, 0:2, :], in_=KTd[64:128, 0:2, :], func=AF.Copy),
                 reads=["KTd"], writes=["KT2"])
            S.op("pool", lambda e: e.tensor_copy(out=KT2[64:128, 2:4, :], in_=KTd[64:128, 2:4, :]),
                 reads=["KTd"], writes=["KT2b"])
            S.op("dve", lambda e: e.memset(KT2[0:64, :, :], 0.0), writes=["KT2c"])
            S.op("dve", lambda e: e.memset(KTd[64:128, :, :], 0.0), reads=["KT2", "KT2b"], writes=["KTd"])
            steps = [(h, qg, kt) for h in range(NH) for qg in range(4) for kt in range(NT)]

            def emit_ST(n):
                h, qg, kt = steps[n]
                g = h // 4
                r0 = (h % 2) * 64
                bank = n % 4
                KK = KTd if h % 2 == 0 else KT2
                S.op("pe", lambda e: e.matmul(
                    ps[:, bank, :], lhsT=KK[:, g, kt * 128:(kt + 1) * 128],
                    rhs=A[:, h // 2, qg * 512:(qg + 1) * 512], start=True, stop=True),
                    reads=["KTd", "KT2", "KT2b", "KT2c"] + [("A", t) for t in range(qg * 4, qg * 4 + 4)], writes=[("ps", bank)])

            def emit_rest(n):
                h, qg, kt = steps[n]
                g = h // 4
                bank = n % 4
                pt = PT[n % 4]
                grp = n // NT
                ob = 4 + (grp % 2)
                S.op("act", lambda e: e.activation(out=pt, in_=ps[:, bank, :], func=AF.Exp, scale=0.125),
                     reads=[("ps", bank)], writes=[("PT", n % 4)])
                for qt in range(4):
                    S.op("pe", lambda e, qt=qt: e.matmul(
                        ps[:, ob, qt * 128:qt * 128 + 65], lhsT=pt[:, qt * 128:(qt + 1) * 128],
                        rhs=V[:, kt, g, 0:65], start=(kt == 0), stop=(kt == NT - 1)),
                        reads=[("PT", n % 4), "V"], writes=[("ps", ob)])
                if kt == NT - 1:
                    rb = grp % 2
                    S.op("dve", lambda e: e.reciprocal(
                        out=rc[rb], in_=ps[:, ob, :].rearrange("p (q n) -> p q n", q=4)[:, :, 64]),
                        reads=[("ps", ob)], writes=[("rc", rb)])
                    for qt in range(4):
                        tt = qg * 4 + qt
                        S.op("dve", lambda e, qt=qt, tt=tt: e.tensor_scalar(
                            out=B[:, tt, h * 64:(h + 1) * 64], in0=ps[:, ob, qt * 128:qt * 128 + 64],
                            scalar1=rc[rb][:, qt:qt + 1], scalar2=None, op0=ALU.mult),
                            reads=[("ps", ob), ("rc", rb)], writes=[("B", tt)])

            emit_ST(0)
            emit_ST(1)
            for n in range(len(steps)):
                if n + 2 < len(steps):
                    emit_ST(n + 2)
                emit_rest(n)

        def wo_ln_phase(i, wo_ap):
            ar.reset(R0)
            WO = ar.alloc([128, KC, D], BF16)
            VB = TMPV + [ar.alloc([128, D]) for _ in range(2)]
            wo_v = wo_ap.rearrange("(k p) n -> p k n", p=128)
            for half in range(2):
                S.dma("pool", lambda e, half=half: e.dma_start(
                    out=WO[:, :, half * 512:(half + 1) * 512], in_=wo_v[:, :, half * 512:(half + 1) * 512]),
                    writes=[("WO", half)])
            load_ln_params(i, 0)
            for half in range(2):
                S.op("dve", lambda e, half=half: e.tensor_tensor(
                    out=WO[:, :, half * 512:(half + 1) * 512], in0=WO[:, :, half * 512:(half + 1) * 512],
                    in1=G1B[:, half * 512:(half + 1) * 512].unsqueeze(1).to_broadcast([128, KC, 512]), op=ALU.mult),
                    reads=[("WO", half), ("GB", 2)], writes=[("WO", half)])
            for tt in range(NT):
                bank = tt % 2
                for kc in range(KC):
                    S.op("pe", lambda e, tt=tt, kc=kc, bank=bank: e.transpose(
                        psb(bank)[:, kc * 128:(kc + 1) * 128], B[:, tt, kc * 128:(kc + 1) * 128], identb),
                        reads=[("B", tt), "identb"], writes=[("ps", bank)])
                eng = "act" if tt % 2 == 0 else "dve"
                if eng == "act":
                    S.op("act", lambda e, tt=tt, bank=bank: e.activation(
                        out=A[:, :, tt * 128:(tt + 1) * 128], in_=psb(bank).rearrange("p (k n) -> p k n", k=8),
                        func=AF.Copy), reads=[("ps", bank)], writes=[("A", tt)])
                else:
                    S.op("dve", lambda e, tt=tt, bank=bank: e.tensor_copy(
                        out=A[:, :, tt * 128:(tt + 1) * 128], in_=psb(bank).rearrange("p (k n) -> p k n", k=8)),
                        reads=[("ps", bank)], writes=[("A", tt)])
            def wo_mm(tt):
                banks = [2 + 2 * (tt % 3), 3 + 2 * (tt % 3)]
                for half in range(2):
                    S.op("pe", lambda e, half=half, bank=banks[half]: e.matmul(
                        ps[:, bank, :], lhsT=alphaI, rhs=X[:, tt, half * 512:(half + 1) * 512], start=True, stop=False),
                        reads=["alphaI", ("X", tt)], writes=[("ps", banks[half])])
                    for kc in range(KC):
                        S.op("pe", lambda e, kc=kc, half=half, bank=banks[half]: e.matmul(
                            ps[:, bank, :], lhsT=A[:, kc, tt * 128:(tt + 1) * 128],
                            rhs=WO[:, kc, half * 512:(half + 1) * 512], start=False, stop=(kc == KC - 1)),
                            reads=[("A", tt), ("WO", half)], writes=[("ps", banks[half])])

            def wo_stats(tt):
                banks = [2 + 2 * (tt % 3), 3 + 2 * (tt % 3)]
                ln_stats(tt, ps[:, banks[0]:banks[0] + 2, :].rearrange("p a n -> p (a n)"),
                         [("ps", banks[0]), ("ps", banks[1])], vbufs=VB)

            wo_mm(0)
            wo_mm(1)
            wo_stats(0)
            for tt in range(NT):
                if tt + 2 < NT:
                    wo_mm(tt + 2)
                if tt + 1 < NT:
                    wo_stats(tt + 1)
                ln_finish(tt, store_out=False, vbufs=VB)

        def moe_phase(i, store_out, next_layer=None):
            ar.reset(A0)
            NWB = 3
            WE = [[ar.alloc([128, KC, 512], BF16), ar.alloc([128, KC, 512], BF16), ar.alloc([128, 4, D], BF16)]
                  for _ in range(NWB)]
            m0 = ar.mark()

            def load_expert(e_, which=(0, 1, 2)):
                wb = e_ % NWB
                srcs = (wg_d, wu_d, wd_d)
                for wi in which:
                    S.dma("pool", lambda e, wi=wi: e.dma_start(
                        out=WE[wb][wi], in_=srcs[wi][i, e_].rearrange("(k p) n -> p k n", p=128)),
                        writes=[("WE", wb, wi)])

            load_ln_params(i, 1)
            WR = ar.alloc([128, KC, 36]); RB = ar.alloc([128, 36])
            HT32 = [ar.alloc([128, KC, 128]) for _ in range(2)]
            L = ar.alloc([128, NT, 36])
            gmax = ar.alloc([128, NT]); gsum = ar.alloc([128, NT]); m1_ = ar.alloc([128, NT]); m2_ = ar.alloc([128, NT])
            dd = ar.alloc([128, NT]); e21 = ar.alloc([128, NT])
            gsel = ar.alloc([128, NT, 4]); gex = ar.alloc([128, NT, 4])
            lem = ar.alloc([128, NT, NE]); lem2 = ar.alloc([128, NT, NE])
            OH1 = ar.alloc([128, NT, NE]); OH2 = ar.alloc([128, NT, NE])
            Mall = ar.alloc([128, NT, NE], BF16)
            PEf = ar.alloc([128, NT, NE]); PR = ar.alloc([128, NT, NE])
            D1f = ar.alloc([128, NT]); D2f = ar.alloc([128, NT])
            HB = [ar.alloc([128, D], BF16) for _ in range(4)]
            HF = [ar.alloc([128, D]) for _ in range(2)]
            SHB, SCB = TMPV[0], TMPV[1]
            S.dma("sp", lambda e: e.dma_start(out=SHB, in_=modrow_d[0:1, :].partition_broadcast(128)),
                  reads=[("modrow", 3, 0), ("modrow", 3, 1)], writes=["SHB"])
            S.dma("sp", lambda e: e.dma_start(out=SCB, in_=modrow_d[1:2, :].partition_broadcast(128)),
                  reads=[("modrow", 4, 0), ("modrow", 4, 1)], writes=["SCB"])
            S.dma("sp", lambda e: e.dma_start(out=WR, in_=wr_d[i].rearrange("(k p) n -> p k n", p=128)), writes=["WR"])
            S.dma("sp", lambda e: e.dma_start(out=RB, in_=br_d[i:i + 1, :].partition_broadcast(128)), writes=["RB"])
            load_expert(0)
            load_expert(1)
            load_expert(2)
            def r_T(tt):
                hb = tt % 2
                for half in range(2):
                    bank = (2 * tt + half) % 4
                    for q in range(4):
                        kc = half * 4 + q
                        S.op("pe", lambda e, q=q, kc=kc, bank=bank: e.transpose(
                            ps[:, bank, q * 128:(q + 1) * 128], X[:, tt, kc * 128:(kc + 1) * 128], ident),
                            reads=[("X", tt), "ident"], writes=[("ps", bank)])
                    for q in range(4):
                        kc = half * 4 + q
                        S.op("act", lambda e, q=q, kc=kc, bank=bank: e.activation(
                            out=HT32[hb][:, kc, :], in_=ps[:, bank, q * 128:(q + 1) * 128],
                            func=AF.Identity, bias=SH2[:, kc:kc + 1], scale=SC2[:, kc:kc + 1]),
                            reads=[("ps", bank), ("modfm", 3), ("modfm", 4)], writes=[("HT32", hb)])

            def r_M(tt):
                hb = tt % 2
                rbank = 4 + tt // 8
                c0 = (tt % 8) * 36
                for kc in range(KC):
                    S.op("pe", lambda e, kc=kc: e.matmul(
                        ps[:, rbank, c0:c0 + 36], lhsT=HT32[hb][:, kc, :], rhs=WR[:, kc, :],
                        start=(kc == 0), stop=(kc == KC - 1)),
                        reads=[("HT32", hb), "WR"], writes=[("ps", rbank)])

            r_T(0)
            for tt in range(NT):
                if tt + 1 < NT:
                    r_T(tt + 1)
                r_M(tt)
            k_ = "rt"
            for hf in range(2):
                S.op("dve", lambda e, hf=hf: e.tensor_tensor(
                    out=L[:, hf * 8:(hf + 1) * 8, :], in0=ps[:, 4 + hf, 0:288].rearrange("p (t x) -> p t x", t=8),
                    in1=RB.unsqueeze(1).to_broadcast([128, 8, 36]), op=ALU.add),
                    reads=[("ps", 4 + hf), "RB"], writes=[k_])
            LG = L[:, :, 0:4]
            LE = L[:, :, 4:36]
            S.op("dve", lambda e: e.tensor_reduce(out=gmax, in_=LG, axis=AX.X, op=ALU.max), reads=[k_], writes=[k_])
            S.op("dve", lambda e: e.tensor_tensor(out=gsel, in0=LG, in1=gmax.unsqueeze(2).to_broadcast([128, NT, 4]),
                                                  op=ALU.is_equal), reads=[k_], writes=[k_])
            S.op("dve", lambda e: e.tensor_tensor(out=gex, in0=LG, in1=gmax.unsqueeze(2).to_broadcast([128, NT, 4]),
                                                  op=ALU.subtract), reads=[k_], writes=[k_])
            S.op("act", lambda e: e.activation(out=gex, in_=gex, func=AF.Exp), reads=[k_], writes=[k_])
            S.op("dve", lambda e: e.tensor_reduce(out=gsum, in_=gex, axis=AX.X, op=ALU.add), reads=[k_], writes=[k_])
            S.op("dve", lambda e: e.reciprocal(out=gsum, in_=gsum), reads=[k_], writes=[k_])
            S.op("dve", lambda e: e.tensor_scalar(out=gsel, in0=gsel, scalar1=BIG, scalar2=-BIG, op0=ALU.mult, op1=ALU.add),
                 reads=[k_], writes=[k_])
            for hf in range(2):
                S.op("dve", lambda e, hf=hf: e.tensor_tensor(
                    out=lem[:, hf * 8:(hf + 1) * 8, :].rearrange("p t (g x) -> p (t g) x", g=4),
                    in0=LE[:, hf * 8:(hf + 1) * 8, :].rearrange("p t (g x) -> p t g x", g=4),
                    in1=gsel[:, hf * 8:(hf + 1) * 8, :].unsqueeze(3).to_broadcast([128, 8, 4, 8]), op=ALU.add),
                    reads=[k_], writes=[k_])
            S.op("dve", lambda e: e.tensor_reduce(out=m1_, in_=lem, axis=AX.X, op=ALU.max), reads=[k_], writes=[k_])
            S.op("dve", lambda e: e.tensor_tensor(out=OH1, in0=lem, in1=m1_.unsqueeze(2).to_broadcast([128, NT, NE]),
                                                  op=ALU.is_equal), reads=[k_], writes=["OH"])
            S.op("dve", lambda e: e.scalar_tensor_tensor(
                out=lem2.rearrange("p t x -> p (t x)"), in0=OH1.rearrange("p t x -> p (t x)"), scalar=-BIG,
                in1=lem.rearrange("p t x -> p (t x)"), op0=ALU.mult, op1=ALU.add), reads=[k_, "OH"], writes=[k_])
            S.op("dve", lambda e: e.tensor_reduce(out=m2_, in_=lem2, axis=AX.X, op=ALU.max), reads=[k_], writes=[k_])
            S.op("dve", lambda e: e.tensor_tensor(out=OH2, in0=lem2, in1=m2_.unsqueeze(2).to_broadcast([128, NT, NE]),
                                                  op=ALU.is_equal), reads=[k_], writes=["OH"])
            S.op("dve", lambda e: e.tensor_tensor(out=Mall, in0=OH1, in1=OH2, op=ALU.add), reads=["OH"], writes=["Mall"])
            S.op("dve", lambda e: e.tensor_tensor(out=dd, in0=m2_, in1=m1_, op=ALU.subtract), reads=[k_], writes=[k_])
            S.op("act", lambda e: e.activation(out=e21, in_=dd, func=AF.Exp), reads=[k_], writes=[k_])
            S.op("dve", lambda e: e.tensor_scalar(out=e21, in0=e21, scalar1=1.0, scalar2=None, op0=ALU.add), reads=[k_], writes=[k_])
            S.op("dve", lambda e: e.reciprocal(out=e21, in_=e21), reads=[k_], writes=[k_])
            S.op("dve", lambda e: e.tensor_tensor(out=G1, in0=e21, in1=gsum, op=ALU.mult), reads=[k_], writes=["G"])
            S.op("dve", lambda e: e.tensor_tensor(out=G2, in0=gsum, in1=G1, op=ALU.subtract), reads=[k_, "G"], writes=["G"])
            for tt in range(NT):
                S.op("pe", lambda e, tt=tt: e.matmul(
                    ps[:, 6, tt * NE:(tt + 1) * NE], lhsT=trib, rhs=Mall[:, tt, :], start=True, stop=(tt == 0)),
                    reads=["trib", "Mall"], writes=[("ps", 6)])
                for t2 in range(tt):
                    S.op("pe", lambda e, tt=tt, t2=t2: e.matmul(
                        ps[:, 6, tt * NE:(tt + 1) * NE], lhsT=onesb, rhs=Mall[:, t2, :], start=False, stop=(t2 == tt - 1)),
                        reads=["onesb", "Mall"], writes=[("ps", 6)])
            S.op("dve", lambda e: e.tensor_tensor(
                out=PEf, in0=ps[:, 6, :].rearrange("p (t x) -> p t x", t=NT),
                in1=iotac.unsqueeze(1).to_broadcast([128, NT, NE]), op=ALU.add),
                reads=[("ps", 6), "iotac"], writes=["PEf"])
            for (OH, Df, Di) in ((OH1, D1f, D1i), (OH2, D2f, D2i)):
                S.op("dve", lambda e, OH=OH: e.tensor_tensor(out=PR, in0=OH, in1=PEf, op=ALU.mult),
                     reads=["OH", "PEf"], writes=["PR"])
                S.op("dve", lambda e, Df=Df: e.tensor_reduce(out=Df, in_=PR, axis=AX.X, op=ALU.add),
                     reads=["PR"], writes=["Df"])
                S.op("dve", lambda e, Df=Df, Di=Di: e.tensor_copy(out=Di, in_=Df), reads=["Df"], writes=["Di"])
            for tt in range(NT):
                hb = tt % 4
                hf = tt % 2
                S.op("dve", lambda e, tt=tt, hf=hf: e.tensor_tensor(out=HF[hf], in0=X[:, tt, :], in1=SCB, op=ALU.mult),
                     reads=[("X", tt), "SCB"], writes=[("HF", hf)])
                S.op("dve", lambda e, hb=hb, hf=hf: e.tensor_tensor(out=HB[hb], in0=HF[hf], in1=SHB, op=ALU.add),
                     reads=[("HF", hf), "SHB"], writes=[("HB", hb)])
                for Di in (D1i, D2i):
                    S.dma("pool", lambda e, tt=tt, Di=Di, hb=hb: e.indirect_dma_start(
                        out=xs_d[:, :], out_offset=bass.IndirectOffsetOnAxis(ap=Di[:, tt:tt + 1], axis=0),
                        in_=HB[hb], in_offset=None),
                        reads=[("HB", hb), "Di"], writes=[("XSw", tt, id(Di))])
            S.barrier()
            ar.reset(m0)
            NXG = 6
            NYO = 4
            XG = [ar.alloc([128, D], BF16) for _ in range(NXG)]
            XeT = [ar.alloc([128, KC, CAP], BF16) for _ in range(2)]
            HTb = [ar.alloc([128, 4, CAP], BF16) for _ in range(2)]
            SG = [ar.alloc([128, CAP]) for _ in range(2)]
            YO = [ar.alloc([128, D], BF16) for _ in range(NYO)]

            def prep_load(e_):
                for s_ in range(NS):
                    xb = (e_ * NS + s_) % NXG
                    r0 = e_ * CAP + s_ * 128
                    S.dma("sp", lambda e, xb=xb, r0=r0: e.dma_start(out=XG[xb], in_=xs_d[r0:r0 + 128, :]),
                          writes=[("XG", xb)])

            def prep(e_):
                wb = e_ % 2
                for s_ in range(NS):
                    xb = (e_ * NS + s_) % NXG
                    bank = (e_ * NS + s_) % 2
                    for kc in range(KC):
                        S.op("pe", lambda e, xb=xb, kc=kc, bank=bank: e.transpose(
                            psb(bank)[:, kc * 128:(kc + 1) * 128], XG[xb][:, kc * 128:(kc + 1) * 128], identb),
                            reads=[("XG", xb), "identb"], writes=[("ps", bank)])
                    S.op("act", lambda e, s_=s_, bank=bank, wb=wb: e.activation(
                        out=XeT[wb][:, :, s_ * 128:(s_ + 1) * 128], in_=psb(bank).rearrange("p (k n) -> p k n", k=KC),
                        func=AF.Copy), reads=[("ps", bank)], writes=[("XeT", wb)])

            def compute(e_):
                wb = e_ % 2
                ww = e_ % NWB
                Wg_, Wu_, Wd_ = WE[ww]
                for fc in range(4):
                    gbank = 2 + 2 * (fc % 2)
                    ubank = gbank + 1
                    for (wi, W_, bank) in ((0, Wg_, gbank), (1, Wu_, ubank)):
                        for kc in range(KC):
                            S.op("pe", lambda e, fc=fc, kc=kc, W_=W_, bank=bank: e.matmul(
                                ps[:, bank, 0:CAP], lhsT=W_[:, kc, fc * 128:(fc + 1) * 128], rhs=XeT[wb][:, kc, :],
                                start=(kc == 0), stop=(kc == KC - 1)),
                                reads=[("WE", ww, wi), ("XeT", wb)], writes=[("ps", bank)])
                    S.op("act", lambda e, fc=fc, gbank=gbank: e.activation(out=SG[fc % 2], in_=ps[:, gbank, 0:CAP], func=AF.Silu),
                         reads=[("ps", gbank)], writes=[("SG", fc % 2)])
                    S.op("dve", lambda e, fc=fc, ubank=ubank: e.tensor_tensor(
                        out=HTb[wb][:, fc, :], in0=SG[fc % 2], in1=ps[:, ubank, 0:CAP], op=ALU.mult),
                        reads=[("SG", fc % 2), ("ps", ubank)], writes=[("HTb", wb)])
                if e_ + NWB < NE:
                    load_expert(e_ + NWB, which=(0, 1))
                for s_ in range(NS):
                    yb_ = (e_ * NS + s_) % NYO
                    for half in range(2):
                        bank = 6 + half
                        for fc in range(4):
                            S.op("pe", lambda e, s_=s_, half=half, fc=fc, bank=bank: e.matmul(
                                ps[:, bank, :], lhsT=HTb[wb][:, fc, s_ * 128:(s_ + 1) * 128],
                                rhs=Wd_[:, fc, half * 512:(half + 1) * 512], start=(fc == 0), stop=(fc == 3)),
                                reads=[("HTb", wb), ("WE", ww, 2)], writes=[("ps", bank)])
                        S.op("dve", lambda e, yb_=yb_, bank=bank, half=half: e.tensor_tensor(
                            out=YO[yb_][:, half * 512:(half + 1) * 512], in0=ps[:, bank, :],
                            in1=G2B[:, half * 512:(half + 1) * 512], op=ALU.mult),
                            reads=[("ps", bank), ("GB", 5)], writes=[("YO", yb_)])
                    r0 = e_ * CAP + s_ * 128
                    S.dma("sp", lambda e, yb_=yb_, r0=r0: e.dma_start(out=ys_d[r0:r0 + 128, :], in_=YO[yb_]),
                          reads=[("YO", yb_)], writes=[("YSw", e_, s_)])
                if e_ + NWB < NE:
                    load_expert(e_ + NWB, which=(2,))

            prep_load(0)
            prep_load(1)
            prep(0)
            for e_ in range(NE):
                if e_ + 2 < NE:
                    prep_load(e_ + 2)
                if e_ + 1 < NE:
                    prep(e_ + 1)
                compute(e_)
            S.barrier()
            ar.reset(A0 + KC * SEQ // 2)
            modg = mod_groups(next_layer, R0, (0,), (1,)) if next_layer is not None else None
            NYB = 4
            Y1 = [ar.alloc([128, D], BF16) for _ in range(NYB)]
            Y2 = [ar.alloc([128, D], BF16) for _ in range(NYB)]
            VB = TMPV + [ar.alloc([128, D]) for _ in range(2)]
            def gather(tt):
                yb = tt % NYB
                S.dma("pool", lambda e: e.indirect_dma_start(
                    out=Y1[yb], out_offset=None, in_=ys_d[:, :],
                    in_offset=bass.IndirectOffsetOnAxis(ap=D1i[:, tt:tt + 1], axis=0)),
                    reads=["Di"], writes=[("Y1", yb)])
                S.dma("pool", lambda e: e.indirect_dma_start(
                    out=Y2[yb], out_offset=None, in_=ys_d[:, :],
                    in_offset=bass.IndirectOffsetOnAxis(ap=D2i[:, tt:tt + 1], axis=0)),
                    reads=["Di"], writes=[("Y2", yb)])

            DG = [[ar.alloc([128, 128], BF16) for _ in range(2)] for _ in range(2)]
            for tt in range(NYB):
                gather(tt)

            def build(tt):
                yb = tt % NYB
                db = tt % 2
                for k_, Gk in enumerate((G1, G2)):
                    S.op("dve", lambda e, k_=k_, Gk=Gk: e.tensor_scalar(
                        out=DG[db][k_], in0=ident, scalar1=Gk[:, tt:tt + 1], scalar2=None, op0=ALU.mult),
                        reads=["ident", "G"], writes=[("DG", db, k_)])
                banks = [2 + 2 * (tt % 3), 3 + 2 * (tt % 3)]
                for half in range(2):
                    hs = slice(half * 512, (half + 1) * 512)
                    S.op("pe", lambda e, half=half, hs=hs: e.matmul(
                        ps[:, banks[half], :], lhsT=alphaI, rhs=X[:, tt, hs], start=True, stop=False),
                        reads=["alphaI", ("X", tt)], writes=[("ps", banks[half])])
                    S.op("pe", lambda e, half=half, hs=hs: e.matmul(
                        ps[:, banks[half], :], lhsT=DG[db][0], rhs=Y1[yb][:, hs], start=False, stop=False),
                        reads=[("DG", db, 0), ("Y1", yb)], writes=[("ps", banks[half])])
                    S.op("pe", lambda e, half=half, hs=hs: e.matmul(
                        ps[:, banks[half], :], lhsT=DG[db][1], rhs=Y2[yb][:, hs], start=False, stop=True),
                        reads=[("DG", db, 1), ("Y2", yb)], writes=[("ps", banks[half])])

            def cstats(tt):
                banks = [2 + 2 * (tt % 3), 3 + 2 * (tt % 3)]
                ln_stats(tt, ps[:, banks[0]:banks[0] + 2, :].rearrange("p a n -> p (a n)"),
                         [("ps", banks[0]), ("ps", banks[1])], vbufs=VB)

            build(0)
            build(1)
            cstats(0)
            for tt in range(NT):
                if tt + 2 < NT:
                    build(tt + 2)
                if tt + 1 < NT:
                    cstats(tt + 1)
                ln_finish(tt, store_out=store_out, vbufs=VB)
                if tt + NYB < NT:
                    gather(tt + NYB)
                if modg is not None:
                    if tt < 12:
                        modg(tt)
                    if tt >= 4:
                        hT_tile(tt - 4, (1,), SC1, SH1, 1, 0)
            if modg is not None:
                for k in range(NT - 4, NT):
                    hT_tile(k, (1,), SC1, SH1, 1, 0)

        def dump_dbg(kind):
            if kind == "mod":
                S.dma("sp", lambda e: e.dma_start(out=out_d[0:128, :], in_=G1B), reads=[("GB", 2)], writes=["o0"])
                S.dma("sp", lambda e: e.dma_start(out=out_d[128:256, :], in_=G2B), reads=[("GB", 5)], writes=["o1"])
                for n_, t_ in enumerate((SC1, SH1, SC2, SH2)):
                    S.dma("sp", lambda e, n_=n_, t_=t_: e.dma_start(out=out_d[256:384, n_ * 8:(n_ + 1) * 8], in_=t_),
                          reads=[("modfm", k) for k in (0, 1, 3, 4)], writes=[("o2", n_)])
            elif kind == "hT":
                for kc in range(KC):
                    S.dma("pool", lambda e, kc=kc: e.dma_start(out=out_d[kc * 128:(kc + 1) * 128, :], in_=A[:, kc, 0:1024]),
                          reads=[("A", t) for t in range(NT)], writes=[("o", kc)])
            elif kind == "attn":
                for tt in range(NT):
                    S.dma("pool", lambda e, tt=tt: e.dma_start(out=out_d[tt * 128:(tt + 1) * 128, :], in_=B[:, tt, :]),
                          reads=[("B", tt)], writes=[("o", tt)])

        def dump_x():
            for tt in range(NT):
                S.dma("sp", lambda e, tt=tt: e.dma_start(out=out_d[tt * 128:(tt + 1) * 128, :], in_=X[:, tt, :]),
                      reads=[("X", tt)], writes=[("out", tt)])

        done = False
        premod = False
        for i in range(n_layers):
            S.barrier()
            if not premod:
                mod_phase(i)
                S.barrier()
                if stop == ("mod", i):
                    dump_dbg("mod")
                    done = True
                    break
                build_hT(SC1, SH1, 1, 0)
                if stop == ("hT", i):
                    S.barrier()
                    dump_dbg("hT")
                    done = True
                    break
            l = i // 2
            if i % 2 == 0:
                na_phase(l)
                wo_ap = nawo_d[l]
            else:
                gqa_phase(l)
                wo_ap = gwo_d[l]
            S.barrier()
            if stop == ("attn", i):
                dump_dbg("attn")
                done = True
                break
            wo_ln_phase(i, wo_ap)
            S.barrier()
            if stop == ("mix", i):
                dump_x()
                done = True
                break
            last = (i == n_layers - 1)
            moe_phase(i, store_out=last, next_layer=(None if last else i + 1))
            premod = not last
            if last:
                done = True
        assert done
        S.barrier()
        S.emit()
        S.close()
    return nc


_CACHE = {}


def _prep_inputs(x, c, ada_w, ada_b, ln_g, ln_b, na_w_qkv, na_rpb, na_w_o, gqa_w_qkv, gqa_q_norm,
                 gqa_k_norm, gqa_w_o, moe_w_group, moe_b_group, moe_w_expert, moe_b_expert,
                 moe_w_gate, moe_w_up, moe_w_down):
    f = lambda a: np.ascontiguousarray(np.asarray(a), dtype=np.float32)
    C64, S64 = _rope_tables()
    shared = {
        "ada_w": f(ada_w), "ada_b": f(ada_b), "ln_g": f(ln_g), "ln_b": f(ln_b),
        "na_w_qkv": f(na_w_qkv), "na_w_o": f(na_w_o), "na_bias": _na_bias_table(f(na_rpb)).reshape(-1, NH, 128, NCLS * 5 * 128),
        "gqa_w_qkv": f(gqa_w_qkv), "gqa_w_o": f(gqa_w_o), "gqa_q_norm": f(gqa_q_norm), "gqa_k_norm": f(gqa_k_norm),
        "rope_c": C64, "rope_s": S64,
        "moe_wr": np.ascontiguousarray(np.concatenate([f(moe_w_group), f(moe_w_expert)], axis=-1)),
        "moe_br": np.ascontiguousarray(np.concatenate([f(moe_b_group), f(moe_b_expert)], axis=-1)),
        "moe_w_gate": f(moe_w_gate), "moe_w_up": f(moe_w_up), "moe_w_down": f(moe_w_down),
        "ident": np.eye(128, dtype=np.float32),
        "tri": np.triu(np.ones((128, 128), np.float32), 1),
        "iotac": np.ascontiguousarray(np.broadcast_to((np.arange(NE) * CAP).astype(np.float32), (128, NE))),
    }
    x = f(x)
    c = f(c)
    in_maps = []
    for b in range(8):
        m = dict(shared)
        m["x"] = x[b]
        m["cfm"] = np.ascontiguousarray(c[b].reshape(KC, 128).T)
        in_maps.append(m)
    return in_maps


def kernel(**inputs):
    in_maps = _prep_inputs(**inputs)
    key = "full"
    if key not in _CACHE:
        _CACHE[key] = build_program()
    nc = _CACHE[key]
    res = run_bass_kernel_spmd(nc, in_maps, core_ids=list(range(8)))
    return np.stack([np.asarray(r["out"], dtype=np.float32) for r in res.results], axis=0)
```

```python
import numpy as np
import concourse.bass as bass
import concourse.mybir as mybir
from concourse.bass_utils import run_bass_kernel_spmd

F32 = mybir.dt.float32
BF16 = mybir.dt.bfloat16
I32 = mybir.dt.int32
AF = mybir.ActivationFunctionType
ALU = mybir.AluOpType
AX = mybir.AxisListType

D = 1024
SEQ = 2048
NT = 16
KC = 8
NH = 16
HD = 64
DEPTH = 4
NE = 32
CAP = 384
NS = CAP // 128
ALPHA = float((2 * DEPTH) ** 0.25)
LN_EPS = 1e-5
RMS_EPS = 1e-6
NEG = -30000.0
BIG = 1.0e4
ARENA_WORDS = 53200


class _Rec:
    def __init__(self):
        self.call = None

    def __getattr__(self, name):
        def f(*a, **k):
            self.call = (name, a, k)
            return self
        return f


def _record(fn):
    r = _Rec()
    fn(r)
    assert r.call is not None
    return r.call


class Sched:
    def __init__(self, nc, n_dma_sems=8, same_engine_sync=True):
        self.nc = nc
        self.prog = {e: [] for e in ("pe", "act", "dve", "pool", "sp")}
        self.cnt = {e: 0 for e in self.prog}
        self.sems = {}
        self.waited = {e: {} for e in self.prog}
        self.last_w = {}
        self.readers = {}
        self.same_engine_sync = same_engine_sync
        self.n_dma_sems = n_dma_sems
        self.dma_ring = {q: {"next": 0, "tot": [0] * n_dma_sems} for q in ("sp", "act", "pool")}
        self._ctx = []

    def open(self):
        nc = self.nc
        for e in self.prog:
            cm = nc.semaphore("s_" + e)
            self.sems["s_" + e] = cm.__enter__()
            self._ctx.append(cm)
        for q in self.dma_ring:
            for i in range(self.n_dma_sems):
                nm = f"d_{q}{i}"
                cm = nc.semaphore(nm)
                self.sems[nm] = cm.__enter__()
                self._ctx.append(cm)

    def close(self):
        for cm in reversed(self._ctx):
            cm.__exit__(None, None, None)

    def _need(self, eng, tok, waits):
        if tok is None:
            return
        sem, val, peng = tok
        if peng == eng and (eng == "pe" or not self.same_engine_sync):
            return
        if self.waited[eng].get(sem, 0) >= val:
            return
        if waits.get(sem, 0) < val:
            waits[sem] = val

    def _collect(self, eng, reads, writes):
        waits = {}
        for k in reads:
            self._need(eng, self.last_w.get(k), waits)
        for k in writes:
            self._need(eng, self.last_w.get(k), waits)
            for t in self.readers.get(k, ()):
                self._need(eng, t, waits)
        for s, v in waits.items():
            self.waited[eng][s] = v
        return list(waits.items())

    def _update(self, tok, reads, writes):
        for k in writes:
            self.last_w[k] = tok
            self.readers[k] = []
        for k in reads:
            if k in writes:
                continue
            lst = self.readers.setdefault(k, [])
            lst.append(tok)
            if len(lst) > 48:
                best = {}
                for t in lst:
                    if t[0] not in best or best[t[0]][1] < t[1]:
                        best[t[0]] = t
                self.readers[k] = list(best.values())

    def op(self, eng, fn, reads=(), writes=()):
        waits = self._collect(eng, reads, writes)
        self.cnt[eng] += 1
        tok = ("s_" + eng, self.cnt[eng], eng)
        self.prog[eng].append((waits, _record(fn), ("s_" + eng, 1)))
        self._update(tok, reads, writes)
        return tok

    def dma(self, q, fn, reads=(), writes=()):
        ring = self.dma_ring[q]
        i = ring["next"]
        ring["next"] = (i + 1) % self.n_dma_sems
        sem = f"d_{q}{i}"
        waits = dict(self._collect(q, reads, writes))
        prev = ring["tot"][i]
        if prev and self.waited[q].get(sem, 0) < prev:
            waits[sem] = prev
            self.waited[q][sem] = prev
        ring["tot"][i] += 16
        tok = (sem, ring["tot"][i], "dma_" + q)
        self.prog[q].append((list(waits.items()), _record(fn), (sem, 16)))
        self._update(tok, reads, writes)
        return tok

    def barrier(self):
        targets = {}
        for e, c in self.cnt.items():
            if c:
                targets["s_" + e] = c
        for q, ring in self.dma_ring.items():
            for i, t in enumerate(ring["tot"]):
                if t:
                    targets[f"d_{q}{i}"] = t
        for eng in self.prog:
            waits = []
            for s, v in targets.items():
                if s == "s_" + eng and eng == "pe":
                    continue
                if self.waited[eng].get(s, 0) < v:
                    waits.append((s, v))
                    self.waited[eng][s] = v
            if waits:
                self.prog[eng].append((waits, None, None))

    def emit(self):
        nc = self.nc
        sems = self.sems
        prog = self.prog

        def run(engh, lst):
            for waits, fn, inc in lst:
                for s, v in waits:
                    engh.wait_ge(sems[s], v)
                if fn is not None:
                    name, a, k = fn
                    ins = getattr(engh, name)(*a, **k)
                    ins.then_inc(sems[inc[0]], inc[1])

        with nc.Block() as block:
            @block.sync
            def _(e):
                run(e, prog["sp"])

            @block.tensor
            def _(e):
                run(e, prog["pe"])

            @block.scalar
            def _(e):
                run(e, prog["act"])

            @block.vector
            def _(e):
                run(e, prog["dve"])

            @block.gpsimd
            def _(e):
                run(e, prog["pool"])


class Arena:
    def __init__(self, t, nwords):
        self.t = t
        self.n = nwords
        self.off = 0

    def alloc(self, shape, dt=F32):
        free = 1
        for s in shape[1:]:
            free *= s
        esz = 4 if dt in (F32, I32) else 2
        words = (free * esz + 3) // 4
        words = (words + 7) // 8 * 8
        assert self.off + words <= self.n, ("arena overflow", self.off, words, self.n)
        v = self.t[:, self.off:self.off + words]
        self.off += words
        if dt != F32:
            v = v.bitcast(dt)
        v = v[:, 0:free]
        if len(shape) == 3:
            v = v.rearrange("p (a b) -> p a b", a=shape[1])
        elif len(shape) == 4:
            v = v.rearrange("p (a b c) -> p a b c", a=shape[1], b=shape[2])
        return v

    def mark(self):
        return self.off

    def reset(self, m):
        self.off = m


def _na_patterns():
    pats = []
    for j in range(NT):
        kt_lo = min(max(j - 2, 0), 11)
        qi = np.arange(128)
        r = 2 * j + qi // 64
        c = qi % 64
        rs = np.clip(r - 4, 0, 24)
        ws = np.clip(c - 8, 0, 48)
        tiles = []
        for i in range(5):
            kt = kt_lo + i
            ki = np.arange(128)
            kr = 2 * kt + ki // 64
            kcol = ki % 64
            vr = (kr[:, None] >= rs[None, :]) & (kr[:, None] < rs[None, :] + 8)
            vc = (kcol[:, None] >= ws[None, :]) & (kcol[:, None] < ws[None, :] + 16)
            dr = np.clip(kr[:, None] - r[None, :] + 7, 0, 14)
            dc = np.clip(kcol[:, None] - c[None, :] + 15, 0, 30)
            valid = vr & vc
            tiles.append((valid, np.where(valid, dr, 0), np.where(valid, dc, 0)))
        pats.append(tiles)
    classes = []
    cls_of_j = []
    for j in range(NT):
        key = b"".join(a.tobytes() for t in pats[j] for a in t)
        found = None
        for ci, (k2, _) in enumerate(classes):
            if k2 == key:
                found = ci
                break
        if found is None:
            classes.append((key, pats[j]))
            found = len(classes) - 1
        cls_of_j.append(found)
    return [c[1] for c in classes], cls_of_j


_NA_CLASSES, _NA_CLS_OF_J = _na_patterns()
NCLS = len(_NA_CLASSES)


def _na_bias_table(rpb):
    L = rpb.shape[0]
    out = np.empty((L, NH, 128, NCLS * 5, 128), np.float32)
    for ci, tiles in enumerate(_NA_CLASSES):
        for i, (valid, dr, dc) in enumerate(tiles):
            g = rpb[:, :, dr, dc]
            out[:, :, :, ci * 5 + i, :] = np.where(valid[None, None], g, np.float32(NEG))
    return out


def _rope_tables():
    t = np.arange(SEQ)
    pos = np.stack([t // 64, t % 64], -1).astype(np.float32)
    inv = (np.float32(10000.0) ** (-np.arange(16, dtype=np.float32) / np.float32(16))).astype(np.float32)
    ang = pos[:, :, None] * inv
    c = np.cos(ang).astype(np.float32)
    s = np.sin(ang).astype(np.float32)
    C64 = np.stack([c, c], 2).reshape(SEQ, 64)
    S64 = np.stack([-s, s], 2).reshape(SEQ, 64)
    C64 = np.ascontiguousarray(C64.reshape(NT, 128, 64).transpose(1, 0, 2))
    S64 = np.ascontiguousarray(S64.reshape(NT, 128, 64).transpose(1, 0, 2))
    return C64, S64


def build_program(n_layers=DEPTH, stop=None, lite=False, decl=None):
    nc = bass.Bass("TRN2", target_bir_lowering=False)
    n_moe = n_layers if stop is None else n_layers - 1
    LD = n_layers if lite else DEPTH
    LNA = max(1, (n_layers + 1) // 2) if lite else 2
    LGQ = max(1, n_layers // 2) if lite else 2
    LMOE = max(1, n_moe) if lite else DEPTH
    EMOE = NE if (n_moe > 0 or not lite) else 1

    def din(name, shape, dt=F32):
        if decl is not None:
            decl[name] = tuple(shape)
        return nc.dram_tensor(name, list(shape), dt, kind="ExternalInput").ap()

    x_d = din("x", [SEQ, D])
    c_d = din("cfm", [128, KC])
    adaw_d = din("ada_w", [LD, D, 6 * D])
    adab_d = din("ada_b", [LD, 6 * D])
    lng_d = din("ln_g", [LD, 2, D])
    lnb_d = din("ln_b", [LD, 2, D])
    nawqkv_d = din("na_w_qkv", [LNA, D, 3 * D])
    nawo_d = din("na_w_o", [LNA, D, D])
    nabias_d = din("na_bias", [LNA, NH, 128, NCLS * 5 * 128])
    gwqkv_d = din("gqa_w_qkv", [LGQ, D, 1536])
    gwo_d = din("gqa_w_o", [LGQ, D, D])
    gqn_d = din("gqa_q_norm", [LGQ, HD])
    gkn_d = din("gqa_k_norm", [LGQ, HD])
    ropec_d = din("rope_c", [128, NT, 64])
    ropes_d = din("rope_s", [128, NT, 64])
    wr_d = din("moe_wr", [LMOE, D, 36])
    br_d = din("moe_br", [LMOE, 36])
    wg_d = din("moe_w_gate", [LMOE, EMOE, D, 512])
    wu_d = din("moe_w_up", [LMOE, EMOE, D, 512])
    wd_d = din("moe_w_down", [LMOE, EMOE, 512, D])
    ident_d = din("ident", [128, 128])
    tri_d = din("tri", [128, 128])
    iotac_d = din("iotac", [128, NE])
    out_d = nc.dram_tensor("out", [SEQ, D], F32, kind="ExternalOutput").ap()
    xs_d = nc.dram_tensor("xs_scr", [NE * CAP + 2 * CAP, D], BF16, kind="Internal").ap()
    ys_d = nc.dram_tensor("ys_scr", [NE * CAP + 2 * CAP, D], BF16, kind="Internal").ap()
    modrow_d = nc.dram_tensor("modrow", [2, D], F32, kind="Internal").ap()

    S = Sched(nc)
    with nc.sbuf_tensor("arena", [128, ARENA_WORDS], F32) as arena_t, \
            nc.psum_tensor("ps", [128, 8, 512], F32) as ps:
        ar = Arena(arena_t, ARENA_WORDS)
        S.open()

        def psb(bank):
            return ps[:, bank, :].bitcast(BF16)

        X = ar.alloc([128, NT, D])
        ident = ar.alloc([128, 128])
        identb = ar.alloc([128, 128], BF16)
        onesb = ar.alloc([128, 128], BF16)
        trib = ar.alloc([128, 128], BF16)
        iotac = ar.alloc([128, NE])
        alphaI = ar.alloc([128, 128])
        cact = ar.alloc([128, KC])
        cA = ar.alloc([128, KC, 128], BF16)
        SC1 = ar.alloc([128, KC]); SH1 = ar.alloc([128, KC])
        SC2 = ar.alloc([128, KC]); SH2 = ar.alloc([128, KC])
        G1B = ar.alloc([128, D]); G2B = ar.alloc([128, D])
        LNG = ar.alloc([128, D]); LNB = ar.alloc([128, D])
        TMPV = [ar.alloc([128, D]) for _ in range(2)]
        st_t = [ar.alloc([128, 12]) for _ in range(4)]
        mv_t = [ar.alloc([128, 2]) for _ in range(4)]
        sd_t = [ar.alloc([128, 2]) for _ in range(4)]
        eps_ln = ar.alloc([128, 1]); eps_rms = ar.alloc([128, 1])
        G1 = ar.alloc([128, NT]); G2 = ar.alloc([128, NT])
        D1i = ar.alloc([128, NT], I32); D2i = ar.alloc([128, NT], I32)
        A0 = ar.mark()
        A = ar.alloc([128, KC, SEQ], BF16)
        B = ar.alloc([128, NT, D], BF16)
        R0 = ar.mark()

        S.dma("sp", lambda e: e.dma_start(out=ident, in_=ident_d[:, :]), writes=["ident"])
        S.dma("pool", lambda e: e.dma_start(out=identb, in_=ident_d[:, :]), writes=["identb"])
        S.dma("pool", lambda e: e.dma_start(out=trib, in_=tri_d[:, :]), writes=["trib"])
        S.dma("sp", lambda e: e.dma_start(out=iotac, in_=iotac_d[:, :]), writes=["iotac"])
        S.dma("sp", lambda e: e.dma_start(out=cact, in_=c_d[:, :]), writes=["cact"])
        S.op("dve", lambda e: e.memset(onesb, 1.0), writes=["onesb"])
        S.op("dve", lambda e: e.tensor_scalar(out=alphaI, in0=ident, scalar1=ALPHA, scalar2=None, op0=ALU.mult),
             reads=["ident"], writes=["alphaI"])
        S.op("dve", lambda e: e.memset(eps_ln, LN_EPS), writes=["eps"])
        S.op("dve", lambda e: e.memset(eps_rms, RMS_EPS), writes=["eps"])
        for g4 in range(4):
            S.dma("sp", lambda e, g4=g4: e.dma_start(
                out=X[:, g4 * 4:(g4 + 1) * 4, :],
                in_=x_d[g4 * 512:(g4 + 1) * 512, :].rearrange("(t p) d -> p t d", p=128)),
                writes=[("X", t) for t in range(g4 * 4, g4 * 4 + 4)])
        S.op("act", lambda e: e.activation(out=cact, in_=cact, func=AF.Silu), reads=["cact"], writes=["cact"])
        S.op("dve", lambda e: e.tensor_copy(out=cA, in_=cact.unsqueeze(2).to_broadcast([128, KC, 128])),
             reads=["cact"], writes=["cA"])

        def mod_groups(i, base, mm_banks, tr_banks):
            arm = Arena(arena_t, ARENA_WORDS)
            arm.off = base
            AW = [arm.alloc([128, KC, 512], BF16) for _ in range(2)]
            ABb = [arm.alloc([128, 512]) for _ in range(2)]
            T = [arm.alloc([128, 512]) for _ in range(2)]
            aw_v = adaw_d[i].rearrange("(k p) n -> p k n", p=128)

            def group(cg):
                b = cg % 2
                S.dma("pool", lambda e: e.dma_start(out=AW[b], in_=aw_v[:, :, cg * 512:(cg + 1) * 512]),
                      writes=[("AW", b)])
                S.dma("sp", lambda e: e.dma_start(
                    out=ABb[b], in_=adab_d[i:i + 1, cg * 512:(cg + 1) * 512].partition_broadcast(128)),
                    writes=[("ABb", b)])
                bank = mm_banks[cg % len(mm_banks)]
                for kc in range(KC):
                    S.op("pe", lambda e, kc=kc: e.matmul(
                        ps[:, bank, :], lhsT=cA[:, kc, :], rhs=AW[b][:, kc, :], start=(kc == 0), stop=(kc == KC - 1)),
                        reads=["cA", ("AW", b)], writes=[("ps", bank)])
                kind = cg // 2
                half = cg % 2
                if kind in (2, 5):
                    GB = G1B if kind == 2 else G2B
                    S.op("dve", lambda e: e.scalar_tensor_tensor(
                        out=GB[:, half * 512:(half + 1) * 512], in0=ps[:, bank, :], scalar=1.0, in1=ABb[b],
                        op0=ALU.add, op1=ALU.add),
                        reads=[("ps", bank), ("ABb", b)], writes=[("GB", kind)])
                else:
                    add1 = 1.0 if kind in (1, 4) else 0.0
                    S.op("dve", lambda e: e.scalar_tensor_tensor(
                        out=T[b], in0=ps[:, bank, :], scalar=add1, in1=ABb[b], op0=ALU.add, op1=ALU.add),
                        reads=[("ps", bank), ("ABb", b)], writes=[("T", b)])
                    tb = tr_banks[b % len(tr_banks)]
                    for q in range(4):
                        S.op("pe", lambda e, q=q: e.transpose(
                            ps[:, tb, q * 128:(q + 1) * 128], T[b][:, q * 128:(q + 1) * 128], ident),
                            reads=[("T", b), "ident"], writes=[("ps", tb)])
                    if kind in (3, 4):
                        S.dma("sp", lambda e: e.dma_start(
                            out=modrow_d[kind - 3:kind - 2, half * 512:(half + 1) * 512], in_=T[b][0:1, :]),
                            reads=[("T", b)], writes=[("modrow", kind, half)])
                    dst = {0: SH1, 1: SC1, 3: SH2, 4: SC2}[kind]
                    S.op("dve", lambda e: e.tensor_copy(
                        out=dst[:, half * 4:(half + 1) * 4],
                        in_=ps[:, tb, :].rearrange("p (q n) -> p q n", q=4)[:, :, 0]),
                        reads=[("ps", tb)], writes=[("modfm", kind)])
            return group

        def mod_phase(i):
            g = mod_groups(i, R0, (0, 1), (2, 3))
            for cg in range(12):
                g(cg)

        def hT_tile(tt, banks, SC, SH, kind_sc, kind_sh):
            for half in range(2):
                bank = banks[half % len(banks)]
                for q in range(4):
                    kc = half * 4 + q
                    S.op("pe", lambda e, q=q, kc=kc: e.transpose(
                        ps[:, bank, q * 128:(q + 1) * 128], X[:, tt, kc * 128:(kc + 1) * 128], ident),
                        reads=[("X", tt), "ident"], writes=[("ps", bank)])
                for q in range(4):
                    kc = half * 4 + q
                    S.op("act", lambda e, q=q, kc=kc: e.activation(
                        out=A[:, kc, tt * 128:(tt + 1) * 128], in_=ps[:, bank, q * 128:(q + 1) * 128],
                        func=AF.Identity, bias=SH[:, kc:kc + 1], scale=SC[:, kc:kc + 1]),
                        reads=[("ps", bank), ("modfm", kind_sc), ("modfm", kind_sh)], writes=[("A", tt)])

        def build_hT(SC, SH, kind_sc, kind_sh):
            for tt in range(NT):
                hT_tile(tt, [(2 * tt) % 4, (2 * tt + 1) % 4], SC, SH, kind_sc, kind_sh)

        def ln_stats(tt, v_ps, vkeys, vbufs):
            nb_ = len(vbufs)
            tb = tt % nb_
            v = vbufs[tb]
            sb_ = tt % 4
            for half in range(2):
                S.op("dve", lambda e, half=half: e.bn_stats(
                    out=st_t[sb_][:, half * 6:(half + 1) * 6], in_=v_ps[:, half * 512:(half + 1) * 512]),
                    reads=[vkeys[half]], writes=[("st", sb_)])
            S.op("dve", lambda e: e.bn_aggr(out=mv_t[sb_], in_=st_t[sb_]), reads=[("st", sb_)], writes=[("mv", sb_)])
            S.op("act", lambda e: e.activation(out=sd_t[sb_][:, 0:1], in_=mv_t[sb_][:, 1:2], func=AF.Sqrt, bias=eps_ln, scale=1.0),
                 reads=[("mv", sb_), "eps"], writes=[("sd", sb_)])
            S.op("dve", lambda e: e.reciprocal(out=sd_t[sb_][:, 0:1], in_=sd_t[sb_][:, 0:1]), reads=[("sd", sb_)], writes=[("sd", sb_)])
            S.op("dve", lambda e: e.scalar_tensor_tensor(
                out=sd_t[sb_][:, 1:2], in0=mv_t[sb_][:, 0:1], scalar=-1.0, in1=sd_t[sb_][:, 0:1], op0=ALU.mult, op1=ALU.mult),
                reads=[("mv", sb_), ("sd", sb_)], writes=[("sd2", sb_)])
            S.op("act", lambda e: e.activation(out=v, in_=v_ps, func=AF.Identity, bias=sd_t[sb_][:, 1:2], scale=sd_t[sb_][:, 0:1]),
                 reads=list(vkeys) + [("sd", sb_), ("sd2", sb_)], writes=[("v", tb)])

        def ln_finish(tt, store_out, vbufs):
            tb = tt % len(vbufs)
            v = vbufs[tb]
            S.op("dve", lambda e: e.tensor_tensor(out=v, in0=v, in1=LNG, op=ALU.mult),
                 reads=[("v", tb), "LNG"], writes=[("v", tb)])
            S.op("dve", lambda e: e.tensor_tensor(out=X[:, tt, :], in0=v, in1=LNB, op=ALU.add),
                 reads=[("v", tb), "LNB"], writes=[("X", tt)])
            if store_out:
                S.dma("sp", lambda e: e.dma_start(out=out_d[tt * 128:(tt + 1) * 128, :], in_=X[:, tt, :]),
                      reads=[("X", tt)], writes=[("out", tt)])

        def load_ln_params(i, which):
            S.dma("sp", lambda e: e.dma_start(out=LNG, in_=lng_d[i, which:which + 1, :].partition_broadcast(128)),
                  writes=["LNG"])
            S.dma("sp", lambda e: e.dma_start(out=LNB, in_=lnb_d[i, which:which + 1, :].partition_broadcast(128)),
                  writes=["LNB"])

        def na_phase(l):
            ar.reset(R0)
            Wb = [[ar.alloc([128, KC, 128], BF16) for _ in range(3)] for _ in range(2)]
            NBI = 2
            BI = [ar.alloc([128, NCLS * 5, 128], BF16) for _ in range(NBI)]
            QT = ar.alloc([128, SEQ], BF16)
            KTlo = ar.alloc([128, SEQ], BF16)
            KThi = ar.alloc([128, SEQ], BF16)
            V = ar.alloc([128, NT, 2, 66], BF16)
            PT = [ar.alloc([128, 640], BF16) for _ in range(3)]
            rc = [ar.alloc([128, 1]) for _ in range(2)]
            w_v = nawqkv_d[l].rearrange("(k p) n -> p k n", p=128)
            S.op("dve", lambda e: e.memset(V[:, :, :, 64:66], 1.0), writes=["V"])
            S.op("dve", lambda e: e.memset(KTlo[64:128, :], 0.0), writes=["KTz0"])
            S.op("dve", lambda e: e.memset(KThi[0:64, :], 0.0), writes=["KTz1"])

            def load_w(p):
                wb = p % 2
                for m in range(3):
                    c0 = m * D + p * 128
                    S.dma("pool", lambda e, wb=wb, m=m, c0=c0: e.dma_start(out=Wb[wb][m], in_=w_v[:, :, c0:c0 + 128]),
                          writes=[("W", wb, m)])

            def load_bias(h):
                nchunk = NCLS * 5 // 5
                S.dma("pool", lambda e, h=h: e.dma_start(
                    out=BI[h % NBI].rearrange("p (a b) n -> p a (b n)", a=nchunk),
                    in_=nabias_d[l, h].rearrange("p (a m) -> p a m", a=nchunk)), writes=[("BI", h % NBI)])

            load_w(0)
            load_bias(0)
            load_bias(1)

            def exp_bias(h):
                S.op("act", lambda e: e.activation(
                    out=BI[h % NBI].rearrange("p a n -> p (a n)"), in_=BI[h % NBI].rearrange("p a n -> p (a n)"), func=AF.Exp),
                    reads=[("BI", h % NBI)], writes=[("BI", h % NBI)])

            def emit_ST(n, h, j):
                hh = h % 2
                r0 = hh * 64
                kt_lo = min(max(j - 2, 0), 11)
                sb = (n % 3) * 2
                for i in range(5):
                    bank = sb + i // 4
                    col = (i % 4) * 128
                    KK = KTlo if hh == 0 else KThi
                    S.op("pe", lambda e, bank=bank, col=col, i=i, KK=KK: e.matmul(
                        ps[:, bank, col:col + 128], lhsT=KK[:, (kt_lo + i) * 128:(kt_lo + i + 1) * 128],
                        rhs=QT[:, j * 128:(j + 1) * 128], start=True, stop=True),
                        reads=["KT", "KTz0", "KTz1", "QT"], writes=[("ps", bank)])

            def emit_B(n, h, j):
                c = _NA_CLS_OF_J[j]
                sb = (n % 3) * 2
                pb = n % 3
                S.op("act", lambda e: e.activation(out=PT[pb][:, 0:512], in_=ps[:, sb, :], func=AF.Exp),
                     reads=[("ps", sb)], writes=[("PT", pb)])
                S.op("act", lambda e: e.activation(out=PT[pb][:, 512:640], in_=ps[:, sb + 1, 0:128], func=AF.Exp),
                     reads=[("ps", sb + 1)], writes=[("PT", pb)])
                S.op("dve", lambda e: e.tensor_tensor(
                    out=PT[pb], in0=PT[pb], in1=BI[h % NBI][:, c * 5:(c + 1) * 5, :].rearrange("p a n -> p (a n)"),
                    op=ALU.mult), reads=[("PT", pb), ("BI", h % NBI)], writes=[("PT", pb)])

            def emit_C(n, h, j):
                hh = h % 2
                kt_lo = min(max(j - 2, 0), 11)
                pb = n % 3
                ob = 6 + (n % 2)
                for i in range(5):
                    S.op("pe", lambda e, i=i: e.matmul(
                        ps[:, ob, 0:65], lhsT=PT[pb][:, i * 128:(i + 1) * 128], rhs=V[:, kt_lo + i, hh, 0:65],
                        start=(i == 0), stop=(i == 4)),
                        reads=[("PT", pb), "V"], writes=[("ps", ob)])
                rb_ = n % 2
                S.op("dve", lambda e: e.reciprocal(out=rc[rb_], in_=ps[:, ob, 64:65]),
                     reads=[("ps", ob)], writes=[("rc", rb_)])
                S.op("dve", lambda e: e.tensor_scalar(
                    out=B[:, j, h * 64:(h + 1) * 64], in0=ps[:, ob, 0:64], scalar1=rc[rb_], scalar2=None, op0=ALU.mult),
                    reads=[("ps", ob), ("rc", rb_)], writes=[("B", j)])

            exp_bias(0)
            exp_bias(1)
            for p in range(8):
                wb = p % 2
                if p + 1 < 8:
                    load_w(p + 1)
                for m in range(2):
                    for tg in range(4):
                        bank = 4 + (tg % 2)
                        for kc in range(KC):
                            S.op("pe", lambda e, m=m, tg=tg, kc=kc, bank=bank: e.matmul(
                                ps[:, bank, :], lhsT=Wb[wb][m][:, kc, :], rhs=A[:, kc, tg * 512:(tg + 1) * 512],
                                start=(kc == 0), stop=(kc == KC - 1)),
                                reads=[("W", wb, m)] + [("A", t) for t in range(tg * 4, tg * 4 + 4)],
                                writes=[("ps", bank)])
                        if m == 0:
                            S.op("act", lambda e, tg=tg, bank=bank: e.activation(
                                out=QT[:, tg * 512:(tg + 1) * 512], in_=ps[:, bank, :], func=AF.Copy, scale=0.125),
                                reads=[("ps", bank)], writes=["QT"])
                        else:
                            S.op("dve", lambda e, tg=tg, bank=bank: e.tensor_copy(
                                out=KTlo[0:64, tg * 512:(tg + 1) * 512], in_=ps[0:64, bank, :]),
                                reads=[("ps", bank)], writes=["KT"])
                            S.op("dve", lambda e, tg=tg, bank=bank: e.tensor_copy(
                                out=KThi[64:128, tg * 512:(tg + 1) * 512], in_=ps[64:128, bank, :]),
                                reads=[("ps", bank)], writes=["KT"])
                for tq in range(4):
                    bank = 6 + (tq % 2)
                    for t4 in range(4):
                        tt = tq * 4 + t4
                        for kc in range(KC):
                            S.op("pe", lambda e, tt=tt, t4=t4, kc=kc, bank=bank: e.matmul(
                                ps[:, bank, t4 * 128:(t4 + 1) * 128], lhsT=A[:, kc, tt * 128:(tt + 1) * 128],
                                rhs=Wb[wb][2][:, kc, :], start=(kc == 0), stop=(kc == KC - 1)),
                                reads=[("W", wb, 2), ("A", tt)], writes=[("ps", bank)])
                    S.op("dve", lambda e, tq=tq, bank=bank: e.tensor_copy(
                        out=V[:, tq * 4:(tq + 1) * 4, :, 0:64],
                        in_=ps[:, bank, :].rearrange("p (t h d) -> p t h d", t=4, h=2)),
                        reads=[("ps", bank)], writes=["V"])
                steps = [(2 * p + hh, j) for hh in range(2) for j in range(NT)]
                emit_ST(0, *steps[0])
                emit_ST(1, *steps[1])
                emit_B(0, *steps[0])
                for n, (h, j) in enumerate(steps):
                    if j == 0 and 0 < h and h + 1 < NH:
                        load_bias(h + 1)
                    if j == 10 and 0 < h and h + 1 < NH:
                        exp_bias(h + 1)
                    if n + 2 < len(steps):
                        emit_ST(n + 2, *steps[n + 2])
                    if n + 1 < len(steps):
                        emit_B(n + 1, *steps[n + 1])
                    emit_C(n, h, j)

        def gqa_phase(l):
            ar.reset(R0)
            KTd = ar.alloc([128, 4, SEQ], BF16)
            V = ar.alloc([128, NT, 4, 66], BF16)
            QN = ar.alloc([128, HD]); KN = ar.alloc([128, HD])
            ssq = ar.alloc([128, 20])
            m1 = ar.mark()
            ar_b = Arena(arena_t, ARENA_WORDS)
            ar_b.off = A0 + KC * SEQ // 2
            Wg = [ar_b.alloc([128, KC, 512], BF16) for _ in range(3)]
            RC = ar_b.alloc([128, NT, 64]); RS = ar_b.alloc([128, NT, 64])
            assert ar_b.off <= R0
            SQ = ar.alloc([128, 1280]); QF = ar.alloc([128, 1280]); T1 = ar.alloc([128, 1280]); T2 = ar.alloc([128, 1280])
            QB = ar.alloc([128, 1024], BF16); KD = ar.alloc([128, 4, 2, 64], BF16)
            w_v = gwqkv_d[l].rearrange("(k p) n -> p k n", p=128)
            for cg in range(3):
                S.dma("pool", lambda e, cg=cg: e.dma_start(out=Wg[cg], in_=w_v[:, :, cg * 512:(cg + 1) * 512]),
                      writes=[("Wg", cg)])
            S.dma("sp", lambda e: e.dma_start(out=RC, in_=ropec_d[:, :, :]), writes=["RC"])
            S.dma("sp", lambda e: e.dma_start(out=RS, in_=ropes_d[:, :, :]), writes=["RS"])
            S.dma("sp", lambda e: e.dma_start(out=QN, in_=gqn_d[l:l + 1, :].partition_broadcast(128)), writes=["QN"])
            S.dma("sp", lambda e: e.dma_start(out=KN, in_=gkn_d[l:l + 1, :].partition_broadcast(128)), writes=["KN"])
            S.op("dve", lambda e: e.memset(V[:, :, :, 64:66], 1.0), writes=["V"])

            def hd(ap, nh):
                return ap.rearrange("p (h d) -> p h d", h=nh)

            def g_mm(tt):
                b0 = (tt % 2) * 3
                for cg in range(3):
                    bank = b0 + cg
                    for kc in range(KC):
                        S.op("pe", lambda e, cg=cg, kc=kc, bank=bank, tt=tt: e.matmul(
                            ps[:, bank, :], lhsT=A[:, kc, tt * 128:(tt + 1) * 128], rhs=Wg[cg][:, kc, :],
                            start=(kc == 0), stop=(kc == KC - 1)),
                            reads=[("A", tt), ("Wg", cg)], writes=[("ps", bank)])

            def g_chain(tt):
                b0 = (tt % 2) * 3
                parts = [(ps[:, b0, :], 0, 8, ("ps", b0)), (ps[:, b0 + 1, :], 512, 8, ("ps", b0 + 1)),
                         (ps[:, b0 + 2, 0:256], 1024, 4, ("ps", b0 + 2))]
                for (pap, co, nh, pk) in parts:
                    S.op("act", lambda e, pap=pap, co=co, nh=nh: e.activation(
                        out=SQ[:, co:co + nh * 64], in_=pap, func=AF.Square),
                        reads=[pk], writes=["SQ"])
                S.op("act", lambda e, tt=tt, b0=b0: e.activation(
                    out=V[:, tt, :, 0:64], in_=hd(ps[:, b0 + 2, 256:512], 4), func=AF.Copy),
                    reads=[("ps", b0 + 2)], writes=["V"])
                S.op("dve", lambda e: e.tensor_reduce(out=ssq, in_=hd(SQ, 20), axis=AX.X, op=ALU.add),
                     reads=["SQ"], writes=["ssq"])
                S.op("act", lambda e: e.activation(out=ssq, in_=ssq, func=AF.Sqrt, bias=eps_rms, scale=1.0 / 64),
                     reads=["ssq", "eps"], writes=["ssq"])
                S.op("dve", lambda e: e.reciprocal(out=ssq, in_=ssq), reads=["ssq"], writes=["ssq"])
                h0 = 0
                for (pap, co, nh, pk) in parts:
                    S.op("dve", lambda e, pap=pap, co=co, nh=nh, h0=h0: e.tensor_tensor(
                        out=hd(QF[:, co:co + nh * 64], nh), in0=hd(pap, nh),
                        in1=ssq[:, h0:h0 + nh].unsqueeze(2).to_broadcast([128, nh, 64]), op=ALU.mult),
                        reads=[pk, "ssq"], writes=["QF"])
                    h0 += nh
                S.op("dve", lambda e: e.tensor_tensor(
                    out=hd(QF[:, 0:1024], 16), in0=hd(QF[:, 0:1024], 16),
                    in1=QN.unsqueeze(1).to_broadcast([128, 16, 64]), op=ALU.mult),
                    reads=["QF", "QN"], writes=["QF"])
                S.op("dve", lambda e: e.tensor_tensor(
                    out=hd(QF[:, 1024:1280], 4), in0=hd(QF[:, 1024:1280], 4),
                    in1=KN.unsqueeze(1).to_broadcast([128, 4, 64]), op=ALU.mult),
                    reads=["QF", "KN"], writes=["QF"])
                S.op("dve", lambda e, tt=tt: e.tensor_tensor(
                    out=hd(T1, 20), in0=hd(QF, 20), in1=RC[:, tt, :].unsqueeze(1).to_broadcast([128, 20, 64]),
                    op=ALU.mult), reads=["QF", "RC"], writes=["T1"])

                def v5(ap):
                    return ap.rearrange("p (h a f q) -> p h a f q", h=20, a=2, f=2)

                for f in range(2):
                    S.op("dve", lambda e, tt=tt, f=f: e.tensor_tensor(
                        out=v5(T2)[:, :, :, f, :], in0=v5(QF)[:, :, :, 1 - f, :],
                        in1=RS[:, tt, :].rearrange("p (a f q) -> p a f q", a=2, f=2)[:, :, f, :]
                        .unsqueeze(1).to_broadcast([128, 20, 2, 16]), op=ALU.mult),
                        reads=["QF", "RS"], writes=["T2"])
                S.op("dve", lambda e: e.tensor_tensor(out=QB, in0=T1[:, 0:1024], in1=T2[:, 0:1024], op=ALU.add),
                     reads=["T1", "T2"], writes=["QB"])
                for dup in range(2):
                    S.op("dve", lambda e, dup=dup: e.tensor_tensor(
                        out=KD[:, :, dup, :], in0=hd(T1[:, 1024:1280], 4), in1=hd(T2[:, 1024:1280], 4), op=ALU.add),
                        reads=["T1", "T2"], writes=["KD"])

            def g_tr(tt):
                for pr in range(8):
                    S.op("pe", lambda e, pr=pr: e.transpose(
                        psb(6)[:, pr * 128:(pr + 1) * 128], QB[:, pr * 128:(pr + 1) * 128], identb),
                        reads=["QB", "identb"], writes=[("ps", 6)])
                for g in range(4):
                    S.op("pe", lambda e, g=g: e.transpose(
                        psb(7)[:, g * 128:(g + 1) * 128], KD[:, g, :, :].rearrange("p a d -> p (a d)"), identb),
                        reads=["KD", "identb"], writes=[("ps", 7)])
                S.op("act", lambda e, tt=tt: e.activation(
                    out=A[:, :, tt * 128:(tt + 1) * 128], in_=psb(6).rearrange("p (k n) -> p k n", k=8), func=AF.Copy),
                    reads=[("ps", 6)], writes=[("A", tt)])
                S.op("dve", lambda e, tt=tt: e.tensor_copy(
                    out=KTd[:, :, tt * 128:(tt + 1) * 128], in_=psb(7)[:, 0:512].rearrange("p (g n) -> p g n", g=4)),
                    reads=[("ps", 7)], writes=["KTd"])

            g_mm(0)
            for tt in range(NT):
                if tt + 1 < NT:
                    g_mm(tt + 1)
                g_chain(tt)
                g_tr(tt)

            S.barrier()
            ar.reset(m1)
            PT = [ar.alloc([128, 512], BF16) for _ in range(4)]
            rc = [ar.alloc([128, 4]) for _ in range(2)]
            KT2 = ar.alloc([128, 4, SEQ], BF16)
            S.op("act", lambda e: e.activation(out=KT2[64:128, 0:2, :], in_=KTd[64:128, 0:2, :], func=AF.Copy),
                 reads=["KTd"], writes=["KT2"])
            S.op("pool", lambda e: e.tensor_copy(out=KT2[64:128, 2:4, :], in_=KTd[64:128, 2:4, :]),
                 reads=["KTd"], writes=["KT2b"])
            S.op("dve", lambda e: e.memset(KT2[0:64, :, :], 0.0), writes=["KT2c"])
            S.op("dve", lambda e: e.memset(KTd[64:128, :, :], 0.0), reads=["KT2", "KT2b"], writes=["KTd"])
            steps = [(h, qg, kt) for h in range(NH) for qg in range(4) for kt in range(NT)]

            def emit_ST(n):
                h, qg, kt = steps[n]
                g = h // 4
                r0 = (h % 2) * 64
                bank = n % 4
                KK = KTd if h % 2 == 0 else KT2
                S.op("pe", lambda e: e.matmul(
                    ps[:, bank, :], lhsT=KK[:, g, kt * 128:(kt + 1) * 128],
                    rhs=A[:, h // 2, qg * 512:(qg + 1) * 512], start=True, stop=True),
                    reads=["KTd", "KT2", "KT2b", "KT2c"] + [("A", t) for t in range(qg * 4, qg * 4 + 4)], writes=[("ps", bank)])

            def emit_rest(n):
                h, qg, kt = steps[n]
                g = h // 4
                bank = n % 4
                pt = PT[n % 4]
                grp = n // NT
                ob = 4 + (grp % 2)
                S.op("act", lambda e: e.activation(out=pt, in_=ps[:, bank, :], func=AF.Exp, scale=0.125),
                     reads=[("ps", bank)], writes=[("PT", n % 4)])
                for qt in range(4):
                    S.op("pe", lambda e, qt=qt: e.matmul(
                        ps[:, ob, qt * 128:qt * 128 + 65], lhsT=pt[:, qt * 128:(qt + 1) * 128],
                        rhs=V[:, kt, g, 0:65], start=(kt == 0), stop=(kt == NT - 1)),
                        reads=[("PT", n % 4), "V"], writes=[("ps", ob)])
                if kt == NT - 1:
                    rb = grp % 2
                    S.op("dve", lambda e: e.reciprocal(
                        out=rc[rb], in_=ps[:, ob, :].rearrange("p (q n) -> p q n", q=4)[:, :, 64]),
                        reads=[("ps", ob)], writes=[("rc", rb)])
                    for qt in range(4):
                        tt = qg * 4 + qt
                        S.op("dve", lambda e, qt=qt, tt=tt: e.tensor_scalar(
                            out=B[:, tt, h * 64:(h + 1) * 64], in0=ps[:, ob, qt * 128:qt * 128 + 64],
                            scalar1=rc[rb][:, qt:qt + 1], scalar2=None, op0=ALU.mult),
                            reads=[("ps", ob), ("rc", rb)], writes=[("B", tt)])

            emit_ST(0)
            emit_ST(1)
            for n in range(len(steps)):
                if n + 2 < len(steps):
                    emit_ST(n + 2)
                emit_rest(n)

        def wo_ln_phase(i, wo_ap):
            ar.reset(R0)
            WO = ar.alloc([128, KC, D], BF16)
            VB = TMPV + [ar.alloc([128, D]) for _ in range(2)]
            wo_v = wo_ap.rearrange("(k p) n -> p k n", p=128)
            for half in range(2):
                S.dma("pool", lambda e, half=half: e.dma_start(
                    out=WO[:, :, half * 512:(half + 1) * 512], in_=wo_v[:, :, half * 512:(half + 1) * 512]),
                    writes=[("WO", half)])
            load_ln_params(i, 0)
            for half in range(2):
                S.op("dve", lambda e, half=half: e.tensor_tensor(
                    out=WO[:, :, half * 512:(half + 1) * 512], in0=WO[:, :, half * 512:(half + 1) * 512],
                    in1=G1B[:, half * 512:(half + 1) * 512].unsqueeze(1).to_broadcast([128, KC, 512]), op=ALU.mult),
                    reads=[("WO", half), ("GB", 2)], writes=[("WO", half)])
            for tt in range(NT):
                bank = tt % 2
                for kc in range(KC):
                    S.op("pe", lambda e, tt=tt, kc=kc, bank=bank: e.transpose(
                        psb(bank)[:, kc * 128:(kc + 1) * 128], B[:, tt, kc * 128:(kc + 1) * 128], identb),
                        reads=[("B", tt), "identb"], writes=[("ps", bank)])
                eng = "act" if tt % 2 == 0 else "dve"
                if eng == "act":
                    S.op("act", lambda e, tt=tt, bank=bank: e.activation(
                        out=A[:, :, tt * 128:(tt + 1) * 128], in_=psb(bank).rearrange("p (k n) -> p k n", k=8),
                        func=AF.Copy), reads=[("ps", bank)], writes=[("A", tt)])
                else:
                    S.op("dve", lambda e, tt=tt, bank=bank: e.tensor_copy(
                        out=A[:, :, tt * 128:(tt + 1) * 128], in_=psb(bank).rearrange("p (k n) -> p k n", k=8)),
                        reads=[("ps", bank)], writes=[("A", tt)])
            def wo_mm(tt):
                banks = [2 + 2 * (tt % 3), 3 + 2 * (tt % 3)]
                for half in range(2):
                    S.op("pe", lambda e, half=half, bank=banks[half]: e.matmul(
                        ps[:, bank, :], lhsT=alphaI, rhs=X[:, tt, half * 512:(half + 1) * 512], start=True, stop=False),
                        reads=["alphaI", ("X", tt)], writes=[("ps", banks[half])])
                    for kc in range(KC):
                        S.op("pe", lambda e, kc=kc, half=half, bank=banks[half]: e.matmul(
                            ps[:, bank, :], lhsT=A[:, kc, tt * 128:(tt + 1) * 128],
                            rhs=WO[:, kc, half * 512:(half + 1) * 512], start=False, stop=(kc == KC - 1)),
                            reads=[("A", tt), ("WO", half)], writes=[("ps", banks[half])])

            def wo_stats(tt):
                banks = [2 + 2 * (tt % 3), 3 + 2 * (tt % 3)]
                ln_stats(tt, ps[:, banks[0]:banks[0] + 2, :].rearrange("p a n -> p (a n)"),
                         [("ps", banks[0]), ("ps", banks[1])], vbufs=VB)

            wo_mm(0)
            wo_mm(1)
            wo_stats(0)
            for tt in range(NT):
                if tt + 2 < NT:
                    wo_mm(tt + 2)
                if tt + 1 < NT:
                    wo_stats(tt + 1)
                ln_finish(tt, store_out=False, vbufs=VB)

        def moe_phase(i, store_out, next_layer=None):
            ar.reset(A0)
            NWB = 3
            WE = [[ar.alloc([128, KC, 512], BF16), ar.alloc([128, KC, 512], BF16), ar.alloc([128, 4, D], BF16)]
                  for _ in range(NWB)]
            m0 = ar.mark()

            def load_expert(e_, which=(0, 1, 2)):
                wb = e_ % NWB
                srcs = (wg_d, wu_d, wd_d)
                for wi in which:
                    S.dma("pool", lambda e, wi=wi: e.dma_start(
                        out=WE[wb][wi], in_=srcs[wi][i, e_].rearrange("(k p) n -> p k n", p=128)),
                        writes=[("WE", wb, wi)])

            load_ln_params(i, 1)
            WR = ar.alloc([128, KC, 36]); RB = ar.alloc([128, 36])
            HT32 = [ar.alloc([128, KC, 128]) for _ in range(2)]
            L = ar.alloc([128, NT, 36])
            gmax = ar.alloc([128, NT]); gsum = ar.alloc([128, NT]); m1_ = ar.alloc([128, NT]); m2_ = ar.alloc([128, NT])
            dd = ar.alloc([128, NT]); e21 = ar.alloc([128, NT])
            gsel = ar.alloc([128, NT, 4]); gex = ar.alloc([128, NT, 4])
            lem = ar.alloc([128, NT, NE]); lem2 = ar.alloc([128, NT, NE])
            OH1 = ar.alloc([128, NT, NE]); OH2 = ar.alloc([128, NT, NE])
            Mall = ar.alloc([128, NT, NE], BF16)
            PEf = ar.alloc([128, NT, NE]); PR = ar.alloc([128, NT, NE])
            D1f = ar.alloc([128, NT]); D2f = ar.alloc([128, NT])
            HB = [ar.alloc([128, D], BF16) for _ in range(4)]
            HF = [ar.alloc([128, D]) for _ in range(2)]
            SHB, SCB = TMPV[0], TMPV[1]
            S.dma("sp", lambda e: e.dma_start(out=SHB, in_=modrow_d[0:1, :].partition_broadcast(128)),
                  reads=[("modrow", 3, 0), ("modrow", 3, 1)], writes=["SHB"])
            S.dma("sp", lambda e: e.dma_start(out=SCB, in_=modrow_d[1:2, :].partition_broadcast(128)),
                  reads=[("modrow", 4, 0), ("modrow", 4, 1)], writes=["SCB"])
            S.dma("sp", lambda e: e.dma_start(out=WR, in_=wr_d[i].rearrange("(k p) n -> p k n", p=128)), writes=["WR"])
            S.dma("sp", lambda e: e.dma_start(out=RB, in_=br_d[i:i + 1, :].partition_broadcast(128)), writes=["RB"])
            load_expert(0)
            load_expert(1)
            load_expert(2)
            def r_T(tt):
                hb = tt % 2
                for half in range(2):
                    bank = (2 * tt + half) % 4
                    for q in range(4):
                        kc = half * 4 + q
                        S.op("pe", lambda e, q=q, kc=kc, bank=bank: e.transpose(
                            ps[:, bank, q * 128:(q + 1) * 128], X[:, tt, kc * 128:(kc + 1) * 128], ident),
                            reads=[("X", tt), "ident"], writes=[("ps", bank)])
                    for q in range(4):
                        kc = half * 4 + q
                        S.op("act", lambda e, q=q, kc=kc, bank=bank: e.activation(
                            out=HT32[hb][:, kc, :], in_=ps[:, bank, q * 128:(q + 1) * 128],
                            func=AF.Identity, bias=SH2[:, kc:kc + 1], scale=SC2[:, kc:kc + 1]),
                            reads=[("ps", bank), ("modfm", 3), ("modfm", 4)], writes=[("HT32", hb)])

            def r_M(tt):
                hb = tt % 2
                rbank = 4 + tt // 8
                c0 = (tt % 8) * 36
                for kc in range(KC):
                    S.op("pe", lambda e, kc=kc: e.matmul(
                        ps[:, rbank, c0:c0 + 36], lhsT=HT32[hb][:, kc, :], rhs=WR[:, kc, :],
                        start=(kc == 0), stop=(kc == KC - 1)),
                        reads=[("HT32", hb), "WR"], writes=[("ps", rbank)])

            r_T(0)
            for tt in range(NT):
                if tt + 1 < NT:
                    r_T(tt + 1)
                r_M(tt)
            k_ = "rt"
            for hf in range(2):
                S.op("dve", lambda e, hf=hf: e.tensor_tensor(
                    out=L[:, hf * 8:(hf + 1) * 8, :], in0=ps[:, 4 + hf, 0:288].rearrange("p (t x) -> p t x", t=8),
                    in1=RB.unsqueeze(1).to_broadcast([128, 8, 36]), op=ALU.add),
                    reads=[("ps", 4 + hf), "RB"], writes=[k_])
            LG = L[:, :, 0:4]
            LE = L[:, :, 4:36]
            S.op("dve", lambda e: e.tensor_reduce(out=gmax, in_=LG, axis=AX.X, op=ALU.max), reads=[k_], writes=[k_])
            S.op("dve", lambda e: e.tensor_tensor(out=gsel, in0=LG, in1=gmax.unsqueeze(2).to_broadcast([128, NT, 4]),
                                                  op=ALU.is_equal), reads=[k_], writes=[k_])
            S.op("dve", lambda e: e.tensor_tensor(out=gex, in0=LG, in1=gmax.unsqueeze(2).to_broadcast([128, NT, 4]),
                                                  op=ALU.subtract), reads=[k_], writes=[k_])
            S.op("act", lambda e: e.activation(out=gex, in_=gex, func=AF.Exp), reads=[k_], writes=[k_])
            S.op("dve", lambda e: e.tensor_reduce(out=gsum, in_=gex, axis=AX.X, op=ALU.add), reads=[k_], writes=[k_])
            S.op("dve", lambda e: e.reciprocal(out=gsum, in_=gsum), reads=[k_], writes=[k_])
            S.op("dve", lambda e: e.tensor_scalar(out=gsel, in0=gsel, scalar1=BIG, scalar2=-BIG, op0=ALU.mult, op1=ALU.add),
                 reads=[k_], writes=[k_])
            for hf in range(2):
                S.op("dve", lambda e, hf=hf: e.tensor_tensor(
                    out=lem[:, hf * 8:(hf + 1) * 8, :].rearrange("p t (g x) -> p (t g) x", g=4),
                    in0=LE[:, hf * 8:(hf + 1) * 8, :].rearrange("p t (g x) -> p t g x", g=4),
                    in1=gsel[:, hf * 8:(hf + 1) * 8, :].unsqueeze(3).to_broadcast([128, 8, 4, 8]), op=ALU.add),
                    reads=[k_], writes=[k_])
            S.op("dve", lambda e: e.tensor_reduce(out=m1_, in_=lem, axis=AX.X, op=ALU.max), reads=[k_], writes=[k_])
            S.op("dve", lambda e: e.tensor_tensor(out=OH1, in0=lem, in1=m1_.unsqueeze(2).to_broadcast([128, NT, NE]),
                                                  op=ALU.is_equal), reads=[k_], writes=["OH"])
            S.op("dve", lambda e: e.scalar_tensor_tensor(
                out=lem2.rearrange("p t x -> p (t x)"), in0=OH1.rearrange("p t x -> p (t x)"), scalar=-BIG,
                in1=lem.rearrange("p t x -> p (t x)"), op0=ALU.mult, op1=ALU.add), reads=[k_, "OH"], writes=[k_])
            S.op("dve", lambda e: e.tensor_reduce(out=m2_, in_=lem2, axis=AX.X, op=ALU.max), reads=[k_], writes=[k_])
            S.op("dve", lambda e: e.tensor_tensor(out=OH2, in0=lem2, in1=m2_.unsqueeze(2).to_broadcast([128, NT, NE]),
                                                  op=ALU.is_equal), reads=[k_], writes=["OH"])
            S.op("dve", lambda e: e.tensor_tensor(out=Mall, in0=OH1, in1=OH2, op=ALU.add), reads=["OH"], writes=["Mall"])
            S.op("dve", lambda e: e.tensor_tensor(out=dd, in0=m2_, in1=m1_, op=ALU.subtract), reads=[k_], writes=[k_])
            S.op("act", lambda e: e.activation(out=e21, in_=dd, func=AF.Exp), reads=[k_], writes=[k_])
            S.op("dve", lambda e: e.tensor_scalar(out=e21, in0=e21, scalar1=1.0, scalar2=None, op0=ALU.add), reads=[k_], writes=[k_])
            S.op("dve", lambda e: e.reciprocal(out=e21, in_=e21), reads=[k_], writes=[k_])
            S.op("dve", lambda e: e.tensor_tensor(out=G1, in0=e21, in1=gsum, op=ALU.mult), reads=[k_], writes=["G"])
            S.op("dve", lambda e: e.tensor_tensor(out=G2, in0=gsum, in1=G1, op=ALU.subtract), reads=[k_, "G"], writes=["G"])
            for tt in range(NT):
                S.op("pe", lambda e, tt=tt: e.matmul(
                    ps[:, 6, tt * NE:(tt + 1) * NE], lhsT=trib, rhs=Mall[:, tt, :], start=True, stop=(tt == 0)),
                    reads=["trib", "Mall"], writes=[("ps", 6)])
                for t2 in range(tt):
                    S.op("pe", lambda e, tt=tt, t2=t2: e.matmul(
                        ps[:, 6, tt * NE:(tt + 1) * NE], lhsT=onesb, rhs=Mall[:, t2, :], start=False, stop=(t2 == tt - 1)),
                        reads=["onesb", "Mall"], writes=[("ps", 6)])
            S.op("dve", lambda e: e.tensor_tensor(
                out=PEf, in0=ps[:, 6, :].rearrange("p (t x) -> p t x", t=NT),
                in1=iotac.unsqueeze(1).to_broadcast([128, NT, NE]), op=ALU.add),
                reads=[("ps", 6), "iotac"], writes=["PEf"])
            for (OH, Df, Di) in ((OH1, D1f, D1i), (OH2, D2f, D2i)):
                S.op("dve", lambda e, OH=OH: e.tensor_tensor(out=PR, in0=OH, in1=PEf, op=ALU.mult),
                     reads=["OH", "PEf"], writes=["PR"])
                S.op("dve", lambda e, Df=Df: e.tensor_reduce(out=Df, in_=PR, axis=AX.X, op=ALU.add),
                     reads=["PR"], writes=["Df"])
                S.op("dve", lambda e, Df=Df, Di=Di: e.tensor_copy(out=Di, in_=Df), reads=["Df"], writes=["Di"])
            for tt in range(NT):
                hb = tt % 4
                hf = tt % 2
                S.op("dve", lambda e, tt=tt, hf=hf: e.tensor_tensor(out=HF[hf], in0=X[:, tt, :], in1=SCB, op=ALU.mult),
                     reads=[("X", tt), "SCB"], writes=[("HF", hf)])
                S.op("dve", lambda e, hb=hb, hf=hf: e.tensor_tensor(out=HB[hb], in0=HF[hf], in1=SHB, op=ALU.add),
                     reads=[("HF", hf), "SHB"], writes=[("HB", hb)])
                for Di in (D1i, D2i):
                    S.dma("pool", lambda e, tt=tt, Di=Di, hb=hb: e.indirect_dma_start(
                        out=xs_d[:, :], out_offset=bass.IndirectOffsetOnAxis(ap=Di[:, tt:tt + 1], axis=0),
                        in_=HB[hb], in_offset=None),
                        reads=[("HB", hb), "Di"], writes=[("XSw", tt, id(Di))])
            S.barrier()
            ar.reset(m0)
            NXG = 6
            NYO = 4
            XG = [ar.alloc([128, D], BF16) for _ in range(NXG)]
            XeT = [ar.alloc([128, KC, CAP], BF16) for _ in range(2)]
            HTb = [ar.alloc([128, 4, CAP], BF16) for _ in range(2)]
            SG = [ar.alloc([128, CAP]) for _ in range(2)]
            YO = [ar.alloc([128, D], BF16) for _ in range(NYO)]

            def prep_load(e_):
                for s_ in range(NS):
                    xb = (e_ * NS + s_) % NXG
                    r0 = e_ * CAP + s_ * 128
                    S.dma("sp", lambda e, xb=xb, r0=r0: e.dma_start(out=XG[xb], in_=xs_d[r0:r0 + 128, :]),
                          writes=[("XG", xb)])

            def prep(e_):
                wb = e_ % 2
                for s_ in range(NS):
                    xb = (e_ * NS + s_) % NXG
                    bank = (e_ * NS + s_) % 2
                    for kc in range(KC):
                        S.op("pe", lambda e, xb=xb, kc=kc, bank=bank: e.transpose(
                            psb(bank)[:, kc * 128:(kc + 1) * 128], XG[xb][:, kc * 128:(kc + 1) * 128], identb),
                            reads=[("XG", xb), "identb"], writes=[("ps", bank)])
                    S.op("act", lambda e, s_=s_, bank=bank, wb=wb: e.activation(
                        out=XeT[wb][:, :, s_ * 128:(s_ + 1) * 128], in_=psb(bank).rearrange("p (k n) -> p k n", k=KC),
                        func=AF.Copy), reads=[("ps", bank)], writes=[("XeT", wb)])

            def compute(e_):
                wb = e_ % 2
                ww = e_ % NWB
                Wg_, Wu_, Wd_ = WE[ww]
                for fc in range(4):
                    gbank = 2 + 2 * (fc % 2)
                    ubank = gbank + 1
                    for (wi, W_, bank) in ((0, Wg_, gbank), (1, Wu_, ubank)):
                        for kc in range(KC):
                            S.op("pe", lambda e, fc=fc, kc=kc, W_=W_, bank=bank: e.matmul(
                                ps[:, bank, 0:CAP], lhsT=W_[:, kc, fc * 128:(fc + 1) * 128], rhs=XeT[wb][:, kc, :],
                                start=(kc == 0), stop=(kc == KC - 1)),
                                reads=[("WE", ww, wi), ("XeT", wb)], writes=[("ps", bank)])
                    S.op("act", lambda e, fc=fc, gbank=gbank: e.activation(out=SG[fc % 2], in_=ps[:, gbank, 0:CAP], func=AF.Silu),
                         reads=[("ps", gbank)], writes=[("SG", fc % 2)])
                    S.op("dve", lambda e, fc=fc, ubank=ubank: e.tensor_tensor(
                        out=HTb[wb][:, fc, :], in0=SG[fc % 2], in1=ps[:, ubank, 0:CAP], op=ALU.mult),
                        reads=[("SG", fc % 2), ("ps", ubank)], writes=[("HTb", wb)])
                if e_ + NWB < NE:
                    load_expert(e_ + NWB, which=(0, 1))
                for s_ in range(NS):
                    yb_ = (e_ * NS + s_) % NYO
                    for half in range(2):
                        bank = 6 + half
                        for fc in range(4):
                            S.op("pe", lambda e, s_=s_, half=half, fc=fc, bank=bank: e.matmul(
                                ps[:, bank, :], lhsT=HTb[wb][:, fc, s_ * 128:(s_ + 1) * 128],
                                rhs=Wd_[:, fc, half * 512:(half + 1) * 512], start=(fc == 0), stop=(fc == 3)),
                                reads=[("HTb", wb), ("WE", ww, 2)], writes=[("ps", bank)])
                        S.op("dve", lambda e, yb_=yb_, bank=bank, half=half: e.tensor_tensor(
                            out=YO[yb_][:, half * 512:(half + 1) * 512], in0=ps[:, bank, :],
                            in1=G2B[:, half * 512:(half + 1) * 512], op=ALU.mult),
                            reads=[("ps", bank), ("GB", 5)], writes=[("YO", yb_)])
                    r0 = e_ * CAP + s_ * 128
                    S.dma("sp", lambda e, yb_=yb_, r0=r0: e.dma_start(out=ys_d[r0:r0 + 128, :], in_=YO[yb_]),
                          reads=[("YO", yb_)], writes=[("YSw", e_, s_)])
                if e_ + NWB < NE:
                    load_expert(e_ + NWB, which=(2,))

            prep_load(0)
            prep_load(1)
            prep(0)
            for e_ in range(NE):
                if e_ + 2 < NE:
                    prep_load(e_ + 2)
                if e_ + 1 < NE:
                    prep(e_ + 1)
                compute(e_)
            S.barrier()
            ar.reset(A0 + KC * SEQ // 2)
            modg = mod_groups(next_layer, R0, (0,), (1,)) if next_layer is not None else None
            NYB = 4
            Y1 = [ar.alloc([128, D], BF16) for _ in range(NYB)]
            Y2 = [ar.alloc([128, D], BF16) for _ in range(NYB)]
            VB = TMPV + [ar.alloc([128, D]) for _ in range(2)]
            def gather(tt):
                yb = tt % NYB
                S.dma("pool", lambda e: e.indirect_dma_start(
                    out=Y1[yb], out_offset=None, in_=ys_d[:, :],
                    in_offset=bass.IndirectOffsetOnAxis(ap=D1i[:, tt:tt + 1], axis=0)),
                    reads=["Di"], writes=[("Y1", yb)])
                S.dma("pool", lambda e: e.indirect_dma_start(
                    out=Y2[yb], out_offset=None, in_=ys_d[:, :],
                    in_offset=bass.IndirectOffsetOnAxis(ap=D2i[:, tt:tt + 1], axis=0)),
                    reads=["Di"], writes=[("Y2", yb)])

            DG = [[ar.alloc([128, 128], BF16) for _ in range(2)] for _ in range(2)]
            for tt in range(NYB):
                gather(tt)

            def build(tt):
                yb = tt % NYB
                db = tt % 2
                for k_, Gk in enumerate((G1, G2)):
                    S.op("dve", lambda e, k_=k_, Gk=Gk: e.tensor_scalar(
                        out=DG[db][k_], in0=ident, scalar1=Gk[:, tt:tt + 1], scalar2=None, op0=ALU.mult),
                        reads=["ident", "G"], writes=[("DG", db, k_)])
                banks = [2 + 2 * (tt % 3), 3 + 2 * (tt % 3)]
                for half in range(2):
                    hs = slice(half * 512, (half + 1) * 512)
                    S.op("pe", lambda e, half=half, hs=hs: e.matmul(
                        ps[:, banks[half], :], lhsT=alphaI, rhs=X[:, tt, hs], start=True, stop=False),
                        reads=["alphaI", ("X", tt)], writes=[("ps", banks[half])])
                    S.op("pe", lambda e, half=half, hs=hs: e.matmul(
                        ps[:, banks[half], :], lhsT=DG[db][0], rhs=Y1[yb][:, hs], start=False, stop=False),
                        reads=[("DG", db, 0), ("Y1", yb)], writes=[("ps", banks[half])])
                    S.op("pe", lambda e, half=half, hs=hs: e.matmul(
                        ps[:, banks[half], :], lhsT=DG[db][1], rhs=Y2[yb][:, hs], start=False, stop=True),
                        reads=[("DG", db, 1), ("Y2", yb)], writes=[("ps", banks[half])])

            def cstats(tt):
                banks = [2 + 2 * (tt % 3), 3 + 2 * (tt % 3)]
                ln_stats(tt, ps[:, banks[0]:banks[0] + 2, :].rearrange("p a n -> p (a n)"),
                         [("ps", banks[0]), ("ps", banks[1])], vbufs=VB)

            build(0)
            build(1)
            cstats(0)
            for tt in range(NT):
                if tt + 2 < NT:
                    build(tt + 2)
                if tt + 1 < NT:
                    cstats(tt + 1)
                ln_finish(tt, store_out=store_out, vbufs=VB)
                if tt + NYB < NT:
                    gather(tt + NYB)
                if modg is not None:
                    if tt < 12:
                        modg(tt)
                    if tt >= 4:
                        hT_tile(tt - 4, (1,), SC1, SH1, 1, 0)
            if modg is not None:
                for k in range(NT - 4, NT):
                    hT_tile(k, (1,), SC1, SH1, 1, 0)

        def dump_dbg(kind):
            if kind == "mod":
                S.dma("sp", lambda e: e.dma_start(out=out_d[0:128, :], in_=G1B), reads=[("GB", 2)], writes=["o0"])
                S.dma("sp", lambda e: e.dma_start(out=out_d[128:256, :], in_=G2B), reads=[("GB", 5)], writes=["o1"])
                for n_, t_ in enumerate((SC1, SH1, SC2, SH2)):
                    S.dma("sp", lambda e, n_=n_, t_=t_: e.dma_start(out=out_d[256:384, n_ * 8:(n_ + 1) * 8], in_=t_),
                          reads=[("modfm", k) for k in (0, 1, 3, 4)], writes=[("o2", n_)])
            elif kind == "hT":
                for kc in range(KC):
                    S.dma("pool", lambda e, kc=kc: e.dma_start(out=out_d[kc * 128:(kc + 1) * 128, :], in_=A[:, kc, 0:1024]),
                          reads=[("A", t) for t in range(NT)], writes=[("o", kc)])
            elif kind == "attn":
                for tt in range(NT):
                    S.dma("pool", lambda e, tt=tt: e.dma_start(out=out_d[tt * 128:(tt + 1) * 128, :], in_=B[:, tt, :]),
                          reads=[("B", tt)], writes=[("o", tt)])

        def dump_x():
            for tt in range(NT):
                S.dma("sp", lambda e, tt=tt: e.dma_start(out=out_d[tt * 128:(tt + 1) * 128, :], in_=X[:, tt, :]),
                      reads=[("X", tt)], writes=[("out", tt)])

        done = False
        premod = False
        for i in range(n_layers):
            S.barrier()
            if not premod:
                mod_phase(i)
                S.barrier()
                if stop == ("mod", i):
                    dump_dbg("mod")
                    done = True
                    break
                build_hT(SC1, SH1, 1, 0)
                if stop == ("hT", i):
                    S.barrier()
                    dump_dbg("hT")
                    done = True
                    break
            l = i // 2
            if i % 2 == 0:
                na_phase(l)
                wo_ap = nawo_d[l]
            else:
                gqa_phase(l)
                wo_ap = gwo_d[l]
            S.barrier()
            if stop == ("attn", i):
                dump_dbg("attn")
                done = True
                break
            wo_ln_phase(i, wo_ap)
            S.barrier()
            if stop == ("mix", i):
                dump_x()
                done = True
                break
            last = (i == n_layers - 1)
            moe_phase(i, store_out=last, next_layer=(None if last else i + 1))
            premod = not last
            if last:
                done = True
        assert done
        S.barrier()
        S.emit()
        S.close()
    return nc


_CACHE = {}


def _prep_inputs(x, c, ada_w, ada_b, ln_g, ln_b, na_w_qkv, na_rpb, na_w_o, gqa_w_qkv, gqa_q_norm,
                 gqa_k_norm, gqa_w_o, moe_w_group, moe_b_group, moe_w_expert, moe_b_expert,
                 moe_w_gate, moe_w_up, moe_w_down):
    f = lambda a: np.ascontiguousarray(np.asarray(a), dtype=np.float32)
    C64, S64 = _rope_tables()
    shared = {
        "ada_w": f(ada_w), "ada_b": f(ada_b), "ln_g": f(ln_g), "ln_b": f(ln_b),
        "na_w_qkv": f(na_w_qkv), "na_w_o": f(na_w_o), "na_bias": _na_bias_table(f(na_rpb)).reshape(-1, NH, 128, NCLS * 5 * 128),
        "gqa_w_qkv": f(gqa_w_qkv), "gqa_w_o": f(gqa_w_o), "gqa_q_norm": f(gqa_q_norm), "gqa_k_norm": f(gqa_k_norm),
        "rope_c": C64, "rope_s": S64,
        "moe_wr": np.ascontiguousarray(np.concatenate([f(moe_w_group), f(moe_w_expert)], axis=-1)),
        "moe_br": np.ascontiguousarray(np.concatenate([f(moe_b_group), f(moe_b_expert)], axis=-1)),
        "moe_w_gate": f(moe_w_gate), "moe_w_up": f(moe_w_up), "moe_w_down": f(moe_w_down),
        "ident": np.eye(128, dtype=np.float32),
        "tri": np.triu(np.ones((128, 128), np.float32), 1),
        "iotac": np.ascontiguousarray(np.broadcast_to((np.arange(NE) * CAP).astype(np.float32), (128, NE))),
    }
    x = f(x)
    c = f(c)
    in_maps = []
    for b in range(8):
        m = dict(shared)
        m["x"] = x[b]
        m["cfm"] = np.ascontiguousarray(c[b].reshape(KC, 128).T)
        in_maps.append(m)
    return in_maps


def kernel(**inputs):
    in_maps = _prep_inputs(**inputs)
    key = "full"
    if key not in _CACHE:
        _CACHE[key] = build_program()
    nc = _CACHE[key]
    res = run_bass_kernel_spmd(nc, in_maps, core_ids=list(range(8)))
    return np.stack([np.asarray(r["out"], dtype=np.float32) for r in res.results], axis=0)
```

```python
import numpy as np
import concourse.bass as bass
import concourse.mybir as mybir
from concourse.bass_utils import run_bass_kernel_spmd

F32 = mybir.dt.float32
BF16 = mybir.dt.bfloat16
I32 = mybir.dt.int32
AF = mybir.ActivationFunctionType
ALU = mybir.AluOpType
AX = mybir.AxisListType

D = 1024
SEQ = 2048
NT = 16
KC = 8
NH = 16
HD = 64
DEPTH = 4
NE = 32
CAP = 384
NS = CAP // 128
ALPHA = float((2 * DEPTH) ** 0.25)
LN_EPS = 1e-5
RMS_EPS = 1e-6
NEG = -30000.0
BIG = 1.0e4
ARENA_WORDS = 53200


class _Rec:
    def __init__(self):
        self.call = None

    def __getattr__(self, name):
        def f(*a, **k):
            self.call = (name, a, k)
            return self
        return f


def _record(fn):
    r = _Rec()
    fn(r)
    assert r.call is not None
    return r.call


class Sched:
    def __init__(self, nc, n_dma_sems=8, same_engine_sync=True):
        self.nc = nc
        self.prog = {e: [] for e in ("pe", "act", "dve", "pool", "sp")}
        self.cnt = {e: 0 for e in self.prog}
        self.sems = {}
        self.waited = {e: {} for e in self.prog}
        self.last_w = {}
        self.readers = {}
        self.same_engine_sync = same_engine_sync
        self.n_dma_sems = n_dma_sems
        self.dma_ring = {q: {"next": 0, "tot": [0] * n_dma_sems} for q in ("sp", "act", "pool")}
        self._ctx = []

    def open(self):
        nc = self.nc
        for e in self.prog:
            cm = nc.semaphore("s_" + e)
            self.sems["s_" + e] = cm.__enter__()
            self._ctx.append(cm)
        for q in self.dma_ring:
            for i in range(self.n_dma_sems):
                nm = f"d_{q}{i}"
                cm = nc.semaphore(nm)
                self.sems[nm] = cm.__enter__()
                self._ctx.append(cm)

    def close(self):
        for cm in reversed(self._ctx):
            cm.__exit__(None, None, None)

    def _need(self, eng, tok, waits):
        if tok is None:
            return
        sem, val, peng = tok
        if peng == eng and (eng == "pe" or not self.same_engine_sync):
            return
        if self.waited[eng].get(sem, 0) >= val:
            return
        if waits.get(sem, 0) < val:
            waits[sem] = val

    def _collect(self, eng, reads, writes):
        waits = {}
        for k in reads:
            self._need(eng, self.last_w.get(k), waits)
        for k in writes:
            self._need(eng, self.last_w.get(k), waits)
            for t in self.readers.get(k, ()):
                self._need(eng, t, waits)
        for s, v in waits.items():
            self.waited[eng][s] = v
        return list(waits.items())

    def _update(self, tok, reads, writes):
        for k in writes:
            self.last_w[k] = tok
            self.readers[k] = []
        for k in reads:
            if k in writes:
                continue
            lst = self.readers.setdefault(k, [])
            lst.append(tok)
            if len(lst) > 48:
                best = {}
                for t in lst:
                    if t[0] not in best or best[t[0]][1] < t[1]:
                        best[t[0]] = t
                self.readers[k] = list(best.values())

    def op(self, eng, fn, reads=(), writes=()):
        waits = self._collect(eng, reads, writes)
        self.cnt[eng] += 1
        tok = ("s_" + eng, self.cnt[eng], eng)
        self.prog[eng].append((waits, _record(fn), ("s_" + eng, 1)))
        self._update(tok, reads, writes)
        return tok

    def dma(self, q, fn, reads=(), writes=()):
        ring = self.dma_ring[q]
        i = ring["next"]
        ring["next"] = (i + 1) % self.n_dma_sems
        sem = f"d_{q}{i}"
        waits = dict(self._collect(q, reads, writes))
        prev = ring["tot"][i]
        if prev and self.waited[q].get(sem, 0) < prev:
            waits[sem] = prev
            self.waited[q][sem] = prev
        ring["tot"][i] += 16
        tok = (sem, ring["tot"][i], "dma_" + q)
        self.prog[q].append((list(waits.items()), _record(fn), (sem, 16)))
        self._update(tok, reads, writes)
        return tok

    def barrier(self):
        targets = {}
        for e, c in self.cnt.items():
            if c:
                targets["s_" + e] = c
        for q, ring in self.dma_ring.items():
            for i, t in enumerate(ring["tot"]):
                if t:
                    targets[f"d_{q}{i}"] = t
        for eng in self.prog:
            waits = []
            for s, v in targets.items():
                if s == "s_" + eng and eng == "pe":
                    continue
                if self.waited[eng].get(s, 0) < v:
                    waits.append((s, v))
                    self.waited[eng][s] = v
            if waits:
                self.prog[eng].append((waits, None, None))

    def emit(self):
        nc = self.nc
        sems = self.sems
        prog = self.prog

        def run(engh, lst):
            for waits, fn, inc in lst:
                for s, v in waits:
                    engh.wait_ge(sems[s], v)
                if fn is not None:
                    name, a, k = fn
                    ins = getattr(engh, name)(*a, **k)
                    ins.then_inc(sems[inc[0]], inc[1])

        with nc.Block() as block:
            @block.sync
            def _(e):
                run(e, prog["sp"])

            @block.tensor
            def _(e):
                run(e, prog["pe"])

            @block.scalar
            def _(e):
                run(e, prog["act"])

            @block.vector
            def _(e):
                run(e, prog["dve"])

            @block.gpsimd
            def _(e):
                run(e, prog["pool"])


class Arena:
    def __init__(self, t, nwords):
        self.t = t
        self.n = nwords
        self.off = 0

    def alloc(self, shape, dt=F32):
        free = 1
        for s in shape[1:]:
            free *= s
        esz = 4 if dt in (F32, I32) else 2
        words = (free * esz + 3) // 4
        words = (words + 7) // 8 * 8
        assert self.off + words <= self.n, ("arena overflow", self.off, words, self.n)
        v = self.t[:, self.off:self.off + words]
        self.off += words
        if dt != F32:
            v = v.bitcast(dt)
        v = v[:, 0:free]
        if len(shape) == 3:
            v = v.rearrange("p (a b) -> p a b", a=shape[1])
        elif len(shape) == 4:
            v = v.rearrange("p (a b c) -> p a b c", a=shape[1], b=shape[2])
        return v

    def mark(self):
        return self.off

    def reset(self, m):
        self.off = m


def _na_patterns():
    pats = []
    for j in range(NT):
        kt_lo = min(max(j - 2, 0), 11)
        qi = np.arange(128)
        r = 2 * j + qi // 64
        c = qi % 64
        rs = np.clip(r - 4, 0, 24)
        ws = np.clip(c - 8, 0, 48)
        tiles = []
        for i in range(5):
            kt = kt_lo + i
            ki = np.arange(128)
            kr = 2 * kt + ki // 64
            kcol = ki % 64
            vr = (kr[:, None] >= rs[None, :]) & (kr[:, None] < rs[None, :] + 8)
            vc = (kcol[:, None] >= ws[None, :]) & (kcol[:, None] < ws[None, :] + 16)
            dr = np.clip(kr[:, None] - r[None, :] + 7, 0, 14)
            dc = np.clip(kcol[:, None] - c[None, :] + 15, 0, 30)
            valid = vr & vc
            tiles.append((valid, np.where(valid, dr, 0), np.where(valid, dc, 0)))
        pats.append(tiles)
    classes = []
    cls_of_j = []
    for j in range(NT):
        key = b"".join(a.tobytes() for t in pats[j] for a in t)
        found = None
        for ci, (k2, _) in enumerate(classes):
            if k2 == key:
                found = ci
                break
        if found is None:
            classes.append((key, pats[j]))
            found = len(classes) - 1
        cls_of_j.append(found)
    return [c[1] for c in classes], cls_of_j


_NA_CLASSES, _NA_CLS_OF_J = _na_patterns()
NCLS = len(_NA_CLASSES)


def _na_bias_table(rpb):
    L = rpb.shape[0]
    out = np.empty((L, NH, 128, NCLS * 5, 128), np.float32)
    for ci, tiles in enumerate(_NA_CLASSES):
        for i, (valid, dr, dc) in enumerate(tiles):
            g = rpb[:, :, dr, dc]
            out[:, :, :, ci * 5 + i, :] = np.where(valid[None, None], g, np.float32(NEG))
    return out


def _rope_tables():
    t = np.arange(SEQ)
    pos = np.stack([t // 64, t % 64], -1).astype(np.float32)
    inv = (np.float32(10000.0) ** (-np.arange(16, dtype=np.float32) / np.float32(16))).astype(np.float32)
    ang = pos[:, :, None] * inv
    c = np.cos(ang).astype(np.float32)
    s = np.sin(ang).astype(np.float32)
    C64 = np.stack([c, c], 2).reshape(SEQ, 64)
    S64 = np.stack([-s, s], 2).reshape(SEQ, 64)
    C64 = np.ascontiguousarray(C64.reshape(NT, 128, 64).transpose(1, 0, 2))
    S64 = np.ascontiguousarray(S64.reshape(NT, 128, 64).transpose(1, 0, 2))
    return C64, S64


def build_program(n_layers=DEPTH, stop=None, lite=False, decl=None):
    nc = bass.Bass("TRN2", target_bir_lowering=False)
    n_moe = n_layers if stop is None else n_layers - 1
    LD = n_layers if lite else DEPTH
    LNA = max(1, (n_layers + 1) // 2) if lite else 2
    LGQ = max(1, n_layers // 2) if lite else 2
    LMOE = max(1, n_moe) if lite else DEPTH
    EMOE = NE if (n_moe > 0 or not lite) else 1

    def din(name, shape, dt=F32):
        if decl is not None:
            decl[name] = tuple(shape)
        return nc.dram_tensor(name, list(shape), dt, kind="ExternalInput").ap()

    x_d = din("x", [SEQ, D])
    c_d = din("cfm", [128, KC])
    adaw_d = din("ada_w", [LD, D, 6 * D])
    adab_d = din("ada_b", [LD, 6 * D])
    lng_d = din("ln_g", [LD, 2, D])
    lnb_d = din("ln_b", [LD, 2, D])
    nawqkv_d = din("na_w_qkv", [LNA, D, 3 * D])
    nawo_d = din("na_w_o", [LNA, D, D])
    nabias_d = din("na_bias", [LNA, NH, 128, NCLS * 5 * 128])
    gwqkv_d = din("gqa_w_qkv", [LGQ, D, 1536])
    gwo_d = din("gqa_w_o", [LGQ, D, D])
    gqn_d = din("gqa_q_norm", [LGQ, HD])
    gkn_d = din("gqa_k_norm", [LGQ, HD])
    ropec_d = din("rope_c", [128, NT, 64])
    ropes_d = din("rope_s", [128, NT, 64])
    wr_d = din("moe_wr", [LMOE, D, 36])
    br_d = din("moe_br", [LMOE, 36])
    wg_d = din("moe_w_gate", [LMOE, EMOE, D, 512])
    wu_d = din("moe_w_up", [LMOE, EMOE, D, 512])
    wd_d = din("moe_w_down", [LMOE, EMOE, 512, D])
    ident_d = din("ident", [128, 128])
    tri_d = din("tri", [128, 128])
    iotac_d = din("iotac", [128, NE])
    out_d = nc.dram_tensor("out", [SEQ, D], F32, kind="ExternalOutput").ap()
    xs_d = nc.dram_tensor("xs_scr", [NE * CAP + 2 * CAP, D], BF16, kind="Internal").ap()
    ys_d = nc.dram_tensor("ys_scr", [NE * CAP + 2 * CAP, D], BF16, kind="Internal").ap()
    modrow_d = nc.dram_tensor("modrow", [2, D], F32, kind="Internal").ap()

    S = Sched(nc)
    with nc.sbuf_tensor("arena", [128, ARENA_WORDS], F32) as arena_t, \
            nc.psum_tensor("ps", [128, 8, 512], F32) as ps:
        ar = Arena(arena_t, ARENA_WORDS)
        S.open()

        def psb(bank):
            return ps[:, bank, :].bitcast(BF16)

        X = ar.alloc([128, NT, D])
        ident = ar.alloc([128, 128])
        identb = ar.alloc([128, 128], BF16)
        onesb = ar.alloc([128, 128], BF16)
        trib = ar.alloc([128, 128], BF16)
        iotac = ar.alloc([128, NE])
        alphaI = ar.alloc([128, 128])
        cact = ar.alloc([128, KC])
        cA = ar.alloc([128, KC, 128], BF16)
        SC1 = ar.alloc([128, KC]); SH1 = ar.alloc([128, KC])
        SC2 = ar.alloc([128, KC]); SH2 = ar.alloc([128, KC])
        G1B = ar.alloc([128, D]); G2B = ar.alloc([128, D])
        LNG = ar.alloc([128, D]); LNB = ar.alloc([128, D])
        TMPV = [ar.alloc([128, D]) for _ in range(2)]
        st_t = [ar.alloc([128, 12]) for _ in range(4)]
        mv_t = [ar.alloc([128, 2]) for _ in range(4)]
        sd_t = [ar.alloc([128, 2]) for _ in range(4)]
        eps_ln = ar.alloc([128, 1]); eps_rms = ar.alloc([128, 1])
        G1 = ar.alloc([128, NT]); G2 = ar.alloc([128, NT])
        D1i = ar.alloc([128, NT], I32); D2i = ar.alloc([128, NT], I32)
        A0 = ar.mark()
        A = ar.alloc([128, KC, SEQ], BF16)
        B = ar.alloc([128, NT, D], BF16)
        R0 = ar.mark()

        S.dma("sp", lambda e: e.dma_start(out=ident, in_=ident_d[:, :]), writes=["ident"])
        S.dma("pool", lambda e: e.dma_start(out=identb, in_=ident_d[:, :]), writes=["identb"])
        S.dma("pool", lambda e: e.dma_start(out=trib, in_=tri_d[:, :]), writes=["trib"])
        S.dma("sp", lambda e: e.dma_start(out=iotac, in_=iotac_d[:, :]), writes=["iotac"])
        S.dma("sp", lambda e: e.dma_start(out=cact, in_=c_d[:, :]), writes=["cact"])
        S.op("dve", lambda e: e.memset(onesb, 1.0), writes=["onesb"])
        S.op("dve", lambda e: e.tensor_scalar(out=alphaI, in0=ident, scalar1=ALPHA, scalar2=None, op0=ALU.mult),
             reads=["ident"], writes=["alphaI"])
        S.op("dve", lambda e: e.memset(eps_ln, LN_EPS), writes=["eps"])
        S.op("dve", lambda e: e.memset(eps_rms, RMS_EPS), writes=["eps"])
        for g4 in range(4):
            S.dma("sp", lambda e, g4=g4: e.dma_start(
                out=X[:, g4 * 4:(g4 + 1) * 4, :],
                in_=x_d[g4 * 512:(g4 + 1) * 512, :].rearrange("(t p) d -> p t d", p=128)),
                writes=[("X", t) for t in range(g4 * 4, g4 * 4 + 4)])
        S.op("act", lambda e: e.activation(out=cact, in_=cact, func=AF.Silu), reads=["cact"], writes=["cact"])
        S.op("dve", lambda e: e.tensor_copy(out=cA, in_=cact.unsqueeze(2).to_broadcast([128, KC, 128])),
             reads=["cact"], writes=["cA"])

        def mod_groups(i, base, mm_banks, tr_banks):
            arm = Arena(arena_t, ARENA_WORDS)
            arm.off = base
            AW = [arm.alloc([128, KC, 512], BF16) for _ in range(3)]
            ABb = [arm.alloc([128, 512]) for _ in range(2)]
            T = [arm.alloc([128, 512]) for _ in range(2)]
            aw_v = adaw_d[i].rearrange("(k p) n -> p k n", p=128)

            def group(cg):
                b = cg % 2
                a3 = cg % 3
                S.dma("pool", lambda e: e.dma_start(out=AW[a3], in_=aw_v[:, :, cg * 512:(cg + 1) * 512]),
                      writes=[("AW", a3)])
                S.dma("sp", lambda e: e.dma_start(
                    out=ABb[b], in_=adab_d[i:i + 1, cg * 512:(cg + 1) * 512].partition_broadcast(128)),
                    writes=[("ABb", b)])
                bank = mm_banks[cg % len(mm_banks)]
                for kc in range(KC):
                    S.op("pe", lambda e, kc=kc: e.matmul(
                        ps[:, bank, :], lhsT=cA[:, kc, :], rhs=AW[a3][:, kc, :], start=(kc == 0), stop=(kc == KC - 1)),
                        reads=["cA", ("AW", a3)], writes=[("ps", bank)])
                kind = cg // 2
                half = cg % 2
                if kind in (2, 5):
                    GB = G1B if kind == 2 else G2B
                    S.op("dve", lambda e: e.scalar_tensor_tensor(
                        out=GB[:, half * 512:(half + 1) * 512], in0=ps[:, bank, :], scalar=1.0, in1=ABb[b],
                        op0=ALU.add, op1=ALU.add),
                        reads=[("ps", bank), ("ABb", b)], writes=[("GB", kind)])
                else:
                    add1 = 1.0 if kind in (1, 4) else 0.0
                    S.op("dve", lambda e: e.scalar_tensor_tensor(
                        out=T[b], in0=ps[:, bank, :], scalar=add1, in1=ABb[b], op0=ALU.add, op1=ALU.add),
                        reads=[("ps", bank), ("ABb", b)], writes=[("T", b)])
                    tb = tr_banks[b % len(tr_banks)]
                    for q in range(4):
                        S.op("pe", lambda e, q=q: e.transpose(
                            ps[:, tb, q * 128:(q + 1) * 128], T[b][:, q * 128:(q + 1) * 128], ident),
                            reads=[("T", b), "ident"], writes=[("ps", tb)])
                    if kind in (3, 4):
                        S.dma("sp", lambda e: e.dma_start(
                            out=modrow_d[kind - 3:kind - 2, half * 512:(half + 1) * 512], in_=T[b][0:1, :]),
                            reads=[("T", b)], writes=[("modrow", kind, half)])
                    dst = {0: SH1, 1: SC1, 3: SH2, 4: SC2}[kind]
                    S.op("dve", lambda e: e.tensor_copy(
                        out=dst[:, half * 4:(half + 1) * 4],
                        in_=ps[:, tb, :].rearrange("p (q n) -> p q n", q=4)[:, :, 0]),
                        reads=[("ps", tb)], writes=[("modfm", kind)])
            return group

        def mod_phase(i):
            g = mod_groups(i, R0, (0, 1), (2, 3))
            for cg in range(12):
                g(cg)

        def hT_tile(tt, banks, SC, SH, kind_sc, kind_sh):
            for half in range(2):
                bank = banks[half % len(banks)]
                for q in range(4):
                    kc = half * 4 + q
                    S.op("pe", lambda e, q=q, kc=kc: e.transpose(
                        ps[:, bank, q * 128:(q + 1) * 128], X[:, tt, kc * 128:(kc + 1) * 128], ident),
                        reads=[("X", tt), "ident"], writes=[("ps", bank)])
                for q in range(4):
                    kc = half * 4 + q
                    S.op("act", lambda e, q=q, kc=kc: e.activation(
                        out=A[:, kc, tt * 128:(tt + 1) * 128], in_=ps[:, bank, q * 128:(q + 1) * 128],
                        func=AF.Identity, bias=SH[:, kc:kc + 1], scale=SC[:, kc:kc + 1]),
                        reads=[("ps", bank), ("modfm", kind_sc), ("modfm", kind_sh)], writes=[("A", tt)])

        def build_hT(SC, SH, kind_sc, kind_sh):
            for tt in range(NT):
                hT_tile(tt, [(2 * tt) % 4, (2 * tt + 1) % 4], SC, SH, kind_sc, kind_sh)

        def ln_stats(tt, v_ps, vkeys, vbufs):
            nb_ = len(vbufs)
            tb = tt % nb_
            v = vbufs[tb]
            sb_ = tt % 4
            for half in range(2):
                S.op("dve", lambda e, half=half: e.bn_stats(
                    out=st_t[sb_][:, half * 6:(half + 1) * 6], in_=v_ps[:, half * 512:(half + 1) * 512]),
                    reads=[vkeys[half]], writes=[("st", sb_)])
            S.op("dve", lambda e: e.bn_aggr(out=mv_t[sb_], in_=st_t[sb_]), reads=[("st", sb_)], writes=[("mv", sb_)])
            S.op("act", lambda e: e.activation(out=sd_t[sb_][:, 0:1], in_=mv_t[sb_][:, 1:2], func=AF.Sqrt, bias=eps_ln, scale=1.0),
                 reads=[("mv", sb_), "eps"], writes=[("sd", sb_)])
            S.op("dve", lambda e: e.reciprocal(out=sd_t[sb_][:, 0:1], in_=sd_t[sb_][:, 0:1]), reads=[("sd", sb_)], writes=[("sd", sb_)])
            S.op("dve", lambda e: e.scalar_tensor_tensor(
                out=sd_t[sb_][:, 1:2], in0=mv_t[sb_][:, 0:1], scalar=-1.0, in1=sd_t[sb_][:, 0:1], op0=ALU.mult, op1=ALU.mult),
                reads=[("mv", sb_), ("sd", sb_)], writes=[("sd2", sb_)])
            S.op("act", lambda e: e.activation(out=v, in_=v_ps, func=AF.Identity, bias=sd_t[sb_][:, 1:2], scale=sd_t[sb_][:, 0:1]),
                 reads=list(vkeys) + [("sd", sb_), ("sd2", sb_)], writes=[("v", tb)])

        def ln_finish(tt, store_out, vbufs):
            tb = tt % len(vbufs)
            v = vbufs[tb]
            S.op("dve", lambda e: e.tensor_tensor(out=v, in0=v, in1=LNG, op=ALU.mult),
                 reads=[("v", tb), "LNG"], writes=[("v", tb)])
            S.op("dve", lambda e: e.tensor_tensor(out=X[:, tt, :], in0=v, in1=LNB, op=ALU.add),
                 reads=[("v", tb), "LNB"], writes=[("X", tt)])
            if store_out:
                S.dma("sp", lambda e: e.dma_start(out=out_d[tt * 128:(tt + 1) * 128, :], in_=X[:, tt, :]),
                      reads=[("X", tt)], writes=[("out", tt)])

        def load_ln_params(i, which):
            S.dma("sp", lambda e: e.dma_start(out=LNG, in_=lng_d[i, which:which + 1, :].partition_broadcast(128)),
                  writes=["LNG"])
            S.dma("sp", lambda e: e.dma_start(out=LNB, in_=lnb_d[i, which:which + 1, :].partition_broadcast(128)),
                  writes=["LNB"])

        def na_phase(l):
            ar.reset(R0)
            Wb = [[ar.alloc([128, KC, 128], BF16) for _ in range(3)] for _ in range(2)]
            NBI = 2
            BI = [ar.alloc([128, NCLS * 5, 128], BF16) for _ in range(NBI)]
            QT = ar.alloc([128, SEQ], BF16)
            KTlo = ar.alloc([128, SEQ], BF16)
            KThi = ar.alloc([128, SEQ], BF16)
            V = ar.alloc([128, NT, 2, 66], BF16)
            PT = [ar.alloc([128, 640], BF16) for _ in range(3)]
            rc = [ar.alloc([128, 1]) for _ in range(2)]
            w_v = nawqkv_d[l].rearrange("(k p) n -> p k n", p=128)
            S.op("dve", lambda e: e.memset(V[:, :, :, 64:66], 1.0), writes=["V"])
            S.op("dve", lambda e: e.memset(KTlo[64:128, :], 0.0), writes=["KTz0"])
            S.op("dve", lambda e: e.memset(KThi[0:64, :], 0.0), writes=["KTz1"])

            def load_w(p):
                wb = p % 2
                for m in range(3):
                    c0 = m * D + p * 128
                    S.dma("pool", lambda e, wb=wb, m=m, c0=c0: e.dma_start(out=Wb[wb][m], in_=w_v[:, :, c0:c0 + 128]),
                          writes=[("W", wb, m)])

            def load_bias(h):
                nchunk = NCLS * 5 // 5
                S.dma("pool", lambda e, h=h: e.dma_start(
                    out=BI[h % NBI].rearrange("p (a b) n -> p a (b n)", a=nchunk),
                    in_=nabias_d[l, h].rearrange("p (a m) -> p a m", a=nchunk)), writes=[("BI", h % NBI)])

            load_w(0)
            load_bias(0)
            load_bias(1)

            def exp_bias(h):
                S.op("act", lambda e: e.activation(
                    out=BI[h % NBI].rearrange("p a n -> p (a n)"), in_=BI[h % NBI].rearrange("p a n -> p (a n)"), func=AF.Exp),
                    reads=[("BI", h % NBI)], writes=[("BI", h % NBI)])

            def emit_ST(n, h, j):
                hh = h % 2
                r0 = hh * 64
                kt_lo = min(max(j - 2, 0), 11)
                sb = (n % 3) * 2
                for i in range(5):
                    bank = sb + i // 4
                    col = (i % 4) * 128
                    KK = KTlo if hh == 0 else KThi
                    S.op("pe", lambda e, bank=bank, col=col, i=i, KK=KK: e.matmul(
                        ps[:, bank, col:col + 128], lhsT=KK[:, (kt_lo + i) * 128:(kt_lo + i + 1) * 128],
                        rhs=QT[:, j * 128:(j + 1) * 128], start=True, stop=True),
                        reads=["KT", "KTz0", "KTz1", "QT"], writes=[("ps", bank)])

            def emit_B(n, h, j):
                c = _NA_CLS_OF_J[j]
                sb = (n % 3) * 2
                pb = n % 3
                S.op("act", lambda e: e.activation(out=PT[pb][:, 0:512], in_=ps[:, sb, :], func=AF.Exp),
                     reads=[("ps", sb)], writes=[("PT", pb)])
                S.op("act", lambda e: e.activation(out=PT[pb][:, 512:640], in_=ps[:, sb + 1, 0:128], func=AF.Exp),
                     reads=[("ps", sb + 1)], writes=[("PT", pb)])
                S.op("dve", lambda e: e.tensor_tensor(
                    out=PT[pb], in0=PT[pb], in1=BI[h % NBI][:, c * 5:(c + 1) * 5, :].rearrange("p a n -> p (a n)"),
                    op=ALU.mult), reads=[("PT", pb), ("BI", h % NBI)], writes=[("PT", pb)])

            def emit_C(n, h, j):
                hh = h % 2
                kt_lo = min(max(j - 2, 0), 11)
                pb = n % 3
                ob = 6 + (n % 2)
                for i in range(5):
                    S.op("pe", lambda e, i=i: e.matmul(
                        ps[:, ob, 0:65], lhsT=PT[pb][:, i * 128:(i + 1) * 128], rhs=V[:, kt_lo + i, hh, 0:65],
                        start=(i == 0), stop=(i == 4)),
                        reads=[("PT", pb), "V"], writes=[("ps", ob)])
                rb_ = n % 2
                S.op("dve", lambda e: e.reciprocal(out=rc[rb_], in_=ps[:, ob, 64:65]),
                     reads=[("ps", ob)], writes=[("rc", rb_)])
                S.op("dve", lambda e: e.tensor_scalar(
                    out=B[:, j, h * 64:(h + 1) * 64], in0=ps[:, ob, 0:64], scalar1=rc[rb_], scalar2=None, op0=ALU.mult),
                    reads=[("ps", ob), ("rc", rb_)], writes=[("B", j)])

            exp_bias(0)
            exp_bias(1)
            for p in range(8):
                wb = p % 2
                if p + 1 < 8:
                    load_w(p + 1)
                for m in range(2):
                    for tg in range(4):
                        bank = 4 + (tg % 2)
                        for kc in range(KC):
                            S.op("pe", lambda e, m=m, tg=tg, kc=kc, bank=bank: e.matmul(
                                ps[:, bank, :], lhsT=Wb[wb][m][:, kc, :], rhs=A[:, kc, tg * 512:(tg + 1) * 512],
                                start=(kc == 0), stop=(kc == KC - 1)),
                                reads=[("W", wb, m)] + [("A", t) for t in range(tg * 4, tg * 4 + 4)],
                                writes=[("ps", bank)])
                        if m == 0:
                            S.op("act", lambda e, tg=tg, bank=bank: e.activation(
                                out=QT[:, tg * 512:(tg + 1) * 512], in_=ps[:, bank, :], func=AF.Copy, scale=0.125),
                                reads=[("ps", bank)], writes=["QT"])
                        else:
                            S.op("dve", lambda e, tg=tg, bank=bank: e.tensor_copy(
                                out=KTlo[0:64, tg * 512:(tg + 1) * 512], in_=ps[0:64, bank, :]),
                                reads=[("ps", bank)], writes=["KT"])
                            S.op("dve", lambda e, tg=tg, bank=bank: e.tensor_copy(
                                out=KThi[64:128, tg * 512:(tg + 1) * 512], in_=ps[64:128, bank, :]),
                                reads=[("ps", bank)], writes=["KT"])
                for tq in range(4):
                    bank = 6 + (tq % 2)
                    for t4 in range(4):
                        tt = tq * 4 + t4
                        for kc in range(KC):
                            S.op("pe", lambda e, tt=tt, t4=t4, kc=kc, bank=bank: e.matmul(
                                ps[:, bank, t4 * 128:(t4 + 1) * 128], lhsT=A[:, kc, tt * 128:(tt + 1) * 128],
                                rhs=Wb[wb][2][:, kc, :], start=(kc == 0), stop=(kc == KC - 1)),
                                reads=[("W", wb, 2), ("A", tt)], writes=[("ps", bank)])
                    S.op("dve", lambda e, tq=tq, bank=bank: e.tensor_copy(
                        out=V[:, tq * 4:(tq + 1) * 4, :, 0:64],
                        in_=ps[:, bank, :].rearrange("p (t h d) -> p t h d", t=4, h=2)),
                        reads=[("ps", bank)], writes=["V"])
                steps = [(2 * p + hh, j) for hh in range(2) for j in range(NT)]
                emit_ST(0, *steps[0])
                emit_ST(1, *steps[1])
                emit_B(0, *steps[0])
                for n, (h, j) in enumerate(steps):
                    if j == 0 and 0 < h and h + 1 < NH:
                        load_bias(h + 1)
                    if j == 10 and 0 < h and h + 1 < NH:
                        exp_bias(h + 1)
                    if n + 2 < len(steps):
                        emit_ST(n + 2, *steps[n + 2])
                    if n + 1 < len(steps):
                        emit_B(n + 1, *steps[n + 1])
                    emit_C(n, h, j)

        def gqa_phase(l):
            ar.reset(R0)
            KTd = ar.alloc([128, 4, SEQ], BF16)
            V = ar.alloc([128, NT, 4, 66], BF16)
            QN = ar.alloc([128, HD]); KN = ar.alloc([128, HD])
            ssq = ar.alloc([128, 20])
            m1 = ar.mark()
            ar_b = Arena(arena_t, ARENA_WORDS)
            ar_b.off = A0 + KC * SEQ // 2
            Wg = [ar_b.alloc([128, KC, 512], BF16) for _ in range(3)]
            RC = ar_b.alloc([128, NT, 64]); RS = ar_b.alloc([128, NT, 64])
            assert ar_b.off <= R0
            SQ = ar.alloc([128, 1280]); QF = ar.alloc([128, 1280]); T1 = ar.alloc([128, 1280]); T2 = ar.alloc([128, 1280])
            QB = ar.alloc([128, 1024], BF16); KD = ar.alloc([128, 4, 2, 64], BF16)
            w_v = gwqkv_d[l].rearrange("(k p) n -> p k n", p=128)
            for cg in range(3):
                S.dma("pool", lambda e, cg=cg: e.dma_start(out=Wg[cg], in_=w_v[:, :, cg * 512:(cg + 1) * 512]),
                      writes=[("Wg", cg)])
            S.dma("sp", lambda e: e.dma_start(out=RC, in_=ropec_d[:, :, :]), writes=["RC"])
            S.dma("sp", lambda e: e.dma_start(out=RS, in_=ropes_d[:, :, :]), writes=["RS"])
            S.dma("sp", lambda e: e.dma_start(out=QN, in_=gqn_d[l:l + 1, :].partition_broadcast(128)), writes=["QN"])
            S.dma("sp", lambda e: e.dma_start(out=KN, in_=gkn_d[l:l + 1, :].partition_broadcast(128)), writes=["KN"])
            S.op("dve", lambda e: e.memset(V[:, :, :, 64:66], 1.0), writes=["V"])

            def hd(ap, nh):
                return ap.rearrange("p (h d) -> p h d", h=nh)

            def g_mm(tt):
                b0 = (tt % 2) * 3
                for cg in range(3):
                    bank = b0 + cg
                    for kc in range(KC):
                        S.op("pe", lambda e, cg=cg, kc=kc, bank=bank, tt=tt: e.matmul(
                            ps[:, bank, :], lhsT=A[:, kc, tt * 128:(tt + 1) * 128], rhs=Wg[cg][:, kc, :],
                            start=(kc == 0), stop=(kc == KC - 1)),
                            reads=[("A", tt), ("Wg", cg)], writes=[("ps", bank)])

            def g_chain(tt):
                b0 = (tt % 2) * 3
                parts = [(ps[:, b0, :], 0, 8, ("ps", b0)), (ps[:, b0 + 1, :], 512, 8, ("ps", b0 + 1)),
                         (ps[:, b0 + 2, 0:256], 1024, 4, ("ps", b0 + 2))]
                for (pap, co, nh, pk) in parts:
                    S.op("act", lambda e, pap=pap, co=co, nh=nh: e.activation(
                        out=SQ[:, co:co + nh * 64], in_=pap, func=AF.Square),
                        reads=[pk], writes=["SQ"])
                S.op("act", lambda e, tt=tt, b0=b0: e.activation(
                    out=V[:, tt, :, 0:64], in_=hd(ps[:, b0 + 2, 256:512], 4), func=AF.Copy),
                    reads=[("ps", b0 + 2)], writes=["V"])
                S.op("dve", lambda e: e.tensor_reduce(out=ssq, in_=hd(SQ, 20), axis=AX.X, op=ALU.add),
                     reads=["SQ"], writes=["ssq"])
                S.op("act", lambda e: e.activation(out=ssq, in_=ssq, func=AF.Sqrt, bias=eps_rms, scale=1.0 / 64),
                     reads=["ssq", "eps"], writes=["ssq"])
                S.op("dve", lambda e: e.reciprocal(out=ssq, in_=ssq), reads=["ssq"], writes=["ssq"])
                h0 = 0
                for (pap, co, nh, pk) in parts:
                    S.op("dve", lambda e, pap=pap, co=co, nh=nh, h0=h0: e.tensor_tensor(
                        out=hd(QF[:, co:co + nh * 64], nh), in0=hd(pap, nh),
                        in1=ssq[:, h0:h0 + nh].unsqueeze(2).to_broadcast([128, nh, 64]), op=ALU.mult),
                        reads=[pk, "ssq"], writes=["QF"])
                    h0 += nh
                S.op("dve", lambda e: e.tensor_tensor(
                    out=hd(QF[:, 0:1024], 16), in0=hd(QF[:, 0:1024], 16),
                    in1=QN.unsqueeze(1).to_broadcast([128, 16, 64]), op=ALU.mult),
                    reads=["QF", "QN"], writes=["QF"])
                S.op("dve", lambda e: e.tensor_tensor(
                    out=hd(QF[:, 1024:1280], 4), in0=hd(QF[:, 1024:1280], 4),
                    in1=KN.unsqueeze(1).to_broadcast([128, 4, 64]), op=ALU.mult),
                    reads=["QF", "KN"], writes=["QF"])
                S.op("dve", lambda e, tt=tt: e.tensor_tensor(
                    out=hd(T1, 20), in0=hd(QF, 20), in1=RC[:, tt, :].unsqueeze(1).to_broadcast([128, 20, 64]),
                    op=ALU.mult), reads=["QF", "RC"], writes=["T1"])

                def v5(ap):
                    return ap.rearrange("p (h a f q) -> p h a f q", h=20, a=2, f=2)

                for f in range(2):
                    S.op("dve", lambda e, tt=tt, f=f: e.tensor_tensor(
                        out=v5(T2)[:, :, :, f, :], in0=v5(QF)[:, :, :, 1 - f, :],
                        in1=RS[:, tt, :].rearrange("p (a f q) -> p a f q", a=2, f=2)[:, :, f, :]
                        .unsqueeze(1).to_broadcast([128, 20, 2, 16]), op=ALU.mult),
                        reads=["QF", "RS"], writes=["T2"])
                S.op("dve", lambda e: e.tensor_tensor(out=QB, in0=T1[:, 0:1024], in1=T2[:, 0:1024], op=ALU.add),
                     reads=["T1", "T2"], writes=["QB"])
                for dup in range(2):
                    S.op("dve", lambda e, dup=dup: e.tensor_tensor(
                        out=KD[:, :, dup, :], in0=hd(T1[:, 1024:1280], 4), in1=hd(T2[:, 1024:1280], 4), op=ALU.add),
                        reads=["T1", "T2"], writes=["KD"])

            def g_tr(tt):
                for pr in range(8):
                    S.op("pe", lambda e, pr=pr: e.transpose(
                        psb(6)[:, pr * 128:(pr + 1) * 128], QB[:, pr * 128:(pr + 1) * 128], identb),
                        reads=["QB", "identb"], writes=[("ps", 6)])
                for g in range(4):
                    S.op("pe", lambda e, g=g: e.transpose(
                        psb(7)[:, g * 128:(g + 1) * 128], KD[:, g, :, :].rearrange("p a d -> p (a d)"), identb),
                        reads=["KD", "identb"], writes=[("ps", 7)])
                S.op("act", lambda e, tt=tt: e.activation(
                    out=A[:, :, tt * 128:(tt + 1) * 128], in_=psb(6).rearrange("p (k n) -> p k n", k=8), func=AF.Copy),
                    reads=[("ps", 6)], writes=[("A", tt)])
                S.op("dve", lambda e, tt=tt: e.tensor_copy(
                    out=KTd[:, :, tt * 128:(tt + 1) * 128], in_=psb(7)[:, 0:512].rearrange("p (g n) -> p g n", g=4)),
                    reads=[("ps", 7)], writes=["KTd"])

            g_mm(0)
            for tt in range(NT):
                if tt + 1 < NT:
                    g_mm(tt + 1)
                g_chain(tt)
                g_tr(tt)

            S.barrier()
            ar.reset(m1)
            PT = [ar.alloc([128, 512], BF16) for _ in range(4)]
            rc = [ar.alloc([128, 4]) for _ in range(2)]
            KT2 = ar.alloc([128, 4, SEQ], BF16)
            S.op("act", lambda e: e.activation(out=KT2[64:128, 0:2, :], in_=KTd[64:128, 0:2, :], func=AF.Copy),
                 reads=["KTd"], writes=["KT2"])
            S.op("pool", lambda e: e.tensor_copy(out=KT2[64:128, 2:4, :], in_=KTd[64:128, 2:4, :]),
                 reads=["KTd"], writes=["KT2b"])
            S.op("dve", lambda e: e.memset(KT2[0:64, :, :], 0.0), writes=["KT2c"])
            S.op("dve", lambda e: e.memset(KTd[64:128, :, :], 0.0), reads=["KT2", "KT2b"], writes=["KTd"])
            steps = [(h, qg, kt) for h in range(NH) for qg in range(4) for kt in range(NT)]

            def emit_ST(n):
                h, qg, kt = steps[n]
                g = h // 4
                r0 = (h % 2) * 64
                bank = n % 4
                KK = KTd if h % 2 == 0 else KT2
                S.op("pe", lambda e: e.matmul(
                    ps[:, bank, :], lhsT=KK[:, g, kt * 128:(kt + 1) * 128],
                    rhs=A[:, h // 2, qg * 512:(qg + 1) * 512], start=True, stop=True),
                    reads=["KTd", "KT2", "KT2b", "KT2c"] + [("A", t) for t in range(qg * 4, qg * 4 + 4)], writes=[("ps", bank)])

            def emit_rest(n):
                h, qg, kt = steps[n]
                g = h // 4
                bank = n % 4
                pt = PT[n % 4]
                grp = n // NT
                ob = 4 + (grp % 2)
                S.op("act", lambda e: e.activation(out=pt, in_=ps[:, bank, :], func=AF.Exp, scale=0.125),
                     reads=[("ps", bank)], writes=[("PT", n % 4)])
                for qt in range(4):
                    S.op("pe", lambda e, qt=qt: e.matmul(
                        ps[:, ob, qt * 128:qt * 128 + 65], lhsT=pt[:, qt * 128:(qt + 1) * 128],
                        rhs=V[:, kt, g, 0:65], start=(kt == 0), stop=(kt == NT - 1)),
                        reads=[("PT", n % 4), "V"], writes=[("ps", ob)])
                if kt == NT - 1:
                    rb = grp % 2
                    S.op("dve", lambda e: e.reciprocal(
                        out=rc[rb], in_=ps[:, ob, :].rearrange("p (q n) -> p q n", q=4)[:, :, 64]),
                        reads=[("ps", ob)], writes=[("rc", rb)])
                    for qt in range(4):
                        tt = qg * 4 + qt
                        S.op("dve", lambda e, qt=qt, tt=tt: e.tensor_scalar(
                            out=B[:, tt, h * 64:(h + 1) * 64], in0=ps[:, ob, qt * 128:qt * 128 + 64],
                            scalar1=rc[rb][:, qt:qt + 1], scalar2=None, op0=ALU.mult),
                            reads=[("ps", ob), ("rc", rb)], writes=[("B", tt)])

            emit_ST(0)
            emit_ST(1)
            for n in range(len(steps)):
                if n + 2 < len(steps):
                    emit_ST(n + 2)
                emit_rest(n)

        def wo_ln_phase(i, wo_ap):
            ar.reset(R0)
            WO = ar.alloc([128, KC, D], BF16)
            VB = TMPV + [ar.alloc([128, D]) for _ in range(2)]
            wo_v = wo_ap.rearrange("(k p) n -> p k n", p=128)
            for half in range(2):
                S.dma("pool", lambda e, half=half: e.dma_start(
                    out=WO[:, :, half * 512:(half + 1) * 512], in_=wo_v[:, :, half * 512:(half + 1) * 512]),
                    writes=[("WO", half)])
            load_ln_params(i, 0)
            for half in range(2):
                S.op("dve", lambda e, half=half: e.tensor_tensor(
                    out=WO[:, :, half * 512:(half + 1) * 512], in0=WO[:, :, half * 512:(half + 1) * 512],
                    in1=G1B[:, half * 512:(half + 1) * 512].unsqueeze(1).to_broadcast([128, KC, 512]), op=ALU.mult),
                    reads=[("WO", half), ("GB", 2)], writes=[("WO", half)])
            for tt in range(NT):
                bank = tt % 2
                for kc in range(KC):
                    S.op("pe", lambda e, tt=tt, kc=kc, bank=bank: e.transpose(
                        psb(bank)[:, kc * 128:(kc + 1) * 128], B[:, tt, kc * 128:(kc + 1) * 128], identb),
                        reads=[("B", tt), "identb"], writes=[("ps", bank)])
                eng = "act" if tt % 2 == 0 else "dve"
                if eng == "act":
                    S.op("act", lambda e, tt=tt, bank=bank: e.activation(
                        out=A[:, :, tt * 128:(tt + 1) * 128], in_=psb(bank).rearrange("p (k n) -> p k n", k=8),
                        func=AF.Copy), reads=[("ps", bank)], writes=[("A", tt)])
                else:
                    S.op("dve", lambda e, tt=tt, bank=bank: e.tensor_copy(
                        out=A[:, :, tt * 128:(tt + 1) * 128], in_=psb(bank).rearrange("p (k n) -> p k n", k=8)),
                        reads=[("ps", bank)], writes=[("A", tt)])
            def wo_mm(tt):
                banks = [2 + 2 * (tt % 3), 3 + 2 * (tt % 3)]
                for half in range(2):
                    S.op("pe", lambda e, half=half, bank=banks[half]: e.matmul(
                        ps[:, bank, :], lhsT=alphaI, rhs=X[:, tt, half * 512:(half + 1) * 512], start=True, stop=False),
                        reads=["alphaI", ("X", tt)], writes=[("ps", banks[half])])
                    for kc in range(KC):
                        S.op("pe", lambda e, kc=kc, half=half, bank=banks[half]: e.matmul(
                            ps[:, bank, :], lhsT=A[:, kc, tt * 128:(tt + 1) * 128],
                            rhs=WO[:, kc, half * 512:(half + 1) * 512], start=False, stop=(kc == KC - 1)),
                            reads=[("A", tt), ("WO", half)], writes=[("ps", banks[half])])

            def wo_stats(tt):
                banks = [2 + 2 * (tt % 3), 3 + 2 * (tt % 3)]
                ln_stats(tt, ps[:, banks[0]:banks[0] + 2, :].rearrange("p a n -> p (a n)"),
                         [("ps", banks[0]), ("ps", banks[1])], vbufs=VB)

            wo_mm(0)
            wo_mm(1)
            wo_stats(0)
            for tt in range(NT):
                if tt + 2 < NT:
                    wo_mm(tt + 2)
                if tt + 1 < NT:
                    wo_stats(tt + 1)
                ln_finish(tt, store_out=False, vbufs=VB)

        def moe_phase(i, store_out, next_layer=None):
            ar.reset(A0)
            NWB = 3
            WE = [[ar.alloc([128, KC, 512], BF16), ar.alloc([128, KC, 512], BF16), ar.alloc([128, 4, D], BF16)]
                  for _ in range(NWB)]
            m0 = ar.mark()

            def load_expert(e_, which=(0, 1, 2)):
                wb = e_ % NWB
                srcs = (wg_d, wu_d, wd_d)
                for wi in which:
                    S.dma("pool", lambda e, wi=wi: e.dma_start(
                        out=WE[wb][wi], in_=srcs[wi][i, e_].rearrange("(k p) n -> p k n", p=128)),
                        writes=[("WE", wb, wi)])

            load_ln_params(i, 1)
            WR = ar.alloc([128, KC, 36]); RB = ar.alloc([128, 36])
            HT32 = [ar.alloc([128, KC, 128]) for _ in range(2)]
            L = ar.alloc([128, NT, 36])
            gmax = ar.alloc([128, NT]); gsum = ar.alloc([128, NT]); m1_ = ar.alloc([128, NT]); m2_ = ar.alloc([128, NT])
            dd = ar.alloc([128, NT]); e21 = ar.alloc([128, NT])
            gsel = ar.alloc([128, NT, 4]); gex = ar.alloc([128, NT, 4])
            lem = ar.alloc([128, NT, NE]); lem2 = ar.alloc([128, NT, NE])
            OH1 = ar.alloc([128, NT, NE]); OH2 = ar.alloc([128, NT, NE])
            Mall = ar.alloc([128, NT, NE], BF16)
            PEf = ar.alloc([128, NT, NE]); PR = ar.alloc([128, NT, NE])
            D1f = ar.alloc([128, NT]); D2f = ar.alloc([128, NT])
            HB = [ar.alloc([128, D], BF16) for _ in range(4)]
            HF = [ar.alloc([128, D]) for _ in range(2)]
            SHB, SCB = TMPV[0], TMPV[1]
            S.dma("sp", lambda e: e.dma_start(out=SHB, in_=modrow_d[0:1, :].partition_broadcast(128)),
                  reads=[("modrow", 3, 0), ("modrow", 3, 1)], writes=["SHB"])
            S.dma("sp", lambda e: e.dma_start(out=SCB, in_=modrow_d[1:2, :].partition_broadcast(128)),
                  reads=[("modrow", 4, 0), ("modrow", 4, 1)], writes=["SCB"])
            S.dma("sp", lambda e: e.dma_start(out=WR, in_=wr_d[i].rearrange("(k p) n -> p k n", p=128)), writes=["WR"])
            S.dma("sp", lambda e: e.dma_start(out=RB, in_=br_d[i:i + 1, :].partition_broadcast(128)), writes=["RB"])
            load_expert(0)
            load_expert(1)
            load_expert(2)
            def r_T(tt):
                hb = tt % 2
                for half in range(2):
                    bank = (2 * tt + half) % 4
                    for q in range(4):
                        kc = half * 4 + q
                        S.op("pe", lambda e, q=q, kc=kc, bank=bank: e.transpose(
                            ps[:, bank, q * 128:(q + 1) * 128], X[:, tt, kc * 128:(kc + 1) * 128], ident),
                            reads=[("X", tt), "ident"], writes=[("ps", bank)])
                    for q in range(4):
                        kc = half * 4 + q
                        S.op("act", lambda e, q=q, kc=kc, bank=bank: e.activation(
                            out=HT32[hb][:, kc, :], in_=ps[:, bank, q * 128:(q + 1) * 128],
                            func=AF.Identity, bias=SH2[:, kc:kc + 1], scale=SC2[:, kc:kc + 1]),
                            reads=[("ps", bank), ("modfm", 3), ("modfm", 4)], writes=[("HT32", hb)])

            def r_M(tt):
                hb = tt % 2
                rbank = 4 + tt // 8
                c0 = (tt % 8) * 36
                for kc in range(KC):
                    S.op("pe", lambda e, kc=kc: e.matmul(
                        ps[:, rbank, c0:c0 + 36], lhsT=HT32[hb][:, kc, :], rhs=WR[:, kc, :],
                        start=(kc == 0), stop=(kc == KC - 1)),
                        reads=[("HT32", hb), "WR"], writes=[("ps", rbank)])

            r_T(0)
            for tt in range(NT):
                if tt + 1 < NT:
                    r_T(tt + 1)
                r_M(tt)
            k_ = "rt"
            for hf in range(2):
                S.op("dve", lambda e, hf=hf: e.tensor_tensor(
                    out=L[:, hf * 8:(hf + 1) * 8, :], in0=ps[:, 4 + hf, 0:288].rearrange("p (t x) -> p t x", t=8),
                    in1=RB.unsqueeze(1).to_broadcast([128, 8, 36]), op=ALU.add),
                    reads=[("ps", 4 + hf), "RB"], writes=[k_])
            LG = L[:, :, 0:4]
            LE = L[:, :, 4:36]
            S.op("dve", lambda e: e.tensor_reduce(out=gmax, in_=LG, axis=AX.X, op=ALU.max), reads=[k_], writes=[k_])
            S.op("dve", lambda e: e.tensor_tensor(out=gsel, in0=LG, in1=gmax.unsqueeze(2).to_broadcast([128, NT, 4]),
                                                  op=ALU.is_equal), reads=[k_], writes=[k_])
            S.op("dve", lambda e: e.tensor_tensor(out=gex, in0=LG, in1=gmax.unsqueeze(2).to_broadcast([128, NT, 4]),
                                                  op=ALU.subtract), reads=[k_], writes=[k_])
            S.op("act", lambda e: e.activation(out=gex, in_=gex, func=AF.Exp), reads=[k_], writes=[k_])
            S.op("dve", lambda e: e.tensor_reduce(out=gsum, in_=gex, axis=AX.X, op=ALU.add), reads=[k_], writes=[k_])
            S.op("dve", lambda e: e.reciprocal(out=gsum, in_=gsum), reads=[k_], writes=[k_])
            S.op("dve", lambda e: e.tensor_scalar(out=gsel, in0=gsel, scalar1=BIG, scalar2=-BIG, op0=ALU.mult, op1=ALU.add),
                 reads=[k_], writes=[k_])
            for hf in range(2):
                S.op("dve", lambda e, hf=hf: e.tensor_tensor(
                    out=lem[:, hf * 8:(hf + 1) * 8, :].rearrange("p t (g x) -> p (t g) x", g=4),
                    in0=LE[:, hf * 8:(hf + 1) * 8, :].rearrange("p t (g x) -> p t g x", g=4),
                    in1=gsel[:, hf * 8:(hf + 1) * 8, :].unsqueeze(3).to_broadcast([128, 8, 4, 8]), op=ALU.add),
                    reads=[k_], writes=[k_])
            S.op("dve", lambda e: e.tensor_reduce(out=m1_, in_=lem, axis=AX.X, op=ALU.max), reads=[k_], writes=[k_])
            S.op("dve", lambda e: e.tensor_tensor(out=OH1, in0=lem, in1=m1_.unsqueeze(2).to_broadcast([128, NT, NE]),
                                                  op=ALU.is_equal), reads=[k_], writes=["OH"])
            S.op("dve", lambda e: e.scalar_tensor_tensor(
                out=lem2.rearrange("p t x -> p (t x)"), in0=OH1.rearrange("p t x -> p (t x)"), scalar=-BIG,
                in1=lem.rearrange("p t x -> p (t x)"), op0=ALU.mult, op1=ALU.add), reads=[k_, "OH"], writes=[k_])
            S.op("dve", lambda e: e.tensor_reduce(out=m2_, in_=lem2, axis=AX.X, op=ALU.max), reads=[k_], writes=[k_])
            S.op("dve", lambda e: e.tensor_tensor(out=OH2, in0=lem2, in1=m2_.unsqueeze(2).to_broadcast([128, NT, NE]),
                                                  op=ALU.is_equal), reads=[k_], writes=["OH"])
            S.op("dve", lambda e: e.tensor_tensor(out=Mall, in0=OH1, in1=OH2, op=ALU.add), reads=["OH"], writes=["Mall"])
            S.op("dve", lambda e: e.tensor_tensor(out=dd, in0=m2_, in1=m1_, op=ALU.subtract), reads=[k_], writes=[k_])
            S.op("act", lambda e: e.activation(out=e21, in_=dd, func=AF.Exp), reads=[k_], writes=[k_])
            S.op("dve", lambda e: e.tensor_scalar(out=e21, in0=e21, scalar1=1.0, scalar2=None, op0=ALU.add), reads=[k_], writes=[k_])
            S.op("dve", lambda e: e.reciprocal(out=e21, in_=e21), reads=[k_], writes=[k_])
            S.op("dve", lambda e: e.tensor_tensor(out=G1, in0=e21, in1=gsum, op=ALU.mult), reads=[k_], writes=["G"])
            S.op("dve", lambda e: e.tensor_tensor(out=G2, in0=gsum, in1=G1, op=ALU.subtract), reads=[k_, "G"], writes=["G"])
            for tt in range(NT):
                S.op("pe", lambda e, tt=tt: e.matmul(
                    ps[:, 6, tt * NE:(tt + 1) * NE], lhsT=trib, rhs=Mall[:, tt, :], start=True, stop=(tt == 0)),
                    reads=["trib", "Mall"], writes=[("ps", 6)])
                for t2 in range(tt):
                    S.op("pe", lambda e, tt=tt, t2=t2: e.matmul(
                        ps[:, 6, tt * NE:(tt + 1) * NE], lhsT=onesb, rhs=Mall[:, t2, :], start=False, stop=(t2 == tt - 1)),
                        reads=["onesb", "Mall"], writes=[("ps", 6)])
            S.op("dve", lambda e: e.tensor_tensor(
                out=PEf, in0=ps[:, 6, :].rearrange("p (t x) -> p t x", t=NT),
                in1=iotac.unsqueeze(1).to_broadcast([128, NT, NE]), op=ALU.add),
                reads=[("ps", 6), "iotac"], writes=["PEf"])
            for (OH, Df, Di) in ((OH1, D1f, D1i), (OH2, D2f, D2i)):
                S.op("dve", lambda e, OH=OH: e.tensor_tensor(out=PR, in0=OH, in1=PEf, op=ALU.mult),
                     reads=["OH", "PEf"], writes=["PR"])
                S.op("dve", lambda e, Df=Df: e.tensor_reduce(out=Df, in_=PR, axis=AX.X, op=ALU.add),
                     reads=["PR"], writes=["Df"])
                S.op("dve", lambda e, Df=Df, Di=Di: e.tensor_copy(out=Di, in_=Df), reads=["Df"], writes=["Di"])
            for tt in range(NT):
                hb = tt % 4
                hf = tt % 2
                S.op("dve", lambda e, tt=tt, hf=hf: e.tensor_tensor(out=HF[hf], in0=X[:, tt, :], in1=SCB, op=ALU.mult),
                     reads=[("X", tt), "SCB"], writes=[("HF", hf)])
                S.op("dve", lambda e, hb=hb, hf=hf: e.tensor_tensor(out=HB[hb], in0=HF[hf], in1=SHB, op=ALU.add),
                     reads=[("HF", hf), "SHB"], writes=[("HB", hb)])
                for Di in (D1i, D2i):
                    S.dma("pool", lambda e, tt=tt, Di=Di, hb=hb: e.indirect_dma_start(
                        out=xs_d[:, :], out_offset=bass.IndirectOffsetOnAxis(ap=Di[:, tt:tt + 1], axis=0),
                        in_=HB[hb], in_offset=None),
                        reads=[("HB", hb), "Di"], writes=[("XSw", tt, id(Di))])
            S.barrier()
            ar.reset(m0)
            NXG = 6
            NYO = 4
            XG = [ar.alloc([128, D], BF16) for _ in range(NXG)]
            XeT = [ar.alloc([128, KC, CAP], BF16) for _ in range(2)]
            HTb = [ar.alloc([128, 4, CAP], BF16) for _ in range(2)]
            SG = [ar.alloc([128, CAP]) for _ in range(2)]
            YO = [ar.alloc([128, D], BF16) for _ in range(NYO)]

            def prep_load(e_):
                for s_ in range(NS):
                    xb = (e_ * NS + s_) % NXG
                    r0 = e_ * CAP + s_ * 128
                    S.dma("sp", lambda e, xb=xb, r0=r0: e.dma_start(out=XG[xb], in_=xs_d[r0:r0 + 128, :]),
                          writes=[("XG", xb)])

            def prep(e_):
                wb = e_ % 2
                for s_ in range(NS):
                    xb = (e_ * NS + s_) % NXG
                    bank = (e_ * NS + s_) % 2
                    for kc in range(KC):
                        S.op("pe", lambda e, xb=xb, kc=kc, bank=bank: e.transpose(
                            psb(bank)[:, kc * 128:(kc + 1) * 128], XG[xb][:, kc * 128:(kc + 1) * 128], identb),
                            reads=[("XG", xb), "identb"], writes=[("ps", bank)])
                    S.op("act", lambda e, s_=s_, bank=bank, wb=wb: e.activation(
                        out=XeT[wb][:, :, s_ * 128:(s_ + 1) * 128], in_=psb(bank).rearrange("p (k n) -> p k n", k=KC),
                        func=AF.Copy), reads=[("ps", bank)], writes=[("XeT", wb)])

            def compute(e_):
                wb = e_ % 2
                ww = e_ % NWB
                Wg_, Wu_, Wd_ = WE[ww]
                for fc in range(4):
                    gbank = 2 + 2 * (fc % 2)
                    ubank = gbank + 1
                    for (wi, W_, bank) in ((0, Wg_, gbank), (1, Wu_, ubank)):
                        for kc in range(KC):
                            S.op("pe", lambda e, fc=fc, kc=kc, W_=W_, bank=bank: e.matmul(
                                ps[:, bank, 0:CAP], lhsT=W_[:, kc, fc * 128:(fc + 1) * 128], rhs=XeT[wb][:, kc, :],
                                start=(kc == 0), stop=(kc == KC - 1)),
                                reads=[("WE", ww, wi), ("XeT", wb)], writes=[("ps", bank)])
                    S.op("act", lambda e, fc=fc, gbank=gbank: e.activation(out=SG[fc % 2], in_=ps[:, gbank, 0:CAP], func=AF.Silu),
                         reads=[("ps", gbank)], writes=[("SG", fc % 2)])
                    S.op("dve", lambda e, fc=fc, ubank=ubank: e.tensor_tensor(
                        out=HTb[wb][:, fc, :], in0=SG[fc % 2], in1=ps[:, ubank, 0:CAP], op=ALU.mult),
                        reads=[("SG", fc % 2), ("ps", ubank)], writes=[("HTb", wb)])
                if e_ + NWB < NE:
                    load_expert(e_ + NWB, which=(0, 1))
                for s_ in range(NS):
                    yb_ = (e_ * NS + s_) % NYO
                    for half in range(2):
                        bank = 6 + half
                        for fc in range(4):
                            S.op("pe", lambda e, s_=s_, half=half, fc=fc, bank=bank: e.matmul(
                                ps[:, bank, :], lhsT=HTb[wb][:, fc, s_ * 128:(s_ + 1) * 128],
                                rhs=Wd_[:, fc, half * 512:(half + 1) * 512], start=(fc == 0), stop=(fc == 3)),
                                reads=[("HTb", wb), ("WE", ww, 2)], writes=[("ps", bank)])
                        S.op("dve", lambda e, yb_=yb_, bank=bank, half=half: e.tensor_tensor(
                            out=YO[yb_][:, half * 512:(half + 1) * 512], in0=ps[:, bank, :],
                            in1=G2B[:, half * 512:(half + 1) * 512], op=ALU.mult),
                            reads=[("ps", bank), ("GB", 5)], writes=[("YO", yb_)])
                    r0 = e_ * CAP + s_ * 128
                    S.dma("sp", lambda e, yb_=yb_, r0=r0: e.dma_start(out=ys_d[r0:r0 + 128, :], in_=YO[yb_]),
                          reads=[("YO", yb_)], writes=[("YSw", e_, s_)])
                if e_ + NWB < NE:
                    load_expert(e_ + NWB, which=(2,))

            prep_load(0)
            prep_load(1)
            prep(0)
            for e_ in range(NE):
                if e_ + 2 < NE:
                    prep_load(e_ + 2)
                if e_ + 1 < NE:
                    prep(e_ + 1)
                compute(e_)
            S.barrier()
            ar.reset(A0 + KC * SEQ // 2)
            modg = mod_groups(next_layer, R0, (0,), (1,)) if next_layer is not None else None
            NYB = 4
            Y1 = [ar.alloc([128, D], BF16) for _ in range(NYB)]
            Y2 = [ar.alloc([128, D], BF16) for _ in range(NYB)]
            VB = TMPV + [ar.alloc([128, D]) for _ in range(2)]
            def gather(tt):
                yb = tt % NYB
                S.dma("pool", lambda e: e.indirect_dma_start(
                    out=Y1[yb], out_offset=None, in_=ys_d[:, :],
                    in_offset=bass.IndirectOffsetOnAxis(ap=D1i[:, tt:tt + 1], axis=0)),
                    reads=["Di"], writes=[("Y1", yb)])
                S.dma("pool", lambda e: e.indirect_dma_start(
                    out=Y2[yb], out_offset=None, in_=ys_d[:, :],
                    in_offset=bass.IndirectOffsetOnAxis(ap=D2i[:, tt:tt + 1], axis=0)),
                    reads=["Di"], writes=[("Y2", yb)])

            DG = [[ar.alloc([128, 128], BF16) for _ in range(2)] for _ in range(2)]
            for tt in range(NYB):
                gather(tt)

            def build(tt):
                yb = tt % NYB
                db = tt % 2
                for k_, Gk in enumerate((G1, G2)):
                    S.op("dve", lambda e, k_=k_, Gk=Gk: e.tensor_scalar(
                        out=DG[db][k_], in0=ident, scalar1=Gk[:, tt:tt + 1], scalar2=None, op0=ALU.mult),
                        reads=["ident", "G"], writes=[("DG", db, k_)])
                banks = [2 + 2 * (tt % 3), 3 + 2 * (tt % 3)]
                for half in range(2):
                    hs = slice(half * 512, (half + 1) * 512)
                    S.op("pe", lambda e, half=half, hs=hs: e.matmul(
                        ps[:, banks[half], :], lhsT=alphaI, rhs=X[:, tt, hs], start=True, stop=False),
                        reads=["alphaI", ("X", tt)], writes=[("ps", banks[half])])
                    S.op("pe", lambda e, half=half, hs=hs: e.matmul(
                        ps[:, banks[half], :], lhsT=DG[db][0], rhs=Y1[yb][:, hs], start=False, stop=False),
                        reads=[("DG", db, 0), ("Y1", yb)], writes=[("ps", banks[half])])
                    S.op("pe", lambda e, half=half, hs=hs: e.matmul(
                        ps[:, banks[half], :], lhsT=DG[db][1], rhs=Y2[yb][:, hs], start=False, stop=True),
                        reads=[("DG", db, 1), ("Y2", yb)], writes=[("ps", banks[half])])

            def cstats(tt):
                banks = [2 + 2 * (tt % 3), 3 + 2 * (tt % 3)]
                ln_stats(tt, ps[:, banks[0]:banks[0] + 2, :].rearrange("p a n -> p (a n)"),
                         [("ps", banks[0]), ("ps", banks[1])], vbufs=VB)

            build(0)
            build(1)
            cstats(0)
            for tt in range(NT):
                if tt + 2 < NT:
                    build(tt + 2)
                if tt + 1 < NT:
                    cstats(tt + 1)
                ln_finish(tt, store_out=store_out, vbufs=VB)
                if tt + NYB < NT:
                    gather(tt + NYB)
                if modg is not None:
                    if tt < 12:
                        modg(tt)
                    if tt >= 4:
                        hT_tile(tt - 4, (1,), SC1, SH1, 1, 0)
            if modg is not None:
                for k in range(NT - 4, NT):
                    hT_tile(k, (1,), SC1, SH1, 1, 0)

        def dump_dbg(kind):
            if kind == "mod":
                S.dma("sp", lambda e: e.dma_start(out=out_d[0:128, :], in_=G1B), reads=[("GB", 2)], writes=["o0"])
                S.dma("sp", lambda e: e.dma_start(out=out_d[128:256, :], in_=G2B), reads=[("GB", 5)], writes=["o1"])
                for n_, t_ in enumerate((SC1, SH1, SC2, SH2)):
                    S.dma("sp", lambda e, n_=n_, t_=t_: e.dma_start(out=out_d[256:384, n_ * 8:(n_ + 1) * 8], in_=t_),
                          reads=[("modfm", k) for k in (0, 1, 3, 4)], writes=[("o2", n_)])
            elif kind == "hT":
                for kc in range(KC):
                    S.dma("pool", lambda e, kc=kc: e.dma_start(out=out_d[kc * 128:(kc + 1) * 128, :], in_=A[:, kc, 0:1024]),
                          reads=[("A", t) for t in range(NT)], writes=[("o", kc)])
            elif kind == "attn":
                for tt in range(NT):
                    S.dma("pool", lambda e, tt=tt: e.dma_start(out=out_d[tt * 128:(tt + 1) * 128, :], in_=B[:, tt, :]),
                          reads=[("B", tt)], writes=[("o", tt)])

        def dump_x():
            for tt in range(NT):
                S.dma("sp", lambda e, tt=tt: e.dma_start(out=out_d[tt * 128:(tt + 1) * 128, :], in_=X[:, tt, :]),
                      reads=[("X", tt)], writes=[("out", tt)])

        done = False
        premod = False
        for i in range(n_layers):
            S.barrier()
            if not premod:
                mod_phase(i)
                S.barrier()
                if stop == ("mod", i):
                    dump_dbg("mod")
                    done = True
                    break
                build_hT(SC1, SH1, 1, 0)
                if stop == ("hT", i):
                    S.barrier()
                    dump_dbg("hT")
                    done = True
                    break
            l = i // 2
            if i % 2 == 0:
                na_phase(l)
                wo_ap = nawo_d[l]
            else:
                gqa_phase(l)
                wo_ap = gwo_d[l]
            S.barrier()
            if stop == ("attn", i):
                dump_dbg("attn")
                done = True
                break
            wo_ln_phase(i, wo_ap)
            S.barrier()
            if stop == ("mix", i):
                dump_x()
                done = True
                break
            last = (i == n_layers - 1)
            moe_phase(i, store_out=last, next_layer=(None if last else i + 1))
            premod = not last
            if last:
                done = True
        assert done
        S.barrier()
        S.emit()
        S.close()
    return nc


_CACHE = {}


def _prep_inputs(x, c, ada_w, ada_b, ln_g, ln_b, na_w_qkv, na_rpb, na_w_o, gqa_w_qkv, gqa_q_norm,
                 gqa_k_norm, gqa_w_o, moe_w_group, moe_b_group, moe_w_expert, moe_b_expert,
                 moe_w_gate, moe_w_up, moe_w_down):
    f = lambda a: np.ascontiguousarray(np.asarray(a), dtype=np.float32)
    C64, S64 = _rope_tables()
    shared = {
        "ada_w": f(ada_w), "ada_b": f(ada_b), "ln_g": f(ln_g), "ln_b": f(ln_b),
        "na_w_qkv": f(na_w_qkv), "na_w_o": f(na_w_o), "na_bias": _na_bias_table(f(na_rpb)).reshape(-1, NH, 128, NCLS * 5 * 128),
        "gqa_w_qkv": f(gqa_w_qkv), "gqa_w_o": f(gqa_w_o), "gqa_q_norm": f(gqa_q_norm), "gqa_k_norm": f(gqa_k_norm),
        "rope_c": C64, "rope_s": S64,
        "moe_wr": np.ascontiguousarray(np.concatenate([f(moe_w_group), f(moe_w_expert)], axis=-1)),
        "moe_br": np.ascontiguousarray(np.concatenate([f(moe_b_group), f(moe_b_expert)], axis=-1)),
        "moe_w_gate": f(moe_w_gate), "moe_w_up": f(moe_w_up), "moe_w_down": f(moe_w_down),
        "ident": np.eye(128, dtype=np.float32),
        "tri": np.triu(np.ones((128, 128), np.float32), 1),
        "iotac": np.ascontiguousarray(np.broadcast_to((np.arange(NE) * CAP).astype(np.float32), (128, NE))),
    }
    x = f(x)
    c = f(c)
    in_maps = []
    for b in range(8):
        m = dict(shared)
        m["x"] = x[b]
        m["cfm"] = np.ascontiguousarray(c[b].reshape(KC, 128).T)
        in_maps.append(m)
    return in_maps


def kernel(**inputs):
    in_maps = _prep_inputs(**inputs)
    key = "full"
    if key not in _CACHE:
        _CACHE[key] = build_program()
    nc = _CACHE[key]
    res = run_bass_kernel_spmd(nc, in_maps, core_ids=list(range(8)))
    return np.stack([np.asarray(r["out"], dtype=np.float32) for r in res.results], axis=0)
```
